# Optimizing a Trainium2 kernel written in Bass

```python
import jax, jax.numpy as jnp
from jax import lax
import numpy as np

D_MODEL = 2048
BATCH = 4
SEQ = 4096
DEPTH = 1

MIX_WIDTH = D_MODEL
HEAD_DIM = 64
NSA_WIDTH = MIX_WIDTH // 2
NSA_HEADS = NSA_WIDTH // HEAD_DIM
NSA_KV_HEADS = 4
NSA_GROUP = NSA_HEADS // NSA_KV_HEADS
NSA_KV_WIDTH = NSA_KV_HEADS * HEAD_DIM
CMP_BLOCK = 32
CMP_STRIDE = 16
SEL_BLOCK = 64
N_SELECT = 16
WINDOW = 512
N_GATES = 3
Q_BLOCK = 64
RWKV_WIDTH = MIX_WIDTH - NSA_WIDTH
RWKV_HEADS = RWKV_WIDTH // HEAD_DIM
DECAY_LORA = 96
ICLR_LORA = 96
GATE_LORA = 256
GN_EPS = 64e-5
N_EXPERTS = 32
TOP_K = 4
D_EXPERT = D_MODEL
SWIGLU_ALPHA = 1.702
SWIGLU_LIMIT = 7.0
MOE_BLOCK = 256
PLE_DIM = 256
LN_EPS = 1e-5
DEEPNORM_ALPHA = (2 * DEPTH) ** 0.25
DEEPNORM_BETA = (8 * DEPTH) ** -0.25
NEG_INF = -1e30

NSA_COLS = NSA_WIDTH + 6 * NSA_KV_WIDTH + NSA_HEADS * N_GATES
RWKV_COLS = 3 * RWKV_WIDTH + DECAY_LORA + ICLR_LORA + GATE_LORA
IN_COLS = NSA_COLS + RWKV_COLS
NSA_SPLIT = tuple(NSA_WIDTH + j * NSA_KV_WIDTH for j in range(7))
RWKV_SPLIT = (RWKV_WIDTH, 2 * RWKV_WIDTH, 3 * RWKV_WIDTH, 3 * RWKV_WIDTH + DECAY_LORA, 3 * RWKV_WIDTH + DECAY_LORA + ICLR_LORA)

kernel_name = "hymba_nsa_rwkv7_moe_deepnorm"


def layer_norm(x, g, b):
    xf = x.astype(jnp.float32)
    mean = xf.mean(-1, keepdims=True)
    var = jnp.square(xf - mean).mean(-1, keepdims=True)
    return ((xf - mean) * lax.rsqrt(var + LN_EPS) * g + b).astype(x.dtype)


def masked_softmax(s, mask):
    s = jnp.where(mask, s, NEG_INF)
    e = jnp.where(mask, jnp.exp(s - jnp.max(s, axis=-1, keepdims=True)), 0.0)
    return e / jnp.maximum(e.sum(-1, keepdims=True), 1e-30)


def alibi_slopes(n):
    return jnp.exp2(-8.0 * jnp.arange(1, n + 1, dtype=jnp.float32) / n)


def compress_kv(kv, pe, w1, w2):
    b, t_len, g, d = kv.shape
    ratio = CMP_BLOCK // CMP_STRIDE
    n_chunks = t_len // CMP_STRIDE
    n_cmp = n_chunks - ratio + 1
    chunks = kv.reshape(b, n_chunks, CMP_STRIDE, g, d)
    blocks = jnp.concatenate([chunks[:, r:r + n_cmp] for r in range(ratio)], axis=2)
    blocks = (blocks + pe[:, None, :]).transpose(0, 1, 3, 2, 4).reshape(b, n_cmp, g, CMP_BLOCK * d)
    return jax.nn.gelu(blocks @ w1) @ w2


def nsa_attention(q, k_cmp, v_cmp, k_slc, v_slc, k_win, v_win, gate_logits):
    b, t_len = q.shape[:2]
    n_cmp = k_cmp.shape[1]
    n_slc = t_len // SEL_BLOCK
    n_sel = min(N_SELECT, n_slc)
    scale = HEAD_DIM ** -0.5
    slopes = alibi_slopes(NSA_HEADS).reshape(NSA_KV_HEADS, NSA_GROUP)[:, :, None, None]
    cmp_end = jnp.arange(n_cmp) * CMP_STRIDE + CMP_BLOCK - 1
    cmp_start = cmp_end - CMP_BLOCK + 1
    slc_start = jnp.arange(n_slc) * SEL_BLOCK
    overlap = jnp.clip(jnp.minimum(cmp_end[:, None], slc_start[None, :] + SEL_BLOCK - 1)
                       - jnp.maximum(cmp_start[:, None], slc_start[None, :]) + 1, 0).astype(jnp.float32)

    def to_blocks(a):
        return a.reshape(b, n_slc, SEL_BLOCK, NSA_KV_HEADS, HEAD_DIM).transpose(0, 3, 1, 2, 4).reshape(
            b, NSA_KV_HEADS, n_slc, SEL_BLOCK * HEAD_DIM)

    k_sb, v_sb = to_blocks(k_slc), to_blocks(v_slc)
    pad = ((0, 0), (WINDOW, 0), (0, 0), (0, 0))
    k_wp, v_wp = jnp.pad(k_win, pad), jnp.pad(v_win, pad)
    b_idx = jnp.arange(b)[:, None, None, None]
    g_idx = jnp.arange(NSA_KV_HEADS)[None, :, None, None]
    blk = jnp.arange(n_slc)
    sel_off = jnp.arange(SEL_BLOCK)
    win_off = jnp.arange(WINDOW + Q_BLOCK) - WINDOW

    def attend(s, mask, dist, v, spec):
        s = s.astype(jnp.float32) * scale - slopes * dist
        probs = masked_softmax(s, mask)
        return probs, jnp.einsum(spec, probs.astype(v.dtype), v)

    def block(qi):
        q0 = qi * Q_BLOCK
        tq = q0 + jnp.arange(Q_BLOCK)
        qb = lax.dynamic_slice_in_dim(q, q0, Q_BLOCK, axis=1)
        qb = qb.reshape(b, Q_BLOCK, NSA_KV_HEADS, NSA_GROUP, HEAD_DIM).transpose(0, 2, 3, 1, 4)
        gb = jax.nn.sigmoid(lax.dynamic_slice_in_dim(gate_logits, q0, Q_BLOCK, axis=1).astype(jnp.float32))
        gb = gb.reshape(b, Q_BLOCK, NSA_KV_HEADS, NSA_GROUP, N_GATES).transpose(0, 2, 3, 1, 4)
        d_cmp = tq[:, None] - cmp_end[None, :]
        p_cmp, o_cmp = attend(jnp.einsum('bgrqd,bcgd->bgrqc', qb, k_cmp), d_cmp >= 0, d_cmp, v_cmp,
                              'bgrqc,bcgd->bgrqd')
        imp = jnp.einsum('bgrqc,cj->bgqj', p_cmp, overlap)
        cur = (tq // SEL_BLOCK)[:, None]
        forced = (blk == 0) | (blk == cur) | (blk == cur - 1)
        imp = jnp.where(forced, jnp.inf, jnp.where(blk > cur, -jnp.inf, imp))
        sel = lax.top_k(imp, n_sel)[1]
        k_g = k_sb[b_idx, g_idx, sel].reshape(b, NSA_KV_HEADS, Q_BLOCK, n_sel * SEL_BLOCK, HEAD_DIM)
        v_g = v_sb[b_idx, g_idx, sel].reshape(b, NSA_KV_HEADS, Q_BLOCK, n_sel * SEL_BLOCK, HEAD_DIM)
        pos = (sel[..., None] * SEL_BLOCK + sel_off).reshape(b, NSA_KV_HEADS, Q_BLOCK, n_sel * SEL_BLOCK)
        d_slc = (tq[:, None] - pos)[:, :, None]
        _, o_slc = attend(jnp.einsum('bgrqd,bgqkd->bgrqk', qb, k_g), d_slc >= 0, d_slc, v_g,
                          'bgrqk,bgqkd->bgrqd')
        kw = lax.dynamic_slice_in_dim(k_wp, q0, WINDOW + Q_BLOCK, axis=1)
        vw = lax.dynamic_slice_in_dim(v_wp, q0, WINDOW + Q_BLOCK, axis=1)
        kpos = q0 + win_off
        d_win = tq[:, None] - kpos[None, :]
        m_win = (d_win >= 0) & (d_win < WINDOW) & (kpos >= 0)[None, :]
        _, o_win = attend(jnp.einsum('bgrqd,bkgd->bgrqk', qb, kw), m_win, d_win, vw, 'bgrqk,bkgd->bgrqd')
        o = gb[..., 0:1] * o_cmp + gb[..., 1:2] * o_slc + gb[..., 2:3] * o_win
        return o.transpose(0, 3, 1, 2, 4).reshape(b, Q_BLOCK, NSA_WIDTH).astype(q.dtype)

    out = lax.map(block, jnp.arange(t_len // Q_BLOCK))
    return out.transpose(1, 0, 2, 3).reshape(b, t_len, NSA_WIDTH)


def rwkv_step(state, inp):
    r, w, k, v, a_vec, b_vec = inp
    state = (state * w[:, :, None, :]
             + jnp.einsum('bhvk,bhk->bhv', state, a_vec)[..., None] * b_vec[:, :, None, :]
             + v[..., None] * k[:, :, None, :])
    return state, jnp.einsum('bhvk,bhk->bhv', state, r)


def rwkv7_time_mix(z, mu, w0, w_up, a0, a_up, g_up, k_k, k_a, r_k, ln_g, ln_b):
    b, t_len, _ = z.shape
    f32 = jnp.float32
    z = z + (jnp.pad(z, ((0, 0), (1, 0), (0, 0)))[:, :-1] - z) * mu
    r, k, v, w_lo, a_lo, g_lo = jnp.split(z, RWKV_SPLIT, axis=-1)
    heads = lambda u: u.astype(f32).reshape(b, t_len, RWKV_HEADS, HEAD_DIM)
    w_raw = -jax.nn.softplus(-(w0 + jnp.tanh(w_lo) @ w_up).astype(f32)) - 0.5
    decay = jnp.exp(-jnp.exp(w_raw))
    a = jax.nn.sigmoid((a0 + a_lo @ a_up).astype(f32))
    g = jax.nn.sigmoid(g_lo) @ g_up
    kk = heads(k * k_k)
    kk = kk / jnp.maximum(jnp.linalg.norm(kk, axis=-1, keepdims=True), 1e-12)
    k = k.astype(f32) * (1.0 + (a - 1.0) * k_a)
    r_h, w_h, k_h, v_h, a_h = heads(r), heads(decay), heads(k), heads(v), heads(a)
    seq_major = lambda u: jnp.swapaxes(u, 0, 1)
    state0 = jnp.zeros((b, RWKV_HEADS, HEAD_DIM, HEAD_DIM), f32)
    _, y = lax.scan(rwkv_step, state0, (seq_major(r_h), seq_major(w_h), seq_major(k_h), seq_major(v_h),
                                         seq_major(-kk), seq_major(kk * a_h)))
    y = jnp.swapaxes(y, 0, 1)
    mean = y.mean(-1, keepdims=True)
    var = jnp.square(y - mean).mean(-1, keepdims=True)
    y = ((y - mean) * lax.rsqrt(var + GN_EPS)).reshape(b, t_len, RWKV_WIDTH) * ln_g + ln_b
    bonus = (jnp.sum(r_h * k_h * r_k, axis=-1, keepdims=True) * v_h).reshape(b, t_len, RWKV_WIDTH)
    return ((y + bonus) * g).astype(z.dtype)


def clamped_swiglu(gate, up):
    gate = jnp.minimum(gate, SWIGLU_LIMIT)
    up = jnp.clip(up, -SWIGLU_LIMIT, SWIGLU_LIMIT)
    return gate * jax.nn.sigmoid(SWIGLU_ALPHA * gate) * (up + 1.0)


def moe_ffn(x, router_w, router_b, w_gate, b_gate, w_up, b_up, w_down, b_down):
    b, t_len, d = x.shape
    xt = x.reshape(-1, d)
    n_tok = xt.shape[0]
    logits = (xt @ router_w + router_b).astype(jnp.float32)
    top_logit, top_idx = lax.top_k(logits, TOP_K)
    top_w = jax.nn.softmax(top_logit, axis=-1)
    n_assign = n_tok * TOP_K
    flat_e = top_idx.reshape(-1)
    order = jnp.argsort(flat_e)
    sorted_e = flat_e[order]
    sorted_tok = order // TOP_K
    sorted_w = top_w.reshape(-1)[order]
    counts = jnp.bincount(flat_e, length=N_EXPERTS)
    padded = (counts + MOE_BLOCK - 1) // MOE_BLOCK * MOE_BLOCK
    start_sorted = jnp.cumsum(counts) - counts
    end_padded = jnp.cumsum(padded)
    start_padded = end_padded - padded
    dest = start_padded[sorted_e] + jnp.arange(n_assign) - start_sorted[sorted_e]
    n_blocks = -(-(n_assign + N_EXPERTS * (MOE_BLOCK - 1)) // MOE_BLOCK)
    n_rows = n_blocks * MOE_BLOCK
    row_tok = jnp.zeros((n_rows,), jnp.int32).at[dest].set(sorted_tok)
    row_w = jnp.zeros((n_rows,), jnp.float32).at[dest].set(sorted_w)
    block_e = jnp.minimum(jnp.searchsorted(end_padded, jnp.arange(n_blocks) * MOE_BLOCK, side='right'),
                          N_EXPERTS - 1)

    def expert_block(args):
        tok, e = args
        xb = xt[tok]
        h = clamped_swiglu(xb @ w_gate[e] + b_gate[e], xb @ w_up[e] + b_up[e])
        return h @ w_down[e] + b_down[e]

    out = lax.map(expert_block, (row_tok.reshape(n_blocks, MOE_BLOCK), block_e))
    y = jnp.zeros((n_tok, d), jnp.float32).at[row_tok].add(out.reshape(n_rows, d).astype(jnp.float32) * row_w[:, None])
    return y.astype(x.dtype).reshape(b, t_len, d)


def setup_inputs(seed: int = 0) -> dict:
    key = jax.random.key(seed)
    keys = iter(jax.random.split(key, 48))
    f32 = jnp.float32
    nrm = lambda shape, s: jax.random.normal(next(keys), shape, f32) * s
    L, D, F, E = DEPTH, D_MODEL, D_EXPERT, N_EXPERTS
    kin = CMP_BLOCK * HEAD_DIM
    return {
        "x": nrm((BATCH, SEQ, D), 1.0),
        "p": nrm((L, BATCH, SEQ, PLE_DIM), 1.0),
        "w_in": nrm((L, D, IN_COLS), D ** -0.5),
        "cmp_pe_k": nrm((L, CMP_BLOCK, HEAD_DIM), 0.5),
        "cmp_w1_k": nrm((L, kin, HEAD_DIM), kin ** -0.5),
        "cmp_w2_k": nrm((L, HEAD_DIM, HEAD_DIM), HEAD_DIM ** -0.5),
        "cmp_pe_v": nrm((L, CMP_BLOCK, HEAD_DIM), 0.5),
        "cmp_w1_v": nrm((L, kin, HEAD_DIM), kin ** -0.5),
        "cmp_w2_v": nrm((L, HEAD_DIM, HEAD_DIM), HEAD_DIM ** -0.5),
        "rwkv_mu": jax.random.uniform(next(keys), (L, RWKV_COLS), f32),
        "rwkv_w0": -1.0 + nrm((L, RWKV_WIDTH), 0.5),
        "rwkv_w_up": nrm((L, DECAY_LORA, RWKV_WIDTH), 0.5 * DECAY_LORA ** -0.5),
        "rwkv_a0": nrm((L, RWKV_WIDTH), 0.5),
        "rwkv_a_up": nrm((L, ICLR_LORA, RWKV_WIDTH), 0.5 * ICLR_LORA ** -0.5),
        "rwkv_g_up": nrm((L, GATE_LORA, RWKV_WIDTH), GATE_LORA ** -0.5),
        "rwkv_k_k": 0.85 + nrm((L, RWKV_WIDTH), 0.05),
        "rwkv_k_a": 1.0 + nrm((L, RWKV_WIDTH), 0.05),
        "rwkv_r_k": nrm((L, RWKV_HEADS, HEAD_DIM), 0.1),
        "rwkv_ln_g": 1.0 + nrm((L, RWKV_WIDTH), 0.05),
        "rwkv_ln_b": nrm((L, RWKV_WIDTH), 0.01),
        "w_out": nrm((L, MIX_WIDTH, D), MIX_WIDTH ** -0.5 * DEEPNORM_BETA),
        "ln1_g": 1.0 + nrm((L, D), 0.05),
        "ln1_b": nrm((L, D), 0.01),
        "router_w": nrm((L, D, E), D ** -0.5),
        "router_b": nrm((L, E), 0.01),
        "exp_w_gate": nrm((L, E, D, F), D ** -0.5),
        "exp_b_gate": nrm((L, E, F), 0.01),
        "exp_w_up": nrm((L, E, D, F), D ** -0.5),
        "exp_b_up": nrm((L, E, F), 0.01),
        "exp_w_down": nrm((L, E, F, D), F ** -0.5 * DEEPNORM_BETA),
        "exp_b_down": nrm((L, E, D), 0.01),
        "ln2_g": 1.0 + nrm((L, D), 0.05),
        "ln2_b": nrm((L, D), 0.01),
        "ple_w": nrm((L, PLE_DIM, D), PLE_DIM ** -0.5),
        "ple_gate_w": nrm((L, D, D), D ** -0.5),
    }


def reference(x, p, w_in, cmp_pe_k, cmp_w1_k, cmp_w2_k, cmp_pe_v, cmp_w1_v, cmp_w2_v,
              rwkv_mu, rwkv_w0, rwkv_w_up, rwkv_a0, rwkv_a_up, rwkv_g_up, rwkv_k_k, rwkv_k_a, rwkv_r_k,
              rwkv_ln_g, rwkv_ln_b, w_out, ln1_g, ln1_b, router_w, router_b,
              exp_w_gate, exp_b_gate, exp_w_up, exp_b_up, exp_w_down, exp_b_down,
              ln2_g, ln2_b, ple_w, ple_gate_w):
    b, t_len, _ = x.shape
    kv_heads = lambda u: u.reshape(b, t_len, NSA_KV_HEADS, HEAD_DIM)
    h = x
    for i in range(DEPTH):
        z = h @ w_in[i]
        z_nsa, z_rwkv = z[..., :NSA_COLS], z[..., NSA_COLS:]
        q, kc, vc, ks, vs, kw, vw, gl = jnp.split(z_nsa, NSA_SPLIT, axis=-1)
        k_cmp = compress_kv(kv_heads(kc), cmp_pe_k[i], cmp_w1_k[i], cmp_w2_k[i])
        v_cmp = compress_kv(kv_heads(vc), cmp_pe_v[i], cmp_w1_v[i], cmp_w2_v[i])
        o_nsa = nsa_attention(q.reshape(b, t_len, NSA_HEADS, HEAD_DIM), k_cmp, v_cmp,
                              kv_heads(ks), kv_heads(vs), kv_heads(kw), kv_heads(vw),
                              gl.reshape(b, t_len, NSA_HEADS, N_GATES))
        o_rwkv = rwkv7_time_mix(z_rwkv, rwkv_mu[i], rwkv_w0[i], rwkv_w_up[i], rwkv_a0[i], rwkv_a_up[i],
                                rwkv_g_up[i], rwkv_k_k[i], rwkv_k_a[i], rwkv_r_k[i], rwkv_ln_g[i], rwkv_ln_b[i])
        mix = jnp.concatenate([o_nsa, o_rwkv], axis=-1) @ w_out[i]
        h = layer_norm(DEEPNORM_ALPHA * h + mix, ln1_g[i], ln1_b[i])
        ffn = moe_ffn(h, router_w[i], router_b[i], exp_w_gate[i], exp_b_gate[i], exp_w_up[i], exp_b_up[i],
                      exp_w_down[i], exp_b_down[i])
        h = layer_norm(DEEPNORM_ALPHA * h + ffn, ln2_g[i], ln2_b[i])
        h = h + jax.nn.sigmoid(h @ ple_gate_w[i]) * (p[i] @ ple_w[i])
    return h
```

```python
import os
import numpy as np
from contextlib import ExitStack
import concourse.bass as bass
import concourse.mybir as mybir
from concourse.bass_utils import run_bass_kernel_spmd

F32 = mybir.dt.float32
BF16 = mybir.dt.bfloat16
ALU = mybir.AluOpType
AF = mybir.ActivationFunctionType
AX = mybir.AxisListType

NCORES = 8
D = 2048
T = 4096
TO = 2048
NSA_COLS = 2608
RW0 = NSA_COLS
IN_COLS = 6128
SEM_ROT = 12000


class Sync:
    ENGS = ("pe", "act", "dve", "pool", "sp")

    def __init__(self, nc, stack):
        self.nc = nc
        self.stack = stack
        self.eng = {"pe": nc.tensor, "act": nc.scalar, "dve": nc.vector,
                    "pool": nc.gpsimd, "sp": nc.sync}
        self.cnt = {e: 0 for e in self.ENGS}
        self.sems = {e: [] for e in self.ENGS}
        self.waited = {e: {} for e in self.ENGS}
        self.last_w = {}
        self.readers = {}
        self.dma_pool = {}
        self.dma_n = {}
        self.all_dma = []
        self.nsem = 0
        for q in ("sp", "pool", "act"):
            self.dma_pool[q] = [self._newsem() for _ in range(12 if q == "sp" else 6)]
            self.dma_n[q] = 0

    def _newsem(self):
        self.nsem += 1
        return self.stack.enter_context(self.nc.semaphore(f"s{self.nsem}"))

    @staticmethod
    def _key(t):
        return t if isinstance(t, (str, tuple)) else t.tensor.name if hasattr(t, "tensor") else t.name

    def _wait(self, e, tok):
        sem, val, src = tok
        if src == e and e == "pe":
            return
        w = self.waited[e]
        k = id(sem)
        if w.get(k, 0) >= val:
            return
        w[k] = val
        self.eng[e].wait_ge(sem, val)

    def _deps(self, e, r, w):
        toks = []
        for t in r:
            k = self._key(t)
            if k in self.last_w:
                toks.append(self.last_w[k])
        for t in w:
            k = self._key(t)
            if k in self.last_w:
                toks.append(self.last_w[k])
            for tk in self.readers.get(k, {}).values():
                if isinstance(tk, list):
                    toks.extend(tk)
                else:
                    toks.append(tk)
        for tk in toks:
            self._wait(e, tk)

    def _record(self, tok, r, w, isdma):
        for t in w:
            k = self._key(t)
            self.last_w[k] = tok
            self.readers[k] = {}
        for t in r:
            k = self._key(t)
            d = self.readers.setdefault(k, {})
            if isdma:
                d.setdefault("dma", []).append(tok)
                if len(d["dma"]) > 24:
                    d["dma"] = d["dma"][-24:]
            else:
                d[tok[2]] = tok

    def op(self, e, fn, r=(), w=()):
        self._deps(e, r, w)
        n = self.cnt[e]
        si, v = divmod(n, SEM_ROT)
        while len(self.sems[e]) <= si:
            self.sems[e].append(self._newsem())
        sem = self.sems[e][si]
        ins = fn()
        ins.then_inc(sem, 1)
        self.cnt[e] = n + 1
        tok = (sem, v + 1, e)
        self._record(tok, r, w, False)
        return tok

    def dma(self, q, out, in_, r=(), w=(), **kw):
        self._deps(q, r, w)
        n = self.dma_n[q]
        pool = self.dma_pool[q]
        sem = pool[n % len(pool)]
        rnd = n // len(pool)
        if rnd > 0:
            self._wait(q, (sem, 16 * rnd, "dma"))
        self.eng[q].dma_start(out=out, in_=in_, **kw).then_inc(sem, 16)
        self.dma_n[q] = n + 1
        tok = (sem, 16 * (rnd + 1), "dma")
        self._record(tok, r, w, True)
        self.all_dma.append(tok)
        if len(self.all_dma) > 64:
            self.all_dma = self.all_dma[-64:]
        return tok

    def barrier(self, scratch):
        for q in self.dma_pool:
            n = self.dma_n[q]
            pool = self.dma_pool[q]
            for i, sem in enumerate(pool):
                uses = (n - i + len(pool) - 1) // len(pool) if n > i else 0
                if uses > 0:
                    self._wait("dve", (sem, 16 * uses, "dma"))
        toks = []
        for e in ("pe", "act", "pool"):
            if self.cnt[e] > 0:
                n = self.cnt[e] - 1
                si, v = divmod(n, SEM_ROT)
                toks.append((self.sems[e][si], v + 1, e))
        for tk in toks:
            self._wait("dve", tk)
        tok = self.op("dve", lambda: self.nc.vector.memset(scratch[0:1, 0:1], 0.0), w=["_bar"])
        for e in ("pe", "act", "pool", "sp"):
            self._wait(e, tok)
        self.last_w = {}
        self.readers = {}

    def finish(self):
        for q in self.dma_pool:
            n = self.dma_n[q]
            pool = self.dma_pool[q]
            for i, sem in enumerate(pool):
                uses = (n - i + len(pool) - 1) // len(pool) if n > i else 0
                if uses > 0:
                    self._wait("sp", (sem, 16 * uses, "dma"))
        for e in ("pe", "act", "dve", "pool"):
            if self.cnt[e] > 0:
                n = self.cnt[e] - 1
                si, v = divmod(n, SEM_ROT)
                self._wait("sp", (self.sems[e][si], v + 1, e))


SLOPES = [2.0 ** (-8.0 * (i + 1) / 16.0) for i in range(16)]
SCALE = 64 ** -0.5
ALPHA = 2.0 ** 0.25
BIG = 1.0e30

INPUT_SPECS = [
    ("xc", [T, D]), ("p_own", [TO, 256]), ("w_in", [D, IN_COLS]),
    ("cmp_pe_k", [32, 64]), ("cmp_w1_k", [2048, 64]), ("cmp_w2_k", [64, 64]),
    ("cmp_pe_v", [32, 64]), ("cmp_w1_v", [2048, 64]), ("cmp_w2_v", [64, 64]),
    ("rwkv_mu", [1, 3520]), ("rwkv_w0", [1, 1024]), ("rwkv_w_up", [96, 1024]),
    ("rwkv_a0", [1, 1024]), ("rwkv_a_up", [96, 1024]), ("rwkv_g_up", [256, 1024]),
    ("rwkv_k_k", [1, 1024]), ("rwkv_k_a", [1, 1024]), ("rwkv_r_k", [1, 1024]),
    ("rwkv_ln_g", [1, 1024]), ("rwkv_ln_b", [1, 1024]),
    ("w_out", [D, D]), ("ln1_g", [1, D]), ("ln1_b", [1, D]),
    ("router_w", [D, 32]), ("router_b", [1, 32]),
    ("exp_w_gate", [32 * D, D]), ("exp_b_gate", [128, 512]),
    ("exp_w_up", [32 * D, D]), ("exp_b_up", [128, 512]),
    ("exp_w_down", [32 * D, D]), ("exp_b_down", [32, D]),
    ("ln2_g", [1, D]), ("ln2_b", [1, D]), ("ple_w", [256, D]), ("ple_gate_w", [D, D]),
    ("c_kvalid", [128, 32]), ("c_ident", [128, 128]), ("c_trile", [128, 128]), ("c_trigt", [128, 128]),
    ("c_abias", [4 * 128, 512]), ("c_cbias", [4 * 2 * 128, 512]), ("c_cmask", [16 * 2 * 128, 128]),
    ("c_expand", [64, T]), ("c_overlap", [256, 64]), ("c_tribias", [128, 256]), ("c_expbig", [64, T]),
    ("c_selmul", [TO, 64]), ("c_seladd", [TO, 64]), ("c_selvalid", [TO, 64]),
    ("c_rw", [64, 448]),
]


class Ctx:
    pass


def build(debug=(), phases="ABCDEF", skip=()):
    nc = bass.Bass("TRN2", target_bir_lowering=False)
    G = Ctx()
    G.nc = nc
    G.I = {}
    for name, shape in INPUT_SPECS:
        if name in skip:
            continue
        G.I[name] = nc.dram_tensor(name, shape, F32, kind="ExternalInput").ap()
    G.out = nc.dram_tensor("out", [TO, D], F32, kind="ExternalOutput").ap()
    G.dbg = {}
    G.debug = debug
    dr = lambda n, s, d: nc.dram_tensor(n, s, d).ap()
    G.vs_tm = dr("vs_tm", [T, 4 * 65], BF16)
    G.vw_tm = dr("vw_tm", [T, 4 * 65], BF16)
    G.gl_tm = dr("gl_tm", [TO, 48], F32)
    G.zr = dr("zr", [T, 3520], F32)
    G.qT = dr("qT", [1024, TO], BF16)
    G.kcT = dr("kcT", [256, T], BF16)
    G.vcT = dr("vcT", [256, T], BF16)
    G.ksT = dr("ksT", [256, T], BF16)
    G.kwT = dr("kwT", [256, T], BF16)
    G.mixT = dr("mixT", [D, TO], BF16)
    G.h1 = dr("h1", [TO, D], F32)
    G.h1T = dr("h1T", [D, TO], BF16)
    G.gw = dr("gw", [TO, 32], F32)
    G.ypre = dr("ypre", [TO, D], F32)
    for n in ("rw_r", "rw_k", "rw_v", "rw_kn", "rw_b", "rw_lw"):
        setattr(G, n, dr(n, [T, 1024], F32))
    G.rw_g = dr("rw_g", [TO, 1024], F32)
    G.rw_y = dr("rw_y", [TO, 1024], F32)
    with ExitStack() as st:
        S = Sync(nc, st)
        G.S = S
        G.bar = st.enter_context(nc.sbuf_tensor("barscr", [128, 8], F32))
        for nm, shp in debug:
            dbg_out(G, nm, shp)
        if "A" in phases:
            phase_a(G)
        if "B" in phases:
            phase_b(G)
        if "C" in phases:
            phase_c(G)
        if "D" in phases:
            phase_d(G)
        if "E" in phases:
            phase_e(G)
        if "F" in phases:
            phase_f(G)
        S.finish()
    return nc, G


def dbg_out(G, name, shape, dt=F32):
    t = G.nc.dram_tensor("dbg_" + name, shape, dt, kind="ExternalOutput").ap()
    G.dbg[name] = t
    return t


_evac_rr = [0]


def evac(G, out, in_, r, w, scale=None):
    nc, S = G.nc, G.S
    _evac_rr[0] ^= 1
    if _evac_rr[0]:
        if scale is None:
            return S.op("act", lambda: nc.scalar.copy(out=out, in_=in_), r=r, w=w)
        return S.op("act", lambda: nc.scalar.mul(out=out, in_=in_, mul=scale), r=r, w=w)
    if scale is None:
        return S.op("dve", lambda: nc.vector.tensor_copy(out=out, in_=in_), r=r, w=w)
    return S.op("dve", lambda: nc.vector.tensor_scalar(out=out, in0=in_, scalar1=scale, scalar2=None, op0=ALU.mult), r=r, w=w)


def run_pipelined(gens, depth):
    active = []
    it = iter(gens)
    more = True
    while True:
        if more and len(active) < depth:
            try:
                active.append(next(it))
            except StopIteration:
                more = False
        if not active:
            break
        for g in list(active):
            try:
                next(g)
            except StopIteration:
                active.remove(g)


def phase_a(G):
    nc, S, I = G.nc, G.S, G.I
    with ExitStack() as st:
        sb = lambda n, s, d: st.enter_context(nc.sbuf_tensor(n, s, d))
        ps = lambda n, s, d: st.enter_context(nc.psum_tensor(n, s, d))
        ident = sb("a_ident", [128, 128], BF16)
        kval = sb("a_kval", [128, 32], F32)
        xb = [sb(f"a_xb{i}", [128, D], BF16) for i in range(2)]
        xT = sb("a_xT", [128, 16, 2048], BF16)
        wch = [sb(f"a_w{i}", [128, 16, 512], BF16) for i in range(2)]
        stf = [sb(f"a_stf{i}", [128, 512], F32) for i in range(3)]
        stb = [sb(f"a_stb{i}", [128, 512], BF16) for i in range(3)]
        vst = [sb(f"a_vst{i}", [128, 4, 65], BF16) for i in range(2)]
        ptr = [ps(f"a_ptr{i}", [128, 1024], BF16) for i in range(2)]
        pac = [ps(f"a_pac{i}", [128, 512], F32) for i in range(4)]
        S.dma("pool", ident[:], I["c_ident"][:, :], w=[ident])
        S.dma("sp", kval[:], I["c_kvalid"][:, :], w=[kval])
        w_in_v = I["w_in"].rearrange("(kc p) c -> p kc c", p=128)
        tm_chunks = [("vs", 1792, 2048), ("vw", 2304, 2560), ("gl", 2560, 2608)]
        c = 0
        while c < 3520:
            n = min(512, 3520 - c)
            tm_chunks.append(("rw", RW0 + c, RW0 + c + n))
            c += n
        fm_chunks = [("q", 128 * j, 128 * j + 128) for j in range(8)]
        for nm, base in (("kc", 1024), ("vc", 1280), ("ks", 1536), ("kw", 2048)):
            fm_chunks += [(nm, base, base + 128), (nm, base + 128, base + 256)]
        fm_dst = {"q": (G.qT, 0), "kc": (G.kcT, 1024), "vc": (G.vcT, 1280), "ks": (G.ksT, 1536), "kw": (G.kwT, 2048)}
        nw = 0
        nst = 0
        npac = 0
        for hf in range(2):
            for tl in range(16):
                tok0 = hf * 2048 + tl * 128
                xbt = xb[tl % 2]
                S.dma("pool", xbt[:], I["xc"][tok0:tok0 + 128, :], w=[xbt])
                for j in range(2):
                    pt = ptr[j]
                    for k8 in range(8):
                        kc = j * 8 + k8
                        S.op("pe", lambda pt=pt, k8=k8, kc=kc, xbt=xbt: nc.tensor.transpose(
                            pt[:, k8 * 128:(k8 + 1) * 128], xbt[:, kc * 128:(kc + 1) * 128], ident[:]),
                            r=[xbt, ident], w=[pt])
                    evac(G, xT[:, j * 8:(j + 1) * 8, tl * 128:(tl + 1) * 128],
                         pt[:].rearrange("p (a b) -> p a b", a=8), r=[pt], w=[xT])
            for (nm, c0, c1) in tm_chunks:
                if nm == "gl" and hf == 0:
                    continue
                ncol = c1 - c0
                wt = wch[nw % 2]
                nw += 1
                S.dma("pool", wt[:, :, 0:ncol], w_in_v[:, :, c0:c1], w=[wt])
                for tl in range(16):
                    tok0 = hf * 2048 + tl * 128
                    pa = pac[npac % 4]
                    npac += 1
                    for kc in range(16):
                        S.op("pe", lambda pa=pa, kc=kc, wt=wt, tl=tl, ncol=ncol: nc.tensor.matmul(
                            pa[:, 0:ncol], lhsT=xT[:, kc, tl * 128:(tl + 1) * 128], rhs=wt[:, kc, 0:ncol],
                            start=(kc == 0), stop=(kc == 15)), r=[xT, wt], w=[pa])
                    if nm in ("vs", "vw"):
                        vt = vst[nst % 2]
                        nst += 1
                        evac(G, vt[:, :, 0:64], pa[:, 0:256].rearrange("p (g d) -> p g d", g=4), r=[pa], w=[vt])
                        tglob = hf * 16 + tl
                        S.op("pool", lambda vt=vt, tglob=tglob: nc.gpsimd.tensor_copy(
                            out=vt[:, :, 64], in_=kval[:, tglob:tglob + 1].to_broadcast([128, 4])),
                            r=[kval, vt], w=[vt])
                        dst = G.vs_tm if nm == "vs" else G.vw_tm
                        S.dma("sp", dst[tok0:tok0 + 128, :], vt[:].rearrange("p g d -> p (g d)"), r=[vt], w=[nm + "_tm"])
                    else:
                        sf = stf[nst % 3]
                        nst += 1
                        evac(G, sf[:, 0:ncol], pa[:, 0:ncol], r=[pa], w=[sf])
                        if nm == "gl":
                            S.dma("sp", G.gl_tm[tl * 128:(tl + 1) * 128, :], sf[:, 0:48], r=[sf], w=["gl_tm"])
                        else:
                            S.dma("sp", G.zr[tok0:tok0 + 128, c0 - RW0:c1 - RW0], sf[:, 0:ncol], r=[sf], w=["zr"])
            for (nm, c0, c1) in fm_chunks:
                if nm == "q" and hf == 0:
                    continue
                wt = wch[nw % 2]
                nw += 1
                S.dma("pool", wt[:, :, 0:128], w_in_v[:, :, c0:c1], w=[wt])
                dst, base = fm_dst[nm]
                for t4 in range(4):
                    pa = pac[npac % 4]
                    npac += 1
                    for kc in range(16):
                        S.op("pe", lambda pa=pa, kc=kc, wt=wt, t4=t4: nc.tensor.matmul(
                            pa[:, :], lhsT=wt[:, kc, 0:128], rhs=xT[:, kc, t4 * 512:(t4 + 1) * 512],
                            start=(kc == 0), stop=(kc == 15)), r=[xT, wt], w=[pa])
                    sbt = stb[nst % 3]
                    nst += 1
                    evac(G, sbt[:, :], pa[:, :], r=[pa], w=[sbt])
                    if nm == "q":
                        col0 = t4 * 512
                    else:
                        col0 = hf * 2048 + t4 * 512
                    S.dma("sp", dst[c0 - base:c1 - base, col0:col0 + 512], sbt[:, :], r=[sbt], w=[nm + "T"])
        S.barrier(G.bar)


def phase_b(G):
    nc, S, I = G.nc, G.S, G.I
    with ExitStack() as st:
        sb = lambda n, s, d: st.enter_context(nc.sbuf_tensor(n, s, d))
        ps = lambda n, s, d: st.enter_context(nc.psum_tensor(n, s, d))
        ident = sb("b_ident", [128, 128], BF16)
        tribias = sb("b_tribias", [128, 256], F32)
        tribias_bf = sb("b_tribias_bf", [128, 256], BF16)
        expbig = sb("b_expbig", [64, T], BF16)
        cmask = sb("b_cmask", [128, 32, 128], BF16)
        abias = sb("b_abias", [128, 512], F32)
        cbias = sb("b_cbias", [128, 2, 512], F32)
        ksT = sb("b_ksT", [64, T], BF16)
        kwT = sb("b_kwT", [64, T], BF16)
        cT = sb("b_cT", [64, T], BF16)
        vs = sb("b_vs", [128, 32, 65], BF16)
        vw = sb("b_vw", [128, 32, 65], BF16)
        qT = sb("b_qT", [64, 4, TO], BF16)
        w1 = [sb(f"b_w1{i}", [128, 16, 64], F32) for i in range(2)]
        w1b = [sb(f"b_w1b{i}", [64, 32, 64], BF16) for i in range(2)]
        w2b = [sb(f"b_w2b{i}", [64, 64], BF16) for i in range(2)]
        pec = [sb(f"b_pec{i}", [128, 16], F32) for i in range(2)]
        b1 = [sb(f"b_b1{i}", [64, 1], F32) for i in range(2)]
        hf = sb("b_hf", [64, 256], F32)
        h2 = sb("b_h2", [64, 256], F32)
        gT = sb("b_gT", [64, 256], BF16)
        kcmpT = sb("b_kcmpT", [64, 256], BF16)
        vcx = sb("b_vcx", [128, 2, 129], BF16)
        tmp = [sb(f"b_tmp{i}", [128, 512], F32) for i in range(2)]
        tmp2 = [sb(f"b_tmq{i}", [128, 512], F32) for i in range(2)]
        Pt = [sb(f"b_P{i}", [128, 512], BF16) for i in range(3)]
        glt = sb("b_gl", [128, 48], F32)
        gsig = sb("b_gsig", [128, 48], F32)
        selc = [sb(f"b_selc{i}", [128, 64], F32) for i in range(3)]
        imp = sb("b_imp", [128, 64], F32)
        imp2 = sb("b_imp2", [128, 64], F32)
        m8 = sb("b_m8", [128, 16], F32)
        selb = sb("b_selb", [128, 64], BF16)
        selT = sb("b_selT", [64, 128], BF16)
        sums = sb("b_sums", [128, 12], F32)
        coef = sb("b_coef", [128, 12], F32)
        oacc = sb("b_oacc", [128, 256], F32)
        obf = sb("b_obf", [128, 256], BF16)
        ost = sb("b_ost", [128, 2, 128], BF16)
        pS = [ps(f"b_pS{i}", [128, 512], F32) for i in range(2)]
        pM = ps("b_pM", [128, 512], F32)
        PMK = [("pM", j) for j in range(4)]
        pAs = ps("b_pAs", [128, 4, 65], F32)
        pAw = ps("b_pAw", [128, 4, 65], F32)
        pAc = [ps(f"b_pAc{i}", [128, 2, 129], F32) for i in range(2)]
        pT = ps("b_pT", [128, 1024], BF16)

        S.dma("pool", ident[:], I["c_ident"][:, :], w=[ident])
        S.dma("sp", tribias[:], I["c_tribias"][:, :], w=[tribias])
        S.dma("pool", tribias_bf[:], I["c_tribias"][:, :], w=[tribias_bf])
        for j in range(4):
            S.dma("pool", expbig[:, j * 1024:(j + 1) * 1024], I["c_expbig"][:, j * 1024:(j + 1) * 1024], w=[expbig])
        cm_v = I["c_cmask"].rearrange("(a p) q -> p a q", p=128)
        for j in range(4):
            S.dma("pool", cmask[:, j * 8:(j + 1) * 8, :], cm_v[:, j * 8:(j + 1) * 8, :], w=[cmask])
        for kv, (nw1, nw2, npe) in enumerate((("cmp_w1_k", "cmp_w2_k", "cmp_pe_k"), ("cmp_w1_v", "cmp_w2_v", "cmp_pe_v"))):
            S.dma("sp", w1[kv][:], I[nw1].rearrange("(p c) o -> p c o", c=16), w=[w1[kv]])
            S.dma("pool", w1b[kv][:], I[nw1].rearrange("(l d) o -> d l o", d=64), w=[w1b[kv]])
            S.dma("pool", w2b[kv][:], I[nw2][:, :], w=[w2b[kv]])
            S.dma("sp", pec[kv][:], I[npe].rearrange("l d -> (l d)").rearrange("(p c) -> p c", c=16), w=[pec[kv]])
            for ch in range(16):
                S.op("pe", lambda kv=kv, ch=ch: nc.tensor.matmul(pM[0:64, 0:1], lhsT=w1[kv][:, ch, :], rhs=pec[kv][:, ch:ch + 1],
                                                                   start=(ch == 0), stop=(ch == 15)), r=[w1[kv], pec[kv]], w=PMK)
            S.op("dve", lambda kv=kv: nc.vector.tensor_copy(out=b1[kv][:], in_=pM[0:64, 0:1]), r=PMK, w=[b1[kv]])
        S.op("dve", lambda: nc.vector.memset(vcx[:], 0.0), w=[vcx])
        S.op("dve", lambda: nc.vector.memset(vcx[:, :, 64:65], 1.0), r=[vcx], w=[vcx])
        S.dma("pool", vcx[:, :, 65:129], I["c_overlap"].rearrange("(ct p) j -> p ct j", p=128), r=[vcx], w=[vcx])
        S.op("dve", lambda: nc.vector.memset(kcmpT[:], 0.0), w=[kcmpT])

        def compress(kv, g):
            src = G.kcT if kv == 0 else G.vcT
            S.dma("sp", cT[:], src[g * 64:(g + 1) * 64, :], r=["kcT", "vcT"], w=[cT])
            cv = cT[:].rearrange("p (c s) -> p c s", s=16)
            for l in range(32):
                rhs = cv[:, 0:255, l] if l < 16 else cv[:, 1:256, l - 16]
                S.op("pe", lambda l=l, rhs=rhs: nc.tensor.matmul(pM[0:64, 0:255], lhsT=w1b[kv][:, l, :], rhs=rhs,
                                                                  start=(l == 0), stop=(l == 31)), r=[w1b[kv], cT], w=PMK)
            S.op("act", lambda: nc.scalar.activation(out=hf[:, 0:255], in_=pM[0:64, 0:255], func=AF.Identity, bias=b1[kv][:, 0:1], scale=1.0),
                 r=PMK + [b1[kv]], w=[hf])
            S.op("dve", lambda: nc.vector.tensor_tensor(out=h2[:, 0:255], in0=hf[:, 0:255], in1=hf[:, 0:255], op=ALU.mult), r=[hf], w=[h2])
            S.op("dve", lambda: nc.vector.tensor_scalar(out=h2[:, 0:255], in0=h2[:, 0:255], scalar1=0.044715, scalar2=1.0, op0=ALU.mult, op1=ALU.add), r=[h2], w=[h2])
            S.op("dve", lambda: nc.vector.tensor_tensor(out=h2[:, 0:255], in0=h2[:, 0:255], in1=hf[:, 0:255], op=ALU.mult), r=[h2, hf], w=[h2])
            S.op("act", lambda: nc.scalar.activation(out=h2[:, 0:255], in_=h2[:, 0:255], func=AF.Tanh, scale=0.7978845608028654), r=[h2], w=[h2])
            S.op("dve", lambda: nc.vector.scalar_tensor_tensor(out=gT[:, 0:255], in0=h2[:, 0:255], scalar=1.0, in1=hf[:, 0:255], op0=ALU.add, op1=ALU.mult),
                 r=[h2, hf], w=[gT])
            if kv == 0:
                S.op("pe", lambda: nc.tensor.matmul(pM[0:64, 0:255], lhsT=w2b[0][:, :], rhs=gT[:, 0:255], start=True, stop=True), r=[w2b[0], gT], w=PMK)
                S.op("act", lambda: nc.scalar.mul(out=kcmpT[:, 0:255], in_=pM[0:64, 0:255], mul=0.5), r=PMK, w=[kcmpT])
            else:
                for ct in range(2):
                    rows = 128 if ct == 0 else 127
                    S.op("pe", lambda ct=ct, rows=rows: nc.tensor.matmul(pM[0:rows, 0:64], lhsT=gT[:, ct * 128:ct * 128 + rows], rhs=w2b[1][:, :],
                                                                          start=True, stop=True), r=[w2b[1], gT], w=PMK)
                    S.op("act", lambda ct=ct, rows=rows: nc.scalar.mul(out=vcx[0:rows, ct, 0:64], in_=pM[0:rows, 0:64], mul=0.5), r=PMK, w=[vcx])

        np_ = [0]
        for g in range(4):
            S.dma("sp", ksT[:], G.ksT[g * 64:(g + 1) * 64, :], r=["ksT"], w=[ksT])
            S.dma("sp", kwT[:], G.kwT[g * 64:(g + 1) * 64, :], r=["kwT"], w=[kwT])
            S.dma("sp", qT[:], G.qT[g * 256:(g + 1) * 256, :].rearrange("(h d) t -> d h t", d=64), r=["qT"], w=[qT])
            vs_v = G.vs_tm.rearrange("(t p) c -> p t c", p=128)
            vw_v = G.vw_tm.rearrange("(t p) c -> p t c", p=128)
            for j in range(4):
                S.dma("sp", vs[:, j * 8:(j + 1) * 8, :], vs_v[:, j * 8:(j + 1) * 8, g * 65:(g + 1) * 65], r=["vs_tm"], w=[vs])
                S.dma("sp", vw[:, j * 8:(j + 1) * 8, :], vw_v[:, j * 8:(j + 1) * 8, g * 65:(g + 1) * 65], r=["vw_tm"], w=[vw])
            S.dma("sp", abias[:], I["c_abias"][g * 128:(g + 1) * 128, :], w=[abias])
            S.dma("sp", cbias[:], I["c_cbias"][g * 256:(g + 1) * 256, :].rearrange("(ct p) x -> p ct x", p=128), w=[cbias])
            compress(0, g)
            compress(1, g)
            for i in range(16):
                q0 = i * 128
                S.dma("sp", glt[:], G.gl_tm[q0:q0 + 128, :], r=["gl_tm"], w=[glt])
                S.op("act", lambda: nc.scalar.activation(out=gsig[:], in_=glt[:], func=AF.Sigmoid), r=[glt], w=[gsig])
                for j, nm in enumerate(("c_selmul", "c_seladd", "c_selvalid")):
                    S.dma("sp", selc[j][:], I[nm][q0:q0 + 128, :], w=[selc[j]])
                gv = gsig[:, 12 * g:12 * g + 12].rearrange("p (h r) -> p h r", r=3)
                qrhs = qT[:, :, q0:q0 + 128]

                def tile_gen(lhsT, bias_ap, mask_fn, offs, pv_fn, rows=128):
                    n = np_[0]
                    np_[0] += 1
                    p_s, t1, t2, P = pS[n % 2], tmp[n % 2], tmp2[n % 2], Pt[n % 3]
                    mk = mask_fn(n) if mask_fn is not None else None
                    S.op("pe", lambda: nc.tensor.matmul(p_s[0:rows, :], lhsT=lhsT, rhs=qrhs, start=True, stop=True), r=[ksT, kwT, kcmpT, qT], w=[p_s])
                    yield
                    S.op("dve", lambda: nc.vector.scalar_tensor_tensor(out=t1[0:rows, :], in0=p_s[0:rows, :], scalar=SCALE, in1=bias_ap,
                                                                        op0=ALU.mult, op1=ALU.add), r=[p_s, abias, cbias], w=[t1])
                    src = t1
                    if mk is not None:
                        mask_ap, mkey = mk
                        S.op("dve", lambda: nc.vector.tensor_tensor(out=t2[0:rows, :].rearrange("p (h q) -> p h q", h=4),
                                                                     in0=t1[0:rows, :].rearrange("p (h q) -> p h q", h=4),
                                                                     in1=mask_ap, op=ALU.add), r=[t1, mkey], w=[t2])
                        src = t2
                    yield
                    for h in range(4):
                        S.op("act", lambda h=h: nc.scalar.activation(out=P[0:rows, h * 128:(h + 1) * 128], in_=src[0:rows, h * 128:(h + 1) * 128],
                                                                      func=AF.Exp, bias=float(offs[h]), scale=1.0), r=[src], w=[P])
                    yield
                    pv_fn(P)

                Pc = []

                def cmp_tile(ct):
                    rows = 128 if ct == 0 else 127
                    offs = [-SLOPES[4 * g + h] * (2048 + 128 * i) for h in range(4)]

                    def pv(P):
                        Pc.append(P)
                        if ct == 0:
                            return
                        for h in range(4):
                            for c2 in range(2):
                                r2 = 128 if c2 == 0 else 127
                                S.op("pe", lambda h=h, c2=c2, r2=r2: nc.tensor.matmul(pAc[h // 2][:, h % 2, :], lhsT=Pc[c2][0:r2, h * 128:(h + 1) * 128],
                                                                                       rhs=vcx[0:r2, c2, :], start=(c2 == 0), stop=(c2 == 1)),
                                     r=[Pc[c2], vcx], w=[pAc[h // 2]])
                    return tile_gen(kcmpT[:, ct * 128:ct * 128 + rows], cbias[0:rows, ct, :],
                                    lambda n: (cmask[0:rows, i * 2 + ct, :].unsqueeze(1).to_broadcast([rows, 4, 128]), cmask), offs, pv, rows)

                def win_tile(wi):
                    kt = 12 + i + wi
                    offs = [-SLOPES[4 * g + h] * 128.0 * (4 - wi) for h in range(4)]
                    if wi == 0:
                        mfn = lambda n: (tribias[:, 128:256].unsqueeze(1).to_broadcast([128, 4, 128]), tribias)
                    elif wi == 4:
                        mfn = lambda n: (tribias[:, 0:128].unsqueeze(1).to_broadcast([128, 4, 128]), tribias)
                    else:
                        mfn = None

                    def pv(P):
                        for h in range(4):
                            S.op("pe", lambda h=h: nc.tensor.matmul(pAw[:, h, :], lhsT=P[:, h * 128:(h + 1) * 128], rhs=vw[:, kt, :],
                                                                     start=(wi == 0), stop=(wi == 4)), r=[P, vw], w=[pAw])
                    return tile_gen(kwT[:, kt * 128:(kt + 1) * 128], abias[:, :], mfn, offs, pv)

                run_pipelined([cmp_tile(0), cmp_tile(1)] + [win_tile(wi) for wi in range(5)], 2)
                for hh in range(2):
                    S.op("dve", lambda hh=hh: nc.vector.tensor_scalar(out=sums[:, 2 * hh:2 * hh + 2], in0=pAc[hh][:, :, 64], scalar1=1e-30, scalar2=None, op0=ALU.max),
                         r=[pAc[hh]], w=[sums])
                S.op("dve", lambda: nc.vector.reciprocal(out=sums[:, 0:4], in_=sums[:, 0:4]), r=[sums], w=[sums])
                S.op("dve", lambda: nc.vector.tensor_tensor(out=coef[:, 0:4], in0=sums[:, 0:4], in1=gv[:, :, 0], op=ALU.mult), r=[sums, gsig], w=[coef])
                for h in range(4):
                    pa = pAc[h // 2]
                    if h == 0:
                        S.op("dve", lambda pa=pa, h=h: nc.vector.tensor_scalar(out=imp[:], in0=pa[:, h % 2, 65:129], scalar1=sums[:, h:h + 1], scalar2=None, op0=ALU.mult),
                             r=[pa, sums], w=[imp])
                    else:
                        S.op("dve", lambda pa=pa, h=h: nc.vector.scalar_tensor_tensor(out=imp[:], in0=pa[:, h % 2, 65:129], scalar=sums[:, h:h + 1], in1=imp[:],
                                                                                       op0=ALU.mult, op1=ALU.add), r=[pa, sums, imp], w=[imp])
                    S.op("dve", lambda pa=pa, h=h: nc.vector.tensor_scalar(out=oacc[:, h * 64:(h + 1) * 64], in0=pa[:, h % 2, 0:64], scalar1=coef[:, h:h + 1], scalar2=None, op0=ALU.mult),
                         r=[pa, coef], w=[oacc])
                S.op("dve", lambda: nc.vector.tensor_tensor(out=imp[:], in0=imp[:], in1=selc[0][:], op=ALU.mult), r=[imp, selc[0]], w=[imp])
                S.op("dve", lambda: nc.vector.tensor_tensor(out=imp[:], in0=imp[:], in1=selc[1][:], op=ALU.add), r=[imp, selc[1]], w=[imp])
                S.op("dve", lambda: nc.vector.max(out=m8[:, 0:8], in_=imp[:]), r=[imp], w=[m8])
                S.op("dve", lambda: nc.vector.match_replace(out=imp2[:], in_to_replace=m8[:, 0:8], in_values=imp[:], imm_value=-3.0e38), r=[imp, m8], w=[imp2])
                S.op("dve", lambda: nc.vector.max(out=m8[:, 8:16], in_=imp2[:]), r=[imp2], w=[m8])
                S.op("dve", lambda: nc.vector.tensor_tensor(out=imp2[:], in0=imp[:], in1=m8[:, 15:16].to_broadcast([128, 64]), op=ALU.is_ge), r=[imp, m8], w=[imp2])
                S.op("dve", lambda: nc.vector.tensor_tensor(out=imp2[:], in0=imp2[:], in1=selc[2][:], op=ALU.mult), r=[imp2, selc[2]], w=[imp2])
                S.op("dve", lambda: nc.vector.tensor_scalar(out=selb[:], in0=imp2[:], scalar1=-1.0, scalar2=None, op0=ALU.add), r=[imp2], w=[selb])
                S.op("pe", lambda: nc.tensor.transpose(pT[0:64, 0:128], selb[:, :], ident[:]), r=[selb, ident], w=[pT])
                S.op("act", lambda: nc.scalar.copy(out=selT[:], in_=pT[0:64, 0:128]), r=[pT], w=[selT])
                nkt = 17 + i

                def slc_tile(kt):
                    diag = (kt == nkt - 1)
                    offs = [-SLOPES[4 * g + h] * 128.0 * (nkt - 1 - kt) for h in range(4)]

                    def mfn(n):
                        j = 0
                        reg = pM[:, j * 128:(j + 1) * 128]
                        S.op("pe", lambda: nc.tensor.matmul(reg, lhsT=expbig[:, kt * 128:(kt + 1) * 128], rhs=selT[:, :], start=True, stop=not diag),
                             r=[expbig, selT], w=[PMK[j]])
                        if diag:
                            S.op("pe", lambda: nc.tensor.matmul(reg, lhsT=ident[:, :], rhs=tribias_bf[:, 0:128], start=False, stop=True), r=[ident, tribias_bf], w=[PMK[j]])
                        return reg.unsqueeze(1).to_broadcast([128, 4, 128]), PMK[j]

                    def pv(P):
                        for h in range(4):
                            S.op("pe", lambda h=h: nc.tensor.matmul(pAs[:, h, :], lhsT=P[:, h * 128:(h + 1) * 128], rhs=vs[:, kt, :],
                                                                     start=(kt == 0), stop=(kt == nkt - 1)), r=[P, vs], w=[pAs])
                    return tile_gen(ksT[:, kt * 128:(kt + 1) * 128], abias[:, :], mfn, offs, pv)

                run_pipelined([slc_tile(kt) for kt in range(nkt)], 2)
                for bi, pa in ((1, pAs), (2, pAw)):
                    S.op("dve", lambda bi=bi, pa=pa: nc.vector.tensor_scalar(out=sums[:, 4 * bi:4 * bi + 4], in0=pa[:, :, 64], scalar1=1e-30, scalar2=None, op0=ALU.max),
                         r=[pa], w=[sums])
                    S.op("dve", lambda bi=bi: nc.vector.reciprocal(out=sums[:, 4 * bi:4 * bi + 4], in_=sums[:, 4 * bi:4 * bi + 4]), r=[sums], w=[sums])
                    S.op("dve", lambda bi=bi: nc.vector.tensor_tensor(out=coef[:, 4 * bi:4 * bi + 4], in0=sums[:, 4 * bi:4 * bi + 4], in1=gv[:, :, bi], op=ALU.mult),
                         r=[sums, gsig], w=[coef])
                    for h in range(4):
                        S.op("dve", lambda bi=bi, pa=pa, h=h: nc.vector.scalar_tensor_tensor(out=oacc[:, h * 64:(h + 1) * 64], in0=pa[:, h, 0:64],
                                                                                              scalar=coef[:, 4 * bi + h:4 * bi + h + 1], in1=oacc[:, h * 64:(h + 1) * 64],
                                                                                              op0=ALU.mult, op1=ALU.add), r=[pa, coef, oacc], w=[oacc])
                S.op("act", lambda: nc.scalar.copy(out=obf[:], in_=oacc[:]), r=[oacc], w=[obf])
                for j in range(2):
                    S.op("pe", lambda j=j: nc.tensor.transpose(pT[:, 128 + j * 128:256 + j * 128], obf[:, j * 128:(j + 1) * 128], ident[:]), r=[obf, ident], w=[pT])
                S.op("act", lambda: nc.scalar.copy(out=ost[:], in_=pT[:, 128:384].rearrange("p (j q) -> p j q", j=2)), r=[pT], w=[ost])
                S.dma("sp", G.mixT[g * 256:(g + 1) * 256, q0:q0 + 128].rearrange("(j p) q -> p j q", p=128), ost[:], r=[ost], w=["mixT"])
        S.barrier(G.bar)


def phase_c(G):
    nc, S, I = G.nc, G.S, G.I
    with ExitStack() as st:
        sb = lambda n, s, d: st.enter_context(nc.sbuf_tensor(n, s, d))
        ps = lambda n, s, d: st.enter_context(nc.psum_tensor(n, s, d))
        identf = sb("c_identf", [128, 128], F32)
        mu = sb("c_mu", [128, 3520], F32)
        bc = {}
        for nm in ("rwkv_w0", "rwkv_a0", "rwkv_k_k", "rwkv_k_a"):
            bc[nm] = sb("c_" + nm, [128, 1024], F32)
        wup = sb("c_wup", [96, 1024], F32)
        aup = sb("c_aup", [96, 1024], F32)
        gup = sb("c_gup", [128, 2, 1024], F32)
        z = [sb(f"c_z{i}", [128, 3520], F32) for i in range(2)]
        zp = [sb(f"c_zp{i}", [128, 3520], F32) for i in range(2)]
        lT = sb("c_lT", [128, 4, 128], F32)
        wv = sb("c_wv", [128, 1024], F32)
        av = sb("c_av", [128, 1024], F32)
        gv = sb("c_gv", [128, 1024], F32)
        kk = sb("c_kk", [128, 1024], F32)
        sq = sb("c_sq", [128, 1024], F32)
        ss = sb("c_ss", [128, 16], F32)
        km = sb("c_km", [128, 1024], F32)
        bv = sb("c_bv", [128, 1024], F32)
        pT = ps("c_pT", [128, 512], F32)
        pW = [ps(f"c_pW{i}", [128, 512], F32) for i in range(6)]
        S.dma("sp", identf[:], I["c_ident"][:, :], w=[identf])
        S.dma("sp", mu[:], I["rwkv_mu"][0:1, :].partition_broadcast(128), w=[mu])
        for nm in bc:
            S.dma("sp", bc[nm][:], I[nm][0:1, :].partition_broadcast(128), w=[bc[nm]])
        S.dma("sp", wup[:], I["rwkv_w_up"][:, :], w=[wup])
        S.dma("sp", aup[:], I["rwkv_a_up"][:, :], w=[aup])
        S.dma("sp", gup[:], I["rwkv_g_up"].rearrange("(c p) n -> p c n", p=128), w=[gup])
        for tl in range(32):
            tok0 = tl * 128
            own = tl >= 16
            zt, zpt = z[tl % 2], zp[tl % 2]
            S.dma("sp", zt[:], G.zr[tok0:tok0 + 128, :], r=["zr"], w=[zt])
            if tl == 0:
                S.op("pool", lambda: nc.gpsimd.memset(zpt[0:1, :], 0.0), w=[zpt])
                S.dma("sp", zpt[1:128, :], G.zr[0:127, :], r=["zr", zpt], w=[zpt])
            else:
                S.dma("sp", zpt[:], G.zr[tok0 - 1:tok0 + 127, :], r=["zr"], w=[zpt])
            S.op("pool", lambda: nc.gpsimd.tensor_tensor(out=zpt[:], in0=zpt[:], in1=zt[:], op=ALU.subtract), r=[zpt, zt], w=[zpt])
            S.op("dve", lambda: nc.vector.tensor_tensor(out=zpt[:], in0=zpt[:], in1=mu[:], op=ALU.mult), r=[zpt, mu], w=[zpt])
            S.op("pool", lambda: nc.gpsimd.tensor_tensor(out=zt[:], in0=zt[:], in1=zpt[:], op=ALU.add), r=[zpt, zt], w=[zt])
            r_ = zt[:, 0:1024]
            k_ = zt[:, 1024:2048]
            v_ = zt[:, 2048:3072]
            S.op("pe", lambda: nc.tensor.transpose(pT[0:96, 0:128], zt[:, 3072:3168], identf[:]), r=[zt, identf], w=[pT])
            S.op("pe", lambda: nc.tensor.transpose(pT[0:96, 128:256], zt[:, 3168:3264], identf[:]), r=[zt, identf], w=[pT])
            S.op("act", lambda: nc.scalar.activation(out=lT[0:96, 0, :], in_=pT[0:96, 0:128], func=AF.Tanh), r=[pT], w=[lT])
            S.op("dve", lambda: nc.vector.tensor_copy(out=lT[0:96, 1, :], in_=pT[0:96, 128:256]), r=[pT], w=[lT])
            if own:
                for j in range(2):
                    S.op("pe", lambda j=j: nc.tensor.transpose(pT[:, 256 + j * 128:384 + j * 128], zt[:, 3264 + j * 128:3392 + j * 128], identf[:]), r=[zt, identf], w=[pT])
                S.op("act", lambda: nc.scalar.activation(out=lT[:, 2:4, :], in_=pT[:, 256:512].rearrange("p (a b) -> p a b", a=2), func=AF.Sigmoid), r=[pT], w=[lT])
            for hh in range(2):
                cs = slice(hh * 512, (hh + 1) * 512)
                S.op("pe", lambda hh=hh, cs=cs: nc.tensor.matmul(pW[hh][:, :], lhsT=lT[0:96, 0, :], rhs=wup[:, cs], start=True, stop=True), r=[lT, wup], w=[pW[hh]])
                S.op("pe", lambda hh=hh, cs=cs: nc.tensor.matmul(pW[2 + hh][:, :], lhsT=lT[0:96, 1, :], rhs=aup[:, cs], start=True, stop=True), r=[lT, aup], w=[pW[2 + hh]])
                S.op("dve", lambda hh=hh, cs=cs: nc.vector.tensor_tensor(out=wv[:, cs], in0=pW[hh][:, :], in1=bc["rwkv_w0"][:, cs], op=ALU.add), r=[pW[hh], bc["rwkv_w0"]], w=[wv])
                S.op("dve", lambda hh=hh, cs=cs: nc.vector.tensor_tensor(out=av[:, cs], in0=pW[2 + hh][:, :], in1=bc["rwkv_a0"][:, cs], op=ALU.add), r=[pW[2 + hh], bc["rwkv_a0"]], w=[av])
                if own:
                    for j in range(2):
                        S.op("pe", lambda hh=hh, cs=cs, j=j: nc.tensor.matmul(pW[4 + hh][:, :], lhsT=lT[:, 2 + j, :], rhs=gup[:, j, cs], start=(j == 0), stop=(j == 1)),
                             r=[lT, gup], w=[pW[4 + hh]])
                    S.op("act", lambda hh=hh, cs=cs: nc.scalar.copy(out=gv[:, cs], in_=pW[4 + hh][:, :]), r=[pW[4 + hh]], w=[gv])
            S.op("act", lambda: nc.scalar.activation(out=wv[:], in_=wv[:], func=AF.Sigmoid), r=[wv], w=[wv])
            S.op("pool", lambda: nc.gpsimd.tensor_scalar(out=wv[:], in0=wv[:], scalar1=-0.6065306597126334, scalar2=None, op0=ALU.mult), r=[wv], w=[wv])
            S.op("act", lambda: nc.scalar.activation(out=av[:], in_=av[:], func=AF.Sigmoid), r=[av], w=[av])
            S.op("dve", lambda: nc.vector.tensor_tensor(out=kk[:], in0=k_, in1=bc["rwkv_k_k"][:], op=ALU.mult), r=[zt, bc["rwkv_k_k"]], w=[kk])
            S.op("pool", lambda: nc.gpsimd.tensor_tensor(out=sq[:], in0=kk[:], in1=kk[:], op=ALU.mult), r=[kk], w=[sq])
            S.op("dve", lambda: nc.vector.tensor_reduce(out=ss[:], in_=sq[:].rearrange("p (h k) -> p h k", k=64), axis=AX.X, op=ALU.add), r=[sq], w=[ss])
            S.op("act", lambda: nc.scalar.activation(out=ss[:], in_=ss[:], func=AF.Sqrt), r=[ss], w=[ss])
            S.op("dve", lambda: nc.vector.tensor_scalar(out=ss[:], in0=ss[:], scalar1=1e-12, scalar2=None, op0=ALU.max), r=[ss], w=[ss])
            S.op("dve", lambda: nc.vector.reciprocal(out=ss[:], in_=ss[:]), r=[ss], w=[ss])
            S.op("dve", lambda: nc.vector.tensor_tensor(out=kk[:].rearrange("p (h k) -> p h k", k=64), in0=kk[:].rearrange("p (h k) -> p h k", k=64),
                                                        in1=ss[:].unsqueeze(2).to_broadcast([128, 16, 64]), op=ALU.mult), r=[kk, ss], w=[kk])
            S.op("dve", lambda: nc.vector.scalar_tensor_tensor(out=km[:], in0=av[:], scalar=-1.0, in1=bc["rwkv_k_a"][:], op0=ALU.add, op1=ALU.mult), r=[av, bc["rwkv_k_a"]], w=[km])
            S.op("dve", lambda: nc.vector.scalar_tensor_tensor(out=km[:], in0=km[:], scalar=1.0, in1=k_, op0=ALU.add, op1=ALU.mult), r=[km, zt], w=[km])
            S.op("pool", lambda: nc.gpsimd.tensor_tensor(out=bv[:], in0=kk[:], in1=av[:], op=ALU.mult), r=[kk, av], w=[bv])
            rows = slice(tok0, tok0 + 128)
            S.dma("sp", G.rw_r[rows, :], r_, r=[zt], w=["rw_r"])
            S.dma("sp", G.rw_v[rows, :], v_, r=[zt], w=["rw_v"])
            S.dma("sp", G.rw_k[rows, :], km[:], r=[km], w=["rw_k"])
            S.dma("sp", G.rw_kn[rows, :], kk[:], r=[kk], w=["rw_kn"])
            S.dma("sp", G.rw_b[rows, :], bv[:], r=[bv], w=["rw_b"])
            S.dma("sp", G.rw_lw[rows, :], wv[:], r=[wv], w=["rw_lw"])
            if own:
                S.dma("sp", G.rw_g[tok0 - 2048:tok0 - 1920, :], gv[:], r=[gv], w=["rw_g"])
        S.barrier(G.bar)
    with ExitStack() as st:
        sb = lambda n, s, d: st.enter_context(nc.sbuf_tensor(n, s, d))
        ps = lambda n, s, d: st.enter_context(nc.psum_tensor(n, s, d))
        crw = sb("s_crw", [64, 448], F32)
        MM = sb("s_MM", [64, 320], F32)
        srcs = [G.rw_lw, G.rw_kn, G.rw_r, G.rw_b, G.rw_k, G.rw_v]
        keys = ["rw_lw", "rw_kn", "rw_r", "rw_b", "rw_k", "rw_v"]
        blk = [[[sb(f"s_in{p}_{hh}_{a}", [64, 8, 64], F32) for a in range(6)] for hh in range(4)] for p in range(2)]
        Hs = [[sb(f"s_H{h}_{p}", [64, 64], F32) for p in range(2)] for h in range(16)]
        NW = 4
        E = [sb(f"s_E{i}", [64, 256], F32) for i in range(NW)]
        FM = [sb(f"s_FM{i}", [64, 256], F32) for i in range(NW)]
        BK = [sb(f"s_BK{i}", [64, 128], F32) for i in range(NW)]
        GM = [sb(f"s_GM{i}", [64, 320], F32) for i in range(NW)]
        XX = [[sb(f"s_XX{i}_{p}", [64, 128], F32) for p in range(2)] for i in range(NW)]
        P2 = [[sb(f"s_P2{i}_{p}", [64, 128], F32) for p in range(2)] for i in range(NW)]
        RU = [sb(f"s_RU{i}", [64, 64], F32) for i in range(NW)]
        U = [sb(f"s_U{i}", [64, 64], F32) for i in range(NW)]
        ybuf = [[sb(f"s_y{p}_{hh}", [64, 8, 64], F32) for hh in range(4)] for p in range(2)]
        pA = ps("s_pA", [64, 256], F32)
        pB = ps("s_pB", [64, 256], F32)
        pC = ps("s_pC", [64, 320], F32)
        pD = ps("s_pD", [64, 128], F32)
        pE = ps("s_pE", [64, 128], F32)
        pF = ps("s_pF", [64, 128], F32)
        pG = ps("s_pG", [64, 64], F32)
        pH = ps("s_pH", [64, 64], F32)
        S.dma("sp", crw[:], I["c_rw"][:, :], w=[crw])
        S.op("dve", lambda: nc.vector.tensor_copy(out=MM[:, 0:128], in_=crw[:, 128:256]), r=[crw], w=[MM])
        S.op("dve", lambda: nc.vector.tensor_copy(out=MM[:, 128:320], in_=crw[:, 128:320]), r=[crw, MM], w=[MM])
        TB = crw[:, 0:128]
        I64 = crw[:, 320:384]
        INC = crw[:, 384:448]
        for h in range(16):
            S.op("pool", lambda h=h: nc.gpsimd.memset(Hs[h][0][:], 0.0), w=[Hs[h][0]])
        nwk = 0
        for hg in range(4):
            for b8 in range(8):
                par = (hg * 8 + b8) % 2
                for hh in range(4):
                    h = hg * 4 + hh
                    for a in range(6):
                        S.dma("sp", blk[par][hh][a][:], srcs[a][b8 * 512:(b8 + 1) * 512, h * 64:(h + 1) * 64].rearrange("(c t) k -> t c k", t=64),
                              r=[keys[a]], w=[blk[par][hh][a]])
                own = b8 >= 4
                for c in range(8):
                    cg = b8 * 8 + c
                    for hh in range(4):
                        h = hg * 4 + hh
                        LW, KN, R, B, K, V = [blk[par][hh][a][:, c, :] for a in range(6)]
                        tl = blk[par][hh]
                        w_ = nwk % NW
                        nwk += 1
                        e_, fm, bk, gm, ru, u_ = E[w_], FM[w_], BK[w_], GM[w_], RU[w_], U[w_]
                        Hc, Hn = Hs[h][cg % 2], Hs[h][(cg + 1) % 2]
                        S.op("pe", lambda: nc.tensor.matmul(pA[:, 0:128], lhsT=LW, rhs=TB, start=True, stop=True), r=[tl[0], crw], w=[pA])
                        S.op("pe", lambda: nc.tensor.matmul(pA[:, 128:192], lhsT=INC, rhs=LW, start=True, stop=True), r=[tl[0], crw], w=[pA])
                        S.op("act", lambda: nc.scalar.activation(out=e_[:, 0:128], in_=pA[:, 0:128], func=AF.Exp), r=[pA], w=[e_])
                        S.op("act", lambda: nc.scalar.activation(out=e_[:, 128:256].rearrange("p (a b) -> p a b", a=2),
                                                                 in_=pA[:, 0:256].rearrange("p (a b) -> p a b", a=2)[:, :, 0:64], func=AF.Exp, scale=-1.0), r=[pA], w=[e_])
                        for j, (src, ti) in enumerate(((KN, 1), (R, 2), (B, 3), (K, 4))):
                            S.op("pe", lambda j=j, src=src: nc.tensor.transpose(pB[:, j * 64:(j + 1) * 64], src, I64), r=[tl[ti], crw], w=[pB])
                        S.op("dve", lambda: nc.vector.scalar_tensor_tensor(out=fm[:, 0:64], in0=pB[:, 0:64], scalar=-1.0, in1=e_[:, 64:128], op0=ALU.mult, op1=ALU.mult),
                             r=[pB, e_], w=[fm])
                        S.op("dve", lambda: nc.vector.tensor_tensor(out=fm[:, 64:128], in0=pB[:, 64:128], in1=e_[:, 0:64], op=ALU.mult), r=[pB, e_, fm], w=[fm])
                        S.op("dve", lambda: nc.vector.tensor_tensor(out=fm[:, 128:256].rearrange("p (a b) -> p a b", a=2), in0=pB[:, 128:256].rearrange("p (a b) -> p a b", a=2),
                                                                    in1=e_[:, 128:192].unsqueeze(1).to_broadcast([64, 2, 64]), op=ALU.mult), r=[pB, e_, fm], w=[fm])
                        S.op("pool", lambda: nc.gpsimd.tensor_tensor(out=bk[:, 0:64], in0=B, in1=e_[:, 192:256], op=ALU.mult), r=[tl[3], e_], w=[bk])
                        S.op("pool", lambda: nc.gpsimd.tensor_tensor(out=bk[:, 64:128], in0=K, in1=e_[:, 192:256], op=ALU.mult), r=[tl[4], e_, bk], w=[bk])
                        AT, RT, BT, KT = fm[:, 0:64], fm[:, 64:128], fm[:, 128:192], fm[:, 192:256]
                        S.op("pe", lambda: nc.tensor.matmul(pC[:, 0:128], lhsT=BT, rhs=fm[:, 0:128], start=True, stop=True), r=[fm], w=[pC])
                        S.op("pe", lambda: nc.tensor.matmul(pC[:, 128:256], lhsT=KT, rhs=fm[:, 0:128], start=True, stop=True), r=[fm], w=[pC])
                        S.op("pe", lambda: nc.tensor.matmul(pC[:, 256:320], lhsT=AT, rhs=BT, start=True, stop=True), r=[fm], w=[pC])
                        S.op("dve", lambda: nc.vector.tensor_tensor(out=gm[:], in0=pC[:, :], in1=MM[:], op=ALU.mult), r=[pC, MM], w=[gm])
                        N_, MrbT, LakT, MrkT, NT = gm[:, 0:64], gm[:, 64:128], gm[:, 128:192], gm[:, 192:256], gm[:, 256:320]
                        xx = XX[w_]
                        p2 = P2[w_]
                        S.op("pool", lambda: nc.gpsimd.tensor_tensor(out=xx[0][:, 0:64], in0=N_, in1=I64, op=ALU.add), r=[gm, crw], w=[xx[0]])
                        S.op("pool", lambda: nc.gpsimd.tensor_tensor(out=xx[0][:, 64:128], in0=NT, in1=I64, op=ALU.add), r=[gm, crw, xx[0]], w=[xx[0]])
                        Pc, PTc, pk = N_, NT, gm
                        for k in range(5):
                            last = (k == 4)
                            pn = p2[k % 2]
                            S.op("pe", lambda Pc=Pc, PTc=PTc: nc.tensor.matmul(pD[:, 0:64], lhsT=PTc, rhs=Pc, start=True, stop=True), r=[pk], w=[pD])
                            if not last:
                                S.op("pe", lambda Pc=Pc, PTc=PTc: nc.tensor.matmul(pD[:, 64:128], lhsT=Pc, rhs=PTc, start=True, stop=True), r=[pk], w=[pD])
                                S.op("act", lambda pn=pn: nc.scalar.copy(out=pn[:], in_=pD[:, :]), r=[pD], w=[pn])
                            else:
                                S.op("act", lambda pn=pn: nc.scalar.copy(out=pn[:, 0:64], in_=pD[:, 0:64]), r=[pD], w=[pn])
                            xc_, xn_ = xx[k % 2], xx[(k + 1) % 2]
                            S.op("pe", lambda xc_=xc_, pn=pn: nc.tensor.matmul(pE[:, 0:64], lhsT=xc_[:, 64:128], rhs=pn[:, 0:64], start=True, stop=True), r=[xc_, pn], w=[pE])
                            if not last:
                                S.op("pe", lambda xc_=xc_, pn=pn: nc.tensor.matmul(pE[:, 64:128], lhsT=pn[:, 0:64], rhs=xc_[:, 64:128], start=True, stop=True), r=[xc_, pn], w=[pE])
                                S.op("dve", lambda xc_=xc_, xn_=xn_: nc.vector.tensor_tensor(out=xn_[:], in0=pE[:, :], in1=xc_[:], op=ALU.add), r=[pE, xc_], w=[xn_])
                            else:
                                S.op("dve", lambda xc_=xc_, xn_=xn_: nc.vector.tensor_tensor(out=xn_[:, 0:64], in0=pE[:, 0:64], in1=xc_[:, 0:64], op=ALU.add), r=[pE, xc_], w=[xn_])
                            Pc, PTc, pk = pn[:, 0:64], pn[:, 64:128], pn
                        X = xx[1][:, 0:64]
                        xk = xx[1]
                        S.op("pe", lambda: nc.tensor.matmul(pF[:, 0:64], lhsT=AT, rhs=Hc[:], start=True, stop=False), r=[fm, Hc], w=[pF])
                        S.op("pe", lambda: nc.tensor.matmul(pF[:, 0:64], lhsT=LakT, rhs=V, start=False, stop=True), r=[gm, tl[5]], w=[pF])
                        S.op("act", lambda: nc.scalar.copy(out=ru[:], in_=pF[:, 0:64]), r=[pF], w=[ru])
                        S.op("pe", lambda: nc.tensor.matmul(pF[:, 64:128], lhsT=X, rhs=ru[:], start=True, stop=True), r=[xk, ru], w=[pF])
                        S.op("dve", lambda: nc.vector.tensor_copy(out=u_[:], in_=pF[:, 64:128]), r=[pF], w=[u_])
                        if own:
                            yb = ybuf[par][hh]
                            S.op("pe", lambda: nc.tensor.matmul(pG[:, :], lhsT=RT, rhs=Hc[:], start=True, stop=False), r=[fm, Hc], w=[pG])
                            S.op("pe", lambda: nc.tensor.matmul(pG[:, :], lhsT=MrbT, rhs=u_[:], start=False, stop=False), r=[gm, u_], w=[pG])
                            S.op("pe", lambda: nc.tensor.matmul(pG[:, :], lhsT=MrkT, rhs=V, start=False, stop=True), r=[gm, tl[5]], w=[pG])
                            S.op("act", lambda: nc.scalar.copy(out=yb[:, c, :], in_=pG[:, :]), r=[pG], w=[yb])
                        S.op("pe", lambda: nc.tensor.matmul(pH[:, :], lhsT=I64, rhs=Hc[:], start=True, stop=False), r=[crw, Hc], w=[pH])
                        S.op("pe", lambda: nc.tensor.matmul(pH[:, :], lhsT=bk[:, 0:64], rhs=u_[:], start=False, stop=False), r=[bk, u_], w=[pH])
                        S.op("pe", lambda: nc.tensor.matmul(pH[:, :], lhsT=bk[:, 64:128], rhs=V, start=False, stop=True), r=[bk, tl[5]], w=[pH])
                        S.op("dve", lambda: nc.vector.tensor_scalar(out=Hn[:], in0=pH[:, :], scalar1=e_[:, 63:64], scalar2=None, op0=ALU.mult), r=[pH, e_], w=[Hn])
                if own:
                    for hh in range(4):
                        h = hg * 4 + hh
                        r0 = (b8 - 4) * 512
                        S.dma("sp", G.rw_y[r0:r0 + 512, h * 64:(h + 1) * 64].rearrange("(c t) k -> t c k", t=64), ybuf[par][hh][:], r=[ybuf[par][hh]], w=["rw_y"])
        S.barrier(G.bar)
    with ExitStack() as st:
        sb = lambda n, s, d: st.enter_context(nc.sbuf_tensor(n, s, d))
        ps = lambda n, s, d: st.enter_context(nc.psum_tensor(n, s, d))
        ident = sb("p_ident", [128, 128], BF16)
        bc = {}
        for nm in ("rwkv_ln_g", "rwkv_ln_b", "rwkv_r_k"):
            bc[nm] = sb("p_" + nm, [128, 1024], F32)
            S.dma("sp", bc[nm][:], I[nm][0:1, :].partition_broadcast(128), w=[bc[nm]])
        S.dma("pool", ident[:], I["c_ident"][:, :], w=[ident])
        y = [sb(f"p_y{i}", [128, 1024], F32) for i in range(2)]
        rr = [sb(f"p_r{i}", [128, 1024], F32) for i in range(2)]
        kq = [sb(f"p_k{i}", [128, 1024], F32) for i in range(2)]
        vv = [sb(f"p_v{i}", [128, 1024], F32) for i in range(2)]
        gg = [sb(f"p_g{i}", [128, 1024], F32) for i in range(2)]
        sq = sb("p_sq", [128, 1024], F32)
        st1 = sb("p_st1", [128, 16], F32)
        st2 = sb("p_st2", [128, 16], F32)
        st3 = sb("p_st3", [128, 16], F32)
        obf = sb("p_obf", [128, 1024], BF16)
        ost = sb("p_ost", [128, 8, 128], BF16)
        pT = ps("p_pT", [128, 1024], BF16)
        v3 = lambda t: t[:].rearrange("p (h k) -> p h k", k=64)
        b3 = lambda t: t[:].unsqueeze(2).to_broadcast([128, 16, 64])
        for tl in range(16):
            p = tl % 2
            rows = slice(tl * 128, (tl + 1) * 128)
            crow = slice(2048 + tl * 128, 2048 + (tl + 1) * 128)
            yt, rt, kt, vt, gt = y[p], rr[p], kq[p], vv[p], gg[p]
            S.dma("sp", yt[:], G.rw_y[rows, :], r=["rw_y"], w=[yt])
            S.dma("sp", rt[:], G.rw_r[crow, :], r=["rw_r"], w=[rt])
            S.dma("sp", kt[:], G.rw_k[crow, :], r=["rw_k"], w=[kt])
            S.dma("sp", vt[:], G.rw_v[crow, :], r=["rw_v"], w=[vt])
            S.dma("sp", gt[:], G.rw_g[rows, :], r=["rw_g"], w=[gt])
            S.op("dve", lambda: nc.vector.tensor_reduce(out=st1[:], in_=v3(yt), axis=AX.X, op=ALU.add), r=[yt], w=[st1])
            S.op("pool", lambda: nc.gpsimd.tensor_tensor(out=sq[:], in0=yt[:], in1=yt[:], op=ALU.mult), r=[yt], w=[sq])
            S.op("dve", lambda: nc.vector.tensor_reduce(out=st2[:], in_=v3(sq), axis=AX.X, op=ALU.add), r=[sq], w=[st2])
            S.op("dve", lambda: nc.vector.tensor_scalar(out=st1[:], in0=st1[:], scalar1=1.0 / 64.0, scalar2=None, op0=ALU.mult), r=[st1], w=[st1])
            S.op("dve", lambda: nc.vector.tensor_tensor(out=st3[:], in0=st1[:], in1=st1[:], op=ALU.mult), r=[st1], w=[st3])
            S.op("dve", lambda: nc.vector.scalar_tensor_tensor(out=st2[:], in0=st2[:], scalar=1.0 / 64.0, in1=st3[:], op0=ALU.mult, op1=ALU.subtract), r=[st2, st3], w=[st2])
            S.op("dve", lambda: nc.vector.tensor_scalar(out=st2[:], in0=st2[:], scalar1=64e-5, scalar2=None, op0=ALU.add), r=[st2], w=[st2])
            S.op("act", lambda: nc.scalar.activation(out=st2[:], in_=st2[:], func=AF.Sqrt), r=[st2], w=[st2])
            S.op("dve", lambda: nc.vector.reciprocal(out=st2[:], in_=st2[:]), r=[st2], w=[st2])
            S.op("dve", lambda: nc.vector.tensor_tensor(out=v3(yt), in0=v3(yt), in1=b3(st1), op=ALU.subtract), r=[yt, st1], w=[yt])
            S.op("dve", lambda: nc.vector.tensor_tensor(out=v3(yt), in0=v3(yt), in1=b3(st2), op=ALU.mult), r=[yt, st2], w=[yt])
            S.op("pool", lambda: nc.gpsimd.tensor_tensor(out=yt[:], in0=yt[:], in1=bc["rwkv_ln_g"][:], op=ALU.mult), r=[yt, bc["rwkv_ln_g"]], w=[yt])
            S.op("pool", lambda: nc.gpsimd.tensor_tensor(out=yt[:], in0=yt[:], in1=bc["rwkv_ln_b"][:], op=ALU.add), r=[yt, bc["rwkv_ln_b"]], w=[yt])
            S.op("pool", lambda: nc.gpsimd.tensor_tensor(out=rt[:], in0=rt[:], in1=kt[:], op=ALU.mult), r=[rt, kt], w=[rt])
            S.op("pool", lambda: nc.gpsimd.tensor_tensor(out=rt[:], in0=rt[:], in1=bc["rwkv_r_k"][:], op=ALU.mult), r=[rt, bc["rwkv_r_k"]], w=[rt])
            S.op("dve", lambda: nc.vector.tensor_reduce(out=st3[:], in_=v3(rt), axis=AX.X, op=ALU.add), r=[rt], w=[st3])
            S.op("dve", lambda: nc.vector.tensor_tensor(out=v3(vt), in0=v3(vt), in1=b3(st3), op=ALU.mult), r=[vt, st3], w=[vt])
            S.op("pool", lambda: nc.gpsimd.tensor_tensor(out=yt[:], in0=yt[:], in1=vt[:], op=ALU.add), r=[yt, vt], w=[yt])
            S.op("dve", lambda: nc.vector.tensor_tensor(out=obf[:], in0=yt[:], in1=gt[:], op=ALU.mult), r=[yt, gt], w=[obf])
            for j in range(8):
                S.op("pe", lambda j=j: nc.tensor.transpose(pT[:, j * 128:(j + 1) * 128], obf[:, j * 128:(j + 1) * 128], ident[:]), r=[obf, ident], w=[pT])
            S.op("act", lambda: nc.scalar.copy(out=ost[:], in_=pT[:, :].rearrange("p (j q) -> p j q", j=8)), r=[pT], w=[ost])
            S.dma("sp", G.mixT[1024:2048, tl * 128:(tl + 1) * 128].rearrange("(j p) q -> p j q", p=128), ost[:], r=[ost], w=["mixT"])
        S.barrier(G.bar)


def layer_norm_tile(G, x, g_bc, b_bc, sq, st, eps=1e-5):
    nc, S = G.nc, G.S
    S.op("dve", lambda: nc.vector.tensor_reduce(out=st[:, 0:1], in_=x[:], axis=AX.X, op=ALU.add), r=[x], w=[st])
    S.op("pool", lambda: nc.gpsimd.tensor_tensor(out=sq[:], in0=x[:], in1=x[:], op=ALU.mult), r=[x], w=[sq])
    S.op("dve", lambda: nc.vector.tensor_reduce(out=st[:, 1:2], in_=sq[:], axis=AX.X, op=ALU.add), r=[sq, st], w=[st])
    S.op("dve", lambda: nc.vector.tensor_scalar(out=st[:, 0:2], in0=st[:, 0:2], scalar1=1.0 / D, scalar2=None, op0=ALU.mult), r=[st], w=[st])
    S.op("dve", lambda: nc.vector.tensor_tensor(out=st[:, 2:3], in0=st[:, 0:1], in1=st[:, 0:1], op=ALU.mult), r=[st], w=[st])
    S.op("dve", lambda: nc.vector.tensor_tensor(out=st[:, 1:2], in0=st[:, 1:2], in1=st[:, 2:3], op=ALU.subtract), r=[st], w=[st])
    S.op("dve", lambda: nc.vector.tensor_scalar(out=st[:, 1:2], in0=st[:, 1:2], scalar1=eps, scalar2=None, op0=ALU.add), r=[st], w=[st])
    S.op("act", lambda: nc.scalar.activation(out=st[:, 1:2], in_=st[:, 1:2], func=AF.Sqrt), r=[st], w=[st])
    S.op("dve", lambda: nc.vector.reciprocal(out=st[:, 1:2], in_=st[:, 1:2]), r=[st], w=[st])
    S.op("dve", lambda: nc.vector.tensor_scalar(out=x[:], in0=x[:], scalar1=st[:, 0:1], scalar2=st[:, 1:2], op0=ALU.subtract, op1=ALU.mult), r=[x, st], w=[x])
    S.op("pool", lambda: nc.gpsimd.tensor_tensor(out=x[:], in0=x[:], in1=g_bc[:], op=ALU.mult), r=[x, g_bc], w=[x])
    S.op("pool", lambda: nc.gpsimd.tensor_tensor(out=x[:], in0=x[:], in1=b_bc[:], op=ALU.add), r=[x, b_bc], w=[x])


def phase_d(G):
    nc, S, I = G.nc, G.S, G.I
    with ExitStack() as st:
        sb = lambda n, s, d: st.enter_context(nc.sbuf_tensor(n, s, d))
        ps = lambda n, s, d: st.enter_context(nc.psum_tensor(n, s, d))
        ident = sb("d_ident", [128, 128], BF16)
        identf = sb("d_identf", [128, 128], F32)
        mixT = sb("d_mixT", [128, 16, TO], BF16)
        wout = sb("d_wout", [128, 16, D], BF16)
        gbc = sb("d_g", [128, D], F32)
        bbc = sb("d_b", [128, D], F32)
        rw = sb("d_rw", [128, 16, 32], F32)
        rb = sb("d_rb", [128, 32], F32)
        xt = [sb(f"d_x{i}", [128, D], F32) for i in range(2)]
        hp = [sb(f"d_hp{i}", [128, D], F32) for i in range(2)]
        sq = sb("d_sq", [128, D], F32)
        stt = sb("d_st", [128, 4], F32)
        hbf = sb("d_hbf", [128, D], BF16)
        hTs = sb("d_hTs", [128, 16, 128], BF16)
        hT32 = sb("d_hT32", [128, 16, 128], F32)
        lg = sb("d_lg", [128, 32], F32)
        ex = sb("d_ex", [128, 32], F32)
        m8 = sb("d_m8", [128, 8], F32)
        sm = sb("d_sm", [128, 2], F32)
        pO = [ps(f"d_pO{i}", [128, 512], F32) for i in range(4)]
        pTf = [ps(f"d_pTf{i}", [128, 512], F32) for i in range(2)]
        pL = ps("d_pL", [128, 32], F32)
        pTb = ps("d_pTb", [128, 1024], BF16)
        S.dma("pool", ident[:], I["c_ident"][:, :], w=[ident])
        S.dma("sp", identf[:], I["c_ident"][:, :], w=[identf])
        mv = G.mixT.rearrange("(kc p) t -> p kc t", p=128)
        wv = I["w_out"].rearrange("(kc p) c -> p kc c", p=128)
        for j in range(4):
            S.dma("sp", mixT[:, j * 4:(j + 1) * 4, :], mv[:, j * 4:(j + 1) * 4, :], r=["mixT"], w=[mixT])
            S.dma("pool", wout[:, :, j * 512:(j + 1) * 512], wv[:, :, j * 512:(j + 1) * 512], w=[wout])
        S.dma("sp", gbc[:], I["ln1_g"][0:1, :].partition_broadcast(128), w=[gbc])
        S.dma("sp", bbc[:], I["ln1_b"][0:1, :].partition_broadcast(128), w=[bbc])
        S.dma("sp", rw[:], I["router_w"].rearrange("(kc p) e -> p kc e", p=128), w=[rw])
        S.dma("sp", rb[:], I["router_b"][0:1, :].partition_broadcast(128), w=[rb])
        for tl in range(16):
            x_ = xt[tl % 2]
            h_ = hp[tl % 2]
            rows = slice(tl * 128, (tl + 1) * 128)
            S.dma("sp", x_[:], I["xc"][2048 + tl * 128:2048 + (tl + 1) * 128, :], w=[x_])
            for c4 in range(4):
                for kc in range(16):
                    S.op("pe", lambda c4=c4, kc=kc: nc.tensor.matmul(pO[c4][:, :], lhsT=mixT[:, kc, tl * 128:(tl + 1) * 128], rhs=wout[:, kc, c4 * 512:(c4 + 1) * 512],
                                                                       start=(kc == 0), stop=(kc == 15)), r=[mixT, wout], w=[pO[c4]])
                S.op("dve", lambda c4=c4: nc.vector.scalar_tensor_tensor(out=h_[:, c4 * 512:(c4 + 1) * 512], in0=x_[:, c4 * 512:(c4 + 1) * 512], scalar=ALPHA, in1=pO[c4][:, :],
                                                                          op0=ALU.mult, op1=ALU.add), r=[x_, pO[c4]], w=[h_])
            layer_norm_tile(G, h_, gbc, bbc, sq, stt)
            S.dma("sp", G.h1[rows, :], h_[:], r=[h_], w=["h1"])
            S.op("act", lambda: nc.scalar.copy(out=hbf[:], in_=h_[:]), r=[h_], w=[hbf])
            for j in range(2):
                for k8 in range(8):
                    kc = j * 8 + k8
                    S.op("pe", lambda k8=k8, kc=kc: nc.tensor.transpose(pTb[:, k8 * 128:(k8 + 1) * 128], hbf[:, kc * 128:(kc + 1) * 128], ident[:]), r=[hbf, ident], w=[pTb])
                evac(G, hTs[:, j * 8:(j + 1) * 8, :], pTb[:].rearrange("p (a b) -> p a b", a=8), r=[pTb], w=[hTs])
            S.dma("sp", G.h1T[:, rows].rearrange("(kc p) t -> p kc t", p=128), hTs[:], r=[hTs], w=["h1T"])
            for j in range(4):
                pt = pTf[j % 2]
                for k4 in range(4):
                    kc = j * 4 + k4
                    S.op("pe", lambda k4=k4, kc=kc, pt=pt: nc.tensor.transpose(pt[:, k4 * 128:(k4 + 1) * 128], h_[:, kc * 128:(kc + 1) * 128], identf[:]), r=[h_, identf], w=[pt])
                evac(G, hT32[:, j * 4:(j + 1) * 4, :], pt[:].rearrange("p (a b) -> p a b", a=4), r=[pt], w=[hT32])
            for kc in range(16):
                S.op("pe", lambda kc=kc: nc.tensor.matmul(pL[:, :], lhsT=hT32[:, kc, :], rhs=rw[:, kc, :], start=(kc == 0), stop=(kc == 15)), r=[hT32, rw], w=[pL])
            S.op("dve", lambda: nc.vector.tensor_tensor(out=lg[:], in0=pL[:, :], in1=rb[:], op=ALU.add), r=[pL, rb], w=[lg])
            S.op("dve", lambda: nc.vector.max(out=m8[:], in_=lg[:]), r=[lg], w=[m8])
            S.op("dve", lambda: nc.vector.tensor_scalar(out=sm[:, 0:1], in0=m8[:, 0:1], scalar1=-1.0, scalar2=None, op0=ALU.mult), r=[m8], w=[sm])
            S.op("act", lambda: nc.scalar.activation(out=ex[:], in_=lg[:], func=AF.Exp, bias=sm[:, 0:1], scale=1.0), r=[lg, sm], w=[ex])
            S.op("dve", lambda: nc.vector.tensor_tensor(out=lg[:], in0=lg[:], in1=m8[:, 3:4].to_broadcast([128, 32]), op=ALU.is_ge), r=[lg, m8], w=[lg])
            S.op("dve", lambda: nc.vector.tensor_tensor(out=ex[:], in0=ex[:], in1=lg[:], op=ALU.mult), r=[ex, lg], w=[ex])
            S.op("dve", lambda: nc.vector.tensor_reduce(out=sm[:, 1:2], in_=ex[:], axis=AX.X, op=ALU.add), r=[ex, sm], w=[sm])
            S.op("dve", lambda: nc.vector.reciprocal(out=sm[:, 1:2], in_=sm[:, 1:2]), r=[sm], w=[sm])
            S.op("dve", lambda: nc.vector.tensor_scalar(out=ex[:], in0=ex[:], scalar1=sm[:, 1:2], scalar2=None, op0=ALU.mult), r=[ex, sm], w=[ex])
            S.dma("sp", G.gw[rows, :], ex[:], r=[ex], w=["gw"])
        S.barrier(G.bar)


def phase_e(G, experts=32):
    nc, S, I = G.nc, G.S, G.I
    LIM = 7.0
    with ExitStack() as st:
        sb = lambda n, s, d: st.enter_context(nc.sbuf_tensor(n, s, d))
        ps = lambda n, s, d: st.enter_context(nc.psum_tensor(n, s, d))
        identf = sb("e_identf", [128, 128], F32)
        h1T = sb("e_h1T", [128, 16, 1024], BF16)
        Y = sb("e_Y", [128, 8, D], F32)
        gw = sb("e_gw", [128, 8, 32], F32)
        gwT = sb("e_gwT", [32, 8, 128], F32)
        bdn = sb("e_bdn", [32, D], F32)
        bg = sb("e_bg", [128, 512], F32)
        bu = sb("e_bu", [128, 512], F32)
        wg = [sb(f"e_wg{i}", [128, 16, 256], BF16) for i in range(2)]
        wu = [sb(f"e_wu{i}", [128, 16, 256], BF16) for i in range(2)]
        wd = [sb(f"e_wd{i}", [128, 2, D], BF16) for i in range(2)]
        hT = [sb(f"e_hT{i}", [128, 2, 1024], BF16) for i in range(2)]
        gt = [sb(f"e_g{i}", [128, 512], F32) for i in range(2)]
        sg = [sb(f"e_sg{i}", [128, 512], F32) for i in range(2)]
        ut = [sb(f"e_u{i}", [128, 512], F32) for i in range(2)]
        ev = [sb(f"e_ev{i}", [128, 512], F32) for i in range(3)]
        h1t = [sb(f"e_h1t{i}", [128, D], F32) for i in range(1)]
        pGU = [ps(f"e_pGU{i}", [128, 512], F32) for i in range(4)]
        pDn = [ps(f"e_pD{i}", [128, 512], F32) for i in range(4)]
        S.dma("sp", identf[:], I["c_ident"][:, :], w=[identf])
        S.dma("sp", bdn[:], I["exp_b_down"][:, :], w=[bdn])
        S.dma("sp", bg[:], I["exp_b_gate"][:, :], w=[bg])
        S.dma("sp", bu[:], I["exp_b_up"][:, :], w=[bu])
        wgv = I["exp_w_gate"].rearrange("(e kc p) f -> e p kc f", p=128, kc=16)
        wuv = I["exp_w_up"].rearrange("(e kc p) f -> e p kc f", p=128, kc=16)
        wdv = I["exp_w_down"].rearrange("(e fc p) d -> e p fc d", p=128, fc=16)
        nst = 0
        nev = 0
        for hf in range(2):
            t0 = hf * 1024
            S.dma("sp", h1T[:], G.h1T[:, t0:t0 + 1024].rearrange("(kc p) t -> p kc t", p=128), r=["h1T"], w=[h1T])
            S.dma("sp", gw[:], G.gw[t0:t0 + 1024, :].rearrange("(t p) e -> p t e", p=128), r=["gw"], w=[gw])
            S.op("pool", lambda: nc.gpsimd.memset(Y[:], 0.0), w=[Y])
            for e in range(experts):
                for fgp in range(8):
                    b = nst % 2
                    nst += 1
                    S.dma("pool", wg[b][:], wgv[e][:, :, fgp * 256:(fgp + 1) * 256], w=[wg[b]])
                    S.dma("pool", wu[b][:], wuv[e][:, :, fgp * 256:(fgp + 1) * 256], w=[wu[b]])
                    S.dma("pool", wd[b][:], wdv[e][:, fgp * 2:(fgp + 1) * 2, :], w=[wd[b]])
                    hb = hT[b]
                    for f2 in range(2):
                        fc = fgp * 2 + f2
                        bcol = e * 16 + fc
                        for tc_ in range(2):
                            pg, pu = pGU[(2 * tc_) % 4], pGU[(2 * tc_ + 1) % 4]
                            for kc in range(16):
                                S.op("pe", lambda kc=kc, pg=pg: nc.tensor.matmul(pg[:, :], lhsT=wg[b][:, kc, f2 * 128:(f2 + 1) * 128], rhs=h1T[:, kc, tc_ * 512:(tc_ + 1) * 512],
                                                                                  start=(kc == 0), stop=(kc == 15)), r=[wg[b], h1T], w=[pg])
                            for kc in range(16):
                                S.op("pe", lambda kc=kc, pu=pu: nc.tensor.matmul(pu[:, :], lhsT=wu[b][:, kc, f2 * 128:(f2 + 1) * 128], rhs=h1T[:, kc, tc_ * 512:(tc_ + 1) * 512],
                                                                                  start=(kc == 0), stop=(kc == 15)), r=[wu[b], h1T], w=[pu])
                            g_, s_, u_ = gt[tc_], sg[tc_], ut[tc_]
                            S.op("dve", lambda: nc.vector.tensor_scalar(out=g_[:], in0=pg[:, :], scalar1=bg[:, bcol:bcol + 1], scalar2=LIM, op0=ALU.add, op1=ALU.min), r=[pg, bg], w=[g_])
                            S.op("act", lambda: nc.scalar.activation(out=s_[:], in_=g_[:], func=AF.Sigmoid, scale=1.702), r=[g_], w=[s_])
                            S.op("dve", lambda: nc.vector.tensor_scalar(out=u_[:], in0=pu[:, :], scalar1=bu[:, bcol:bcol + 1], scalar2=LIM, op0=ALU.add, op1=ALU.min), r=[pu, bu], w=[u_])
                            S.op("dve", lambda: nc.vector.tensor_scalar(out=u_[:], in0=u_[:], scalar1=-LIM, scalar2=1.0, op0=ALU.max, op1=ALU.add), r=[u_], w=[u_])
                            S.op("dve", lambda: nc.vector.tensor_tensor(out=g_[:], in0=g_[:], in1=s_[:], op=ALU.mult), r=[g_, s_], w=[g_])
                            S.op("dve", lambda: nc.vector.tensor_tensor(out=hb[:, f2, tc_ * 512:(tc_ + 1) * 512], in0=g_[:], in1=u_[:], op=ALU.mult), r=[g_, u_], w=[hb])
                    for tl in range(8):
                        for d4 in range(4):
                            pd = pDn[(tl * 4 + d4) % 4]
                            for f2 in range(2):
                                S.op("pe", lambda f2=f2, pd=pd, d4=d4, tl=tl: nc.tensor.matmul(pd[:, :], lhsT=hb[:, f2, tl * 128:(tl + 1) * 128], rhs=wd[b][:, f2, d4 * 512:(d4 + 1) * 512],
                                                                                                 start=(f2 == 0), stop=(f2 == 1)), r=[hb, wd[b]], w=[pd])
                            et = ev[nev % 3]
                            nev += 1
                            S.op("act", lambda pd=pd, et=et, tl=tl: nc.scalar.mul(out=et[:], in_=pd[:, :], mul=gw[:, tl, e:e + 1]), r=[pd, gw], w=[et])
                            S.op("dve", lambda et=et, tl=tl, d4=d4: nc.vector.tensor_tensor(out=Y[:, tl, d4 * 512:(d4 + 1) * 512], in0=Y[:, tl, d4 * 512:(d4 + 1) * 512], in1=et[:], op=ALU.add),
                                 r=[et, Y], w=[Y])
            for tl in range(8):
                S.op("pe", lambda tl=tl: nc.tensor.transpose(pGU[0][0:32, 0:128], gw[:, tl, :], identf[:]), r=[gw, identf], w=[pGU[0]])
                S.op("dve", lambda tl=tl: nc.vector.tensor_copy(out=gwT[:, tl, :], in_=pGU[0][0:32, 0:128]), r=[pGU[0]], w=[gwT])
                ht = h1t[0]
                rows = slice(t0 + tl * 128, t0 + (tl + 1) * 128)
                S.dma("sp", ht[:], G.h1[rows, :], r=["h1"], w=[ht])
                for d4 in range(4):
                    pd = pDn[d4]
                    S.op("pe", lambda pd=pd, d4=d4, tl=tl: nc.tensor.matmul(pd[:, :], lhsT=gwT[:, tl, :], rhs=bdn[:, d4 * 512:(d4 + 1) * 512], start=True, stop=True), r=[gwT, bdn], w=[pd])
                    S.op("dve", lambda pd=pd, d4=d4, tl=tl: nc.vector.tensor_tensor(out=Y[:, tl, d4 * 512:(d4 + 1) * 512], in0=Y[:, tl, d4 * 512:(d4 + 1) * 512], in1=pd[:, :], op=ALU.add),
                         r=[pd, Y], w=[Y])
                S.op("dve", lambda tl=tl, ht=ht: nc.vector.scalar_tensor_tensor(out=ht[:], in0=ht[:], scalar=ALPHA, in1=Y[:, tl, :], op0=ALU.mult, op1=ALU.add), r=[ht, Y], w=[ht])
                S.dma("sp", G.ypre[rows, :], ht[:], r=[ht], w=["ypre"])
        S.barrier(G.bar)


def phase_f(G):
    nc, S, I = G.nc, G.S, G.I
    with ExitStack() as st:
        sb = lambda n, s, d: st.enter_context(nc.sbuf_tensor(n, s, d))
        ps = lambda n, s, d: st.enter_context(nc.psum_tensor(n, s, d))
        ident = sb("f_ident", [128, 128], BF16)
        pgw = sb("f_pgw", [128, 16, D], BF16)
        plw = sb("f_plw", [128, 2, D], BF16)
        gbc = sb("f_g", [128, D], F32)
        bbc = sb("f_b", [128, D], F32)
        yt = [sb(f"f_y{i}", [128, D], F32) for i in range(2)]
        ot = [sb(f"f_o{i}", [128, D], F32) for i in range(2)]
        sq = sb("f_sq", [128, D], F32)
        stt = sb("f_st", [128, 4], F32)
        hbf = sb("f_hbf", [128, D], BF16)
        pb = [sb(f"f_pb{i}", [128, 256], BF16) for i in range(2)]
        hTs = sb("f_hTs", [128, 16, 128], BF16)
        pTs = sb("f_pTs", [128, 2, 128], BF16)
        sgt = [sb(f"f_sg{i}", [128, 512], F32) for i in range(2)]
        pGt = [ps(f"f_pG{i}", [128, 512], F32) for i in range(2)]
        pPt = [ps(f"f_pP{i}", [128, 512], F32) for i in range(2)]
        pTb = ps("f_pTb", [128, 1024], BF16)
        S.dma("pool", ident[:], I["c_ident"][:, :], w=[ident])
        wv = I["ple_gate_w"].rearrange("(kc p) c -> p kc c", p=128)
        for j in range(4):
            S.dma("pool", pgw[:, :, j * 512:(j + 1) * 512], wv[:, :, j * 512:(j + 1) * 512], w=[pgw])
        S.dma("pool", plw[:], I["ple_w"].rearrange("(kc p) c -> p kc c", p=128), w=[plw])
        S.dma("sp", gbc[:], I["ln2_g"][0:1, :].partition_broadcast(128), w=[gbc])
        S.dma("sp", bbc[:], I["ln2_b"][0:1, :].partition_broadcast(128), w=[bbc])
        for tl in range(16):
            rows = slice(tl * 128, (tl + 1) * 128)
            y_ = yt[tl % 2]
            o_ = ot[tl % 2]
            p_ = pb[tl % 2]
            S.dma("sp", y_[:], G.ypre[rows, :], r=["ypre"], w=[y_])
            S.dma("pool", p_[:], I["p_own"][rows, :], w=[p_])
            layer_norm_tile(G, y_, gbc, bbc, sq, stt)
            S.op("act", lambda: nc.scalar.copy(out=hbf[:], in_=y_[:]), r=[y_], w=[hbf])
            for j in range(2):
                for k8 in range(8):
                    kc = j * 8 + k8
                    S.op("pe", lambda k8=k8, kc=kc: nc.tensor.transpose(pTb[:, k8 * 128:(k8 + 1) * 128], hbf[:, kc * 128:(kc + 1) * 128], ident[:]), r=[hbf, ident], w=[pTb])
                evac(G, hTs[:, j * 8:(j + 1) * 8, :], pTb[:].rearrange("p (a b) -> p a b", a=8), r=[pTb], w=[hTs])
            for j in range(2):
                S.op("pe", lambda j=j: nc.tensor.transpose(pTb[:, j * 128:(j + 1) * 128], p_[:, j * 128:(j + 1) * 128], ident[:]), r=[p_, ident], w=[pTb])
            evac(G, pTs[:], pTb[:, 0:256].rearrange("p (a b) -> p a b", a=2), r=[pTb], w=[pTs])
            for d4 in range(4):
                cs = slice(d4 * 512, (d4 + 1) * 512)
                pg, pp, s_ = pGt[d4 % 2], pPt[d4 % 2], sgt[d4 % 2]
                for kc in range(16):
                    S.op("pe", lambda kc=kc, pg=pg, cs=cs: nc.tensor.matmul(pg[:, :], lhsT=hTs[:, kc, :], rhs=pgw[:, kc, cs], start=(kc == 0), stop=(kc == 15)), r=[hTs, pgw], w=[pg])
                for kc in range(2):
                    S.op("pe", lambda kc=kc, pp=pp, cs=cs: nc.tensor.matmul(pp[:, :], lhsT=pTs[:, kc, :], rhs=plw[:, kc, cs], start=(kc == 0), stop=(kc == 1)), r=[pTs, plw], w=[pp])
                S.op("act", lambda pg=pg, s_=s_: nc.scalar.activation(out=s_[:], in_=pg[:, :], func=AF.Sigmoid), r=[pg], w=[s_])
                S.op("dve", lambda pp=pp, s_=s_: nc.vector.tensor_tensor(out=s_[:], in0=s_[:], in1=pp[:, :], op=ALU.mult), r=[pp, s_], w=[s_])
                S.op("dve", lambda s_=s_, cs=cs: nc.vector.tensor_tensor(out=o_[:, cs], in0=y_[:, cs], in1=s_[:], op=ALU.add), r=[y_, s_], w=[o_])
            S.dma("sp", G.out[rows, :], o_[:], r=[o_], w=["out"])


def make_consts(s):
    c = {}
    kv = np.ones((T,), np.float32)
    if s == 0:
        kv[:2048] = 0.0
    c["c_kvalid"] = np.ascontiguousarray(kv.reshape(32, 128).T)
    c["c_ident"] = np.eye(128, dtype=np.float32)
    k = np.arange(128)[:, None]
    q = np.arange(128)[None, :]
    c["c_trile"] = (k <= q).astype(np.float32)
    c["c_trigt"] = (k > q).astype(np.float32)
    ab = np.zeros((4, 128, 4, 128), np.float32)
    cb = np.zeros((4, 2, 128, 4, 128), np.float32)
    for g in range(4):
        for h in range(4):
            sl = SLOPES[4 * g + h]
            ab[g, :, h, :] = -sl * (q - k)
            for ct in range(2):
                cb[g, ct, :, h, :] = -sl * (q - 16 * (k + 128 * ct) - 31)
    c["c_abias"] = ab.reshape(4 * 128, 512)
    c["c_cbias"] = cb.reshape(4 * 2 * 128, 512)
    cval = np.zeros((256, 1), np.float32)
    cval[(128 if s == 0 else 0):255] = 1.0
    cm = np.zeros((16, 2, 128, 128), np.float32)
    for i in range(16):
        for ct in range(2):
            cc = k + 128 * ct
            d = 2048 + 128 * i + q - 16 * cc - 31
            cm[i, ct] = (d >= 0) * cval[cc[:, 0]]
    c["c_cmask"] = ((cm - 1.0) * BIG).reshape(16 * 2 * 128, 128).astype(np.float32)
    c["c_tribias"] = np.concatenate([(c["c_trile"] - 1.0) * BIG, (c["c_trigt"] - 1.0) * BIG], axis=1).astype(np.float32)
    c["c_expand"] = (np.arange(T)[None, :] // 64 == np.arange(64)[:, None]).astype(np.float32)
    c["c_expbig"] = c["c_expand"] * BIG
    ce = np.arange(256)[:, None] * 16 + 31
    cs = ce - 31
    ss = np.arange(64)[None, :] * 64
    c["c_overlap"] = np.clip(np.minimum(ce, ss + 63) - np.maximum(cs, ss) + 1, 0, None).astype(np.float32)
    j0 = 32 if s == 0 else 0
    qi = np.arange(TO)[:, None]
    cur = 32 + qi // 64
    j = np.arange(64)[None, :]
    invalid = (j < j0) | (j > cur)
    forced = ((j == j0) | (j == cur) | (j == cur - 1)) & ~invalid
    c["c_selmul"] = (~invalid & ~forced).astype(np.float32)
    c["c_seladd"] = np.where(invalid, -BIG, np.where(forced, BIG, 0.0)).astype(np.float32)
    c["c_selvalid"] = (~invalid).astype(np.float32)
    s_ = np.arange(64)[:, None]
    t_ = np.arange(64)[None, :]
    incl = (s_ <= t_).astype(np.float32)
    strict = (s_ < t_).astype(np.float32)
    low = (t_ < s_).astype(np.float32)
    c["c_rw"] = np.concatenate([incl, strict, strict, incl, low, np.eye(64, dtype=np.float32), incl], axis=1)
    return c


def prep_core_inputs(inputs, c, consts_cache={}):
    b, s = c // 2, c % 2
    m = {}
    x = inputs["x"]
    if s == 1:
        m["xc"] = np.ascontiguousarray(x[b])
    else:
        m["xc"] = np.concatenate([np.zeros((2048, D), np.float32), x[b, :2048]], axis=0)
    m["p_own"] = np.ascontiguousarray(inputs["p"][0, b, 2048 * s:2048 * s + 2048])
    for name, shape in INPUT_SPECS:
        if name in ("xc", "p_own") or name.startswith("c_"):
            continue
        a = np.asarray(inputs[name], np.float32)
        if name in ("exp_b_gate", "exp_b_up"):
            a = np.ascontiguousarray(a.reshape(32, 16, 128).transpose(2, 0, 1))
        m[name] = a.reshape(shape)
    if s not in consts_cache:
        consts_cache[s] = make_consts(s)
    m.update(consts_cache[s])
    return m


def kernel(**inputs):
    nc, G = build()
    in_maps = [prep_core_inputs(inputs, c) for c in range(NCORES)]
    res = run_bass_kernel_spmd(nc, in_maps, core_ids=list(range(NCORES)))
    out = np.zeros((4, 4096, D), np.float32)
    for c in range(NCORES):
        b, s = c // 2, c % 2
        out[b, 2048 * s:2048 * s + 2048] = res.results[c]["out"]
    return out
```

```python
import os
import numpy as np
from contextlib import ExitStack
import concourse.bass as bass
import concourse.mybir as mybir
from concourse.bass_utils import run_bass_kernel_spmd

F32 = mybir.dt.float32
BF16 = mybir.dt.bfloat16
ALU = mybir.AluOpType
AF = mybir.ActivationFunctionType
AX = mybir.AxisListType

NCORES = 8
D = 2048
T = 4096
TO = 2048
NSA_COLS = 2608
RW0 = NSA_COLS
IN_COLS = 6128
SEM_ROT = 12000


class Sync:
    ENGS = ("pe", "act", "dve", "pool", "sp")

    def __init__(self, nc, stack):
        self.nc = nc
        self.stack = stack
        self.eng = {"pe": nc.tensor, "act": nc.scalar, "dve": nc.vector,
                    "pool": nc.gpsimd, "sp": nc.sync}
        self.cnt = {e: 0 for e in self.ENGS}
        self.sems = {e: [] for e in self.ENGS}
        self.waited = {e: {} for e in self.ENGS}
        self.last_w = {}
        self.readers = {}
        self.dma_pool = {}
        self.dma_n = {}
        self.all_dma = []
        self.nsem = 0
        for q in ("sp", "pool", "act"):
            self.dma_pool[q] = [self._newsem() for _ in range(12 if q == "sp" else 6)]
            self.dma_n[q] = 0

    def _newsem(self):
        self.nsem += 1
        return self.stack.enter_context(self.nc.semaphore(f"s{self.nsem}"))

    @staticmethod
    def _key(t):
        return t if isinstance(t, (str, tuple)) else t.tensor.name if hasattr(t, "tensor") else t.name

    def _wait(self, e, tok):
        sem, val, src = tok
        if src == e and e == "pe":
            return
        w = self.waited[e]
        k = id(sem)
        if w.get(k, 0) >= val:
            return
        w[k] = val
        self.eng[e].wait_ge(sem, val)

    def _deps(self, e, r, w):
        toks = []
        for t in r:
            k = self._key(t)
            if k in self.last_w:
                toks.append(self.last_w[k])
        for t in w:
            k = self._key(t)
            if k in self.last_w:
                toks.append(self.last_w[k])
            for tk in self.readers.get(k, {}).values():
                if isinstance(tk, list):
                    toks.extend(tk)
                else:
                    toks.append(tk)
        for tk in toks:
            self._wait(e, tk)

    def _record(self, tok, r, w, isdma):
        for t in w:
            k = self._key(t)
            self.last_w[k] = tok
            self.readers[k] = {}
        for t in r:
            k = self._key(t)
            d = self.readers.setdefault(k, {})
            if isdma:
                d.setdefault("dma", []).append(tok)
                if len(d["dma"]) > 24:
                    d["dma"] = d["dma"][-24:]
            else:
                d[tok[2]] = tok

    def op(self, e, fn, r=(), w=()):
        self._deps(e, r, w)
        n = self.cnt[e]
        si, v = divmod(n, SEM_ROT)
        while len(self.sems[e]) <= si:
            self.sems[e].append(self._newsem())
        sem = self.sems[e][si]
        ins = fn()
        ins.then_inc(sem, 1)
        self.cnt[e] = n + 1
        tok = (sem, v + 1, e)
        self._record(tok, r, w, False)
        return tok

    def dma(self, q, out, in_, r=(), w=(), **kw):
        self._deps(q, r, w)
        n = self.dma_n[q]
        pool = self.dma_pool[q]
        sem = pool[n % len(pool)]
        rnd = n // len(pool)
        if rnd > 0:
            self._wait(q, (sem, 16 * rnd, "dma"))
        self.eng[q].dma_start(out=out, in_=in_, **kw).then_inc(sem, 16)
        self.dma_n[q] = n + 1
        tok = (sem, 16 * (rnd + 1), "dma")
        self._record(tok, r, w, True)
        self.all_dma.append(tok)
        if len(self.all_dma) > 64:
            self.all_dma = self.all_dma[-64:]
        return tok

    def barrier(self, scratch):
        for q in self.dma_pool:
            n = self.dma_n[q]
            pool = self.dma_pool[q]
            for i, sem in enumerate(pool):
                uses = (n - i + len(pool) - 1) // len(pool) if n > i else 0
                if uses > 0:
                    self._wait("dve", (sem, 16 * uses, "dma"))
        toks = []
        for e in ("pe", "act", "pool"):
            if self.cnt[e] > 0:
                n = self.cnt[e] - 1
                si, v = divmod(n, SEM_ROT)
                toks.append((self.sems[e][si], v + 1, e))
        for tk in toks:
            self._wait("dve", tk)
        tok = self.op("dve", lambda: self.nc.vector.memset(scratch[0:1, 0:1], 0.0), w=["_bar"])
        for e in ("pe", "act", "pool", "sp"):
            self._wait(e, tok)
        self.last_w = {}
        self.readers = {}

    def finish(self):
        for q in self.dma_pool:
            n = self.dma_n[q]
            pool = self.dma_pool[q]
            for i, sem in enumerate(pool):
                uses = (n - i + len(pool) - 1) // len(pool) if n > i else 0
                if uses > 0:
                    self._wait("sp", (sem, 16 * uses, "dma"))
        for e in ("pe", "act", "dve", "pool"):
            if self.cnt[e] > 0:
                n = self.cnt[e] - 1
                si, v = divmod(n, SEM_ROT)
                self._wait("sp", (self.sems[e][si], v + 1, e))


SLOPES = [2.0 ** (-8.0 * (i + 1) / 16.0) for i in range(16)]
SCALE = 64 ** -0.5
ALPHA = 2.0 ** 0.25
BIG = 1.0e30

INPUT_SPECS = [
    ("xc", [T, D]), ("p_own", [TO, 256]), ("w_in", [D, IN_COLS]),
    ("cmp_pe_k", [32, 64]), ("cmp_w1_k", [2048, 64]), ("cmp_w2_k", [64, 64]),
    ("cmp_pe_v", [32, 64]), ("cmp_w1_v", [2048, 64]), ("cmp_w2_v", [64, 64]),
    ("rwkv_mu", [1, 3520]), ("rwkv_w0", [1, 1024]), ("rwkv_w_up", [96, 1024]),
    ("rwkv_a0", [1, 1024]), ("rwkv_a_up", [96, 1024]), ("rwkv_g_up", [256, 1024]),
    ("rwkv_k_k", [1, 1024]), ("rwkv_k_a", [1, 1024]), ("rwkv_r_k", [1, 1024]),
    ("rwkv_ln_g", [1, 1024]), ("rwkv_ln_b", [1, 1024]),
    ("w_out", [D, D]), ("ln1_g", [1, D]), ("ln1_b", [1, D]),
    ("router_w", [D, 32]), ("router_b", [1, 32]),
    ("exp_w_gate", [32 * D, D]), ("exp_b_gate", [128, 512]),
    ("exp_w_up", [32 * D, D]), ("exp_b_up", [128, 512]),
    ("exp_w_down", [32 * D, D]), ("exp_b_down", [32, D]),
    ("ln2_g", [1, D]), ("ln2_b", [1, D]), ("ple_w", [256, D]), ("ple_gate_w", [D, D]),
    ("c_kvalid", [128, 32]), ("c_ident", [128, 128]), ("c_trile", [128, 128]), ("c_trigt", [128, 128]),
    ("c_abias", [4 * 128, 512]), ("c_cbias", [4 * 2 * 128, 512]), ("c_cmask", [16 * 2 * 128, 128]),
    ("c_expand", [64, T]), ("c_overlap", [256, 64]), ("c_tribias", [128, 256]), ("c_expbig", [64, T]),
    ("c_selmul", [TO, 64]), ("c_seladd", [TO, 64]), ("c_selvalid", [TO, 64]),
    ("c_rw", [64, 448]),
]


class Ctx:
    pass


def build(debug=(), phases="ABCDEF", skip=()):
    nc = bass.Bass("TRN2", target_bir_lowering=False)
    G = Ctx()
    G.nc = nc
    G.I = {}
    for name, shape in INPUT_SPECS:
        if name in skip:
            continue
        G.I[name] = nc.dram_tensor(name, shape, F32, kind="ExternalInput").ap()
    G.out = nc.dram_tensor("out", [TO, D], F32, kind="ExternalOutput").ap()
    G.dbg = {}
    G.debug = debug
    dr = lambda n, s, d: nc.dram_tensor(n, s, d).ap()
    G.vs_tm = dr("vs_tm", [T, 4 * 65], BF16)
    G.vw_tm = dr("vw_tm", [T, 4 * 65], BF16)
    G.gl_tm = dr("gl_tm", [TO, 48], F32)
    G.zr = dr("zr", [T, 3520], F32)
    G.qT = dr("qT", [1024, TO], BF16)
    G.kcT = dr("kcT", [256, T], BF16)
    G.vcT = dr("vcT", [256, T], BF16)
    G.ksT = dr("ksT", [256, T], BF16)
    G.kwT = dr("kwT", [256, T], BF16)
    G.mixT = dr("mixT", [D, TO], BF16)
    G.h1 = dr("h1", [TO, D], F32)
    G.h1T = dr("h1T", [D, TO], BF16)
    G.gw = dr("gw", [TO, 32], F32)
    G.ypre = dr("ypre", [TO, D], F32)
    for n in ("rw_r", "rw_k", "rw_v", "rw_kn", "rw_b", "rw_lw"):
        setattr(G, n, dr(n, [T, 1024], F32))
    G.rw_g = dr("rw_g", [TO, 1024], F32)
    G.rw_y = dr("rw_y", [TO, 1024], F32)
    with ExitStack() as st:
        S = Sync(nc, st)
        G.S = S
        G.bar = st.enter_context(nc.sbuf_tensor("barscr", [128, 8], F32))
        for nm, shp in debug:
            dbg_out(G, nm, shp)
        if "A" in phases:
            phase_a(G)
        if "B" in phases:
            phase_b(G)
        if "C" in phases:
            phase_c(G)
        if "D" in phases:
            phase_d(G)
        if "E" in phases:
            phase_e(G)
        if "F" in phases:
            phase_f(G)
        S.finish()
    return nc, G


def dbg_out(G, name, shape, dt=F32):
    t = G.nc.dram_tensor("dbg_" + name, shape, dt, kind="ExternalOutput").ap()
    G.dbg[name] = t
    return t


_evac_rr = [0]


def evac(G, out, in_, r, w, scale=None):
    nc, S = G.nc, G.S
    _evac_rr[0] ^= 1
    if _evac_rr[0]:
        if scale is None:
            return S.op("act", lambda: nc.scalar.copy(out=out, in_=in_), r=r, w=w)
        return S.op("act", lambda: nc.scalar.mul(out=out, in_=in_, mul=scale), r=r, w=w)
    if scale is None:
        return S.op("dve", lambda: nc.vector.tensor_copy(out=out, in_=in_), r=r, w=w)
    return S.op("dve", lambda: nc.vector.tensor_scalar(out=out, in0=in_, scalar1=scale, scalar2=None, op0=ALU.mult), r=r, w=w)


def run_pipelined(gens, depth):
    active = []
    it = iter(gens)
    more = True
    while True:
        if more and len(active) < depth:
            try:
                active.append(next(it))
            except StopIteration:
                more = False
        if not active:
            break
        for g in list(active):
            try:
                next(g)
            except StopIteration:
                active.remove(g)


def phase_a(G):
    nc, S, I = G.nc, G.S, G.I
    with ExitStack() as st:
        sb = lambda n, s, d: st.enter_context(nc.sbuf_tensor(n, s, d))
        ps = lambda n, s, d: st.enter_context(nc.psum_tensor(n, s, d))
        ident = sb("a_ident", [128, 128], BF16)
        kval = sb("a_kval", [128, 32], F32)
        xb = [sb(f"a_xb{i}", [128, D], BF16) for i in range(2)]
        xT = sb("a_xT", [128, 16, 2048], BF16)
        wch = [sb(f"a_w{i}", [128, 16, 512], BF16) for i in range(2)]
        stf = [sb(f"a_stf{i}", [128, 512], F32) for i in range(3)]
        stb = [sb(f"a_stb{i}", [128, 512], BF16) for i in range(3)]
        vst = [sb(f"a_vst{i}", [128, 4, 65], BF16) for i in range(2)]
        ptr = [ps(f"a_ptr{i}", [128, 1024], BF16) for i in range(2)]
        pac = [ps(f"a_pac{i}", [128, 512], F32) for i in range(4)]
        S.dma("pool", ident[:], I["c_ident"][:, :], w=[ident])
        S.dma("sp", kval[:], I["c_kvalid"][:, :], w=[kval])
        w_in_v = I["w_in"].rearrange("(kc p) c -> p kc c", p=128)
        tm_chunks = [("vs", 1792, 2048), ("vw", 2304, 2560), ("gl", 2560, 2608)]
        c = 0
        while c < 3520:
            n = min(512, 3520 - c)
            tm_chunks.append(("rw", RW0 + c, RW0 + c + n))
            c += n
        fm_chunks = [("q", 128 * j, 128 * j + 128) for j in range(8)]
        for nm, base in (("kc", 1024), ("vc", 1280), ("ks", 1536), ("kw", 2048)):
            fm_chunks += [(nm, base, base + 128), (nm, base + 128, base + 256)]
        fm_dst = {"q": (G.qT, 0), "kc": (G.kcT, 1024), "vc": (G.vcT, 1280), "ks": (G.ksT, 1536), "kw": (G.kwT, 2048)}
        nw = 0
        nst = 0
        npac = 0
        for hf in range(2):
            for tl in range(16):
                tok0 = hf * 2048 + tl * 128
                xbt = xb[tl % 2]
                S.dma("pool", xbt[:], I["xc"][tok0:tok0 + 128, :], w=[xbt])
                for j in range(2):
                    pt = ptr[j]
                    for k8 in range(8):
                        kc = j * 8 + k8
                        S.op("pe", lambda pt=pt, k8=k8, kc=kc, xbt=xbt: nc.tensor.transpose(
                            pt[:, k8 * 128:(k8 + 1) * 128], xbt[:, kc * 128:(kc + 1) * 128], ident[:]),
                            r=[xbt, ident], w=[pt])
                    evac(G, xT[:, j * 8:(j + 1) * 8, tl * 128:(tl + 1) * 128],
                         pt[:].rearrange("p (a b) -> p a b", a=8), r=[pt], w=[xT])
            for (nm, c0, c1) in tm_chunks:
                if nm == "gl" and hf == 0:
                    continue
                ncol = c1 - c0
                wt = wch[nw % 2]
                nw += 1
                S.dma("pool", wt[:, :, 0:ncol], w_in_v[:, :, c0:c1], w=[wt])
                for tl in range(16):
                    tok0 = hf * 2048 + tl * 128
                    pa = pac[npac % 4]
                    npac += 1
                    for kc in range(16):
                        S.op("pe", lambda pa=pa, kc=kc, wt=wt, tl=tl, ncol=ncol: nc.tensor.matmul(
                            pa[:, 0:ncol], lhsT=xT[:, kc, tl * 128:(tl + 1) * 128], rhs=wt[:, kc, 0:ncol],
                            start=(kc == 0), stop=(kc == 15)), r=[xT, wt], w=[pa])
                    if nm in ("vs", "vw"):
                        vt = vst[nst % 2]
                        nst += 1
                        evac(G, vt[:, :, 0:64], pa[:, 0:256].rearrange("p (g d) -> p g d", g=4), r=[pa], w=[vt])
                        tglob = hf * 16 + tl
                        S.op("pool", lambda vt=vt, tglob=tglob: nc.gpsimd.tensor_copy(
                            out=vt[:, :, 64], in_=kval[:, tglob:tglob + 1].to_broadcast([128, 4])),
                            r=[kval, vt], w=[vt])
                        dst = G.vs_tm if nm == "vs" else G.vw_tm
                        S.dma("sp", dst[tok0:tok0 + 128, :], vt[:].rearrange("p g d -> p (g d)"), r=[vt], w=[nm + "_tm"])
                    else:
                        sf = stf[nst % 3]
                        nst += 1
                        evac(G, sf[:, 0:ncol], pa[:, 0:ncol], r=[pa], w=[sf])
                        if nm == "gl":
                            S.dma("sp", G.gl_tm[tl * 128:(tl + 1) * 128, :], sf[:, 0:48], r=[sf], w=["gl_tm"])
                        else:
                            S.dma("sp", G.zr[tok0:tok0 + 128, c0 - RW0:c1 - RW0], sf[:, 0:ncol], r=[sf], w=["zr"])
            for (nm, c0, c1) in fm_chunks:
                if nm == "q" and hf == 0:
                    continue
                wt = wch[nw % 2]
                nw += 1
                S.dma("pool", wt[:, :, 0:128], w_in_v[:, :, c0:c1], w=[wt])
                dst, base = fm_dst[nm]
                for t4 in range(4):
                    pa = pac[npac % 4]
                    npac += 1
                    for kc in range(16):
                        S.op("pe", lambda pa=pa, kc=kc, wt=wt, t4=t4: nc.tensor.matmul(
                            pa[:, :], lhsT=wt[:, kc, 0:128], rhs=xT[:, kc, t4 * 512:(t4 + 1) * 512],
                            start=(kc == 0), stop=(kc == 15)), r=[xT, wt], w=[pa])
                    sbt = stb[nst % 3]
                    nst += 1
                    evac(G, sbt[:, :], pa[:, :], r=[pa], w=[sbt])
                    if nm == "q":
                        col0 = t4 * 512
                    else:
                        col0 = hf * 2048 + t4 * 512
                    S.dma("sp", dst[c0 - base:c1 - base, col0:col0 + 512], sbt[:, :], r=[sbt], w=[nm + "T"])
        S.barrier(G.bar)


def phase_b(G):
    nc, S, I = G.nc, G.S, G.I
    with ExitStack() as st:
        sb = lambda n, s, d: st.enter_context(nc.sbuf_tensor(n, s, d))
        ps = lambda n, s, d: st.enter_context(nc.psum_tensor(n, s, d))
        ident = sb("b_ident", [128, 128], BF16)
        tribias = sb("b_tribias", [128, 256], F32)
        tribias_bf = sb("b_tribias_bf", [128, 256], BF16)
        expbig = sb("b_expbig", [64, T], BF16)
        cmask = sb("b_cmask", [128, 32, 128], BF16)
        abias = sb("b_abias", [128, 512], F32)
        cbias = sb("b_cbias", [128, 2, 512], F32)
        ksT = sb("b_ksT", [64, T], BF16)
        kwT = sb("b_kwT", [64, T], BF16)
        cT = sb("b_cT", [64, T], BF16)
        vs = sb("b_vs", [128, 32, 65], BF16)
        vw = sb("b_vw", [128, 32, 65], BF16)
        qT = sb("b_qT", [64, 4, TO], BF16)
        w1 = [sb(f"b_w1{i}", [128, 16, 64], F32) for i in range(2)]
        w1b = [sb(f"b_w1b{i}", [64, 32, 64], BF16) for i in range(2)]
        w2b = [sb(f"b_w2b{i}", [64, 64], BF16) for i in range(2)]
        pec = [sb(f"b_pec{i}", [128, 16], F32) for i in range(2)]
        b1 = [sb(f"b_b1{i}", [64, 1], F32) for i in range(2)]
        hf = sb("b_hf", [64, 256], F32)
        h2 = sb("b_h2", [64, 256], F32)
        gT = sb("b_gT", [64, 256], BF16)
        kcmpT = sb("b_kcmpT", [64, 256], BF16)
        vcx = sb("b_vcx", [128, 2, 129], BF16)
        tmp = [sb(f"b_tmp{i}", [128, 512], F32) for i in range(2)]
        tmp2 = [sb(f"b_tmq{i}", [128, 512], F32) for i in range(2)]
        Pt = [sb(f"b_P{i}", [128, 512], BF16) for i in range(3)]
        glt = sb("b_gl", [128, 48], F32)
        gsig = sb("b_gsig", [128, 48], F32)
        selc = [sb(f"b_selc{i}", [128, 64], F32) for i in range(3)]
        imp = sb("b_imp", [128, 64], F32)
        imp2 = sb("b_imp2", [128, 64], F32)
        m8 = sb("b_m8", [128, 16], F32)
        selb = sb("b_selb", [128, 64], BF16)
        selT = sb("b_selT", [64, 128], BF16)
        sums = sb("b_sums", [128, 12], F32)
        coef = sb("b_coef", [128, 12], F32)
        oacc = sb("b_oacc", [128, 256], F32)
        obf = sb("b_obf", [128, 256], BF16)
        ost = sb("b_ost", [128, 2, 128], BF16)
        pS = [ps(f"b_pS{i}", [128, 512], F32) for i in range(2)]
        pM = ps("b_pM", [128, 512], F32)
        PMK = [("pM", j) for j in range(4)]
        pAs = ps("b_pAs", [128, 4, 65], F32)
        pAw = ps("b_pAw", [128, 4, 65], F32)
        pAc = [ps(f"b_pAc{i}", [128, 2, 129], F32) for i in range(2)]
        pT = ps("b_pT", [128, 1024], BF16)

        S.dma("pool", ident[:], I["c_ident"][:, :], w=[ident])
        S.dma("sp", tribias[:], I["c_tribias"][:, :], w=[tribias])
        S.dma("pool", tribias_bf[:], I["c_tribias"][:, :], w=[tribias_bf])
        for j in range(4):
            S.dma("pool", expbig[:, j * 1024:(j + 1) * 1024], I["c_expbig"][:, j * 1024:(j + 1) * 1024], w=[expbig])
        cm_v = I["c_cmask"].rearrange("(a p) q -> p a q", p=128)
        for j in range(4):
            S.dma("pool", cmask[:, j * 8:(j + 1) * 8, :], cm_v[:, j * 8:(j + 1) * 8, :], w=[cmask])
        for kv, (nw1, nw2, npe) in enumerate((("cmp_w1_k", "cmp_w2_k", "cmp_pe_k"), ("cmp_w1_v", "cmp_w2_v", "cmp_pe_v"))):
            S.dma("sp", w1[kv][:], I[nw1].rearrange("(p c) o -> p c o", c=16), w=[w1[kv]])
            S.dma("pool", w1b[kv][:], I[nw1].rearrange("(l d) o -> d l o", d=64), w=[w1b[kv]])
            S.dma("pool", w2b[kv][:], I[nw2][:, :], w=[w2b[kv]])
            S.dma("sp", pec[kv][:], I[npe].rearrange("l d -> (l d)").rearrange("(p c) -> p c", c=16), w=[pec[kv]])
            for ch in range(16):
                S.op("pe", lambda kv=kv, ch=ch: nc.tensor.matmul(pM[0:64, 0:1], lhsT=w1[kv][:, ch, :], rhs=pec[kv][:, ch:ch + 1],
                                                                   start=(ch == 0), stop=(ch == 15)), r=[w1[kv], pec[kv]], w=PMK)
            S.op("dve", lambda kv=kv: nc.vector.tensor_copy(out=b1[kv][:], in_=pM[0:64, 0:1]), r=PMK, w=[b1[kv]])
        S.op("dve", lambda: nc.vector.memset(vcx[:], 0.0), w=[vcx])
        S.op("dve", lambda: nc.vector.memset(vcx[:, :, 64:65], 1.0), r=[vcx], w=[vcx])
        S.dma("pool", vcx[:, :, 65:129], I["c_overlap"].rearrange("(ct p) j -> p ct j", p=128), r=[vcx], w=[vcx])
        S.op("dve", lambda: nc.vector.memset(kcmpT[:], 0.0), w=[kcmpT])

        def compress(kv, g):
            src = G.kcT if kv == 0 else G.vcT
            S.dma("sp", cT[:], src[g * 64:(g + 1) * 64, :], r=["kcT", "vcT"], w=[cT])
            cv = cT[:].rearrange("p (c s) -> p c s", s=16)
            for l in range(32):
                rhs = cv[:, 0:255, l] if l < 16 else cv[:, 1:256, l - 16]
                S.op("pe", lambda l=l, rhs=rhs: nc.tensor.matmul(pM[0:64, 0:255], lhsT=w1b[kv][:, l, :], rhs=rhs,
                                                                  start=(l == 0), stop=(l == 31)), r=[w1b[kv], cT], w=PMK)
            S.op("act", lambda: nc.scalar.activation(out=hf[:, 0:255], in_=pM[0:64, 0:255], func=AF.Identity, bias=b1[kv][:, 0:1], scale=1.0),
                 r=PMK + [b1[kv]], w=[hf])
            S.op("dve", lambda: nc.vector.tensor_tensor(out=h2[:, 0:255], in0=hf[:, 0:255], in1=hf[:, 0:255], op=ALU.mult), r=[hf], w=[h2])
            S.op("dve", lambda: nc.vector.tensor_scalar(out=h2[:, 0:255], in0=h2[:, 0:255], scalar1=0.044715, scalar2=1.0, op0=ALU.mult, op1=ALU.add), r=[h2], w=[h2])
            S.op("dve", lambda: nc.vector.tensor_tensor(out=h2[:, 0:255], in0=h2[:, 0:255], in1=hf[:, 0:255], op=ALU.mult), r=[h2, hf], w=[h2])
            S.op("act", lambda: nc.scalar.activation(out=h2[:, 0:255], in_=h2[:, 0:255], func=AF.Tanh, scale=0.7978845608028654), r=[h2], w=[h2])
            S.op("dve", lambda: nc.vector.scalar_tensor_tensor(out=gT[:, 0:255], in0=h2[:, 0:255], scalar=1.0, in1=hf[:, 0:255], op0=ALU.add, op1=ALU.mult),
                 r=[h2, hf], w=[gT])
            if kv == 0:
                S.op("pe", lambda: nc.tensor.matmul(pM[0:64, 0:255], lhsT=w2b[0][:, :], rhs=gT[:, 0:255], start=True, stop=True), r=[w2b[0], gT], w=PMK)
                S.op("act", lambda: nc.scalar.mul(out=kcmpT[:, 0:255], in_=pM[0:64, 0:255], mul=0.5), r=PMK, w=[kcmpT])
            else:
                for ct in range(2):
                    rows = 128 if ct == 0 else 127
                    S.op("pe", lambda ct=ct, rows=rows: nc.tensor.matmul(pM[0:rows, 0:64], lhsT=gT[:, ct * 128:ct * 128 + rows], rhs=w2b[1][:, :],
                                                                          start=True, stop=True), r=[w2b[1], gT], w=PMK)
                    S.op("act", lambda ct=ct, rows=rows: nc.scalar.mul(out=vcx[0:rows, ct, 0:64], in_=pM[0:rows, 0:64], mul=0.5), r=PMK, w=[vcx])

        np_ = [0]
        for g in range(4):
            S.dma("sp", ksT[:], G.ksT[g * 64:(g + 1) * 64, :], r=["ksT"], w=[ksT])
            S.dma("sp", kwT[:], G.kwT[g * 64:(g + 1) * 64, :], r=["kwT"], w=[kwT])
            S.dma("sp", qT[:], G.qT[g * 256:(g + 1) * 256, :].rearrange("(h d) t -> d h t", d=64), r=["qT"], w=[qT])
            vs_v = G.vs_tm.rearrange("(t p) c -> p t c", p=128)
            vw_v = G.vw_tm.rearrange("(t p) c -> p t c", p=128)
            for j in range(4):
                S.dma("sp", vs[:, j * 8:(j + 1) * 8, :], vs_v[:, j * 8:(j + 1) * 8, g * 65:(g + 1) * 65], r=["vs_tm"], w=[vs])
                S.dma("sp", vw[:, j * 8:(j + 1) * 8, :], vw_v[:, j * 8:(j + 1) * 8, g * 65:(g + 1) * 65], r=["vw_tm"], w=[vw])
            S.dma("sp", abias[:], I["c_abias"][g * 128:(g + 1) * 128, :], w=[abias])
            S.dma("sp", cbias[:], I["c_cbias"][g * 256:(g + 1) * 256, :].rearrange("(ct p) x -> p ct x", p=128), w=[cbias])
            compress(0, g)
            compress(1, g)
            for i in range(16):
                q0 = i * 128
                S.dma("sp", glt[:], G.gl_tm[q0:q0 + 128, :], r=["gl_tm"], w=[glt])
                S.op("act", lambda: nc.scalar.activation(out=gsig[:], in_=glt[:], func=AF.Sigmoid), r=[glt], w=[gsig])
                for j, nm in enumerate(("c_selmul", "c_seladd", "c_selvalid")):
                    S.dma("sp", selc[j][:], I[nm][q0:q0 + 128, :], w=[selc[j]])
                gv = gsig[:, 12 * g:12 * g + 12].rearrange("p (h r) -> p h r", r=3)
                qrhs = qT[:, :, q0:q0 + 128]

                def tile_gen(lhsT, bias_ap, mask_fn, offs, pv_fn, rows=128):
                    n = np_[0]
                    np_[0] += 1
                    p_s, t1, t2, P = pS[n % 2], tmp[n % 2], tmp2[n % 2], Pt[n % 3]
                    mk = mask_fn(n) if mask_fn is not None else None
                    S.op("pe", lambda: nc.tensor.matmul(p_s[0:rows, :], lhsT=lhsT, rhs=qrhs, start=True, stop=True), r=[ksT, kwT, kcmpT, qT], w=[p_s])
                    yield
                    S.op("dve", lambda: nc.vector.scalar_tensor_tensor(out=t1[0:rows, :], in0=p_s[0:rows, :], scalar=SCALE, in1=bias_ap,
                                                                        op0=ALU.mult, op1=ALU.add), r=[p_s, abias, cbias], w=[t1])
                    src = t1
                    if mk is not None:
                        mask_ap, mkey = mk
                        S.op("dve", lambda: nc.vector.tensor_tensor(out=t2[0:rows, :].rearrange("p (h q) -> p h q", h=4),
                                                                     in0=t1[0:rows, :].rearrange("p (h q) -> p h q", h=4),
                                                                     in1=mask_ap, op=ALU.add), r=[t1, mkey], w=[t2])
                        src = t2
                    yield
                    for h in range(4):
                        S.op("act", lambda h=h: nc.scalar.activation(out=P[0:rows, h * 128:(h + 1) * 128], in_=src[0:rows, h * 128:(h + 1) * 128],
                                                                      func=AF.Exp, bias=float(offs[h]), scale=1.0), r=[src], w=[P])
                    yield
                    pv_fn(P)

                Pc = []

                def cmp_tile(ct):
                    rows = 128 if ct == 0 else 127
                    offs = [-SLOPES[4 * g + h] * (2048 + 128 * i) for h in range(4)]

                    def pv(P):
                        Pc.append(P)
                        if ct == 0:
                            return
                        for h in range(4):
                            for c2 in range(2):
                                r2 = 128 if c2 == 0 else 127
                                S.op("pe", lambda h=h, c2=c2, r2=r2: nc.tensor.matmul(pAc[h // 2][:, h % 2, :], lhsT=Pc[c2][0:r2, h * 128:(h + 1) * 128],
                                                                                       rhs=vcx[0:r2, c2, :], start=(c2 == 0), stop=(c2 == 1)),
                                     r=[Pc[c2], vcx], w=[pAc[h // 2]])
                    return tile_gen(kcmpT[:, ct * 128:ct * 128 + rows], cbias[0:rows, ct, :],
                                    lambda n: (cmask[0:rows, i * 2 + ct, :].unsqueeze(1).to_broadcast([rows, 4, 128]), cmask), offs, pv, rows)

                def win_tile(wi):
                    kt = 12 + i + wi
                    offs = [-SLOPES[4 * g + h] * 128.0 * (4 - wi) for h in range(4)]
                    if wi == 0:
                        mfn = lambda n: (tribias[:, 128:256].unsqueeze(1).to_broadcast([128, 4, 128]), tribias)
                    elif wi == 4:
                        mfn = lambda n: (tribias[:, 0:128].unsqueeze(1).to_broadcast([128, 4, 128]), tribias)
                    else:
                        mfn = None

                    def pv(P):
                        for h in range(4):
                            S.op("pe", lambda h=h: nc.tensor.matmul(pAw[:, h, :], lhsT=P[:, h * 128:(h + 1) * 128], rhs=vw[:, kt, :],
                                                                     start=(wi == 0), stop=(wi == 4)), r=[P, vw], w=[pAw])
                    return tile_gen(kwT[:, kt * 128:(kt + 1) * 128], abias[:, :], mfn, offs, pv)

                run_pipelined([cmp_tile(0), cmp_tile(1)] + [win_tile(wi) for wi in range(5)], 2)
                for hh in range(2):
                    S.op("dve", lambda hh=hh: nc.vector.tensor_scalar(out=sums[:, 2 * hh:2 * hh + 2], in0=pAc[hh][:, :, 64], scalar1=1e-30, scalar2=None, op0=ALU.max),
                         r=[pAc[hh]], w=[sums])
                S.op("dve", lambda: nc.vector.reciprocal(out=sums[:, 0:4], in_=sums[:, 0:4]), r=[sums], w=[sums])
                S.op("dve", lambda: nc.vector.tensor_tensor(out=coef[:, 0:4], in0=sums[:, 0:4], in1=gv[:, :, 0], op=ALU.mult), r=[sums, gsig], w=[coef])
                for h in range(4):
                    pa = pAc[h // 2]
                    if h == 0:
                        S.op("dve", lambda pa=pa, h=h: nc.vector.tensor_scalar(out=imp[:], in0=pa[:, h % 2, 65:129], scalar1=sums[:, h:h + 1], scalar2=None, op0=ALU.mult),
                             r=[pa, sums], w=[imp])
                    else:
                        S.op("dve", lambda pa=pa, h=h: nc.vector.scalar_tensor_tensor(out=imp[:], in0=pa[:, h % 2, 65:129], scalar=sums[:, h:h + 1], in1=imp[:],
                                                                                       op0=ALU.mult, op1=ALU.add), r=[pa, sums, imp], w=[imp])
                    S.op("dve", lambda pa=pa, h=h: nc.vector.tensor_scalar(out=oacc[:, h * 64:(h + 1) * 64], in0=pa[:, h % 2, 0:64], scalar1=coef[:, h:h + 1], scalar2=None, op0=ALU.mult),
                         r=[pa, coef], w=[oacc])
                S.op("dve", lambda: nc.vector.tensor_tensor(out=imp[:], in0=imp[:], in1=selc[0][:], op=ALU.mult), r=[imp, selc[0]], w=[imp])
                S.op("dve", lambda: nc.vector.tensor_tensor(out=imp[:], in0=imp[:], in1=selc[1][:], op=ALU.add), r=[imp, selc[1]], w=[imp])
                S.op("dve", lambda: nc.vector.max(out=m8[:, 0:8], in_=imp[:]), r=[imp], w=[m8])
                S.op("dve", lambda: nc.vector.match_replace(out=imp2[:], in_to_replace=m8[:, 0:8], in_values=imp[:], imm_value=-3.0e38), r=[imp, m8], w=[imp2])
                S.op("dve", lambda: nc.vector.max(out=m8[:, 8:16], in_=imp2[:]), r=[imp2], w=[m8])
                S.op("dve", lambda: nc.vector.tensor_tensor(out=imp2[:], in0=imp[:], in1=m8[:, 15:16].to_broadcast([128, 64]), op=ALU.is_ge), r=[imp, m8], w=[imp2])
                S.op("dve", lambda: nc.vector.tensor_tensor(out=imp2[:], in0=imp2[:], in1=selc[2][:], op=ALU.mult), r=[imp2, selc[2]], w=[imp2])
                S.op("dve", lambda: nc.vector.tensor_scalar(out=selb[:], in0=imp2[:], scalar1=-1.0, scalar2=None, op0=ALU.add), r=[imp2], w=[selb])
                S.op("pe", lambda: nc.tensor.transpose(pT[0:64, 0:128], selb[:, :], ident[:]), r=[selb, ident], w=[pT])
                S.op("act", lambda: nc.scalar.copy(out=selT[:], in_=pT[0:64, 0:128]), r=[pT], w=[selT])
                nkt = 17 + i

                def slc_tile(kt):
                    diag = (kt == nkt - 1)
                    offs = [-SLOPES[4 * g + h] * 128.0 * (nkt - 1 - kt) for h in range(4)]

                    def mfn(n):
                        j = 0
                        reg = pM[:, j * 128:(j + 1) * 128]
                        S.op("pe", lambda: nc.tensor.matmul(reg, lhsT=expbig[:, kt * 128:(kt + 1) * 128], rhs=selT[:, :], start=True, stop=not diag),
                             r=[expbig, selT], w=[PMK[j]])
                        if diag:
                            S.op("pe", lambda: nc.tensor.matmul(reg, lhsT=ident[:, :], rhs=tribias_bf[:, 0:128], start=False, stop=True), r=[ident, tribias_bf], w=[PMK[j]])
                        return reg.unsqueeze(1).to_broadcast([128, 4, 128]), PMK[j]

                    def pv(P):
                        for h in range(4):
                            S.op("pe", lambda h=h: nc.tensor.matmul(pAs[:, h, :], lhsT=P[:, h * 128:(h + 1) * 128], rhs=vs[:, kt, :],
                                                                     start=(kt == 0), stop=(kt == nkt - 1)), r=[P, vs], w=[pAs])
                    return tile_gen(ksT[:, kt * 128:(kt + 1) * 128], abias[:, :], mfn, offs, pv)

                run_pipelined([slc_tile(kt) for kt in range(nkt)], 2)
                for bi, pa in ((1, pAs), (2, pAw)):
                    S.op("dve", lambda bi=bi, pa=pa: nc.vector.tensor_scalar(out=sums[:, 4 * bi:4 * bi + 4], in0=pa[:, :, 64], scalar1=1e-30, scalar2=None, op0=ALU.max),
                         r=[pa], w=[sums])
                    S.op("dve", lambda bi=bi: nc.vector.reciprocal(out=sums[:, 4 * bi:4 * bi + 4], in_=sums[:, 4 * bi:4 * bi + 4]), r=[sums], w=[sums])
                    S.op("dve", lambda bi=bi: nc.vector.tensor_tensor(out=coef[:, 4 * bi:4 * bi + 4], in0=sums[:, 4 * bi:4 * bi + 4], in1=gv[:, :, bi], op=ALU.mult),
                         r=[sums, gsig], w=[coef])
                    for h in range(4):
                        S.op("dve", lambda bi=bi, pa=pa, h=h: nc.vector.scalar_tensor_tensor(out=oacc[:, h * 64:(h + 1) * 64], in0=pa[:, h, 0:64],
                                                                                              scalar=coef[:, 4 * bi + h:4 * bi + h + 1], in1=oacc[:, h * 64:(h + 1) * 64],
                                                                                              op0=ALU.mult, op1=ALU.add), r=[pa, coef, oacc], w=[oacc])
                S.op("act", lambda: nc.scalar.copy(out=obf[:], in_=oacc[:]), r=[oacc], w=[obf])
                for j in range(2):
                    S.op("pe", lambda j=j: nc.tensor.transpose(pT[:, 128 + j * 128:256 + j * 128], obf[:, j * 128:(j + 1) * 128], ident[:]), r=[obf, ident], w=[pT])
                S.op("act", lambda: nc.scalar.copy(out=ost[:], in_=pT[:, 128:384].rearrange("p (j q) -> p j q", j=2)), r=[pT], w=[ost])
                S.dma("sp", G.mixT[g * 256:(g + 1) * 256, q0:q0 + 128].rearrange("(j p) q -> p j q", p=128), ost[:], r=[ost], w=["mixT"])
        S.barrier(G.bar)


def phase_c(G):
    nc, S, I = G.nc, G.S, G.I
    with ExitStack() as st:
        sb = lambda n, s, d: st.enter_context(nc.sbuf_tensor(n, s, d))
        ps = lambda n, s, d: st.enter_context(nc.psum_tensor(n, s, d))
        identf = sb("c_identf", [128, 128], F32)
        mu = sb("c_mu", [128, 3520], F32)
        bc = {}
        for nm in ("rwkv_w0", "rwkv_a0", "rwkv_k_k", "rwkv_k_a"):
            bc[nm] = sb("c_" + nm, [128, 1024], F32)
        wup = sb("c_wup", [96, 1024], F32)
        aup = sb("c_aup", [96, 1024], F32)
        gup = sb("c_gup", [128, 2, 1024], F32)
        z = [sb(f"c_z{i}", [128, 3520], F32) for i in range(2)]
        zp = [sb(f"c_zp{i}", [128, 3520], F32) for i in range(2)]
        lT = sb("c_lT", [128, 4, 128], F32)
        wv = sb("c_wv", [128, 1024], F32)
        av = sb("c_av", [128, 1024], F32)
        gv = sb("c_gv", [128, 1024], F32)
        kk = sb("c_kk", [128, 1024], F32)
        sq = sb("c_sq", [128, 1024], F32)
        ss = sb("c_ss", [128, 16], F32)
        km = sb("c_km", [128, 1024], F32)
        bv = sb("c_bv", [128, 1024], F32)
        pT = ps("c_pT", [128, 512], F32)
        pW = [ps(f"c_pW{i}", [128, 512], F32) for i in range(6)]
        S.dma("sp", identf[:], I["c_ident"][:, :], w=[identf])
        S.dma("sp", mu[:], I["rwkv_mu"][0:1, :].partition_broadcast(128), w=[mu])
        for nm in bc:
            S.dma("sp", bc[nm][:], I[nm][0:1, :].partition_broadcast(128), w=[bc[nm]])
        S.dma("sp", wup[:], I["rwkv_w_up"][:, :], w=[wup])
        S.dma("sp", aup[:], I["rwkv_a_up"][:, :], w=[aup])
        S.dma("sp", gup[:], I["rwkv_g_up"].rearrange("(c p) n -> p c n", p=128), w=[gup])
        for tl in range(32):
            tok0 = tl * 128
            own = tl >= 16
            zt, zpt = z[tl % 2], zp[tl % 2]
            S.dma("sp", zt[:], G.zr[tok0:tok0 + 128, :], r=["zr"], w=[zt])
            if tl == 0:
                S.op("pool", lambda: nc.gpsimd.memset(zpt[0:1, :], 0.0), w=[zpt])
                S.dma("sp", zpt[1:128, :], G.zr[0:127, :], r=["zr", zpt], w=[zpt])
            else:
                S.dma("sp", zpt[:], G.zr[tok0 - 1:tok0 + 127, :], r=["zr"], w=[zpt])
            S.op("pool", lambda: nc.gpsimd.tensor_tensor(out=zpt[:], in0=zpt[:], in1=zt[:], op=ALU.subtract), r=[zpt, zt], w=[zpt])
            S.op("dve", lambda: nc.vector.tensor_tensor(out=zpt[:], in0=zpt[:], in1=mu[:], op=ALU.mult), r=[zpt, mu], w=[zpt])
            S.op("pool", lambda: nc.gpsimd.tensor_tensor(out=zt[:], in0=zt[:], in1=zpt[:], op=ALU.add), r=[zpt, zt], w=[zt])
            r_ = zt[:, 0:1024]
            k_ = zt[:, 1024:2048]
            v_ = zt[:, 2048:3072]
            S.op("pe", lambda: nc.tensor.transpose(pT[0:96, 0:128], zt[:, 3072:3168], identf[:]), r=[zt, identf], w=[pT])
            S.op("pe", lambda: nc.tensor.transpose(pT[0:96, 128:256], zt[:, 3168:3264], identf[:]), r=[zt, identf], w=[pT])
            S.op("act", lambda: nc.scalar.activation(out=lT[0:96, 0, :], in_=pT[0:96, 0:128], func=AF.Tanh), r=[pT], w=[lT])
            S.op("dve", lambda: nc.vector.tensor_copy(out=lT[0:96, 1, :], in_=pT[0:96, 128:256]), r=[pT], w=[lT])
            if own:
                for j in range(2):
                    S.op("pe", lambda j=j: nc.tensor.transpose(pT[:, 256 + j * 128:384 + j * 128], zt[:, 3264 + j * 128:3392 + j * 128], identf[:]), r=[zt, identf], w=[pT])
                S.op("act", lambda: nc.scalar.activation(out=lT[:, 2:4, :], in_=pT[:, 256:512].rearrange("p (a b) -> p a b", a=2), func=AF.Sigmoid), r=[pT], w=[lT])
            for hh in range(2):
                cs = slice(hh * 512, (hh + 1) * 512)
                S.op("pe", lambda hh=hh, cs=cs: nc.tensor.matmul(pW[hh][:, :], lhsT=lT[0:96, 0, :], rhs=wup[:, cs], start=True, stop=True), r=[lT, wup], w=[pW[hh]])
                S.op("pe", lambda hh=hh, cs=cs: nc.tensor.matmul(pW[2 + hh][:, :], lhsT=lT[0:96, 1, :], rhs=aup[:, cs], start=True, stop=True), r=[lT, aup], w=[pW[2 + hh]])
                S.op("dve", lambda hh=hh, cs=cs: nc.vector.tensor_tensor(out=wv[:, cs], in0=pW[hh][:, :], in1=bc["rwkv_w0"][:, cs], op=ALU.add), r=[pW[hh], bc["rwkv_w0"]], w=[wv])
                S.op("dve", lambda hh=hh, cs=cs: nc.vector.tensor_tensor(out=av[:, cs], in0=pW[2 + hh][:, :], in1=bc["rwkv_a0"][:, cs], op=ALU.add), r=[pW[2 + hh], bc["rwkv_a0"]], w=[av])
                if own:
                    for j in range(2):
                        S.op("pe", lambda hh=hh, cs=cs, j=j: nc.tensor.matmul(pW[4 + hh][:, :], lhsT=lT[:, 2 + j, :], rhs=gup[:, j, cs], start=(j == 0), stop=(j == 1)),
                             r=[lT, gup], w=[pW[4 + hh]])
                    S.op("act", lambda hh=hh, cs=cs: nc.scalar.copy(out=gv[:, cs], in_=pW[4 + hh][:, :]), r=[pW[4 + hh]], w=[gv])
            S.op("act", lambda: nc.scalar.activation(out=wv[:], in_=wv[:], func=AF.Sigmoid), r=[wv], w=[wv])
            S.op("pool", lambda: nc.gpsimd.tensor_scalar(out=wv[:], in0=wv[:], scalar1=-0.6065306597126334, scalar2=None, op0=ALU.mult), r=[wv], w=[wv])
            S.op("act", lambda: nc.scalar.activation(out=av[:], in_=av[:], func=AF.Sigmoid), r=[av], w=[av])
            S.op("dve", lambda: nc.vector.tensor_tensor(out=kk[:], in0=k_, in1=bc["rwkv_k_k"][:], op=ALU.mult), r=[zt, bc["rwkv_k_k"]], w=[kk])
            S.op("pool", lambda: nc.gpsimd.tensor_tensor(out=sq[:], in0=kk[:], in1=kk[:], op=ALU.mult), r=[kk], w=[sq])
            S.op("dve", lambda: nc.vector.tensor_reduce(out=ss[:], in_=sq[:].rearrange("p (h k) -> p h k", k=64), axis=AX.X, op=ALU.add), r=[sq], w=[ss])
            S.op("act", lambda: nc.scalar.activation(out=ss[:], in_=ss[:], func=AF.Sqrt), r=[ss], w=[ss])
            S.op("dve", lambda: nc.vector.tensor_scalar(out=ss[:], in0=ss[:], scalar1=1e-12, scalar2=None, op0=ALU.max), r=[ss], w=[ss])
            S.op("dve", lambda: nc.vector.reciprocal(out=ss[:], in_=ss[:]), r=[ss], w=[ss])
            S.op("dve", lambda: nc.vector.tensor_tensor(out=kk[:].rearrange("p (h k) -> p h k", k=64), in0=kk[:].rearrange("p (h k) -> p h k", k=64),
                                                        in1=ss[:].unsqueeze(2).to_broadcast([128, 16, 64]), op=ALU.mult), r=[kk, ss], w=[kk])
            S.op("dve", lambda: nc.vector.scalar_tensor_tensor(out=km[:], in0=av[:], scalar=-1.0, in1=bc["rwkv_k_a"][:], op0=ALU.add, op1=ALU.mult), r=[av, bc["rwkv_k_a"]], w=[km])
            S.op("dve", lambda: nc.vector.scalar_tensor_tensor(out=km[:], in0=km[:], scalar=1.0, in1=k_, op0=ALU.add, op1=ALU.mult), r=[km, zt], w=[km])
            S.op("pool", lambda: nc.gpsimd.tensor_tensor(out=bv[:], in0=kk[:], in1=av[:], op=ALU.mult), r=[kk, av], w=[bv])
            rows = slice(tok0, tok0 + 128)
            S.dma("sp", G.rw_r[rows, :], r_, r=[zt], w=["rw_r"])
            S.dma("sp", G.rw_v[rows, :], v_, r=[zt], w=["rw_v"])
            S.dma("sp", G.rw_k[rows, :], km[:], r=[km], w=["rw_k"])
            S.dma("sp", G.rw_kn[rows, :], kk[:], r=[kk], w=["rw_kn"])
            S.dma("sp", G.rw_b[rows, :], bv[:], r=[bv], w=["rw_b"])
            S.dma("sp", G.rw_lw[rows, :], wv[:], r=[wv], w=["rw_lw"])
            if own:
                S.dma("sp", G.rw_g[tok0 - 2048:tok0 - 1920, :], gv[:], r=[gv], w=["rw_g"])
        S.barrier(G.bar)
    with ExitStack() as st:
        sb = lambda n, s, d: st.enter_context(nc.sbuf_tensor(n, s, d))
        ps = lambda n, s, d: st.enter_context(nc.psum_tensor(n, s, d))
        crw = sb("s_crw", [64, 448], F32)
        MM = sb("s_MM", [64, 320], F32)
        srcs = [G.rw_lw, G.rw_kn, G.rw_r, G.rw_b, G.rw_k, G.rw_v]
        keys = ["rw_lw", "rw_kn", "rw_r", "rw_b", "rw_k", "rw_v"]
        blk = [[[sb(f"s_in{p}_{hh}_{a}", [64, 8, 64], F32) for a in range(6)] for hh in range(4)] for p in range(2)]
        Hs = [[sb(f"s_H{h}_{p}", [64, 64], F32) for p in range(2)] for h in range(16)]
        NW = 4
        E = [sb(f"s_E{i}", [64, 256], F32) for i in range(NW)]
        FM = [sb(f"s_FM{i}", [64, 256], F32) for i in range(NW)]
        BK = [sb(f"s_BK{i}", [64, 128], F32) for i in range(NW)]
        GM = [sb(f"s_GM{i}", [64, 320], F32) for i in range(NW)]
        XX = [[sb(f"s_XX{i}_{p}", [64, 128], F32) for p in range(2)] for i in range(NW)]
        P2 = [[sb(f"s_P2{i}_{p}", [64, 128], F32) for p in range(2)] for i in range(NW)]
        RU = [sb(f"s_RU{i}", [64, 64], F32) for i in range(NW)]
        U = [sb(f"s_U{i}", [64, 64], F32) for i in range(NW)]
        ybuf = [[sb(f"s_y{p}_{hh}", [64, 8, 64], F32) for hh in range(4)] for p in range(2)]
        pA = ps("s_pA", [64, 256], F32)
        pB = ps("s_pB", [64, 256], F32)
        pC = ps("s_pC", [64, 320], F32)
        pD = ps("s_pD", [64, 128], F32)
        pE = ps("s_pE", [64, 128], F32)
        pF = ps("s_pF", [64, 128], F32)
        pG = ps("s_pG", [64, 64], F32)
        pH = ps("s_pH", [64, 64], F32)
        S.dma("sp", crw[:], I["c_rw"][:, :], w=[crw])
        S.op("dve", lambda: nc.vector.tensor_copy(out=MM[:, 0:128], in_=crw[:, 128:256]), r=[crw], w=[MM])
        S.op("dve", lambda: nc.vector.tensor_copy(out=MM[:, 128:320], in_=crw[:, 128:320]), r=[crw, MM], w=[MM])
        TB = crw[:, 0:128]
        I64 = crw[:, 320:384]
        INC = crw[:, 384:448]
        for h in range(16):
            S.op("pool", lambda h=h: nc.gpsimd.memset(Hs[h][0][:], 0.0), w=[Hs[h][0]])
        nwk = 0
        for hg in range(4):
            for b8 in range(8):
                par = (hg * 8 + b8) % 2
                for hh in range(4):
                    h = hg * 4 + hh
                    for a in range(6):
                        S.dma("sp", blk[par][hh][a][:], srcs[a][b8 * 512:(b8 + 1) * 512, h * 64:(h + 1) * 64].rearrange("(c t) k -> t c k", t=64),
                              r=[keys[a]], w=[blk[par][hh][a]])
                own = b8 >= 4
                for c in range(8):
                    cg = b8 * 8 + c
                    for hh in range(4):
                        h = hg * 4 + hh
                        LW, KN, R, B, K, V = [blk[par][hh][a][:, c, :] for a in range(6)]
                        tl = blk[par][hh]
                        w_ = nwk % NW
                        nwk += 1
                        e_, fm, bk, gm, ru, u_ = E[w_], FM[w_], BK[w_], GM[w_], RU[w_], U[w_]
                        Hc, Hn = Hs[h][cg % 2], Hs[h][(cg + 1) % 2]
                        S.op("pe", lambda: nc.tensor.matmul(pA[:, 0:128], lhsT=LW, rhs=TB, start=True, stop=True), r=[tl[0], crw], w=[pA])
                        S.op("pe", lambda: nc.tensor.matmul(pA[:, 128:192], lhsT=INC, rhs=LW, start=True, stop=True), r=[tl[0], crw], w=[pA])
                        S.op("act", lambda: nc.scalar.activation(out=e_[:, 0:128], in_=pA[:, 0:128], func=AF.Exp), r=[pA], w=[e_])
                        S.op("act", lambda: nc.scalar.activation(out=e_[:, 128:256].rearrange("p (a b) -> p a b", a=2),
                                                                 in_=pA[:, 0:256].rearrange("p (a b) -> p a b", a=2)[:, :, 0:64], func=AF.Exp, scale=-1.0), r=[pA], w=[e_])
                        for j, (src, ti) in enumerate(((KN, 1), (R, 2), (B, 3), (K, 4))):
                            S.op("pe", lambda j=j, src=src: nc.tensor.transpose(pB[:, j * 64:(j + 1) * 64], src, I64), r=[tl[ti], crw], w=[pB])
                        S.op("dve", lambda: nc.vector.scalar_tensor_tensor(out=fm[:, 0:64], in0=pB[:, 0:64], scalar=-1.0, in1=e_[:, 64:128], op0=ALU.mult, op1=ALU.mult),
                             r=[pB, e_], w=[fm])
                        S.op("dve", lambda: nc.vector.tensor_tensor(out=fm[:, 64:128], in0=pB[:, 64:128], in1=e_[:, 0:64], op=ALU.mult), r=[pB, e_, fm], w=[fm])
                        S.op("dve", lambda: nc.vector.tensor_tensor(out=fm[:, 128:256].rearrange("p (a b) -> p a b", a=2), in0=pB[:, 128:256].rearrange("p (a b) -> p a b", a=2),
                                                                    in1=e_[:, 128:192].unsqueeze(1).to_broadcast([64, 2, 64]), op=ALU.mult), r=[pB, e_, fm], w=[fm])
                        S.op("pool", lambda: nc.gpsimd.tensor_tensor(out=bk[:, 0:64], in0=B, in1=e_[:, 192:256], op=ALU.mult), r=[tl[3], e_], w=[bk])
                        S.op("pool", lambda: nc.gpsimd.tensor_tensor(out=bk[:, 64:128], in0=K, in1=e_[:, 192:256], op=ALU.mult), r=[tl[4], e_, bk], w=[bk])
                        AT, RT, BT, KT = fm[:, 0:64], fm[:, 64:128], fm[:, 128:192], fm[:, 192:256]
                        S.op("pe", lambda: nc.tensor.matmul(pC[:, 0:128], lhsT=BT, rhs=fm[:, 0:128], start=True, stop=True), r=[fm], w=[pC])
                        S.op("pe", lambda: nc.tensor.matmul(pC[:, 128:256], lhsT=KT, rhs=fm[:, 0:128], start=True, stop=True), r=[fm], w=[pC])
                        S.op("pe", lambda: nc.tensor.matmul(pC[:, 256:320], lhsT=AT, rhs=BT, start=True, stop=True), r=[fm], w=[pC])
                        S.op("dve", lambda: nc.vector.tensor_tensor(out=gm[:], in0=pC[:, :], in1=MM[:], op=ALU.mult), r=[pC, MM], w=[gm])
                        N_, MrbT, LakT, MrkT, NT = gm[:, 0:64], gm[:, 64:128], gm[:, 128:192], gm[:, 192:256], gm[:, 256:320]
                        xx = XX[w_]
                        p2 = P2[w_]
                        S.op("pool", lambda: nc.gpsimd.tensor_tensor(out=xx[0][:, 0:64], in0=N_, in1=I64, op=ALU.add), r=[gm, crw], w=[xx[0]])
                        S.op("pool", lambda: nc.gpsimd.tensor_tensor(out=xx[0][:, 64:128], in0=NT, in1=I64, op=ALU.add), r=[gm, crw, xx[0]], w=[xx[0]])
                        Pc, PTc, pk = N_, NT, gm
                        for k in range(5):
                            last = (k == 4)
                            pn = p2[k % 2]
                            S.op("pe", lambda Pc=Pc, PTc=PTc: nc.tensor.matmul(pD[:, 0:64], lhsT=PTc, rhs=Pc, start=True, stop=True), r=[pk], w=[pD])
                            if not last:
                                S.op("pe", lambda Pc=Pc, PTc=PTc: nc.tensor.matmul(pD[:, 64:128], lhsT=Pc, rhs=PTc, start=True, stop=True), r=[pk], w=[pD])
                                S.op("act", lambda pn=pn: nc.scalar.copy(out=pn[:], in_=pD[:, :]), r=[pD], w=[pn])
                            else:
                                S.op("act", lambda pn=pn: nc.scalar.copy(out=pn[:, 0:64], in_=pD[:, 0:64]), r=[pD], w=[pn])
                            xc_, xn_ = xx[k % 2], xx[(k + 1) % 2]
                            S.op("pe", lambda xc_=xc_, pn=pn: nc.tensor.matmul(pE[:, 0:64], lhsT=xc_[:, 64:128], rhs=pn[:, 0:64], start=True, stop=True), r=[xc_, pn], w=[pE])
                            if not last:
                                S.op("pe", lambda xc_=xc_, pn=pn: nc.tensor.matmul(pE[:, 64:128], lhsT=pn[:, 0:64], rhs=xc_[:, 64:128], start=True, stop=True), r=[xc_, pn], w=[pE])
                                S.op("dve", lambda xc_=xc_, xn_=xn_: nc.vector.tensor_tensor(out=xn_[:], in0=pE[:, :], in1=xc_[:], op=ALU.add), r=[pE, xc_], w=[xn_])
                            else:
                                S.op("dve", lambda xc_=xc_, xn_=xn_: nc.vector.tensor_tensor(out=xn_[:, 0:64], in0=pE[:, 0:64], in1=xc_[:, 0:64], op=ALU.add), r=[pE, xc_], w=[xn_])
                            Pc, PTc, pk = pn[:, 0:64], pn[:, 64:128], pn
                        X = xx[1][:, 0:64]
                        xk = xx[1]
                        S.op("pe", lambda: nc.tensor.matmul(pF[:, 0:64], lhsT=AT, rhs=Hc[:], start=True, stop=False), r=[fm, Hc], w=[pF])
                        S.op("pe", lambda: nc.tensor.matmul(pF[:, 0:64], lhsT=LakT, rhs=V, start=False, stop=True), r=[gm, tl[5]], w=[pF])
                        S.op("act", lambda: nc.scalar.copy(out=ru[:], in_=pF[:, 0:64]), r=[pF], w=[ru])
                        S.op("pe", lambda: nc.tensor.matmul(pF[:, 64:128], lhsT=X, rhs=ru[:], start=True, stop=True), r=[xk, ru], w=[pF])
                        S.op("dve", lambda: nc.vector.tensor_copy(out=u_[:], in_=pF[:, 64:128]), r=[pF], w=[u_])
                        if own:
                            yb = ybuf[par][hh]
                            S.op("pe", lambda: nc.tensor.matmul(pG[:, :], lhsT=RT, rhs=Hc[:], start=True, stop=False), r=[fm, Hc], w=[pG])
                            S.op("pe", lambda: nc.tensor.matmul(pG[:, :], lhsT=MrbT, rhs=u_[:], start=False, stop=False), r=[gm, u_], w=[pG])
                            S.op("pe", lambda: nc.tensor.matmul(pG[:, :], lhsT=MrkT, rhs=V, start=False, stop=True), r=[gm, tl[5]], w=[pG])
                            S.op("act", lambda: nc.scalar.copy(out=yb[:, c, :], in_=pG[:, :]), r=[pG], w=[yb])
                        S.op("pe", lambda: nc.tensor.matmul(pH[:, :], lhsT=I64, rhs=Hc[:], start=True, stop=False), r=[crw, Hc], w=[pH])
                        S.op("pe", lambda: nc.tensor.matmul(pH[:, :], lhsT=bk[:, 0:64], rhs=u_[:], start=False, stop=False), r=[bk, u_], w=[pH])
                        S.op("pe", lambda: nc.tensor.matmul(pH[:, :], lhsT=bk[:, 64:128], rhs=V, start=False, stop=True), r=[bk, tl[5]], w=[pH])
                        S.op("dve", lambda: nc.vector.tensor_scalar(out=Hn[:], in0=pH[:, :], scalar1=e_[:, 63:64], scalar2=None, op0=ALU.mult), r=[pH, e_], w=[Hn])
                if own:
                    for hh in range(4):
                        h = hg * 4 + hh
                        r0 = (b8 - 4) * 512
                        S.dma("sp", G.rw_y[r0:r0 + 512, h * 64:(h + 1) * 64].rearrange("(c t) k -> t c k", t=64), ybuf[par][hh][:], r=[ybuf[par][hh]], w=["rw_y"])
        S.barrier(G.bar)
    with ExitStack() as st:
        sb = lambda n, s, d: st.enter_context(nc.sbuf_tensor(n, s, d))
        ps = lambda n, s, d: st.enter_context(nc.psum_tensor(n, s, d))
        ident = sb("p_ident", [128, 128], BF16)
        bc = {}
        for nm in ("rwkv_ln_g", "rwkv_ln_b", "rwkv_r_k"):
            bc[nm] = sb("p_" + nm, [128, 1024], F32)
            S.dma("sp", bc[nm][:], I[nm][0:1, :].partition_broadcast(128), w=[bc[nm]])
        S.dma("pool", ident[:], I["c_ident"][:, :], w=[ident])
        y = [sb(f"p_y{i}", [128, 1024], F32) for i in range(2)]
        rr = [sb(f"p_r{i}", [128, 1024], F32) for i in range(2)]
        kq = [sb(f"p_k{i}", [128, 1024], F32) for i in range(2)]
        vv = [sb(f"p_v{i}", [128, 1024], F32) for i in range(2)]
        gg = [sb(f"p_g{i}", [128, 1024], F32) for i in range(2)]
        sq = sb("p_sq", [128, 1024], F32)
        st1 = sb("p_st1", [128, 16], F32)
        st2 = sb("p_st2", [128, 16], F32)
        st3 = sb("p_st3", [128, 16], F32)
        obf = sb("p_obf", [128, 1024], BF16)
        ost = sb("p_ost", [128, 8, 128], BF16)
        pT = ps("p_pT", [128, 1024], BF16)
        v3 = lambda t: t[:].rearrange("p (h k) -> p h k", k=64)
        b3 = lambda t: t[:].unsqueeze(2).to_broadcast([128, 16, 64])
        for tl in range(16):
            p = tl % 2
            rows = slice(tl * 128, (tl + 1) * 128)
            crow = slice(2048 + tl * 128, 2048 + (tl + 1) * 128)
            yt, rt, kt, vt, gt = y[p], rr[p], kq[p], vv[p], gg[p]
            S.dma("sp", yt[:], G.rw_y[rows, :], r=["rw_y"], w=[yt])
            S.dma("sp", rt[:], G.rw_r[crow, :], r=["rw_r"], w=[rt])
            S.dma("sp", kt[:], G.rw_k[crow, :], r=["rw_k"], w=[kt])
            S.dma("sp", vt[:], G.rw_v[crow, :], r=["rw_v"], w=[vt])
            S.dma("sp", gt[:], G.rw_g[rows, :], r=["rw_g"], w=[gt])
            S.op("dve", lambda: nc.vector.tensor_reduce(out=st1[:], in_=v3(yt), axis=AX.X, op=ALU.add), r=[yt], w=[st1])
            S.op("pool", lambda: nc.gpsimd.tensor_tensor(out=sq[:], in0=yt[:], in1=yt[:], op=ALU.mult), r=[yt], w=[sq])
            S.op("dve", lambda: nc.vector.tensor_reduce(out=st2[:], in_=v3(sq), axis=AX.X, op=ALU.add), r=[sq], w=[st2])
            S.op("dve", lambda: nc.vector.tensor_scalar(out=st1[:], in0=st1[:], scalar1=1.0 / 64.0, scalar2=None, op0=ALU.mult), r=[st1], w=[st1])
            S.op("dve", lambda: nc.vector.tensor_tensor(out=st3[:], in0=st1[:], in1=st1[:], op=ALU.mult), r=[st1], w=[st3])
            S.op("dve", lambda: nc.vector.scalar_tensor_tensor(out=st2[:], in0=st2[:], scalar=1.0 / 64.0, in1=st3[:], op0=ALU.mult, op1=ALU.subtract), r=[st2, st3], w=[st2])
            S.op("dve", lambda: nc.vector.tensor_scalar(out=st2[:], in0=st2[:], scalar1=64e-5, scalar2=None, op0=ALU.add), r=[st2], w=[st2])
            S.op("act", lambda: nc.scalar.activation(out=st2[:], in_=st2[:], func=AF.Sqrt), r=[st2], w=[st2])
            S.op("dve", lambda: nc.vector.reciprocal(out=st2[:], in_=st2[:]), r=[st2], w=[st2])
            S.op("dve", lambda: nc.vector.tensor_tensor(out=v3(yt), in0=v3(yt), in1=b3(st1), op=ALU.subtract), r=[yt, st1], w=[yt])
            S.op("dve", lambda: nc.vector.tensor_tensor(out=v3(yt), in0=v3(yt), in1=b3(st2), op=ALU.mult), r=[yt, st2], w=[yt])
            S.op("pool", lambda: nc.gpsimd.tensor_tensor(out=yt[:], in0=yt[:], in1=bc["rwkv_ln_g"][:], op=ALU.mult), r=[yt, bc["rwkv_ln_g"]], w=[yt])
            S.op("pool", lambda: nc.gpsimd.tensor_tensor(out=yt[:], in0=yt[:], in1=bc["rwkv_ln_b"][:], op=ALU.add), r=[yt, bc["rwkv_ln_b"]], w=[yt])
            S.op("pool", lambda: nc.gpsimd.tensor_tensor(out=rt[:], in0=rt[:], in1=kt[:], op=ALU.mult), r=[rt, kt], w=[rt])
            S.op("pool", lambda: nc.gpsimd.tensor_tensor(out=rt[:], in0=rt[:], in1=bc["rwkv_r_k"][:], op=ALU.mult), r=[rt, bc["rwkv_r_k"]], w=[rt])
            S.op("dve", lambda: nc.vector.tensor_reduce(out=st3[:], in_=v3(rt), axis=AX.X, op=ALU.add), r=[rt], w=[st3])
            S.op("dve", lambda: nc.vector.tensor_tensor(out=v3(vt), in0=v3(vt), in1=b3(st3), op=ALU.mult), r=[vt, st3], w=[vt])
            S.op("pool", lambda: nc.gpsimd.tensor_tensor(out=yt[:], in0=yt[:], in1=vt[:], op=ALU.add), r=[yt, vt], w=[yt])
            S.op("dve", lambda: nc.vector.tensor_tensor(out=obf[:], in0=yt[:], in1=gt[:], op=ALU.mult), r=[yt, gt], w=[obf])
            for j in range(8):
                S.op("pe", lambda j=j: nc.tensor.transpose(pT[:, j * 128:(j + 1) * 128], obf[:, j * 128:(j + 1) * 128], ident[:]), r=[obf, ident], w=[pT])
            S.op("act", lambda: nc.scalar.copy(out=ost[:], in_=pT[:, :].rearrange("p (j q) -> p j q", j=8)), r=[pT], w=[ost])
            S.dma("sp", G.mixT[1024:2048, tl * 128:(tl + 1) * 128].rearrange("(j p) q -> p j q", p=128), ost[:], r=[ost], w=["mixT"])
        S.barrier(G.bar)


def layer_norm_tile(G, x, g_bc, b_bc, sq, st, eps=1e-5):
    nc, S = G.nc, G.S
    S.op("dve", lambda: nc.vector.tensor_reduce(out=st[:, 0:1], in_=x[:], axis=AX.X, op=ALU.add), r=[x], w=[st])
    S.op("pool", lambda: nc.gpsimd.tensor_tensor(out=sq[:], in0=x[:], in1=x[:], op=ALU.mult), r=[x], w=[sq])
    S.op("dve", lambda: nc.vector.tensor_reduce(out=st[:, 1:2], in_=sq[:], axis=AX.X, op=ALU.add), r=[sq, st], w=[st])
    S.op("dve", lambda: nc.vector.tensor_scalar(out=st[:, 0:2], in0=st[:, 0:2], scalar1=1.0 / D, scalar2=None, op0=ALU.mult), r=[st], w=[st])
    S.op("dve", lambda: nc.vector.tensor_tensor(out=st[:, 2:3], in0=st[:, 0:1], in1=st[:, 0:1], op=ALU.mult), r=[st], w=[st])
    S.op("dve", lambda: nc.vector.tensor_tensor(out=st[:, 1:2], in0=st[:, 1:2], in1=st[:, 2:3], op=ALU.subtract), r=[st], w=[st])
    S.op("dve", lambda: nc.vector.tensor_scalar(out=st[:, 1:2], in0=st[:, 1:2], scalar1=eps, scalar2=None, op0=ALU.add), r=[st], w=[st])
    S.op("act", lambda: nc.scalar.activation(out=st[:, 1:2], in_=st[:, 1:2], func=AF.Sqrt), r=[st], w=[st])
    S.op("dve", lambda: nc.vector.reciprocal(out=st[:, 1:2], in_=st[:, 1:2]), r=[st], w=[st])
    S.op("dve", lambda: nc.vector.tensor_scalar(out=x[:], in0=x[:], scalar1=st[:, 0:1], scalar2=st[:, 1:2], op0=ALU.subtract, op1=ALU.mult), r=[x, st], w=[x])
    S.op("pool", lambda: nc.gpsimd.tensor_tensor(out=x[:], in0=x[:], in1=g_bc[:], op=ALU.mult), r=[x, g_bc], w=[x])
    S.op("pool", lambda: nc.gpsimd.tensor_tensor(out=x[:], in0=x[:], in1=b_bc[:], op=ALU.add), r=[x, b_bc], w=[x])


def phase_d(G):
    nc, S, I = G.nc, G.S, G.I
    with ExitStack() as st:
        sb = lambda n, s, d: st.enter_context(nc.sbuf_tensor(n, s, d))
        ps = lambda n, s, d: st.enter_context(nc.psum_tensor(n, s, d))
        ident = sb("d_ident", [128, 128], BF16)
        identf = sb("d_identf", [128, 128], F32)
        mixT = sb("d_mixT", [128, 16, TO], BF16)
        wout = sb("d_wout", [128, 16, D], BF16)
        gbc = sb("d_g", [128, D], F32)
        bbc = sb("d_b", [128, D], F32)
        rw = sb("d_rw", [128, 16, 32], F32)
        rb = sb("d_rb", [128, 32], F32)
        xt = [sb(f"d_x{i}", [128, D], F32) for i in range(2)]
        hp = [sb(f"d_hp{i}", [128, D], F32) for i in range(2)]
        sq = sb("d_sq", [128, D], F32)
        stt = sb("d_st", [128, 4], F32)
        hbf = sb("d_hbf", [128, D], BF16)
        hTs = sb("d_hTs", [128, 16, 128], BF16)
        hT32 = sb("d_hT32", [128, 16, 128], F32)
        lg = sb("d_lg", [128, 32], F32)
        ex = sb("d_ex", [128, 32], F32)
        m8 = sb("d_m8", [128, 8], F32)
        sm = sb("d_sm", [128, 2], F32)
        pO = [ps(f"d_pO{i}", [128, 512], F32) for i in range(4)]
        pTf = [ps(f"d_pTf{i}", [128, 512], F32) for i in range(2)]
        pL = ps("d_pL", [128, 32], F32)
        pTb = ps("d_pTb", [128, 1024], BF16)
        S.dma("pool", ident[:], I["c_ident"][:, :], w=[ident])
        S.dma("sp", identf[:], I["c_ident"][:, :], w=[identf])
        mv = G.mixT.rearrange("(kc p) t -> p kc t", p=128)
        wv = I["w_out"].rearrange("(kc p) c -> p kc c", p=128)
        for j in range(4):
            S.dma("sp", mixT[:, j * 4:(j + 1) * 4, :], mv[:, j * 4:(j + 1) * 4, :], r=["mixT"], w=[mixT])
            S.dma("pool", wout[:, :, j * 512:(j + 1) * 512], wv[:, :, j * 512:(j + 1) * 512], w=[wout])
        S.dma("sp", gbc[:], I["ln1_g"][0:1, :].partition_broadcast(128), w=[gbc])
        S.dma("sp", bbc[:], I["ln1_b"][0:1, :].partition_broadcast(128), w=[bbc])
        S.dma("sp", rw[:], I["router_w"].rearrange("(kc p) e -> p kc e", p=128), w=[rw])
        S.dma("sp", rb[:], I["router_b"][0:1, :].partition_broadcast(128), w=[rb])
        for tl in range(16):
            x_ = xt[tl % 2]
            h_ = hp[tl % 2]
            rows = slice(tl * 128, (tl + 1) * 128)
            S.dma("sp", x_[:], I["xc"][2048 + tl * 128:2048 + (tl + 1) * 128, :], w=[x_])
            for c4 in range(4):
                for kc in range(16):
                    S.op("pe", lambda c4=c4, kc=kc: nc.tensor.matmul(pO[c4][:, :], lhsT=mixT[:, kc, tl * 128:(tl + 1) * 128], rhs=wout[:, kc, c4 * 512:(c4 + 1) * 512],
                                                                       start=(kc == 0), stop=(kc == 15)), r=[mixT, wout], w=[pO[c4]])
                S.op("dve", lambda c4=c4: nc.vector.scalar_tensor_tensor(out=h_[:, c4 * 512:(c4 + 1) * 512], in0=x_[:, c4 * 512:(c4 + 1) * 512], scalar=ALPHA, in1=pO[c4][:, :],
                                                                          op0=ALU.mult, op1=ALU.add), r=[x_, pO[c4]], w=[h_])
            layer_norm_tile(G, h_, gbc, bbc, sq, stt)
            S.dma("sp", G.h1[rows, :], h_[:], r=[h_], w=["h1"])
            S.op("act", lambda: nc.scalar.copy(out=hbf[:], in_=h_[:]), r=[h_], w=[hbf])
            for j in range(2):
                for k8 in range(8):
                    kc = j * 8 + k8
                    S.op("pe", lambda k8=k8, kc=kc: nc.tensor.transpose(pTb[:, k8 * 128:(k8 + 1) * 128], hbf[:, kc * 128:(kc + 1) * 128], ident[:]), r=[hbf, ident], w=[pTb])
                evac(G, hTs[:, j * 8:(j + 1) * 8, :], pTb[:].rearrange("p (a b) -> p a b", a=8), r=[pTb], w=[hTs])
            S.dma("sp", G.h1T[:, rows].rearrange("(kc p) t -> p kc t", p=128), hTs[:], r=[hTs], w=["h1T"])
            for j in range(4):
                pt = pTf[j % 2]
                for k4 in range(4):
                    kc = j * 4 + k4
                    S.op("pe", lambda k4=k4, kc=kc, pt=pt: nc.tensor.transpose(pt[:, k4 * 128:(k4 + 1) * 128], h_[:, kc * 128:(kc + 1) * 128], identf[:]), r=[h_, identf], w=[pt])
                evac(G, hT32[:, j * 4:(j + 1) * 4, :], pt[:].rearrange("p (a b) -> p a b", a=4), r=[pt], w=[hT32])
            for kc in range(16):
                S.op("pe", lambda kc=kc: nc.tensor.matmul(pL[:, :], lhsT=hT32[:, kc, :], rhs=rw[:, kc, :], start=(kc == 0), stop=(kc == 15)), r=[hT32, rw], w=[pL])
            S.op("dve", lambda: nc.vector.tensor_tensor(out=lg[:], in0=pL[:, :], in1=rb[:], op=ALU.add), r=[pL, rb], w=[lg])
            S.op("dve", lambda: nc.vector.max(out=m8[:], in_=lg[:]), r=[lg], w=[m8])
            S.op("dve", lambda: nc.vector.tensor_scalar(out=sm[:, 0:1], in0=m8[:, 0:1], scalar1=-1.0, scalar2=None, op0=ALU.mult), r=[m8], w=[sm])
            S.op("act", lambda: nc.scalar.activation(out=ex[:], in_=lg[:], func=AF.Exp, bias=sm[:, 0:1], scale=1.0), r=[lg, sm], w=[ex])
            S.op("dve", lambda: nc.vector.tensor_tensor(out=lg[:], in0=lg[:], in1=m8[:, 3:4].to_broadcast([128, 32]), op=ALU.is_ge), r=[lg, m8], w=[lg])
            S.op("dve", lambda: nc.vector.tensor_tensor(out=ex[:], in0=ex[:], in1=lg[:], op=ALU.mult), r=[ex, lg], w=[ex])
            S.op("dve", lambda: nc.vector.tensor_reduce(out=sm[:, 1:2], in_=ex[:], axis=AX.X, op=ALU.add), r=[ex, sm], w=[sm])
            S.op("dve", lambda: nc.vector.reciprocal(out=sm[:, 1:2], in_=sm[:, 1:2]), r=[sm], w=[sm])
            S.op("dve", lambda: nc.vector.tensor_scalar(out=ex[:], in0=ex[:], scalar1=sm[:, 1:2], scalar2=None, op0=ALU.mult), r=[ex, sm], w=[ex])
            S.dma("sp", G.gw[rows, :], ex[:], r=[ex], w=["gw"])
        S.barrier(G.bar)


def phase_e(G, experts=32):
    nc, S, I = G.nc, G.S, G.I
    LIM = 7.0
    with ExitStack() as st:
        sb = lambda n, s, d: st.enter_context(nc.sbuf_tensor(n, s, d))
        ps = lambda n, s, d: st.enter_context(nc.psum_tensor(n, s, d))
        identf = sb("e_identf", [128, 128], F32)
        h1T = sb("e_h1T", [128, 16, 1024], BF16)
        Y = sb("e_Y", [128, 8, D], F32)
        gw = sb("e_gw", [128, 8, 32], F32)
        gwT = sb("e_gwT", [32, 8, 128], F32)
        bdn = sb("e_bdn", [32, D], F32)
        bg = sb("e_bg", [128, 512], F32)
        bu = sb("e_bu", [128, 512], F32)
        wg = [sb(f"e_wg{i}", [128, 16, 256], BF16) for i in range(2)]
        wu = [sb(f"e_wu{i}", [128, 16, 256], BF16) for i in range(2)]
        wd = [sb(f"e_wd{i}", [128, 2, D], BF16) for i in range(2)]
        hT = [sb(f"e_hT{i}", [128, 2, 1024], BF16) for i in range(2)]
        gt = [sb(f"e_g{i}", [128, 512], F32) for i in range(2)]
        sg = [sb(f"e_sg{i}", [128, 512], F32) for i in range(2)]
        ut = [sb(f"e_u{i}", [128, 512], F32) for i in range(2)]
        ev = [sb(f"e_ev{i}", [128, 512], F32) for i in range(3)]
        h1t = [sb(f"e_h1t{i}", [128, D], F32) for i in range(1)]
        pGU = [ps(f"e_pGU{i}", [128, 512], F32) for i in range(4)]
        pDn = [ps(f"e_pD{i}", [128, 512], F32) for i in range(4)]
        S.dma("sp", identf[:], I["c_ident"][:, :], w=[identf])
        S.dma("sp", bdn[:], I["exp_b_down"][:, :], w=[bdn])
        S.dma("sp", bg[:], I["exp_b_gate"][:, :], w=[bg])
        S.dma("sp", bu[:], I["exp_b_up"][:, :], w=[bu])
        wgv = I["exp_w_gate"].rearrange("(e kc p) f -> e p kc f", p=128, kc=16)
        wuv = I["exp_w_up"].rearrange("(e kc p) f -> e p kc f", p=128, kc=16)
        wdv = I["exp_w_down"].rearrange("(e fc p) d -> e p fc d", p=128, fc=16)
        nst = 0
        YK = [[("Y", tl, d4) for d4 in range(4)] for tl in range(8)]
        YALL = [k for row in YK for k in row]
        for hf in range(2):
            t0 = hf * 1024
            S.dma("sp", h1T[:], G.h1T[:, t0:t0 + 1024].rearrange("(kc p) t -> p kc t", p=128), r=["h1T"], w=[h1T])
            S.dma("sp", gw[:], G.gw[t0:t0 + 1024, :].rearrange("(t p) e -> p t e", p=128), r=["gw"], w=[gw])
            S.op("pool", lambda: nc.gpsimd.memset(Y[:], 0.0), w=YALL)
            for e in range(experts):
                for fgp in range(8):
                    b = nst % 2
                    nst += 1
                    S.dma("pool", wg[b][:], wgv[e][:, :, fgp * 256:(fgp + 1) * 256], w=[wg[b]])
                    S.dma("pool", wu[b][:], wuv[e][:, :, fgp * 256:(fgp + 1) * 256], w=[wu[b]])
                    S.dma("pool", wd[b][:], wdv[e][:, fgp * 2:(fgp + 1) * 2, :], w=[wd[b]])
                    hb = hT[b]
                    for f2 in range(2):
                        fc = fgp * 2 + f2
                        bcol = e * 16 + fc
                        for tc_ in range(2):
                            pg, pu = pGU[(2 * tc_) % 4], pGU[(2 * tc_ + 1) % 4]
                            for kc in range(16):
                                S.op("pe", lambda kc=kc, pg=pg: nc.tensor.matmul(pg[:, :], lhsT=wg[b][:, kc, f2 * 128:(f2 + 1) * 128], rhs=h1T[:, kc, tc_ * 512:(tc_ + 1) * 512],
                                                                                  start=(kc == 0), stop=(kc == 15)), r=[wg[b], h1T], w=[pg])
                            for kc in range(16):
                                S.op("pe", lambda kc=kc, pu=pu: nc.tensor.matmul(pu[:, :], lhsT=wu[b][:, kc, f2 * 128:(f2 + 1) * 128], rhs=h1T[:, kc, tc_ * 512:(tc_ + 1) * 512],
                                                                                  start=(kc == 0), stop=(kc == 15)), r=[wu[b], h1T], w=[pu])
                            g_, s_, u_ = gt[tc_], sg[tc_], ut[tc_]
                            S.op("dve", lambda: nc.vector.tensor_scalar(out=g_[:], in0=pg[:, :], scalar1=bg[:, bcol:bcol + 1], scalar2=LIM, op0=ALU.add, op1=ALU.min), r=[pg, bg], w=[g_])
                            S.op("act", lambda: nc.scalar.activation(out=s_[:], in_=g_[:], func=AF.Sigmoid, scale=1.702), r=[g_], w=[s_])
                            S.op("dve", lambda: nc.vector.tensor_scalar(out=u_[:], in0=pu[:, :], scalar1=bu[:, bcol:bcol + 1], scalar2=LIM, op0=ALU.add, op1=ALU.min), r=[pu, bu], w=[u_])
                            S.op("dve", lambda: nc.vector.tensor_scalar(out=u_[:], in0=u_[:], scalar1=-LIM, scalar2=1.0, op0=ALU.max, op1=ALU.add), r=[u_], w=[u_])
                            S.op("dve", lambda: nc.vector.tensor_tensor(out=g_[:], in0=g_[:], in1=s_[:], op=ALU.mult), r=[g_, s_], w=[g_])
                            S.op("dve", lambda: nc.vector.tensor_tensor(out=hb[:, f2, tc_ * 512:(tc_ + 1) * 512], in0=g_[:], in1=u_[:], op=ALU.mult), r=[g_, u_], w=[hb])
                    for tl in range(8):
                        for d4 in range(4):
                            pd = pDn[(tl * 4 + d4) % 4]
                            for f2 in range(2):
                                S.op("pe", lambda f2=f2, pd=pd, d4=d4, tl=tl: nc.tensor.matmul(pd[:, :], lhsT=hb[:, f2, tl * 128:(tl + 1) * 128], rhs=wd[b][:, f2, d4 * 512:(d4 + 1) * 512],
                                                                                                 start=(f2 == 0), stop=(f2 == 1)), r=[hb, wd[b]], w=[pd])
                            S.op("dve", lambda pd=pd, tl=tl, d4=d4: nc.vector.scalar_tensor_tensor(out=Y[:, tl, d4 * 512:(d4 + 1) * 512], in0=pd[:, :], scalar=gw[:, tl, e:e + 1],
                                                                                                     in1=Y[:, tl, d4 * 512:(d4 + 1) * 512], op0=ALU.mult, op1=ALU.add),
                                 r=[pd, gw, YK[tl][d4]], w=[YK[tl][d4]])
            for tl in range(8):
                S.op("pe", lambda tl=tl: nc.tensor.transpose(pGU[0][0:32, 0:128], gw[:, tl, :], identf[:]), r=[gw, identf], w=[pGU[0]])
                S.op("dve", lambda tl=tl: nc.vector.tensor_copy(out=gwT[:, tl, :], in_=pGU[0][0:32, 0:128]), r=[pGU[0]], w=[gwT])
                ht = h1t[0]
                rows = slice(t0 + tl * 128, t0 + (tl + 1) * 128)
                S.dma("sp", ht[:], G.h1[rows, :], r=["h1"], w=[ht])
                for d4 in range(4):
                    pd = pDn[d4]
                    S.op("pe", lambda pd=pd, d4=d4, tl=tl: nc.tensor.matmul(pd[:, :], lhsT=gwT[:, tl, :], rhs=bdn[:, d4 * 512:(d4 + 1) * 512], start=True, stop=True), r=[gwT, bdn], w=[pd])
                    S.op("dve", lambda pd=pd, d4=d4, tl=tl: nc.vector.tensor_tensor(out=Y[:, tl, d4 * 512:(d4 + 1) * 512], in0=Y[:, tl, d4 * 512:(d4 + 1) * 512], in1=pd[:, :], op=ALU.add),
                         r=[pd, YK[tl][d4]], w=[YK[tl][d4]])
                S.op("dve", lambda tl=tl, ht=ht: nc.vector.scalar_tensor_tensor(out=ht[:], in0=ht[:], scalar=ALPHA, in1=Y[:, tl, :], op0=ALU.mult, op1=ALU.add), r=[ht] + YK[tl], w=[ht])
                S.dma("sp", G.ypre[rows, :], ht[:], r=[ht], w=["ypre"])
        S.barrier(G.bar)


def phase_f(G):
    nc, S, I = G.nc, G.S, G.I
    with ExitStack() as st:
        sb = lambda n, s, d: st.enter_context(nc.sbuf_tensor(n, s, d))
        ps = lambda n, s, d: st.enter_context(nc.psum_tensor(n, s, d))
        ident = sb("f_ident", [128, 128], BF16)
        pgw = sb("f_pgw", [128, 16, D], BF16)
        plw = sb("f_plw", [128, 2, D], BF16)
        gbc = sb("f_g", [128, D], F32)
        bbc = sb("f_b", [128, D], F32)
        yt = [sb(f"f_y{i}", [128, D], F32) for i in range(2)]
        ot = [sb(f"f_o{i}", [128, D], F32) for i in range(2)]
        sq = sb("f_sq", [128, D], F32)
        stt = sb("f_st", [128, 4], F32)
        hbf = sb("f_hbf", [128, D], BF16)
        pb = [sb(f"f_pb{i}", [128, 256], BF16) for i in range(2)]
        hTs = sb("f_hTs", [128, 16, 128], BF16)
        pTs = sb("f_pTs", [128, 2, 128], BF16)
        sgt = [sb(f"f_sg{i}", [128, 512], F32) for i in range(2)]
        pGt = [ps(f"f_pG{i}", [128, 512], F32) for i in range(2)]
        pPt = [ps(f"f_pP{i}", [128, 512], F32) for i in range(2)]
        pTb = ps("f_pTb", [128, 1024], BF16)
        S.dma("pool", ident[:], I["c_ident"][:, :], w=[ident])
        wv = I["ple_gate_w"].rearrange("(kc p) c -> p kc c", p=128)
        for j in range(4):
            S.dma("pool", pgw[:, :, j * 512:(j + 1) * 512], wv[:, :, j * 512:(j + 1) * 512], w=[pgw])
        S.dma("pool", plw[:], I["ple_w"].rearrange("(kc p) c -> p kc c", p=128), w=[plw])
        S.dma("sp", gbc[:], I["ln2_g"][0:1, :].partition_broadcast(128), w=[gbc])
        S.dma("sp", bbc[:], I["ln2_b"][0:1, :].partition_broadcast(128), w=[bbc])
        for tl in range(16):
            rows = slice(tl * 128, (tl + 1) * 128)
            y_ = yt[tl % 2]
            o_ = ot[tl % 2]
            p_ = pb[tl % 2]
            S.dma("sp", y_[:], G.ypre[rows, :], r=["ypre"], w=[y_])
            S.dma("pool", p_[:], I["p_own"][rows, :], w=[p_])
            layer_norm_tile(G, y_, gbc, bbc, sq, stt)
            S.op("act", lambda: nc.scalar.copy(out=hbf[:], in_=y_[:]), r=[y_], w=[hbf])
            for j in range(2):
                for k8 in range(8):
                    kc = j * 8 + k8
                    S.op("pe", lambda k8=k8, kc=kc: nc.tensor.transpose(pTb[:, k8 * 128:(k8 + 1) * 128], hbf[:, kc * 128:(kc + 1) * 128], ident[:]), r=[hbf, ident], w=[pTb])
                evac(G, hTs[:, j * 8:(j + 1) * 8, :], pTb[:].rearrange("p (a b) -> p a b", a=8), r=[pTb], w=[hTs])
            for j in range(2):
                S.op("pe", lambda j=j: nc.tensor.transpose(pTb[:, j * 128:(j + 1) * 128], p_[:, j * 128:(j + 1) * 128], ident[:]), r=[p_, ident], w=[pTb])
            evac(G, pTs[:], pTb[:, 0:256].rearrange("p (a b) -> p a b", a=2), r=[pTb], w=[pTs])
            for d4 in range(4):
                cs = slice(d4 * 512, (d4 + 1) * 512)
                pg, pp, s_ = pGt[d4 % 2], pPt[d4 % 2], sgt[d4 % 2]
                for kc in range(16):
                    S.op("pe", lambda kc=kc, pg=pg, cs=cs: nc.tensor.matmul(pg[:, :], lhsT=hTs[:, kc, :], rhs=pgw[:, kc, cs], start=(kc == 0), stop=(kc == 15)), r=[hTs, pgw], w=[pg])
                for kc in range(2):
                    S.op("pe", lambda kc=kc, pp=pp, cs=cs: nc.tensor.matmul(pp[:, :], lhsT=pTs[:, kc, :], rhs=plw[:, kc, cs], start=(kc == 0), stop=(kc == 1)), r=[pTs, plw], w=[pp])
                S.op("act", lambda pg=pg, s_=s_: nc.scalar.activation(out=s_[:], in_=pg[:, :], func=AF.Sigmoid), r=[pg], w=[s_])
                S.op("dve", lambda pp=pp, s_=s_: nc.vector.tensor_tensor(out=s_[:], in0=s_[:], in1=pp[:, :], op=ALU.mult), r=[pp, s_], w=[s_])
                S.op("dve", lambda s_=s_, cs=cs: nc.vector.tensor_tensor(out=o_[:, cs], in0=y_[:, cs], in1=s_[:], op=ALU.add), r=[y_, s_], w=[o_])
            S.dma("sp", G.out[rows, :], o_[:], r=[o_], w=["out"])


def make_consts(s):
    c = {}
    kv = np.ones((T,), np.float32)
    if s == 0:
        kv[:2048] = 0.0
    c["c_kvalid"] = np.ascontiguousarray(kv.reshape(32, 128).T)
    c["c_ident"] = np.eye(128, dtype=np.float32)
    k = np.arange(128)[:, None]
    q = np.arange(128)[None, :]
    c["c_trile"] = (k <= q).astype(np.float32)
    c["c_trigt"] = (k > q).astype(np.float32)
    ab = np.zeros((4, 128, 4, 128), np.float32)
    cb = np.zeros((4, 2, 128, 4, 128), np.float32)
    for g in range(4):
        for h in range(4):
            sl = SLOPES[4 * g + h]
            ab[g, :, h, :] = -sl * (q - k)
            for ct in range(2):
                cb[g, ct, :, h, :] = -sl * (q - 16 * (k + 128 * ct) - 31)
    c["c_abias"] = ab.reshape(4 * 128, 512)
    c["c_cbias"] = cb.reshape(4 * 2 * 128, 512)
    cval = np.zeros((256, 1), np.float32)
    cval[(128 if s == 0 else 0):255] = 1.0
    cm = np.zeros((16, 2, 128, 128), np.float32)
    for i in range(16):
        for ct in range(2):
            cc = k + 128 * ct
            d = 2048 + 128 * i + q - 16 * cc - 31
            cm[i, ct] = (d >= 0) * cval[cc[:, 0]]
    c["c_cmask"] = ((cm - 1.0) * BIG).reshape(16 * 2 * 128, 128).astype(np.float32)
    c["c_tribias"] = np.concatenate([(c["c_trile"] - 1.0) * BIG, (c["c_trigt"] - 1.0) * BIG], axis=1).astype(np.float32)
    c["c_expand"] = (np.arange(T)[None, :] // 64 == np.arange(64)[:, None]).astype(np.float32)
    c["c_expbig"] = c["c_expand"] * BIG
    ce = np.arange(256)[:, None] * 16 + 31
    cs = ce - 31
    ss = np.arange(64)[None, :] * 64
    c["c_overlap"] = np.clip(np.minimum(ce, ss + 63) - np.maximum(cs, ss) + 1, 0, None).astype(np.float32)
    j0 = 32 if s == 0 else 0
    qi = np.arange(TO)[:, None]
    cur = 32 + qi // 64
    j = np.arange(64)[None, :]
    invalid = (j < j0) | (j > cur)
    forced = ((j == j0) | (j == cur) | (j == cur - 1)) & ~invalid
    c["c_selmul"] = (~invalid & ~forced).astype(np.float32)
    c["c_seladd"] = np.where(invalid, -BIG, np.where(forced, BIG, 0.0)).astype(np.float32)
    c["c_selvalid"] = (~invalid).astype(np.float32)
    s_ = np.arange(64)[:, None]
    t_ = np.arange(64)[None, :]
    incl = (s_ <= t_).astype(np.float32)
    strict = (s_ < t_).astype(np.float32)
    low = (t_ < s_).astype(np.float32)
    c["c_rw"] = np.concatenate([incl, strict, strict, incl, low, np.eye(64, dtype=np.float32), incl], axis=1)
    return c


def prep_core_inputs(inputs, c, consts_cache={}):
    b, s = c // 2, c % 2
    m = {}
    x = inputs["x"]
    if s == 1:
        m["xc"] = np.ascontiguousarray(x[b])
    else:
        m["xc"] = np.concatenate([np.zeros((2048, D), np.float32), x[b, :2048]], axis=0)
    m["p_own"] = np.ascontiguousarray(inputs["p"][0, b, 2048 * s:2048 * s + 2048])
    for name, shape in INPUT_SPECS:
        if name in ("xc", "p_own") or name.startswith("c_"):
            continue
        a = np.asarray(inputs[name], np.float32)
        if name in ("exp_b_gate", "exp_b_up"):
            a = np.ascontiguousarray(a.reshape(32, 16, 128).transpose(2, 0, 1))
        m[name] = a.reshape(shape)
    if s not in consts_cache:
        consts_cache[s] = make_consts(s)
    m.update(consts_cache[s])
    return m


def kernel(**inputs):
    nc, G = build()
    in_maps = [prep_core_inputs(inputs, c) for c in range(NCORES)]
    res = run_bass_kernel_spmd(nc, in_maps, core_ids=list(range(NCORES)))
    out = np.zeros((4, 4096, D), np.float32)
    for c in range(NCORES):
        b, s = c // 2, c % 2
        out[b, 2048 * s:2048 * s + 2048] = res.results[c]["out"]
    return out
```

```python
import os
import numpy as np
from contextlib import ExitStack
import concourse.bass as bass
import concourse.mybir as mybir
from concourse.bass_utils import run_bass_kernel_spmd

F32 = mybir.dt.float32
BF16 = mybir.dt.bfloat16
ALU = mybir.AluOpType
AF = mybir.ActivationFunctionType
AX = mybir.AxisListType

NCORES = 8
D = 2048
T = 4096
TO = 2048
NSA_COLS = 2608
RW0 = NSA_COLS
IN_COLS = 6128
SEM_ROT = 12000


class Sync:
    ENGS = ("pe", "act", "dve", "pool", "sp")

    def __init__(self, nc, stack):
        self.nc = nc
        self.stack = stack
        self.eng = {"pe": nc.tensor, "act": nc.scalar, "dve": nc.vector,
                    "pool": nc.gpsimd, "sp": nc.sync}
        self.cnt = {e: 0 for e in self.ENGS}
        self.sems = {e: [] for e in self.ENGS}
        self.waited = {e: {} for e in self.ENGS}
        self.last_w = {}
        self.readers = {}
        self.dma_pool = {}
        self.dma_n = {}
        self.all_dma = []
        self.nsem = 0
        for q in ("sp", "pool", "act"):
            self.dma_pool[q] = [self._newsem() for _ in range(12 if q == "sp" else 6)]
            self.dma_n[q] = 0

    def _newsem(self):
        self.nsem += 1
        return self.stack.enter_context(self.nc.semaphore(f"s{self.nsem}"))

    @staticmethod
    def _key(t):
        return t if isinstance(t, (str, tuple)) else t.tensor.name if hasattr(t, "tensor") else t.name

    def _wait(self, e, tok):
        sem, val, src = tok
        if src == e and e == "pe":
            return
        w = self.waited[e]
        k = id(sem)
        if w.get(k, 0) >= val:
            return
        w[k] = val
        self.eng[e].wait_ge(sem, val)

    def _deps(self, e, r, w):
        toks = []
        for t in r:
            k = self._key(t)
            if k in self.last_w:
                toks.append(self.last_w[k])
        for t in w:
            k = self._key(t)
            if k in self.last_w:
                toks.append(self.last_w[k])
            for tk in self.readers.get(k, {}).values():
                if isinstance(tk, list):
                    toks.extend(tk)
                else:
                    toks.append(tk)
        for tk in toks:
            self._wait(e, tk)

    def _record(self, tok, r, w, isdma):
        for t in w:
            k = self._key(t)
            self.last_w[k] = tok
            self.readers[k] = {}
        for t in r:
            k = self._key(t)
            d = self.readers.setdefault(k, {})
            if isdma:
                d.setdefault("dma", []).append(tok)
                if len(d["dma"]) > 24:
                    d["dma"] = d["dma"][-24:]
            else:
                d[tok[2]] = tok

    def op(self, e, fn, r=(), w=()):
        self._deps(e, r, w)
        n = self.cnt[e]
        si, v = divmod(n, SEM_ROT)
        while len(self.sems[e]) <= si:
            self.sems[e].append(self._newsem())
        sem = self.sems[e][si]
        ins = fn()
        ins.then_inc(sem, 1)
        self.cnt[e] = n + 1
        tok = (sem, v + 1, e)
        self._record(tok, r, w, False)
        return tok

    def dma(self, q, out, in_, r=(), w=(), **kw):
        self._deps(q, r, w)
        n = self.dma_n[q]
        pool = self.dma_pool[q]
        sem = pool[n % len(pool)]
        rnd = n // len(pool)
        if rnd > 0:
            self._wait(q, (sem, 16 * rnd, "dma"))
        self.eng[q].dma_start(out=out, in_=in_, **kw).then_inc(sem, 16)
        self.dma_n[q] = n + 1
        tok = (sem, 16 * (rnd + 1), "dma")
        self._record(tok, r, w, True)
        self.all_dma.append(tok)
        if len(self.all_dma) > 64:
            self.all_dma = self.all_dma[-64:]
        return tok

    def barrier(self, scratch):
        for q in self.dma_pool:
            n = self.dma_n[q]
            pool = self.dma_pool[q]
            for i, sem in enumerate(pool):
                uses = (n - i + len(pool) - 1) // len(pool) if n > i else 0
                if uses > 0:
                    self._wait("dve", (sem, 16 * uses, "dma"))
        toks = []
        for e in ("pe", "act", "pool"):
            if self.cnt[e] > 0:
                n = self.cnt[e] - 1
                si, v = divmod(n, SEM_ROT)
                toks.append((self.sems[e][si], v + 1, e))
        for tk in toks:
            self._wait("dve", tk)
        tok = self.op("dve", lambda: self.nc.vector.memset(scratch[0:1, 0:1], 0.0), w=["_bar"])
        for e in ("pe", "act", "pool", "sp"):
            self._wait(e, tok)
        self.last_w = {}
        self.readers = {}

    def finish(self):
        for q in self.dma_pool:
            n = self.dma_n[q]
            pool = self.dma_pool[q]
            for i, sem in enumerate(pool):
                uses = (n - i + len(pool) - 1) // len(pool) if n > i else 0
                if uses > 0:
                    self._wait("sp", (sem, 16 * uses, "dma"))
        for e in ("pe", "act", "dve", "pool"):
            if self.cnt[e] > 0:
                n = self.cnt[e] - 1
                si, v = divmod(n, SEM_ROT)
                self._wait("sp", (self.sems[e][si], v + 1, e))


SLOPES = [2.0 ** (-8.0 * (i + 1) / 16.0) for i in range(16)]
SCALE = 64 ** -0.5
ALPHA = 2.0 ** 0.25
BIG = 1.0e30

INPUT_SPECS = [
    ("xc", [T, D]), ("p_own", [TO, 256]), ("w_in", [D, IN_COLS]),
    ("cmp_pe_k", [32, 64]), ("cmp_w1_k", [2048, 64]), ("cmp_w2_k", [64, 64]),
    ("cmp_pe_v", [32, 64]), ("cmp_w1_v", [2048, 64]), ("cmp_w2_v", [64, 64]),
    ("rwkv_mu", [1, 3520]), ("rwkv_w0", [1, 1024]), ("rwkv_w_up", [96, 1024]),
    ("rwkv_a0", [1, 1024]), ("rwkv_a_up", [96, 1024]), ("rwkv_g_up", [256, 1024]),
    ("rwkv_k_k", [1, 1024]), ("rwkv_k_a", [1, 1024]), ("rwkv_r_k", [1, 1024]),
    ("rwkv_ln_g", [1, 1024]), ("rwkv_ln_b", [1, 1024]),
    ("w_out", [D, D]), ("ln1_g", [1, D]), ("ln1_b", [1, D]),
    ("router_w", [D, 32]), ("router_b", [1, 32]),
    ("exp_w_gate", [32 * D, D]), ("exp_b_gate", [128, 512]),
    ("exp_w_up", [32 * D, D]), ("exp_b_up", [128, 512]),
    ("exp_w_down", [32 * D, D]), ("exp_b_down", [32, D]),
    ("ln2_g", [1, D]), ("ln2_b", [1, D]), ("ple_w", [256, D]), ("ple_gate_w", [D, D]),
    ("c_kvalid", [128, 32]), ("c_ident", [128, 128]), ("c_trile", [128, 128]), ("c_trigt", [128, 128]),
    ("c_abias", [4 * 128, 512]), ("c_cbias", [4 * 2 * 128, 512]), ("c_cmask", [16 * 2 * 128, 128]),
    ("c_expand", [64, T]), ("c_overlap", [256, 64]), ("c_tribias", [128, 256]), ("c_expbig", [64, T]),
    ("c_selmul", [TO, 64]), ("c_seladd", [TO, 64]), ("c_selvalid", [TO, 64]),
    ("c_rw", [64, 448]),
]


class Ctx:
    pass


def build(debug=(), phases="ABCDEF", skip=()):
    nc = bass.Bass("TRN2", target_bir_lowering=False)
    G = Ctx()
    G.nc = nc
    G.I = {}
    for name, shape in INPUT_SPECS:
        if name in skip:
            continue
        G.I[name] = nc.dram_tensor(name, shape, F32, kind="ExternalInput").ap()
    G.out = nc.dram_tensor("out", [TO, D], F32, kind="ExternalOutput").ap()
    G.dbg = {}
    G.debug = debug
    dr = lambda n, s, d: nc.dram_tensor(n, s, d).ap()
    G.vs_tm = dr("vs_tm", [T, 4 * 65], BF16)
    G.vw_tm = dr("vw_tm", [T, 4 * 65], BF16)
    G.gl_tm = dr("gl_tm", [TO, 48], F32)
    G.zr = dr("zr", [T, 3520], F32)
    G.qT = dr("qT", [1024, TO], BF16)
    G.kcT = dr("kcT", [256, T], BF16)
    G.vcT = dr("vcT", [256, T], BF16)
    G.ksT = dr("ksT", [256, T], BF16)
    G.kwT = dr("kwT", [256, T], BF16)
    G.mixT = dr("mixT", [D, TO], BF16)
    G.h1 = dr("h1", [TO, D], F32)
    G.h1T = dr("h1T", [D, TO], BF16)
    G.gw = dr("gw", [TO, 32], F32)
    G.ypre = dr("ypre", [TO, D], F32)
    for n in ("rw_r", "rw_k", "rw_v", "rw_kn", "rw_b", "rw_lw"):
        setattr(G, n, dr(n, [T, 1024], F32))
    G.rw_g = dr("rw_g", [TO, 1024], F32)
    G.rw_y = dr("rw_y", [TO, 1024], F32)
    with ExitStack() as st:
        S = Sync(nc, st)
        G.S = S
        G.bar = st.enter_context(nc.sbuf_tensor("barscr", [128, 8], F32))
        for nm, shp in debug:
            dbg_out(G, nm, shp)
        if "A" in phases:
            phase_a(G)
        if "B" in phases:
            phase_b(G)
        if "C" in phases:
            phase_c(G)
        if "D" in phases:
            phase_d(G)
        if "E" in phases:
            phase_e(G)
        if "F" in phases:
            phase_f(G)
        S.finish()
    return nc, G


def dbg_out(G, name, shape, dt=F32):
    t = G.nc.dram_tensor("dbg_" + name, shape, dt, kind="ExternalOutput").ap()
    G.dbg[name] = t
    return t


_evac_rr = [0]


def evac(G, out, in_, r, w, scale=None):
    nc, S = G.nc, G.S
    _evac_rr[0] ^= 1
    if _evac_rr[0]:
        if scale is None:
            return S.op("act", lambda: nc.scalar.copy(out=out, in_=in_), r=r, w=w)
        return S.op("act", lambda: nc.scalar.mul(out=out, in_=in_, mul=scale), r=r, w=w)
    if scale is None:
        return S.op("dve", lambda: nc.vector.tensor_copy(out=out, in_=in_), r=r, w=w)
    return S.op("dve", lambda: nc.vector.tensor_scalar(out=out, in0=in_, scalar1=scale, scalar2=None, op0=ALU.mult), r=r, w=w)


def run_pipelined(gens, depth):
    active = []
    it = iter(gens)
    more = True
    while True:
        if more and len(active) < depth:
            try:
                active.append(next(it))
            except StopIteration:
                more = False
        if not active:
            break
        for g in list(active):
            try:
                next(g)
            except StopIteration:
                active.remove(g)


def phase_a(G):
    nc, S, I = G.nc, G.S, G.I
    with ExitStack() as st:
        sb = lambda n, s, d: st.enter_context(nc.sbuf_tensor(n, s, d))
        ps = lambda n, s, d: st.enter_context(nc.psum_tensor(n, s, d))
        ident = sb("a_ident", [128, 128], BF16)
        kval = sb("a_kval", [128, 32], F32)
        xb = [sb(f"a_xb{i}", [128, D], BF16) for i in range(2)]
        xT = sb("a_xT", [128, 16, 2048], BF16)
        wch = [sb(f"a_w{i}", [128, 16, 512], BF16) for i in range(2)]
        stf = [sb(f"a_stf{i}", [128, 512], F32) for i in range(3)]
        stb = [sb(f"a_stb{i}", [128, 512], BF16) for i in range(3)]
        vst = [sb(f"a_vst{i}", [128, 4, 65], BF16) for i in range(2)]
        ptr = [ps(f"a_ptr{i}", [128, 1024], BF16) for i in range(2)]
        pac = [ps(f"a_pac{i}", [128, 512], F32) for i in range(4)]
        S.dma("pool", ident[:], I["c_ident"][:, :], w=[ident])
        S.dma("sp", kval[:], I["c_kvalid"][:, :], w=[kval])
        w_in_v = I["w_in"].rearrange("(kc p) c -> p kc c", p=128)
        tm_chunks = [("vs", 1792, 2048), ("vw", 2304, 2560), ("gl", 2560, 2608)]
        c = 0
        while c < 3520:
            n = min(512, 3520 - c)
            tm_chunks.append(("rw", RW0 + c, RW0 + c + n))
            c += n
        fm_chunks = [("q", 128 * j, 128 * j + 128) for j in range(8)]
        for nm, base in (("kc", 1024), ("vc", 1280), ("ks", 1536), ("kw", 2048)):
            fm_chunks += [(nm, base, base + 128), (nm, base + 128, base + 256)]
        fm_dst = {"q": (G.qT, 0), "kc": (G.kcT, 1024), "vc": (G.vcT, 1280), "ks": (G.ksT, 1536), "kw": (G.kwT, 2048)}
        nw = 0
        nst = 0
        npac = 0
        for hf in range(2):
            for tl in range(16):
                tok0 = hf * 2048 + tl * 128
                xbt = xb[tl % 2]
                S.dma("pool", xbt[:], I["xc"][tok0:tok0 + 128, :], w=[xbt])
                for j in range(2):
                    pt = ptr[j]
                    for k8 in range(8):
                        kc = j * 8 + k8
                        S.op("pe", lambda pt=pt, k8=k8, kc=kc, xbt=xbt: nc.tensor.transpose(
                            pt[:, k8 * 128:(k8 + 1) * 128], xbt[:, kc * 128:(kc + 1) * 128], ident[:]),
                            r=[xbt, ident], w=[pt])
                    evac(G, xT[:, j * 8:(j + 1) * 8, tl * 128:(tl + 1) * 128],
                         pt[:].rearrange("p (a b) -> p a b", a=8), r=[pt], w=[xT])
            for (nm, c0, c1) in tm_chunks:
                if nm == "gl" and hf == 0:
                    continue
                ncol = c1 - c0
                wt = wch[nw % 2]
                nw += 1
                S.dma("pool", wt[:, :, 0:ncol], w_in_v[:, :, c0:c1], w=[wt])
                for tl in range(16):
                    tok0 = hf * 2048 + tl * 128
                    pa = pac[npac % 4]
                    npac += 1
                    for kc in range(16):
                        S.op("pe", lambda pa=pa, kc=kc, wt=wt, tl=tl, ncol=ncol: nc.tensor.matmul(
                            pa[:, 0:ncol], lhsT=xT[:, kc, tl * 128:(tl + 1) * 128], rhs=wt[:, kc, 0:ncol],
                            start=(kc == 0), stop=(kc == 15)), r=[xT, wt], w=[pa])
                    if nm in ("vs", "vw"):
                        vt = vst[nst % 2]
                        nst += 1
                        evac(G, vt[:, :, 0:64], pa[:, 0:256].rearrange("p (g d) -> p g d", g=4), r=[pa], w=[vt])
                        tglob = hf * 16 + tl
                        S.op("pool", lambda vt=vt, tglob=tglob: nc.gpsimd.tensor_copy(
                            out=vt[:, :, 64], in_=kval[:, tglob:tglob + 1].to_broadcast([128, 4])),
                            r=[kval, vt], w=[vt])
                        dst = G.vs_tm if nm == "vs" else G.vw_tm
                        S.dma("sp", dst[tok0:tok0 + 128, :], vt[:].rearrange("p g d -> p (g d)"), r=[vt], w=[nm + "_tm"])
                    else:
                        sf = stf[nst % 3]
                        nst += 1
                        evac(G, sf[:, 0:ncol], pa[:, 0:ncol], r=[pa], w=[sf])
                        if nm == "gl":
                            S.dma("sp", G.gl_tm[tl * 128:(tl + 1) * 128, :], sf[:, 0:48], r=[sf], w=["gl_tm"])
                        else:
                            S.dma("sp", G.zr[tok0:tok0 + 128, c0 - RW0:c1 - RW0], sf[:, 0:ncol], r=[sf], w=["zr"])
            for (nm, c0, c1) in fm_chunks:
                if nm == "q" and hf == 0:
                    continue
                wt = wch[nw % 2]
                nw += 1
                S.dma("pool", wt[:, :, 0:128], w_in_v[:, :, c0:c1], w=[wt])
                dst, base = fm_dst[nm]
                for t4 in range(4):
                    pa = pac[npac % 4]
                    npac += 1
                    for kc in range(16):
                        S.op("pe", lambda pa=pa, kc=kc, wt=wt, t4=t4: nc.tensor.matmul(
                            pa[:, :], lhsT=wt[:, kc, 0:128], rhs=xT[:, kc, t4 * 512:(t4 + 1) * 512],
                            start=(kc == 0), stop=(kc == 15)), r=[xT, wt], w=[pa])
                    sbt = stb[nst % 3]
                    nst += 1
                    evac(G, sbt[:, :], pa[:, :], r=[pa], w=[sbt])
                    if nm == "q":
                        col0 = t4 * 512
                    else:
                        col0 = hf * 2048 + t4 * 512
                    S.dma("sp", dst[c0 - base:c1 - base, col0:col0 + 512], sbt[:, :], r=[sbt], w=[nm + "T"])
        S.barrier(G.bar)


def phase_b(G):
    nc, S, I = G.nc, G.S, G.I
    with ExitStack() as st:
        sb = lambda n, s, d: st.enter_context(nc.sbuf_tensor(n, s, d))
        ps = lambda n, s, d: st.enter_context(nc.psum_tensor(n, s, d))
        ident = sb("b_ident", [128, 128], BF16)
        tribias = sb("b_tribias", [128, 256], F32)
        tribias_bf = sb("b_tribias_bf", [128, 256], BF16)
        expbig = sb("b_expbig", [64, T], BF16)
        cmask = sb("b_cmask", [128, 32, 128], BF16)
        abias = sb("b_abias", [128, 512], F32)
        cbias = sb("b_cbias", [128, 2, 512], F32)
        ksT = sb("b_ksT", [64, T], BF16)
        kwT = sb("b_kwT", [64, T], BF16)
        cT = sb("b_cT", [64, T], BF16)
        vs = sb("b_vs", [128, 32, 65], BF16)
        vw = sb("b_vw", [128, 32, 65], BF16)
        qT = sb("b_qT", [64, 4, TO], BF16)
        w1 = [sb(f"b_w1{i}", [128, 16, 64], F32) for i in range(2)]
        w1b = [sb(f"b_w1b{i}", [64, 32, 64], BF16) for i in range(2)]
        w2b = [sb(f"b_w2b{i}", [64, 64], BF16) for i in range(2)]
        pec = [sb(f"b_pec{i}", [128, 16], F32) for i in range(2)]
        b1 = [sb(f"b_b1{i}", [64, 1], F32) for i in range(2)]
        hf = sb("b_hf", [64, 256], F32)
        h2 = sb("b_h2", [64, 256], F32)
        gT = sb("b_gT", [64, 256], BF16)
        kcmpT = sb("b_kcmpT", [64, 256], BF16)
        vcx = sb("b_vcx", [128, 2, 129], BF16)
        tmp = [sb(f"b_tmp{i}", [128, 512], F32) for i in range(2)]
        tmp2 = [sb(f"b_tmq{i}", [128, 512], F32) for i in range(2)]
        Pt = [sb(f"b_P{i}", [128, 512], BF16) for i in range(3)]
        glt = sb("b_gl", [128, 48], F32)
        gsig = sb("b_gsig", [128, 48], F32)
        selc = [sb(f"b_selc{i}", [128, 64], F32) for i in range(3)]
        imp = sb("b_imp", [128, 64], F32)
        imp2 = sb("b_imp2", [128, 64], F32)
        m8 = sb("b_m8", [128, 16], F32)
        selb = sb("b_selb", [128, 64], BF16)
        selT = sb("b_selT", [64, 512], BF16)
        tri4 = sb("b_tri4", [128, 512], BF16)
        sums = sb("b_sums", [128, 12], F32)
        coef = sb("b_coef", [128, 12], F32)
        oacc = sb("b_oacc", [128, 256], F32)
        obf = sb("b_obf", [128, 256], BF16)
        ost = sb("b_ost", [128, 2, 128], BF16)
        pS = [ps(f"b_pS{i}", [128, 512], F32) for i in range(2)]
        pM = ps("b_pM", [128, 512], F32)
        PMK = [("pM", j) for j in range(4)]
        pAs = ps("b_pAs", [128, 4, 65], F32)
        pAw = ps("b_pAw", [128, 4, 65], F32)
        pAc = [ps(f"b_pAc{i}", [128, 2, 129], F32) for i in range(2)]
        pT = ps("b_pT", [128, 1024], BF16)

        S.dma("pool", ident[:], I["c_ident"][:, :], w=[ident])
        S.dma("sp", tribias[:], I["c_tribias"][:, :], w=[tribias])
        S.dma("pool", tribias_bf[:], I["c_tribias"][:, :], w=[tribias_bf])
        S.op("pool", lambda: nc.gpsimd.tensor_copy(out=tri4[:].rearrange("p (h q) -> p h q", h=4), in_=tribias_bf[:, 0:128].unsqueeze(1).to_broadcast([128, 4, 128])), r=[tribias_bf], w=[tri4])
        for j in range(4):
            S.dma("pool", expbig[:, j * 1024:(j + 1) * 1024], I["c_expbig"][:, j * 1024:(j + 1) * 1024], w=[expbig])
        cm_v = I["c_cmask"].rearrange("(a p) q -> p a q", p=128)
        for j in range(4):
            S.dma("pool", cmask[:, j * 8:(j + 1) * 8, :], cm_v[:, j * 8:(j + 1) * 8, :], w=[cmask])
        for kv, (nw1, nw2, npe) in enumerate((("cmp_w1_k", "cmp_w2_k", "cmp_pe_k"), ("cmp_w1_v", "cmp_w2_v", "cmp_pe_v"))):
            S.dma("sp", w1[kv][:], I[nw1].rearrange("(p c) o -> p c o", c=16), w=[w1[kv]])
            S.dma("pool", w1b[kv][:], I[nw1].rearrange("(l d) o -> d l o", d=64), w=[w1b[kv]])
            S.dma("pool", w2b[kv][:], I[nw2][:, :], w=[w2b[kv]])
            S.dma("sp", pec[kv][:], I[npe].rearrange("l d -> (l d)").rearrange("(p c) -> p c", c=16), w=[pec[kv]])
            for ch in range(16):
                S.op("pe", lambda kv=kv, ch=ch: nc.tensor.matmul(pM[0:64, 0:1], lhsT=w1[kv][:, ch, :], rhs=pec[kv][:, ch:ch + 1],
                                                                   start=(ch == 0), stop=(ch == 15)), r=[w1[kv], pec[kv]], w=PMK)
            S.op("dve", lambda kv=kv: nc.vector.tensor_copy(out=b1[kv][:], in_=pM[0:64, 0:1]), r=PMK, w=[b1[kv]])
        S.op("dve", lambda: nc.vector.memset(vcx[:], 0.0), w=[vcx])
        S.op("dve", lambda: nc.vector.memset(vcx[:, :, 64:65], 1.0), r=[vcx], w=[vcx])
        S.dma("pool", vcx[:, :, 65:129], I["c_overlap"].rearrange("(ct p) j -> p ct j", p=128), r=[vcx], w=[vcx])
        S.op("dve", lambda: nc.vector.memset(kcmpT[:], 0.0), w=[kcmpT])

        def compress(kv, g):
            src = G.kcT if kv == 0 else G.vcT
            S.dma("sp", cT[:], src[g * 64:(g + 1) * 64, :], r=["kcT", "vcT"], w=[cT])
            cv = cT[:].rearrange("p (c s) -> p c s", s=16)
            for l in range(32):
                rhs = cv[:, 0:255, l] if l < 16 else cv[:, 1:256, l - 16]
                S.op("pe", lambda l=l, rhs=rhs: nc.tensor.matmul(pM[0:64, 0:255], lhsT=w1b[kv][:, l, :], rhs=rhs,
                                                                  start=(l == 0), stop=(l == 31)), r=[w1b[kv], cT], w=PMK)
            S.op("act", lambda: nc.scalar.activation(out=hf[:, 0:255], in_=pM[0:64, 0:255], func=AF.Identity, bias=b1[kv][:, 0:1], scale=1.0),
                 r=PMK + [b1[kv]], w=[hf])
            S.op("dve", lambda: nc.vector.tensor_tensor(out=h2[:, 0:255], in0=hf[:, 0:255], in1=hf[:, 0:255], op=ALU.mult), r=[hf], w=[h2])
            S.op("dve", lambda: nc.vector.tensor_scalar(out=h2[:, 0:255], in0=h2[:, 0:255], scalar1=0.044715, scalar2=1.0, op0=ALU.mult, op1=ALU.add), r=[h2], w=[h2])
            S.op("dve", lambda: nc.vector.tensor_tensor(out=h2[:, 0:255], in0=h2[:, 0:255], in1=hf[:, 0:255], op=ALU.mult), r=[h2, hf], w=[h2])
            S.op("act", lambda: nc.scalar.activation(out=h2[:, 0:255], in_=h2[:, 0:255], func=AF.Tanh, scale=0.7978845608028654), r=[h2], w=[h2])
            S.op("dve", lambda: nc.vector.scalar_tensor_tensor(out=gT[:, 0:255], in0=h2[:, 0:255], scalar=1.0, in1=hf[:, 0:255], op0=ALU.add, op1=ALU.mult),
                 r=[h2, hf], w=[gT])
            if kv == 0:
                S.op("pe", lambda: nc.tensor.matmul(pM[0:64, 0:255], lhsT=w2b[0][:, :], rhs=gT[:, 0:255], start=True, stop=True), r=[w2b[0], gT], w=PMK)
                S.op("act", lambda: nc.scalar.mul(out=kcmpT[:, 0:255], in_=pM[0:64, 0:255], mul=0.5), r=PMK, w=[kcmpT])
            else:
                for ct in range(2):
                    rows = 128 if ct == 0 else 127
                    S.op("pe", lambda ct=ct, rows=rows: nc.tensor.matmul(pM[0:rows, 0:64], lhsT=gT[:, ct * 128:ct * 128 + rows], rhs=w2b[1][:, :],
                                                                          start=True, stop=True), r=[w2b[1], gT], w=PMK)
                    S.op("act", lambda ct=ct, rows=rows: nc.scalar.mul(out=vcx[0:rows, ct, 0:64], in_=pM[0:rows, 0:64], mul=0.5), r=PMK, w=[vcx])

        np_ = [0]
        for g in range(4):
            S.dma("sp", ksT[:], G.ksT[g * 64:(g + 1) * 64, :], r=["ksT"], w=[ksT])
            S.dma("sp", kwT[:], G.kwT[g * 64:(g + 1) * 64, :], r=["kwT"], w=[kwT])
            S.dma("sp", qT[:], G.qT[g * 256:(g + 1) * 256, :].rearrange("(h d) t -> d h t", d=64), r=["qT"], w=[qT])
            vs_v = G.vs_tm.rearrange("(t p) c -> p t c", p=128)
            vw_v = G.vw_tm.rearrange("(t p) c -> p t c", p=128)
            for j in range(4):
                S.dma("sp", vs[:, j * 8:(j + 1) * 8, :], vs_v[:, j * 8:(j + 1) * 8, g * 65:(g + 1) * 65], r=["vs_tm"], w=[vs])
                S.dma("sp", vw[:, j * 8:(j + 1) * 8, :], vw_v[:, j * 8:(j + 1) * 8, g * 65:(g + 1) * 65], r=["vw_tm"], w=[vw])
            S.dma("sp", abias[:], I["c_abias"][g * 128:(g + 1) * 128, :], w=[abias])
            S.dma("sp", cbias[:], I["c_cbias"][g * 256:(g + 1) * 256, :].rearrange("(ct p) x -> p ct x", p=128), w=[cbias])
            compress(0, g)
            compress(1, g)
            for i in range(16):
                q0 = i * 128
                S.dma("sp", glt[:], G.gl_tm[q0:q0 + 128, :], r=["gl_tm"], w=[glt])
                S.op("act", lambda: nc.scalar.activation(out=gsig[:], in_=glt[:], func=AF.Sigmoid), r=[glt], w=[gsig])
                for j, nm in enumerate(("c_selmul", "c_seladd", "c_selvalid")):
                    S.dma("sp", selc[j][:], I[nm][q0:q0 + 128, :], w=[selc[j]])
                gv = gsig[:, 12 * g:12 * g + 12].rearrange("p (h r) -> p h r", r=3)
                qrhs = qT[:, :, q0:q0 + 128]

                def tile_gen(lhsT, bias_ap, mask_fn, offs, pv_fn, rows=128, extra=()):
                    n = np_[0]
                    np_[0] += 1
                    p_s, t1, t2, P = pS[n % 2], tmp[n % 2], tmp2[n % 2], Pt[n % 3]
                    mk = mask_fn(n) if mask_fn is not None else None
                    S.op("pe", lambda: nc.tensor.matmul(p_s[0:rows, :], lhsT=lhsT, rhs=qrhs, start=True, stop=(len(extra) == 0)), r=[ksT, kwT, kcmpT, qT], w=[p_s])
                    for xi, (xl, xr) in enumerate(extra):
                        S.op("pe", lambda xl=xl, xr=xr, xi=xi: nc.tensor.matmul(p_s[0:rows, :], lhsT=xl, rhs=xr, start=False, stop=(xi == len(extra) - 1)),
                             r=[expbig, selT, ident, tri4], w=[p_s])
                    yield
                    S.op("dve", lambda: nc.vector.scalar_tensor_tensor(out=t1[0:rows, :], in0=p_s[0:rows, :], scalar=SCALE, in1=bias_ap,
                                                                        op0=ALU.mult, op1=ALU.add), r=[p_s, abias, cbias], w=[t1])
                    src = t1
                    if mk is not None:
                        mask_ap, mkey = mk
                        S.op("dve", lambda: nc.vector.tensor_tensor(out=t2[0:rows, :].rearrange("p (h q) -> p h q", h=4),
                                                                     in0=t1[0:rows, :].rearrange("p (h q) -> p h q", h=4),
                                                                     in1=mask_ap, op=ALU.add), r=[t1, mkey], w=[t2])
                        src = t2
                    yield
                    for h in range(4):
                        S.op("act", lambda h=h: nc.scalar.activation(out=P[0:rows, h * 128:(h + 1) * 128], in_=src[0:rows, h * 128:(h + 1) * 128],
                                                                      func=AF.Exp, bias=float(offs[h]), scale=1.0), r=[src], w=[P])
                    yield
                    pv_fn(P)

                Pc = []

                def cmp_tile(ct):
                    rows = 128 if ct == 0 else 127
                    offs = [-SLOPES[4 * g + h] * (2048 + 128 * i) for h in range(4)]

                    def pv(P):
                        Pc.append(P)
                        if ct == 0:
                            return
                        for h in range(4):
                            for c2 in range(2):
                                r2 = 128 if c2 == 0 else 127
                                S.op("pe", lambda h=h, c2=c2, r2=r2: nc.tensor.matmul(pAc[h // 2][:, h % 2, :], lhsT=Pc[c2][0:r2, h * 128:(h + 1) * 128],
                                                                                       rhs=vcx[0:r2, c2, :], start=(c2 == 0), stop=(c2 == 1)),
                                     r=[Pc[c2], vcx], w=[pAc[h // 2]])
                    return tile_gen(kcmpT[:, ct * 128:ct * 128 + rows], cbias[0:rows, ct, :],
                                    lambda n: (cmask[0:rows, i * 2 + ct, :].unsqueeze(1).to_broadcast([rows, 4, 128]), cmask), offs, pv, rows)

                def win_tile(wi):
                    kt = 12 + i + wi
                    offs = [-SLOPES[4 * g + h] * 128.0 * (4 - wi) for h in range(4)]
                    if wi == 0:
                        mfn = lambda n: (tribias[:, 128:256].unsqueeze(1).to_broadcast([128, 4, 128]), tribias)
                    elif wi == 4:
                        mfn = lambda n: (tribias[:, 0:128].unsqueeze(1).to_broadcast([128, 4, 128]), tribias)
                    else:
                        mfn = None

                    def pv(P):
                        for h in range(4):
                            S.op("pe", lambda h=h: nc.tensor.matmul(pAw[:, h, :], lhsT=P[:, h * 128:(h + 1) * 128], rhs=vw[:, kt, :],
                                                                     start=(wi == 0), stop=(wi == 4)), r=[P, vw], w=[pAw])
                    return tile_gen(kwT[:, kt * 128:(kt + 1) * 128], abias[:, :], mfn, offs, pv)

                run_pipelined([cmp_tile(0), cmp_tile(1)] + [win_tile(wi) for wi in range(5)], 2)
                for hh in range(2):
                    S.op("dve", lambda hh=hh: nc.vector.tensor_scalar(out=sums[:, 2 * hh:2 * hh + 2], in0=pAc[hh][:, :, 64], scalar1=1e-30, scalar2=None, op0=ALU.max),
                         r=[pAc[hh]], w=[sums])
                S.op("dve", lambda: nc.vector.reciprocal(out=sums[:, 0:4], in_=sums[:, 0:4]), r=[sums], w=[sums])
                S.op("dve", lambda: nc.vector.tensor_tensor(out=coef[:, 0:4], in0=sums[:, 0:4], in1=gv[:, :, 0], op=ALU.mult), r=[sums, gsig], w=[coef])
                for h in range(4):
                    pa = pAc[h // 2]
                    if h == 0:
                        S.op("dve", lambda pa=pa, h=h: nc.vector.tensor_scalar(out=imp[:], in0=pa[:, h % 2, 65:129], scalar1=sums[:, h:h + 1], scalar2=None, op0=ALU.mult),
                             r=[pa, sums], w=[imp])
                    else:
                        S.op("dve", lambda pa=pa, h=h: nc.vector.scalar_tensor_tensor(out=imp[:], in0=pa[:, h % 2, 65:129], scalar=sums[:, h:h + 1], in1=imp[:],
                                                                                       op0=ALU.mult, op1=ALU.add), r=[pa, sums, imp], w=[imp])
                    S.op("dve", lambda pa=pa, h=h: nc.vector.tensor_scalar(out=oacc[:, h * 64:(h + 1) * 64], in0=pa[:, h % 2, 0:64], scalar1=coef[:, h:h + 1], scalar2=None, op0=ALU.mult),
                         r=[pa, coef], w=[oacc])
                S.op("dve", lambda: nc.vector.tensor_tensor(out=imp[:], in0=imp[:], in1=selc[0][:], op=ALU.mult), r=[imp, selc[0]], w=[imp])
                S.op("dve", lambda: nc.vector.tensor_tensor(out=imp[:], in0=imp[:], in1=selc[1][:], op=ALU.add), r=[imp, selc[1]], w=[imp])
                S.op("dve", lambda: nc.vector.max(out=m8[:, 0:8], in_=imp[:]), r=[imp], w=[m8])
                S.op("dve", lambda: nc.vector.match_replace(out=imp2[:], in_to_replace=m8[:, 0:8], in_values=imp[:], imm_value=-3.0e38), r=[imp, m8], w=[imp2])
                S.op("dve", lambda: nc.vector.max(out=m8[:, 8:16], in_=imp2[:]), r=[imp2], w=[m8])
                S.op("dve", lambda: nc.vector.tensor_tensor(out=imp2[:], in0=imp[:], in1=m8[:, 15:16].to_broadcast([128, 64]), op=ALU.is_ge), r=[imp, m8], w=[imp2])
                S.op("dve", lambda: nc.vector.tensor_tensor(out=imp2[:], in0=imp2[:], in1=selc[2][:], op=ALU.mult), r=[imp2, selc[2]], w=[imp2])
                S.op("dve", lambda: nc.vector.tensor_scalar(out=selb[:], in0=imp2[:], scalar1=-1.0, scalar2=None, op0=ALU.add), r=[imp2], w=[selb])
                S.op("pe", lambda: nc.tensor.transpose(pT[0:64, 0:128], selb[:, :], ident[:]), r=[selb, ident], w=[pT])
                S.op("act", lambda: nc.scalar.copy(out=selT[:].rearrange("p (h q) -> p h q", h=4), in_=pT[0:64, 0:128].unsqueeze(1).to_broadcast([64, 4, 128])), r=[pT], w=[selT])
                nkt = 17 + i

                def slc_tile(kt):
                    diag = (kt == nkt - 1)
                    offs = [-SLOPES[4 * g + h] * 128.0 * (nkt - 1 - kt) for h in range(4)]

                    extra = [(expbig[:, kt * 128:(kt + 1) * 128], selT[:, :])]
                    if diag:
                        extra.append((ident[:, :], tri4[:, :]))

                    def pv(P):
                        for h in range(4):
                            S.op("pe", lambda h=h: nc.tensor.matmul(pAs[:, h, :], lhsT=P[:, h * 128:(h + 1) * 128], rhs=vs[:, kt, :],
                                                                     start=(kt == 0), stop=(kt == nkt - 1)), r=[P, vs], w=[pAs])
                    return tile_gen(ksT[:, kt * 128:(kt + 1) * 128], abias[:, :], None, offs, pv, extra=extra)

                run_pipelined([slc_tile(kt) for kt in range(nkt)], 2)
                for bi, pa in ((1, pAs), (2, pAw)):
                    S.op("dve", lambda bi=bi, pa=pa: nc.vector.tensor_scalar(out=sums[:, 4 * bi:4 * bi + 4], in0=pa[:, :, 64], scalar1=1e-30, scalar2=None, op0=ALU.max),
                         r=[pa], w=[sums])
                    S.op("dve", lambda bi=bi: nc.vector.reciprocal(out=sums[:, 4 * bi:4 * bi + 4], in_=sums[:, 4 * bi:4 * bi + 4]), r=[sums], w=[sums])
                    S.op("dve", lambda bi=bi: nc.vector.tensor_tensor(out=coef[:, 4 * bi:4 * bi + 4], in0=sums[:, 4 * bi:4 * bi + 4], in1=gv[:, :, bi], op=ALU.mult),
                         r=[sums, gsig], w=[coef])
                    for h in range(4):
                        S.op("dve", lambda bi=bi, pa=pa, h=h: nc.vector.scalar_tensor_tensor(out=oacc[:, h * 64:(h + 1) * 64], in0=pa[:, h, 0:64],
                                                                                              scalar=coef[:, 4 * bi + h:4 * bi + h + 1], in1=oacc[:, h * 64:(h + 1) * 64],
                                                                                              op0=ALU.mult, op1=ALU.add), r=[pa, coef, oacc], w=[oacc])
                S.op("act", lambda: nc.scalar.copy(out=obf[:], in_=oacc[:]), r=[oacc], w=[obf])
                for j in range(2):
                    S.op("pe", lambda j=j: nc.tensor.transpose(pT[:, 128 + j * 128:256 + j * 128], obf[:, j * 128:(j + 1) * 128], ident[:]), r=[obf, ident], w=[pT])
                S.op("act", lambda: nc.scalar.copy(out=ost[:], in_=pT[:, 128:384].rearrange("p (j q) -> p j q", j=2)), r=[pT], w=[ost])
                S.dma("sp", G.mixT[g * 256:(g + 1) * 256, q0:q0 + 128].rearrange("(j p) q -> p j q", p=128), ost[:], r=[ost], w=["mixT"])
        S.barrier(G.bar)


def phase_c(G):
    nc, S, I = G.nc, G.S, G.I
    with ExitStack() as st:
        sb = lambda n, s, d: st.enter_context(nc.sbuf_tensor(n, s, d))
        ps = lambda n, s, d: st.enter_context(nc.psum_tensor(n, s, d))
        identf = sb("c_identf", [128, 128], F32)
        mu = sb("c_mu", [128, 3520], F32)
        bc = {}
        for nm in ("rwkv_w0", "rwkv_a0", "rwkv_k_k", "rwkv_k_a"):
            bc[nm] = sb("c_" + nm, [128, 1024], F32)
        wup = sb("c_wup", [96, 1024], F32)
        aup = sb("c_aup", [96, 1024], F32)
        gup = sb("c_gup", [128, 2, 1024], F32)
        z = [sb(f"c_z{i}", [128, 3520], F32) for i in range(2)]
        zp = [sb(f"c_zp{i}", [128, 3520], F32) for i in range(2)]
        lT = sb("c_lT", [128, 4, 128], F32)
        wv = sb("c_wv", [128, 1024], F32)
        av = sb("c_av", [128, 1024], F32)
        gv = sb("c_gv", [128, 1024], F32)
        kk = sb("c_kk", [128, 1024], F32)
        sq = sb("c_sq", [128, 1024], F32)
        ss = sb("c_ss", [128, 16], F32)
        km = sb("c_km", [128, 1024], F32)
        bv = sb("c_bv", [128, 1024], F32)
        pT = ps("c_pT", [128, 512], F32)
        pW = [ps(f"c_pW{i}", [128, 512], F32) for i in range(6)]
        S.dma("sp", identf[:], I["c_ident"][:, :], w=[identf])
        S.dma("sp", mu[:], I["rwkv_mu"][0:1, :].partition_broadcast(128), w=[mu])
        for nm in bc:
            S.dma("sp", bc[nm][:], I[nm][0:1, :].partition_broadcast(128), w=[bc[nm]])
        S.dma("sp", wup[:], I["rwkv_w_up"][:, :], w=[wup])
        S.dma("sp", aup[:], I["rwkv_a_up"][:, :], w=[aup])
        S.dma("sp", gup[:], I["rwkv_g_up"].rearrange("(c p) n -> p c n", p=128), w=[gup])
        for tl in range(32):
            tok0 = tl * 128
            own = tl >= 16
            zt, zpt = z[tl % 2], zp[tl % 2]
            S.dma("sp", zt[:], G.zr[tok0:tok0 + 128, :], r=["zr"], w=[zt])
            if tl == 0:
                S.op("pool", lambda: nc.gpsimd.memset(zpt[0:1, :], 0.0), w=[zpt])
                S.dma("sp", zpt[1:128, :], G.zr[0:127, :], r=["zr", zpt], w=[zpt])
            else:
                S.dma("sp", zpt[:], G.zr[tok0 - 1:tok0 + 127, :], r=["zr"], w=[zpt])
            S.op("pool", lambda: nc.gpsimd.tensor_tensor(out=zpt[:], in0=zpt[:], in1=zt[:], op=ALU.subtract), r=[zpt, zt], w=[zpt])
            S.op("dve", lambda: nc.vector.tensor_tensor(out=zpt[:], in0=zpt[:], in1=mu[:], op=ALU.mult), r=[zpt, mu], w=[zpt])
            S.op("pool", lambda: nc.gpsimd.tensor_tensor(out=zt[:], in0=zt[:], in1=zpt[:], op=ALU.add), r=[zpt, zt], w=[zt])
            r_ = zt[:, 0:1024]
            k_ = zt[:, 1024:2048]
            v_ = zt[:, 2048:3072]
            S.op("pe", lambda: nc.tensor.transpose(pT[0:96, 0:128], zt[:, 3072:3168], identf[:]), r=[zt, identf], w=[pT])
            S.op("pe", lambda: nc.tensor.transpose(pT[0:96, 128:256], zt[:, 3168:3264], identf[:]), r=[zt, identf], w=[pT])
            S.op("act", lambda: nc.scalar.activation(out=lT[0:96, 0, :], in_=pT[0:96, 0:128], func=AF.Tanh), r=[pT], w=[lT])
            S.op("dve", lambda: nc.vector.tensor_copy(out=lT[0:96, 1, :], in_=pT[0:96, 128:256]), r=[pT], w=[lT])
            if own:
                for j in range(2):
                    S.op("pe", lambda j=j: nc.tensor.transpose(pT[:, 256 + j * 128:384 + j * 128], zt[:, 3264 + j * 128:3392 + j * 128], identf[:]), r=[zt, identf], w=[pT])
                S.op("act", lambda: nc.scalar.activation(out=lT[:, 2:4, :], in_=pT[:, 256:512].rearrange("p (a b) -> p a b", a=2), func=AF.Sigmoid), r=[pT], w=[lT])
            for hh in range(2):
                cs = slice(hh * 512, (hh + 1) * 512)
                S.op("pe", lambda hh=hh, cs=cs: nc.tensor.matmul(pW[hh][:, :], lhsT=lT[0:96, 0, :], rhs=wup[:, cs], start=True, stop=True), r=[lT, wup], w=[pW[hh]])
                S.op("pe", lambda hh=hh, cs=cs: nc.tensor.matmul(pW[2 + hh][:, :], lhsT=lT[0:96, 1, :], rhs=aup[:, cs], start=True, stop=True), r=[lT, aup], w=[pW[2 + hh]])
                S.op("dve", lambda hh=hh, cs=cs: nc.vector.tensor_tensor(out=wv[:, cs], in0=pW[hh][:, :], in1=bc["rwkv_w0"][:, cs], op=ALU.add), r=[pW[hh], bc["rwkv_w0"]], w=[wv])
                S.op("dve", lambda hh=hh, cs=cs: nc.vector.tensor_tensor(out=av[:, cs], in0=pW[2 + hh][:, :], in1=bc["rwkv_a0"][:, cs], op=ALU.add), r=[pW[2 + hh], bc["rwkv_a0"]], w=[av])
                if own:
                    for j in range(2):
                        S.op("pe", lambda hh=hh, cs=cs, j=j: nc.tensor.matmul(pW[4 + hh][:, :], lhsT=lT[:, 2 + j, :], rhs=gup[:, j, cs], start=(j == 0), stop=(j == 1)),
                             r=[lT, gup], w=[pW[4 + hh]])
                    S.op("act", lambda hh=hh, cs=cs: nc.scalar.copy(out=gv[:, cs], in_=pW[4 + hh][:, :]), r=[pW[4 + hh]], w=[gv])
            S.op("act", lambda: nc.scalar.activation(out=wv[:], in_=wv[:], func=AF.Sigmoid), r=[wv], w=[wv])
            S.op("pool", lambda: nc.gpsimd.tensor_scalar(out=wv[:], in0=wv[:], scalar1=-0.6065306597126334, scalar2=None, op0=ALU.mult), r=[wv], w=[wv])
            S.op("act", lambda: nc.scalar.activation(out=av[:], in_=av[:], func=AF.Sigmoid), r=[av], w=[av])
            S.op("dve", lambda: nc.vector.tensor_tensor(out=kk[:], in0=k_, in1=bc["rwkv_k_k"][:], op=ALU.mult), r=[zt, bc["rwkv_k_k"]], w=[kk])
            S.op("pool", lambda: nc.gpsimd.tensor_tensor(out=sq[:], in0=kk[:], in1=kk[:], op=ALU.mult), r=[kk], w=[sq])
            S.op("dve", lambda: nc.vector.tensor_reduce(out=ss[:], in_=sq[:].rearrange("p (h k) -> p h k", k=64), axis=AX.X, op=ALU.add), r=[sq], w=[ss])
            S.op("act", lambda: nc.scalar.activation(out=ss[:], in_=ss[:], func=AF.Sqrt), r=[ss], w=[ss])
            S.op("dve", lambda: nc.vector.tensor_scalar(out=ss[:], in0=ss[:], scalar1=1e-12, scalar2=None, op0=ALU.max), r=[ss], w=[ss])
            S.op("dve", lambda: nc.vector.reciprocal(out=ss[:], in_=ss[:]), r=[ss], w=[ss])
            S.op("dve", lambda: nc.vector.tensor_tensor(out=kk[:].rearrange("p (h k) -> p h k", k=64), in0=kk[:].rearrange("p (h k) -> p h k", k=64),
                                                        in1=ss[:].unsqueeze(2).to_broadcast([128, 16, 64]), op=ALU.mult), r=[kk, ss], w=[kk])
            S.op("dve", lambda: nc.vector.scalar_tensor_tensor(out=km[:], in0=av[:], scalar=-1.0, in1=bc["rwkv_k_a"][:], op0=ALU.add, op1=ALU.mult), r=[av, bc["rwkv_k_a"]], w=[km])
            S.op("dve", lambda: nc.vector.scalar_tensor_tensor(out=km[:], in0=km[:], scalar=1.0, in1=k_, op0=ALU.add, op1=ALU.mult), r=[km, zt], w=[km])
            S.op("pool", lambda: nc.gpsimd.tensor_tensor(out=bv[:], in0=kk[:], in1=av[:], op=ALU.mult), r=[kk, av], w=[bv])
            rows = slice(tok0, tok0 + 128)
            S.dma("sp", G.rw_r[rows, :], r_, r=[zt], w=["rw_r"])
            S.dma("sp", G.rw_v[rows, :], v_, r=[zt], w=["rw_v"])
            S.dma("sp", G.rw_k[rows, :], km[:], r=[km], w=["rw_k"])
            S.dma("sp", G.rw_kn[rows, :], kk[:], r=[kk], w=["rw_kn"])
            S.dma("sp", G.rw_b[rows, :], bv[:], r=[bv], w=["rw_b"])
            S.dma("sp", G.rw_lw[rows, :], wv[:], r=[wv], w=["rw_lw"])
            if own:
                S.dma("sp", G.rw_g[tok0 - 2048:tok0 - 1920, :], gv[:], r=[gv], w=["rw_g"])
        S.barrier(G.bar)
    with ExitStack() as st:
        sb = lambda n, s, d: st.enter_context(nc.sbuf_tensor(n, s, d))
        ps = lambda n, s, d: st.enter_context(nc.psum_tensor(n, s, d))
        crw = sb("s_crw", [64, 448], F32)
        MM = sb("s_MM", [64, 320], F32)
        srcs = [G.rw_lw, G.rw_kn, G.rw_r, G.rw_b, G.rw_k, G.rw_v]
        keys = ["rw_lw", "rw_kn", "rw_r", "rw_b", "rw_k", "rw_v"]
        blk = [[[sb(f"s_in{p}_{hh}_{a}", [64, 8, 64], F32) for a in range(6)] for hh in range(4)] for p in range(2)]
        Hs = [[sb(f"s_H{h}_{p}", [64, 64], F32) for p in range(2)] for h in range(16)]
        NW = 4
        E = [sb(f"s_E{i}", [64, 256], F32) for i in range(NW)]
        FM = [sb(f"s_FM{i}", [64, 256], F32) for i in range(NW)]
        BK = [sb(f"s_BK{i}", [64, 128], F32) for i in range(NW)]
        GM = [sb(f"s_GM{i}", [64, 320], F32) for i in range(NW)]
        XX = [[sb(f"s_XX{i}_{p}", [64, 128], F32) for p in range(2)] for i in range(NW)]
        P2 = [[sb(f"s_P2{i}_{p}", [64, 128], F32) for p in range(2)] for i in range(NW)]
        RU = [sb(f"s_RU{i}", [64, 64], F32) for i in range(NW)]
        U = [sb(f"s_U{i}", [64, 64], F32) for i in range(NW)]
        ybuf = [[sb(f"s_y{p}_{hh}", [64, 8, 64], F32) for hh in range(4)] for p in range(2)]
        pA = ps("s_pA", [64, 256], F32)
        pB = ps("s_pB", [64, 256], F32)
        pC = ps("s_pC", [64, 320], F32)
        pD = ps("s_pD", [64, 128], F32)
        pE = ps("s_pE", [64, 128], F32)
        pF = ps("s_pF", [64, 128], F32)
        pG = ps("s_pG", [64, 64], F32)
        pH = ps("s_pH", [64, 64], F32)
        S.dma("sp", crw[:], I["c_rw"][:, :], w=[crw])
        S.op("dve", lambda: nc.vector.tensor_copy(out=MM[:, 0:128], in_=crw[:, 128:256]), r=[crw], w=[MM])
        S.op("dve", lambda: nc.vector.tensor_copy(out=MM[:, 128:320], in_=crw[:, 128:320]), r=[crw, MM], w=[MM])
        TB = crw[:, 0:128]
        I64 = crw[:, 320:384]
        INC = crw[:, 384:448]
        for h in range(16):
            S.op("pool", lambda h=h: nc.gpsimd.memset(Hs[h][0][:], 0.0), w=[Hs[h][0]])
        nwk = 0
        for hg in range(4):
            for b8 in range(8):
                par = (hg * 8 + b8) % 2
                for hh in range(4):
                    h = hg * 4 + hh
                    for a in range(6):
                        S.dma("sp", blk[par][hh][a][:], srcs[a][b8 * 512:(b8 + 1) * 512, h * 64:(h + 1) * 64].rearrange("(c t) k -> t c k", t=64),
                              r=[keys[a]], w=[blk[par][hh][a]])
                own = b8 >= 4
                for c in range(8):
                    cg = b8 * 8 + c
                    for hh in range(4):
                        h = hg * 4 + hh
                        LW, KN, R, B, K, V = [blk[par][hh][a][:, c, :] for a in range(6)]
                        tl = blk[par][hh]
                        w_ = nwk % NW
                        nwk += 1
                        e_, fm, bk, gm, ru, u_ = E[w_], FM[w_], BK[w_], GM[w_], RU[w_], U[w_]
                        Hc, Hn = Hs[h][cg % 2], Hs[h][(cg + 1) % 2]
                        S.op("pe", lambda: nc.tensor.matmul(pA[:, 0:128], lhsT=LW, rhs=TB, start=True, stop=True), r=[tl[0], crw], w=[pA])
                        S.op("pe", lambda: nc.tensor.matmul(pA[:, 128:192], lhsT=INC, rhs=LW, start=True, stop=True), r=[tl[0], crw], w=[pA])
                        S.op("act", lambda: nc.scalar.activation(out=e_[:, 0:128], in_=pA[:, 0:128], func=AF.Exp), r=[pA], w=[e_])
                        S.op("act", lambda: nc.scalar.activation(out=e_[:, 128:256].rearrange("p (a b) -> p a b", a=2),
                                                                 in_=pA[:, 0:256].rearrange("p (a b) -> p a b", a=2)[:, :, 0:64], func=AF.Exp, scale=-1.0), r=[pA], w=[e_])
                        for j, (src, ti) in enumerate(((KN, 1), (R, 2), (B, 3), (K, 4))):
                            S.op("pe", lambda j=j, src=src: nc.tensor.transpose(pB[:, j * 64:(j + 1) * 64], src, I64), r=[tl[ti], crw], w=[pB])
                        S.op("dve", lambda: nc.vector.scalar_tensor_tensor(out=fm[:, 0:64], in0=pB[:, 0:64], scalar=-1.0, in1=e_[:, 64:128], op0=ALU.mult, op1=ALU.mult),
                             r=[pB, e_], w=[fm])
                        S.op("dve", lambda: nc.vector.tensor_tensor(out=fm[:, 64:128], in0=pB[:, 64:128], in1=e_[:, 0:64], op=ALU.mult), r=[pB, e_, fm], w=[fm])
                        S.op("dve", lambda: nc.vector.tensor_tensor(out=fm[:, 128:256].rearrange("p (a b) -> p a b", a=2), in0=pB[:, 128:256].rearrange("p (a b) -> p a b", a=2),
                                                                    in1=e_[:, 128:192].unsqueeze(1).to_broadcast([64, 2, 64]), op=ALU.mult), r=[pB, e_, fm], w=[fm])
                        S.op("pool", lambda: nc.gpsimd.tensor_tensor(out=bk[:, 0:64], in0=B, in1=e_[:, 192:256], op=ALU.mult), r=[tl[3], e_], w=[bk])
                        S.op("pool", lambda: nc.gpsimd.tensor_tensor(out=bk[:, 64:128], in0=K, in1=e_[:, 192:256], op=ALU.mult), r=[tl[4], e_, bk], w=[bk])
                        AT, RT, BT, KT = fm[:, 0:64], fm[:, 64:128], fm[:, 128:192], fm[:, 192:256]
                        S.op("pe", lambda: nc.tensor.matmul(pC[:, 0:128], lhsT=BT, rhs=fm[:, 0:128], start=True, stop=True), r=[fm], w=[pC])
                        S.op("pe", lambda: nc.tensor.matmul(pC[:, 128:256], lhsT=KT, rhs=fm[:, 0:128], start=True, stop=True), r=[fm], w=[pC])
                        S.op("pe", lambda: nc.tensor.matmul(pC[:, 256:320], lhsT=AT, rhs=BT, start=True, stop=True), r=[fm], w=[pC])
                        S.op("dve", lambda: nc.vector.tensor_tensor(out=gm[:], in0=pC[:, :], in1=MM[:], op=ALU.mult), r=[pC, MM], w=[gm])
                        N_, MrbT, LakT, MrkT, NT = gm[:, 0:64], gm[:, 64:128], gm[:, 128:192], gm[:, 192:256], gm[:, 256:320]
                        xx = XX[w_]
                        p2 = P2[w_]
                        S.op("pool", lambda: nc.gpsimd.tensor_tensor(out=xx[0][:, 0:64], in0=N_, in1=I64, op=ALU.add), r=[gm, crw], w=[xx[0]])
                        S.op("pool", lambda: nc.gpsimd.tensor_tensor(out=xx[0][:, 64:128], in0=NT, in1=I64, op=ALU.add), r=[gm, crw, xx[0]], w=[xx[0]])
                        Pc, PTc, pk = N_, NT, gm
                        for k in range(5):
                            last = (k == 4)
                            pn = p2[k % 2]
                            S.op("pe", lambda Pc=Pc, PTc=PTc: nc.tensor.matmul(pD[:, 0:64], lhsT=PTc, rhs=Pc, start=True, stop=True), r=[pk], w=[pD])
                            if not last:
                                S.op("pe", lambda Pc=Pc, PTc=PTc: nc.tensor.matmul(pD[:, 64:128], lhsT=Pc, rhs=PTc, start=True, stop=True), r=[pk], w=[pD])
                                S.op("act", lambda pn=pn: nc.scalar.copy(out=pn[:], in_=pD[:, :]), r=[pD], w=[pn])
                            else:
                                S.op("act", lambda pn=pn: nc.scalar.copy(out=pn[:, 0:64], in_=pD[:, 0:64]), r=[pD], w=[pn])
                            xc_, xn_ = xx[k % 2], xx[(k + 1) % 2]
                            S.op("pe", lambda xc_=xc_, pn=pn: nc.tensor.matmul(pE[:, 0:64], lhsT=xc_[:, 64:128], rhs=pn[:, 0:64], start=True, stop=True), r=[xc_, pn], w=[pE])
                            if not last:
                                S.op("pe", lambda xc_=xc_, pn=pn: nc.tensor.matmul(pE[:, 64:128], lhsT=pn[:, 0:64], rhs=xc_[:, 64:128], start=True, stop=True), r=[xc_, pn], w=[pE])
                                S.op("dve", lambda xc_=xc_, xn_=xn_: nc.vector.tensor_tensor(out=xn_[:], in0=pE[:, :], in1=xc_[:], op=ALU.add), r=[pE, xc_], w=[xn_])
                            else:
                                S.op("dve", lambda xc_=xc_, xn_=xn_: nc.vector.tensor_tensor(out=xn_[:, 0:64], in0=pE[:, 0:64], in1=xc_[:, 0:64], op=ALU.add), r=[pE, xc_], w=[xn_])
                            Pc, PTc, pk = pn[:, 0:64], pn[:, 64:128], pn
                        X = xx[1][:, 0:64]
                        xk = xx[1]
                        S.op("pe", lambda: nc.tensor.matmul(pF[:, 0:64], lhsT=AT, rhs=Hc[:], start=True, stop=False), r=[fm, Hc], w=[pF])
                        S.op("pe", lambda: nc.tensor.matmul(pF[:, 0:64], lhsT=LakT, rhs=V, start=False, stop=True), r=[gm, tl[5]], w=[pF])
                        S.op("act", lambda: nc.scalar.copy(out=ru[:], in_=pF[:, 0:64]), r=[pF], w=[ru])
                        S.op("pe", lambda: nc.tensor.matmul(pF[:, 64:128], lhsT=X, rhs=ru[:], start=True, stop=True), r=[xk, ru], w=[pF])
                        S.op("dve", lambda: nc.vector.tensor_copy(out=u_[:], in_=pF[:, 64:128]), r=[pF], w=[u_])
                        if own:
                            yb = ybuf[par][hh]
                            S.op("pe", lambda: nc.tensor.matmul(pG[:, :], lhsT=RT, rhs=Hc[:], start=True, stop=False), r=[fm, Hc], w=[pG])
                            S.op("pe", lambda: nc.tensor.matmul(pG[:, :], lhsT=MrbT, rhs=u_[:], start=False, stop=False), r=[gm, u_], w=[pG])
                            S.op("pe", lambda: nc.tensor.matmul(pG[:, :], lhsT=MrkT, rhs=V, start=False, stop=True), r=[gm, tl[5]], w=[pG])
                            S.op("act", lambda: nc.scalar.copy(out=yb[:, c, :], in_=pG[:, :]), r=[pG], w=[yb])
                        S.op("pe", lambda: nc.tensor.matmul(pH[:, :], lhsT=I64, rhs=Hc[:], start=True, stop=False), r=[crw, Hc], w=[pH])
                        S.op("pe", lambda: nc.tensor.matmul(pH[:, :], lhsT=bk[:, 0:64], rhs=u_[:], start=False, stop=False), r=[bk, u_], w=[pH])
                        S.op("pe", lambda: nc.tensor.matmul(pH[:, :], lhsT=bk[:, 64:128], rhs=V, start=False, stop=True), r=[bk, tl[5]], w=[pH])
                        S.op("dve", lambda: nc.vector.tensor_scalar(out=Hn[:], in0=pH[:, :], scalar1=e_[:, 63:64], scalar2=None, op0=ALU.mult), r=[pH, e_], w=[Hn])
                if own:
                    for hh in range(4):
                        h = hg * 4 + hh
                        r0 = (b8 - 4) * 512
                        S.dma("sp", G.rw_y[r0:r0 + 512, h * 64:(h + 1) * 64].rearrange("(c t) k -> t c k", t=64), ybuf[par][hh][:], r=[ybuf[par][hh]], w=["rw_y"])
        S.barrier(G.bar)
    with ExitStack() as st:
        sb = lambda n, s, d: st.enter_context(nc.sbuf_tensor(n, s, d))
        ps = lambda n, s, d: st.enter_context(nc.psum_tensor(n, s, d))
        ident = sb("p_ident", [128, 128], BF16)
        bc = {}
        for nm in ("rwkv_ln_g", "rwkv_ln_b", "rwkv_r_k"):
            bc[nm] = sb("p_" + nm, [128, 1024], F32)
            S.dma("sp", bc[nm][:], I[nm][0:1, :].partition_broadcast(128), w=[bc[nm]])
        S.dma("pool", ident[:], I["c_ident"][:, :], w=[ident])
        y = [sb(f"p_y{i}", [128, 1024], F32) for i in range(2)]
        rr = [sb(f"p_r{i}", [128, 1024], F32) for i in range(2)]
        kq = [sb(f"p_k{i}", [128, 1024], F32) for i in range(2)]
        vv = [sb(f"p_v{i}", [128, 1024], F32) for i in range(2)]
        gg = [sb(f"p_g{i}", [128, 1024], F32) for i in range(2)]
        sq = sb("p_sq", [128, 1024], F32)
        st1 = sb("p_st1", [128, 16], F32)
        st2 = sb("p_st2", [128, 16], F32)
        st3 = sb("p_st3", [128, 16], F32)
        obf = sb("p_obf", [128, 1024], BF16)
        ost = sb("p_ost", [128, 8, 128], BF16)
        pT = ps("p_pT", [128, 1024], BF16)
        v3 = lambda t: t[:].rearrange("p (h k) -> p h k", k=64)
        b3 = lambda t: t[:].unsqueeze(2).to_broadcast([128, 16, 64])
        for tl in range(16):
            p = tl % 2
            rows = slice(tl * 128, (tl + 1) * 128)
            crow = slice(2048 + tl * 128, 2048 + (tl + 1) * 128)
            yt, rt, kt, vt, gt = y[p], rr[p], kq[p], vv[p], gg[p]
            S.dma("sp", yt[:], G.rw_y[rows, :], r=["rw_y"], w=[yt])
            S.dma("sp", rt[:], G.rw_r[crow, :], r=["rw_r"], w=[rt])
            S.dma("sp", kt[:], G.rw_k[crow, :], r=["rw_k"], w=[kt])
            S.dma("sp", vt[:], G.rw_v[crow, :], r=["rw_v"], w=[vt])
            S.dma("sp", gt[:], G.rw_g[rows, :], r=["rw_g"], w=[gt])
            S.op("dve", lambda: nc.vector.tensor_reduce(out=st1[:], in_=v3(yt), axis=AX.X, op=ALU.add), r=[yt], w=[st1])
            S.op("pool", lambda: nc.gpsimd.tensor_tensor(out=sq[:], in0=yt[:], in1=yt[:], op=ALU.mult), r=[yt], w=[sq])
            S.op("dve", lambda: nc.vector.tensor_reduce(out=st2[:], in_=v3(sq), axis=AX.X, op=ALU.add), r=[sq], w=[st2])
            S.op("dve", lambda: nc.vector.tensor_scalar(out=st1[:], in0=st1[:], scalar1=1.0 / 64.0, scalar2=None, op0=ALU.mult), r=[st1], w=[st1])
            S.op("dve", lambda: nc.vector.tensor_tensor(out=st3[:], in0=st1[:], in1=st1[:], op=ALU.mult), r=[st1], w=[st3])
            S.op("dve", lambda: nc.vector.scalar_tensor_tensor(out=st2[:], in0=st2[:], scalar=1.0 / 64.0, in1=st3[:], op0=ALU.mult, op1=ALU.subtract), r=[st2, st3], w=[st2])
            S.op("dve", lambda: nc.vector.tensor_scalar(out=st2[:], in0=st2[:], scalar1=64e-5, scalar2=None, op0=ALU.add), r=[st2], w=[st2])
            S.op("act", lambda: nc.scalar.activation(out=st2[:], in_=st2[:], func=AF.Sqrt), r=[st2], w=[st2])
            S.op("dve", lambda: nc.vector.reciprocal(out=st2[:], in_=st2[:]), r=[st2], w=[st2])
            S.op("dve", lambda: nc.vector.tensor_tensor(out=v3(yt), in0=v3(yt), in1=b3(st1), op=ALU.subtract), r=[yt, st1], w=[yt])
            S.op("dve", lambda: nc.vector.tensor_tensor(out=v3(yt), in0=v3(yt), in1=b3(st2), op=ALU.mult), r=[yt, st2], w=[yt])
            S.op("pool", lambda: nc.gpsimd.tensor_tensor(out=yt[:], in0=yt[:], in1=bc["rwkv_ln_g"][:], op=ALU.mult), r=[yt, bc["rwkv_ln_g"]], w=[yt])
            S.op("pool", lambda: nc.gpsimd.tensor_tensor(out=yt[:], in0=yt[:], in1=bc["rwkv_ln_b"][:], op=ALU.add), r=[yt, bc["rwkv_ln_b"]], w=[yt])
            S.op("pool", lambda: nc.gpsimd.tensor_tensor(out=rt[:], in0=rt[:], in1=kt[:], op=ALU.mult), r=[rt, kt], w=[rt])
            S.op("pool", lambda: nc.gpsimd.tensor_tensor(out=rt[:], in0=rt[:], in1=bc["rwkv_r_k"][:], op=ALU.mult), r=[rt, bc["rwkv_r_k"]], w=[rt])
            S.op("dve", lambda: nc.vector.tensor_reduce(out=st3[:], in_=v3(rt), axis=AX.X, op=ALU.add), r=[rt], w=[st3])
            S.op("dve", lambda: nc.vector.tensor_tensor(out=v3(vt), in0=v3(vt), in1=b3(st3), op=ALU.mult), r=[vt, st3], w=[vt])
            S.op("pool", lambda: nc.gpsimd.tensor_tensor(out=yt[:], in0=yt[:], in1=vt[:], op=ALU.add), r=[yt, vt], w=[yt])
            S.op("dve", lambda: nc.vector.tensor_tensor(out=obf[:], in0=yt[:], in1=gt[:], op=ALU.mult), r=[yt, gt], w=[obf])
            for j in range(8):
                S.op("pe", lambda j=j: nc.tensor.transpose(pT[:, j * 128:(j + 1) * 128], obf[:, j * 128:(j + 1) * 128], ident[:]), r=[obf, ident], w=[pT])
            S.op("act", lambda: nc.scalar.copy(out=ost[:], in_=pT[:, :].rearrange("p (j q) -> p j q", j=8)), r=[pT], w=[ost])
            S.dma("sp", G.mixT[1024:2048, tl * 128:(tl + 1) * 128].rearrange("(j p) q -> p j q", p=128), ost[:], r=[ost], w=["mixT"])
        S.barrier(G.bar)


def layer_norm_tile(G, x, g_bc, b_bc, sq, st, eps=1e-5):
    nc, S = G.nc, G.S
    S.op("dve", lambda: nc.vector.tensor_reduce(out=st[:, 0:1], in_=x[:], axis=AX.X, op=ALU.add), r=[x], w=[st])
    S.op("pool", lambda: nc.gpsimd.tensor_tensor(out=sq[:], in0=x[:], in1=x[:], op=ALU.mult), r=[x], w=[sq])
    S.op("dve", lambda: nc.vector.tensor_reduce(out=st[:, 1:2], in_=sq[:], axis=AX.X, op=ALU.add), r=[sq, st], w=[st])
    S.op("dve", lambda: nc.vector.tensor_scalar(out=st[:, 0:2], in0=st[:, 0:2], scalar1=1.0 / D, scalar2=None, op0=ALU.mult), r=[st], w=[st])
    S.op("dve", lambda: nc.vector.tensor_tensor(out=st[:, 2:3], in0=st[:, 0:1], in1=st[:, 0:1], op=ALU.mult), r=[st], w=[st])
    S.op("dve", lambda: nc.vector.tensor_tensor(out=st[:, 1:2], in0=st[:, 1:2], in1=st[:, 2:3], op=ALU.subtract), r=[st], w=[st])
    S.op("dve", lambda: nc.vector.tensor_scalar(out=st[:, 1:2], in0=st[:, 1:2], scalar1=eps, scalar2=None, op0=ALU.add), r=[st], w=[st])
    S.op("act", lambda: nc.scalar.activation(out=st[:, 1:2], in_=st[:, 1:2], func=AF.Sqrt), r=[st], w=[st])
    S.op("dve", lambda: nc.vector.reciprocal(out=st[:, 1:2], in_=st[:, 1:2]), r=[st], w=[st])
    S.op("dve", lambda: nc.vector.tensor_scalar(out=x[:], in0=x[:], scalar1=st[:, 0:1], scalar2=st[:, 1:2], op0=ALU.subtract, op1=ALU.mult), r=[x, st], w=[x])
    S.op("pool", lambda: nc.gpsimd.tensor_tensor(out=x[:], in0=x[:], in1=g_bc[:], op=ALU.mult), r=[x, g_bc], w=[x])
    S.op("pool", lambda: nc.gpsimd.tensor_tensor(out=x[:], in0=x[:], in1=b_bc[:], op=ALU.add), r=[x, b_bc], w=[x])


def phase_d(G):
    nc, S, I = G.nc, G.S, G.I
    with ExitStack() as st:
        sb = lambda n, s, d: st.enter_context(nc.sbuf_tensor(n, s, d))
        ps = lambda n, s, d: st.enter_context(nc.psum_tensor(n, s, d))
        ident = sb("d_ident", [128, 128], BF16)
        identf = sb("d_identf", [128, 128], F32)
        mixT = sb("d_mixT", [128, 16, TO], BF16)
        wout = sb("d_wout", [128, 16, D], BF16)
        gbc = sb("d_g", [128, D], F32)
        bbc = sb("d_b", [128, D], F32)
        rw = sb("d_rw", [128, 16, 32], F32)
        rb = sb("d_rb", [128, 32], F32)
        xt = [sb(f"d_x{i}", [128, D], F32) for i in range(2)]
        hp = [sb(f"d_hp{i}", [128, D], F32) for i in range(2)]
        sq = sb("d_sq", [128, D], F32)
        stt = sb("d_st", [128, 4], F32)
        hbf = sb("d_hbf", [128, D], BF16)
        hTs = sb("d_hTs", [128, 16, 128], BF16)
        hT32 = sb("d_hT32", [128, 16, 128], F32)
        lg = sb("d_lg", [128, 32], F32)
        ex = sb("d_ex", [128, 32], F32)
        m8 = sb("d_m8", [128, 8], F32)
        sm = sb("d_sm", [128, 2], F32)
        pO = [ps(f"d_pO{i}", [128, 512], F32) for i in range(4)]
        pTf = [ps(f"d_pTf{i}", [128, 512], F32) for i in range(2)]
        pL = ps("d_pL", [128, 32], F32)
        pTb = ps("d_pTb", [128, 1024], BF16)
        S.dma("pool", ident[:], I["c_ident"][:, :], w=[ident])
        S.dma("sp", identf[:], I["c_ident"][:, :], w=[identf])
        mv = G.mixT.rearrange("(kc p) t -> p kc t", p=128)
        wv = I["w_out"].rearrange("(kc p) c -> p kc c", p=128)
        for j in range(4):
            S.dma("sp", mixT[:, j * 4:(j + 1) * 4, :], mv[:, j * 4:(j + 1) * 4, :], r=["mixT"], w=[mixT])
            S.dma("pool", wout[:, :, j * 512:(j + 1) * 512], wv[:, :, j * 512:(j + 1) * 512], w=[wout])
        S.dma("sp", gbc[:], I["ln1_g"][0:1, :].partition_broadcast(128), w=[gbc])
        S.dma("sp", bbc[:], I["ln1_b"][0:1, :].partition_broadcast(128), w=[bbc])
        S.dma("sp", rw[:], I["router_w"].rearrange("(kc p) e -> p kc e", p=128), w=[rw])
        S.dma("sp", rb[:], I["router_b"][0:1, :].partition_broadcast(128), w=[rb])
        for tl in range(16):
            x_ = xt[tl % 2]
            h_ = hp[tl % 2]
            rows = slice(tl * 128, (tl + 1) * 128)
            S.dma("sp", x_[:], I["xc"][2048 + tl * 128:2048 + (tl + 1) * 128, :], w=[x_])
            for c4 in range(4):
                for kc in range(16):
                    S.op("pe", lambda c4=c4, kc=kc: nc.tensor.matmul(pO[c4][:, :], lhsT=mixT[:, kc, tl * 128:(tl + 1) * 128], rhs=wout[:, kc, c4 * 512:(c4 + 1) * 512],
                                                                       start=(kc == 0), stop=(kc == 15)), r=[mixT, wout], w=[pO[c4]])
                S.op("dve", lambda c4=c4: nc.vector.scalar_tensor_tensor(out=h_[:, c4 * 512:(c4 + 1) * 512], in0=x_[:, c4 * 512:(c4 + 1) * 512], scalar=ALPHA, in1=pO[c4][:, :],
                                                                          op0=ALU.mult, op1=ALU.add), r=[x_, pO[c4]], w=[h_])
            layer_norm_tile(G, h_, gbc, bbc, sq, stt)
            S.dma("sp", G.h1[rows, :], h_[:], r=[h_], w=["h1"])
            S.op("act", lambda: nc.scalar.copy(out=hbf[:], in_=h_[:]), r=[h_], w=[hbf])
            for j in range(2):
                for k8 in range(8):
                    kc = j * 8 + k8
                    S.op("pe", lambda k8=k8, kc=kc: nc.tensor.transpose(pTb[:, k8 * 128:(k8 + 1) * 128], hbf[:, kc * 128:(kc + 1) * 128], ident[:]), r=[hbf, ident], w=[pTb])
                evac(G, hTs[:, j * 8:(j + 1) * 8, :], pTb[:].rearrange("p (a b) -> p a b", a=8), r=[pTb], w=[hTs])
            S.dma("sp", G.h1T[:, rows].rearrange("(kc p) t -> p kc t", p=128), hTs[:], r=[hTs], w=["h1T"])
            for j in range(4):
                pt = pTf[j % 2]
                for k4 in range(4):
                    kc = j * 4 + k4
                    S.op("pe", lambda k4=k4, kc=kc, pt=pt: nc.tensor.transpose(pt[:, k4 * 128:(k4 + 1) * 128], h_[:, kc * 128:(kc + 1) * 128], identf[:]), r=[h_, identf], w=[pt])
                evac(G, hT32[:, j * 4:(j + 1) * 4, :], pt[:].rearrange("p (a b) -> p a b", a=4), r=[pt], w=[hT32])
            for kc in range(16):
                S.op("pe", lambda kc=kc: nc.tensor.matmul(pL[:, :], lhsT=hT32[:, kc, :], rhs=rw[:, kc, :], start=(kc == 0), stop=(kc == 15)), r=[hT32, rw], w=[pL])
            S.op("dve", lambda: nc.vector.tensor_tensor(out=lg[:], in0=pL[:, :], in1=rb[:], op=ALU.add), r=[pL, rb], w=[lg])
            S.op("dve", lambda: nc.vector.max(out=m8[:], in_=lg[:]), r=[lg], w=[m8])
            S.op("dve", lambda: nc.vector.tensor_scalar(out=sm[:, 0:1], in0=m8[:, 0:1], scalar1=-1.0, scalar2=None, op0=ALU.mult), r=[m8], w=[sm])
            S.op("act", lambda: nc.scalar.activation(out=ex[:], in_=lg[:], func=AF.Exp, bias=sm[:, 0:1], scale=1.0), r=[lg, sm], w=[ex])
            S.op("dve", lambda: nc.vector.tensor_tensor(out=lg[:], in0=lg[:], in1=m8[:, 3:4].to_broadcast([128, 32]), op=ALU.is_ge), r=[lg, m8], w=[lg])
            S.op("dve", lambda: nc.vector.tensor_tensor(out=ex[:], in0=ex[:], in1=lg[:], op=ALU.mult), r=[ex, lg], w=[ex])
            S.op("dve", lambda: nc.vector.tensor_reduce(out=sm[:, 1:2], in_=ex[:], axis=AX.X, op=ALU.add), r=[ex, sm], w=[sm])
            S.op("dve", lambda: nc.vector.reciprocal(out=sm[:, 1:2], in_=sm[:, 1:2]), r=[sm], w=[sm])
            S.op("dve", lambda: nc.vector.tensor_scalar(out=ex[:], in0=ex[:], scalar1=sm[:, 1:2], scalar2=None, op0=ALU.mult), r=[ex, sm], w=[ex])
            S.dma("sp", G.gw[rows, :], ex[:], r=[ex], w=["gw"])
        S.barrier(G.bar)


def phase_e(G, experts=32):
    nc, S, I = G.nc, G.S, G.I
    LIM = 7.0
    with ExitStack() as st:
        sb = lambda n, s, d: st.enter_context(nc.sbuf_tensor(n, s, d))
        ps = lambda n, s, d: st.enter_context(nc.psum_tensor(n, s, d))
        identf = sb("e_identf", [128, 128], F32)
        h1T = sb("e_h1T", [128, 16, 1024], BF16)
        Y = sb("e_Y", [128, 8, D], F32)
        gw = sb("e_gw", [128, 8, 32], F32)
        gwT = sb("e_gwT", [32, 8, 128], F32)
        bdn = sb("e_bdn", [32, D], F32)
        bg = sb("e_bg", [128, 512], F32)
        bu = sb("e_bu", [128, 512], F32)
        wg = [sb(f"e_wg{i}", [128, 16, 256], BF16) for i in range(2)]
        wu = [sb(f"e_wu{i}", [128, 16, 256], BF16) for i in range(2)]
        wd = [sb(f"e_wd{i}", [128, 2, D], BF16) for i in range(2)]
        hT = [sb(f"e_hT{i}", [128, 2, 1024], BF16) for i in range(2)]
        gt = [sb(f"e_g{i}", [128, 512], F32) for i in range(2)]
        sg = [sb(f"e_sg{i}", [128, 512], F32) for i in range(2)]
        ut = [sb(f"e_u{i}", [128, 512], F32) for i in range(2)]
        ev = [sb(f"e_ev{i}", [128, 512], F32) for i in range(3)]
        h1t = [sb(f"e_h1t{i}", [128, D], F32) for i in range(1)]
        pGU = [ps(f"e_pGU{i}", [128, 512], F32) for i in range(4)]
        pDn = [ps(f"e_pD{i}", [128, 512], F32) for i in range(4)]
        S.dma("sp", identf[:], I["c_ident"][:, :], w=[identf])
        S.dma("sp", bdn[:], I["exp_b_down"][:, :], w=[bdn])
        S.dma("sp", bg[:], I["exp_b_gate"][:, :], w=[bg])
        S.dma("sp", bu[:], I["exp_b_up"][:, :], w=[bu])
        wgv = I["exp_w_gate"].rearrange("(e kc p) f -> e p kc f", p=128, kc=16)
        wuv = I["exp_w_up"].rearrange("(e kc p) f -> e p kc f", p=128, kc=16)
        wdv = I["exp_w_down"].rearrange("(e fc p) d -> e p fc d", p=128, fc=16)
        nst = 0
        YK = [[("Y", tl, d4) for d4 in range(4)] for tl in range(8)]
        YALL = [k for row in YK for k in row]
        for hf in range(2):
            t0 = hf * 1024
            S.dma("sp", h1T[:], G.h1T[:, t0:t0 + 1024].rearrange("(kc p) t -> p kc t", p=128), r=["h1T"], w=[h1T])
            S.dma("sp", gw[:], G.gw[t0:t0 + 1024, :].rearrange("(t p) e -> p t e", p=128), r=["gw"], w=[gw])
            S.op("pool", lambda: nc.gpsimd.memset(Y[:], 0.0), w=YALL)
            for e in range(experts):
                for fgp in range(8):
                    b = nst % 2
                    nst += 1
                    S.dma("pool", wg[b][:], wgv[e][:, :, fgp * 256:(fgp + 1) * 256], w=[wg[b]])
                    S.dma("pool", wu[b][:], wuv[e][:, :, fgp * 256:(fgp + 1) * 256], w=[wu[b]])
                    S.dma("pool", wd[b][:], wdv[e][:, fgp * 2:(fgp + 1) * 2, :], w=[wd[b]])
                    hb = hT[b]
                    for f2 in range(2):
                        fc = fgp * 2 + f2
                        bcol = e * 16 + fc
                        for tc_ in range(2):
                            pg, pu = pGU[(2 * tc_) % 4], pGU[(2 * tc_ + 1) % 4]
                            for kc in range(16):
                                S.op("pe", lambda kc=kc, pg=pg: nc.tensor.matmul(pg[:, :], lhsT=wg[b][:, kc, f2 * 128:(f2 + 1) * 128], rhs=h1T[:, kc, tc_ * 512:(tc_ + 1) * 512],
                                                                                  start=(kc == 0), stop=(kc == 15)), r=[wg[b], h1T], w=[pg])
                            for kc in range(16):
                                S.op("pe", lambda kc=kc, pu=pu: nc.tensor.matmul(pu[:, :], lhsT=wu[b][:, kc, f2 * 128:(f2 + 1) * 128], rhs=h1T[:, kc, tc_ * 512:(tc_ + 1) * 512],
                                                                                  start=(kc == 0), stop=(kc == 15)), r=[wu[b], h1T], w=[pu])
                            g_, s_, u_ = gt[tc_], sg[tc_], ut[tc_]
                            S.op("dve", lambda: nc.vector.tensor_scalar(out=g_[:], in0=pg[:, :], scalar1=bg[:, bcol:bcol + 1], scalar2=LIM, op0=ALU.add, op1=ALU.min), r=[pg, bg], w=[g_])
                            S.op("act", lambda: nc.scalar.activation(out=s_[:], in_=g_[:], func=AF.Sigmoid, scale=1.702), r=[g_], w=[s_])
                            S.op("dve", lambda: nc.vector.tensor_scalar(out=u_[:], in0=pu[:, :], scalar1=bu[:, bcol:bcol + 1], scalar2=LIM, op0=ALU.add, op1=ALU.min), r=[pu, bu], w=[u_])
                            S.op("dve", lambda: nc.vector.tensor_scalar(out=u_[:], in0=u_[:], scalar1=-LIM, scalar2=1.0, op0=ALU.max, op1=ALU.add), r=[u_], w=[u_])
                            S.op("dve", lambda: nc.vector.tensor_tensor(out=g_[:], in0=g_[:], in1=s_[:], op=ALU.mult), r=[g_, s_], w=[g_])
                            S.op("dve", lambda: nc.vector.tensor_tensor(out=hb[:, f2, tc_ * 512:(tc_ + 1) * 512], in0=g_[:], in1=u_[:], op=ALU.mult), r=[g_, u_], w=[hb])
                    for tl in range(8):
                        for d4 in range(4):
                            pd = pDn[(tl * 4 + d4) % 4]
                            for f2 in range(2):
                                S.op("pe", lambda f2=f2, pd=pd, d4=d4, tl=tl: nc.tensor.matmul(pd[:, :], lhsT=hb[:, f2, tl * 128:(tl + 1) * 128], rhs=wd[b][:, f2, d4 * 512:(d4 + 1) * 512],
                                                                                                 start=(f2 == 0), stop=(f2 == 1)), r=[hb, wd[b]], w=[pd])
                            S.op("dve", lambda pd=pd, tl=tl, d4=d4: nc.vector.scalar_tensor_tensor(out=Y[:, tl, d4 * 512:(d4 + 1) * 512], in0=pd[:, :], scalar=gw[:, tl, e:e + 1],
                                                                                                     in1=Y[:, tl, d4 * 512:(d4 + 1) * 512], op0=ALU.mult, op1=ALU.add),
                                 r=[pd, gw, YK[tl][d4]], w=[YK[tl][d4]])
            for tl in range(8):
                S.op("pe", lambda tl=tl: nc.tensor.transpose(pGU[0][0:32, 0:128], gw[:, tl, :], identf[:]), r=[gw, identf], w=[pGU[0]])
                S.op("dve", lambda tl=tl: nc.vector.tensor_copy(out=gwT[:, tl, :], in_=pGU[0][0:32, 0:128]), r=[pGU[0]], w=[gwT])
                ht = h1t[0]
                rows = slice(t0 + tl * 128, t0 + (tl + 1) * 128)
                S.dma("sp", ht[:], G.h1[rows, :], r=["h1"], w=[ht])
                for d4 in range(4):
                    pd = pDn[d4]
                    S.op("pe", lambda pd=pd, d4=d4, tl=tl: nc.tensor.matmul(pd[:, :], lhsT=gwT[:, tl, :], rhs=bdn[:, d4 * 512:(d4 + 1) * 512], start=True, stop=True), r=[gwT, bdn], w=[pd])
                    S.op("dve", lambda pd=pd, d4=d4, tl=tl: nc.vector.tensor_tensor(out=Y[:, tl, d4 * 512:(d4 + 1) * 512], in0=Y[:, tl, d4 * 512:(d4 + 1) * 512], in1=pd[:, :], op=ALU.add),
                         r=[pd, YK[tl][d4]], w=[YK[tl][d4]])
                S.op("dve", lambda tl=tl, ht=ht: nc.vector.scalar_tensor_tensor(out=ht[:], in0=ht[:], scalar=ALPHA, in1=Y[:, tl, :], op0=ALU.mult, op1=ALU.add), r=[ht] + YK[tl], w=[ht])
                S.dma("sp", G.ypre[rows, :], ht[:], r=[ht], w=["ypre"])
        S.barrier(G.bar)


def phase_f(G):
    nc, S, I = G.nc, G.S, G.I
    with ExitStack() as st:
        sb = lambda n, s, d: st.enter_context(nc.sbuf_tensor(n, s, d))
        ps = lambda n, s, d: st.enter_context(nc.psum_tensor(n, s, d))
        ident = sb("f_ident", [128, 128], BF16)
        pgw = sb("f_pgw", [128, 16, D], BF16)
        plw = sb("f_plw", [128, 2, D], BF16)
        gbc = sb("f_g", [128, D], F32)
        bbc = sb("f_b", [128, D], F32)
        yt = [sb(f"f_y{i}", [128, D], F32) for i in range(2)]
        ot = [sb(f"f_o{i}", [128, D], F32) for i in range(2)]
        sq = sb("f_sq", [128, D], F32)
        stt = sb("f_st", [128, 4], F32)
        hbf = sb("f_hbf", [128, D], BF16)
        pb = [sb(f"f_pb{i}", [128, 256], BF16) for i in range(2)]
        hTs = sb("f_hTs", [128, 16, 128], BF16)
        pTs = sb("f_pTs", [128, 2, 128], BF16)
        sgt = [sb(f"f_sg{i}", [128, 512], F32) for i in range(2)]
        pGt = [ps(f"f_pG{i}", [128, 512], F32) for i in range(2)]
        pPt = [ps(f"f_pP{i}", [128, 512], F32) for i in range(2)]
        pTb = ps("f_pTb", [128, 1024], BF16)
        S.dma("pool", ident[:], I["c_ident"][:, :], w=[ident])
        wv = I["ple_gate_w"].rearrange("(kc p) c -> p kc c", p=128)
        for j in range(4):
            S.dma("pool", pgw[:, :, j * 512:(j + 1) * 512], wv[:, :, j * 512:(j + 1) * 512], w=[pgw])
        S.dma("pool", plw[:], I["ple_w"].rearrange("(kc p) c -> p kc c", p=128), w=[plw])
        S.dma("sp", gbc[:], I["ln2_g"][0:1, :].partition_broadcast(128), w=[gbc])
        S.dma("sp", bbc[:], I["ln2_b"][0:1, :].partition_broadcast(128), w=[bbc])
        for tl in range(16):
            rows = slice(tl * 128, (tl + 1) * 128)
            y_ = yt[tl % 2]
            o_ = ot[tl % 2]
            p_ = pb[tl % 2]
            S.dma("sp", y_[:], G.ypre[rows, :], r=["ypre"], w=[y_])
            S.dma("pool", p_[:], I["p_own"][rows, :], w=[p_])
            layer_norm_tile(G, y_, gbc, bbc, sq, stt)
            S.op("act", lambda: nc.scalar.copy(out=hbf[:], in_=y_[:]), r=[y_], w=[hbf])
            for j in range(2):
                for k8 in range(8):
                    kc = j * 8 + k8
                    S.op("pe", lambda k8=k8, kc=kc: nc.tensor.transpose(pTb[:, k8 * 128:(k8 + 1) * 128], hbf[:, kc * 128:(kc + 1) * 128], ident[:]), r=[hbf, ident], w=[pTb])
                evac(G, hTs[:, j * 8:(j + 1) * 8, :], pTb[:].rearrange("p (a b) -> p a b", a=8), r=[pTb], w=[hTs])
            for j in range(2):
                S.op("pe", lambda j=j: nc.tensor.transpose(pTb[:, j * 128:(j + 1) * 128], p_[:, j * 128:(j + 1) * 128], ident[:]), r=[p_, ident], w=[pTb])
            evac(G, pTs[:], pTb[:, 0:256].rearrange("p (a b) -> p a b", a=2), r=[pTb], w=[pTs])
            for d4 in range(4):
                cs = slice(d4 * 512, (d4 + 1) * 512)
                pg, pp, s_ = pGt[d4 % 2], pPt[d4 % 2], sgt[d4 % 2]
                for kc in range(16):
                    S.op("pe", lambda kc=kc, pg=pg, cs=cs: nc.tensor.matmul(pg[:, :], lhsT=hTs[:, kc, :], rhs=pgw[:, kc, cs], start=(kc == 0), stop=(kc == 15)), r=[hTs, pgw], w=[pg])
                for kc in range(2):
                    S.op("pe", lambda kc=kc, pp=pp, cs=cs: nc.tensor.matmul(pp[:, :], lhsT=pTs[:, kc, :], rhs=plw[:, kc, cs], start=(kc == 0), stop=(kc == 1)), r=[pTs, plw], w=[pp])
                S.op("act", lambda pg=pg, s_=s_: nc.scalar.activation(out=s_[:], in_=pg[:, :], func=AF.Sigmoid), r=[pg], w=[s_])
                S.op("dve", lambda pp=pp, s_=s_: nc.vector.tensor_tensor(out=s_[:], in0=s_[:], in1=pp[:, :], op=ALU.mult), r=[pp, s_], w=[s_])
                S.op("dve", lambda s_=s_, cs=cs: nc.vector.tensor_tensor(out=o_[:, cs], in0=y_[:, cs], in1=s_[:], op=ALU.add), r=[y_, s_], w=[o_])
            S.dma("sp", G.out[rows, :], o_[:], r=[o_], w=["out"])


def make_consts(s):
    c = {}
    kv = np.ones((T,), np.float32)
    if s == 0:
        kv[:2048] = 0.0
    c["c_kvalid"] = np.ascontiguousarray(kv.reshape(32, 128).T)
    c["c_ident"] = np.eye(128, dtype=np.float32)
    k = np.arange(128)[:, None]
    q = np.arange(128)[None, :]
    c["c_trile"] = (k <= q).astype(np.float32)
    c["c_trigt"] = (k > q).astype(np.float32)
    ab = np.zeros((4, 128, 4, 128), np.float32)
    cb = np.zeros((4, 2, 128, 4, 128), np.float32)
    for g in range(4):
        for h in range(4):
            sl = SLOPES[4 * g + h]
            ab[g, :, h, :] = -sl * (q - k)
            for ct in range(2):
                cb[g, ct, :, h, :] = -sl * (q - 16 * (k + 128 * ct) - 31)
    c["c_abias"] = ab.reshape(4 * 128, 512)
    c["c_cbias"] = cb.reshape(4 * 2 * 128, 512)
    cval = np.zeros((256, 1), np.float32)
    cval[(128 if s == 0 else 0):255] = 1.0
    cm = np.zeros((16, 2, 128, 128), np.float32)
    for i in range(16):
        for ct in range(2):
            cc = k + 128 * ct
            d = 2048 + 128 * i + q - 16 * cc - 31
            cm[i, ct] = (d >= 0) * cval[cc[:, 0]]
    c["c_cmask"] = ((cm - 1.0) * BIG).reshape(16 * 2 * 128, 128).astype(np.float32)
    c["c_tribias"] = np.concatenate([(c["c_trile"] - 1.0) * BIG, (c["c_trigt"] - 1.0) * BIG], axis=1).astype(np.float32)
    c["c_expand"] = (np.arange(T)[None, :] // 64 == np.arange(64)[:, None]).astype(np.float32)
    c["c_expbig"] = c["c_expand"] * BIG
    ce = np.arange(256)[:, None] * 16 + 31
    cs = ce - 31
    ss = np.arange(64)[None, :] * 64
    c["c_overlap"] = np.clip(np.minimum(ce, ss + 63) - np.maximum(cs, ss) + 1, 0, None).astype(np.float32)
    j0 = 32 if s == 0 else 0
    qi = np.arange(TO)[:, None]
    cur = 32 + qi // 64
    j = np.arange(64)[None, :]
    invalid = (j < j0) | (j > cur)
    forced = ((j == j0) | (j == cur) | (j == cur - 1)) & ~invalid
    c["c_selmul"] = (~invalid & ~forced).astype(np.float32)
    c["c_seladd"] = np.where(invalid, -BIG, np.where(forced, BIG, 0.0)).astype(np.float32)
    c["c_selvalid"] = (~invalid).astype(np.float32)
    s_ = np.arange(64)[:, None]
    t_ = np.arange(64)[None, :]
    incl = (s_ <= t_).astype(np.float32)
    strict = (s_ < t_).astype(np.float32)
    low = (t_ < s_).astype(np.float32)
    c["c_rw"] = np.concatenate([incl, strict, strict, incl, low, np.eye(64, dtype=np.float32), incl], axis=1)
    return c


def prep_core_inputs(inputs, c, consts_cache={}):
    b, s = c // 2, c % 2
    m = {}
    x = inputs["x"]
    if s == 1:
        m["xc"] = np.ascontiguousarray(x[b])
    else:
        m["xc"] = np.concatenate([np.zeros((2048, D), np.float32), x[b, :2048]], axis=0)
    m["p_own"] = np.ascontiguousarray(inputs["p"][0, b, 2048 * s:2048 * s + 2048])
    for name, shape in INPUT_SPECS:
        if name in ("xc", "p_own") or name.startswith("c_"):
            continue
        a = np.asarray(inputs[name], np.float32)
        if name in ("exp_b_gate", "exp_b_up"):
            a = np.ascontiguousarray(a.reshape(32, 16, 128).transpose(2, 0, 1))
        m[name] = a.reshape(shape)
    if s not in consts_cache:
        consts_cache[s] = make_consts(s)
    m.update(consts_cache[s])
    return m


def kernel(**inputs):
    nc, G = build()
    in_maps = [prep_core_inputs(inputs, c) for c in range(NCORES)]
    res = run_bass_kernel_spmd(nc, in_maps, core_ids=list(range(NCORES)))
    out = np.zeros((4, 4096, D), np.float32)
    for c in range(NCORES):
        b, s = c // 2, c % 2
        out[b, 2048 * s:2048 * s + 2048] = res.results[c]["out"]
    return out
```

```python
import os
import numpy as np
from contextlib import ExitStack
import concourse.bass as bass
import concourse.mybir as mybir
from concourse.bass_utils import run_bass_kernel_spmd

F32 = mybir.dt.float32
BF16 = mybir.dt.bfloat16
ALU = mybir.AluOpType
AF = mybir.ActivationFunctionType
AX = mybir.AxisListType

NCORES = 8
D = 2048
T = 4096
TO = 2048
NSA_COLS = 2608
RW0 = NSA_COLS
IN_COLS = 6128
SEM_ROT = 12000


class Sync:
    ENGS = ("pe", "act", "dve", "pool", "sp")

    def __init__(self, nc, stack):
        self.nc = nc
        self.stack = stack
        self.eng = {"pe": nc.tensor, "act": nc.scalar, "dve": nc.vector,
                    "pool": nc.gpsimd, "sp": nc.sync}
        self.cnt = {e: 0 for e in self.ENGS}
        self.sems = {e: [] for e in self.ENGS}
        self.waited = {e: {} for e in self.ENGS}
        self.last_w = {}
        self.readers = {}
        self.dma_pool = {}
        self.dma_n = {}
        self.all_dma = []
        self.nsem = 0
        for q in ("sp", "pool", "act"):
            self.dma_pool[q] = [self._newsem() for _ in range(12 if q == "sp" else 6)]
            self.dma_n[q] = 0

    def _newsem(self):
        self.nsem += 1
        return self.stack.enter_context(self.nc.semaphore(f"s{self.nsem}"))

    @staticmethod
    def _key(t):
        return t if isinstance(t, (str, tuple)) else t.tensor.name if hasattr(t, "tensor") else t.name

    def _wait(self, e, tok):
        sem, val, src = tok
        if src == e and e == "pe":
            return
        w = self.waited[e]
        k = id(sem)
        if w.get(k, 0) >= val:
            return
        w[k] = val
        self.eng[e].wait_ge(sem, val)

    def _deps(self, e, r, w):
        toks = []
        for t in r:
            k = self._key(t)
            if k in self.last_w:
                toks.append(self.last_w[k])
        for t in w:
            k = self._key(t)
            if k in self.last_w:
                toks.append(self.last_w[k])
            for tk in self.readers.get(k, {}).values():
                if isinstance(tk, list):
                    toks.extend(tk)
                else:
                    toks.append(tk)
        for tk in toks:
            self._wait(e, tk)

    def _record(self, tok, r, w, isdma):
        for t in w:
            k = self._key(t)
            self.last_w[k] = tok
            self.readers[k] = {}
        for t in r:
            k = self._key(t)
            d = self.readers.setdefault(k, {})
            if isdma:
                d.setdefault("dma", []).append(tok)
                if len(d["dma"]) > 24:
                    d["dma"] = d["dma"][-24:]
            else:
                d[tok[2]] = tok

    def op(self, e, fn, r=(), w=()):
        self._deps(e, r, w)
        n = self.cnt[e]
        si, v = divmod(n, SEM_ROT)
        while len(self.sems[e]) <= si:
            self.sems[e].append(self._newsem())
        sem = self.sems[e][si]
        ins = fn()
        ins.then_inc(sem, 1)
        self.cnt[e] = n + 1
        tok = (sem, v + 1, e)
        self._record(tok, r, w, False)
        return tok

    def dma(self, q, out, in_, r=(), w=(), **kw):
        self._deps(q, r, w)
        n = self.dma_n[q]
        pool = self.dma_pool[q]
        sem = pool[n % len(pool)]
        rnd = n // len(pool)
        if rnd > 0:
            self._wait(q, (sem, 16 * rnd, "dma"))
        self.eng[q].dma_start(out=out, in_=in_, **kw).then_inc(sem, 16)
        self.dma_n[q] = n + 1
        tok = (sem, 16 * (rnd + 1), "dma")
        self._record(tok, r, w, True)
        self.all_dma.append(tok)
        if len(self.all_dma) > 64:
            self.all_dma = self.all_dma[-64:]
        return tok

    def barrier(self, scratch):
        for q in self.dma_pool:
            n = self.dma_n[q]
            pool = self.dma_pool[q]
            for i, sem in enumerate(pool):
                uses = (n - i + len(pool) - 1) // len(pool) if n > i else 0
                if uses > 0:
                    self._wait("dve", (sem, 16 * uses, "dma"))
        toks = []
        for e in ("pe", "act", "pool"):
            if self.cnt[e] > 0:
                n = self.cnt[e] - 1
                si, v = divmod(n, SEM_ROT)
                toks.append((self.sems[e][si], v + 1, e))
        for tk in toks:
            self._wait("dve", tk)
        tok = self.op("dve", lambda: self.nc.vector.memset(scratch[0:1, 0:1], 0.0), w=["_bar"])
        for e in ("pe", "act", "pool", "sp"):
            self._wait(e, tok)
        self.last_w = {}
        self.readers = {}

    def finish(self):
        for q in self.dma_pool:
            n = self.dma_n[q]
            pool = self.dma_pool[q]
            for i, sem in enumerate(pool):
                uses = (n - i + len(pool) - 1) // len(pool) if n > i else 0
                if uses > 0:
                    self._wait("sp", (sem, 16 * uses, "dma"))
        for e in ("pe", "act", "dve", "pool"):
            if self.cnt[e] > 0:
                n = self.cnt[e] - 1
                si, v = divmod(n, SEM_ROT)
                self._wait("sp", (self.sems[e][si], v + 1, e))


SLOPES = [2.0 ** (-8.0 * (i + 1) / 16.0) for i in range(16)]
SCALE = 64 ** -0.5
ALPHA = 2.0 ** 0.25
BIG = 1.0e30

INPUT_SPECS = [
    ("xc", [T, D]), ("p_own", [TO, 256]), ("w_in", [D, IN_COLS]),
    ("cmp_pe_k", [32, 64]), ("cmp_w1_k", [2048, 64]), ("cmp_w2_k", [64, 64]),
    ("cmp_pe_v", [32, 64]), ("cmp_w1_v", [2048, 64]), ("cmp_w2_v", [64, 64]),
    ("rwkv_mu", [1, 3520]), ("rwkv_w0", [1, 1024]), ("rwkv_w_up", [96, 1024]),
    ("rwkv_a0", [1, 1024]), ("rwkv_a_up", [96, 1024]), ("rwkv_g_up", [256, 1024]),
    ("rwkv_k_k", [1, 1024]), ("rwkv_k_a", [1, 1024]), ("rwkv_r_k", [1, 1024]),
    ("rwkv_ln_g", [1, 1024]), ("rwkv_ln_b", [1, 1024]),
    ("w_out", [D, D]), ("ln1_g", [1, D]), ("ln1_b", [1, D]),
    ("router_w", [D, 32]), ("router_b", [1, 32]),
    ("exp_w_gate", [32 * D, D]), ("exp_b_gate", [128, 512]),
    ("exp_w_up", [32 * D, D]), ("exp_b_up", [128, 512]),
    ("exp_w_down", [32 * D, D]), ("exp_b_down", [32, D]),
    ("ln2_g", [1, D]), ("ln2_b", [1, D]), ("ple_w", [256, D]), ("ple_gate_w", [D, D]),
    ("c_kvalid", [128, 32]), ("c_ident", [128, 128]), ("c_trile", [128, 128]), ("c_trigt", [128, 128]),
    ("c_abias", [4 * 128, 512]), ("c_cbias", [4 * 2 * 128, 512]), ("c_cmask", [16 * 2 * 128, 128]),
    ("c_expand", [64, T]), ("c_overlap", [256, 64]), ("c_tribias", [128, 256]), ("c_expbig", [64, T]),
    ("c_selmul", [TO, 64]), ("c_seladd", [TO, 64]), ("c_selvalid", [TO, 64]),
    ("c_rw", [64, 448]),
]


class Ctx:
    pass


def build(debug=(), phases="ABCDEF", skip=()):
    nc = bass.Bass("TRN2", target_bir_lowering=False)
    G = Ctx()
    G.nc = nc
    G.I = {}
    for name, shape in INPUT_SPECS:
        if name in skip:
            continue
        G.I[name] = nc.dram_tensor(name, shape, F32, kind="ExternalInput").ap()
    G.out = nc.dram_tensor("out", [TO, D], F32, kind="ExternalOutput").ap()
    G.dbg = {}
    G.debug = debug
    dr = lambda n, s, d: nc.dram_tensor(n, s, d).ap()
    G.vs_tm = dr("vs_tm", [T, 4 * 65], BF16)
    G.vw_tm = dr("vw_tm", [T, 4 * 65], BF16)
    G.gl_tm = dr("gl_tm", [TO, 48], F32)
    G.zr = dr("zr", [T, 3520], F32)
    G.qT = dr("qT", [1024, TO], BF16)
    G.kcT = dr("kcT", [256, T], BF16)
    G.vcT = dr("vcT", [256, T], BF16)
    G.ksT = dr("ksT", [256, T], BF16)
    G.kwT = dr("kwT", [256, T], BF16)
    G.mixT = dr("mixT", [D, TO], BF16)
    G.h1 = dr("h1", [TO, D], F32)
    G.h1T = dr("h1T", [D, TO], BF16)
    G.gw = dr("gw", [TO, 32], F32)
    G.ypre = dr("ypre", [TO, D], F32)
    for n in ("rw_r", "rw_k", "rw_v", "rw_kn", "rw_b", "rw_lw"):
        setattr(G, n, dr(n, [T, 1024], F32))
    G.rw_g = dr("rw_g", [TO, 1024], F32)
    G.rw_y = dr("rw_y", [TO, 1024], F32)
    with ExitStack() as st:
        S = Sync(nc, st)
        G.S = S
        G.bar = st.enter_context(nc.sbuf_tensor("barscr", [128, 8], F32))
        for nm, shp in debug:
            dbg_out(G, nm, shp)
        if "A" in phases:
            phase_a(G)
        if "B" in phases:
            phase_b(G)
        if "C" in phases:
            phase_c(G)
        if "D" in phases:
            phase_d(G)
        if "E" in phases:
            phase_e(G)
        if "F" in phases:
            phase_f(G)
        S.finish()
    return nc, G


def dbg_out(G, name, shape, dt=F32):
    t = G.nc.dram_tensor("dbg_" + name, shape, dt, kind="ExternalOutput").ap()
    G.dbg[name] = t
    return t


_evac_rr = [0]


def evac(G, out, in_, r, w, scale=None):
    nc, S = G.nc, G.S
    _evac_rr[0] ^= 1
    if _evac_rr[0]:
        if scale is None:
            return S.op("act", lambda: nc.scalar.copy(out=out, in_=in_), r=r, w=w)
        return S.op("act", lambda: nc.scalar.mul(out=out, in_=in_, mul=scale), r=r, w=w)
    if scale is None:
        return S.op("dve", lambda: nc.vector.tensor_copy(out=out, in_=in_), r=r, w=w)
    return S.op("dve", lambda: nc.vector.tensor_scalar(out=out, in0=in_, scalar1=scale, scalar2=None, op0=ALU.mult), r=r, w=w)


def run_pipelined(gens, depth):
    active = []
    it = iter(gens)
    more = True
    while True:
        if more and len(active) < depth:
            try:
                active.append(next(it))
            except StopIteration:
                more = False
        if not active:
            break
        for g in list(active):
            try:
                next(g)
            except StopIteration:
                active.remove(g)


def phase_a(G):
    nc, S, I = G.nc, G.S, G.I
    with ExitStack() as st:
        sb = lambda n, s, d: st.enter_context(nc.sbuf_tensor(n, s, d))
        ps = lambda n, s, d: st.enter_context(nc.psum_tensor(n, s, d))
        ident = sb("a_ident", [128, 128], BF16)
        kval = sb("a_kval", [128, 32], F32)
        xb = [sb(f"a_xb{i}", [128, D], BF16) for i in range(2)]
        xT = sb("a_xT", [128, 16, 2048], BF16)
        wch = [sb(f"a_w{i}", [128, 16, 512], BF16) for i in range(2)]
        stf = [sb(f"a_stf{i}", [128, 512], F32) for i in range(3)]
        stb = [sb(f"a_stb{i}", [128, 512], BF16) for i in range(3)]
        vst = [sb(f"a_vst{i}", [128, 4, 65], BF16) for i in range(2)]
        ptr = [ps(f"a_ptr{i}", [128, 1024], BF16) for i in range(2)]
        pac = [ps(f"a_pac{i}", [128, 512], F32) for i in range(4)]
        S.dma("pool", ident[:], I["c_ident"][:, :], w=[ident])
        S.dma("sp", kval[:], I["c_kvalid"][:, :], w=[kval])
        w_in_v = I["w_in"].rearrange("(kc p) c -> p kc c", p=128)
        tm_chunks = [("vs", 1792, 2048), ("vw", 2304, 2560), ("gl", 2560, 2608)]
        c = 0
        while c < 3520:
            n = min(512, 3520 - c)
            tm_chunks.append(("rw", RW0 + c, RW0 + c + n))
            c += n
        fm_chunks = [("q", 128 * j, 128 * j + 128) for j in range(8)]
        for nm, base in (("kc", 1024), ("vc", 1280), ("ks", 1536), ("kw", 2048)):
            fm_chunks += [(nm, base, base + 128), (nm, base + 128, base + 256)]
        fm_dst = {"q": (G.qT, 0), "kc": (G.kcT, 1024), "vc": (G.vcT, 1280), "ks": (G.ksT, 1536), "kw": (G.kwT, 2048)}
        nw = 0
        nst = 0
        npac = 0
        for hf in range(2):
            for tl in range(16):
                tok0 = hf * 2048 + tl * 128
                xbt = xb[tl % 2]
                S.dma("pool", xbt[:], I["xc"][tok0:tok0 + 128, :], w=[xbt])
                for j in range(2):
                    pt = ptr[j]
                    for k8 in range(8):
                        kc = j * 8 + k8
                        S.op("pe", lambda pt=pt, k8=k8, kc=kc, xbt=xbt: nc.tensor.transpose(
                            pt[:, k8 * 128:(k8 + 1) * 128], xbt[:, kc * 128:(kc + 1) * 128], ident[:]),
                            r=[xbt, ident], w=[pt])
                    evac(G, xT[:, j * 8:(j + 1) * 8, tl * 128:(tl + 1) * 128],
                         pt[:].rearrange("p (a b) -> p a b", a=8), r=[pt], w=[xT])
            for (nm, c0, c1) in tm_chunks:
                if nm == "gl" and hf == 0:
                    continue
                ncol = c1 - c0
                wt = wch[nw % 2]
                nw += 1
                S.dma("pool", wt[:, :, 0:ncol], w_in_v[:, :, c0:c1], w=[wt])
                for tl in range(16):
                    tok0 = hf * 2048 + tl * 128
                    pa = pac[npac % 4]
                    npac += 1
                    for kc in range(16):
                        S.op("pe", lambda pa=pa, kc=kc, wt=wt, tl=tl, ncol=ncol: nc.tensor.matmul(
                            pa[:, 0:ncol], lhsT=xT[:, kc, tl * 128:(tl + 1) * 128], rhs=wt[:, kc, 0:ncol],
                            start=(kc == 0), stop=(kc == 15)), r=[xT, wt], w=[pa])
                    if nm in ("vs", "vw"):
                        vt = vst[nst % 2]
                        nst += 1
                        evac(G, vt[:, :, 0:64], pa[:, 0:256].rearrange("p (g d) -> p g d", g=4), r=[pa], w=[vt])
                        tglob = hf * 16 + tl
                        S.op("pool", lambda vt=vt, tglob=tglob: nc.gpsimd.tensor_copy(
                            out=vt[:, :, 64], in_=kval[:, tglob:tglob + 1].to_broadcast([128, 4])),
                            r=[kval, vt], w=[vt])
                        dst = G.vs_tm if nm == "vs" else G.vw_tm
                        S.dma("sp", dst[tok0:tok0 + 128, :], vt[:].rearrange("p g d -> p (g d)"), r=[vt], w=[nm + "_tm"])
                    else:
                        sf = stf[nst % 3]
                        nst += 1
                        evac(G, sf[:, 0:ncol], pa[:, 0:ncol], r=[pa], w=[sf])
                        if nm == "gl":
                            S.dma("sp", G.gl_tm[tl * 128:(tl + 1) * 128, :], sf[:, 0:48], r=[sf], w=["gl_tm"])
                        else:
                            S.dma("sp", G.zr[tok0:tok0 + 128, c0 - RW0:c1 - RW0], sf[:, 0:ncol], r=[sf], w=["zr"])
            for (nm, c0, c1) in fm_chunks:
                if nm == "q" and hf == 0:
                    continue
                wt = wch[nw % 2]
                nw += 1
                S.dma("pool", wt[:, :, 0:128], w_in_v[:, :, c0:c1], w=[wt])
                dst, base = fm_dst[nm]
                for t4 in range(4):
                    pa = pac[npac % 4]
                    npac += 1
                    for kc in range(16):
                        S.op("pe", lambda pa=pa, kc=kc, wt=wt, t4=t4: nc.tensor.matmul(
                            pa[:, :], lhsT=wt[:, kc, 0:128], rhs=xT[:, kc, t4 * 512:(t4 + 1) * 512],
                            start=(kc == 0), stop=(kc == 15)), r=[xT, wt], w=[pa])
                    sbt = stb[nst % 3]
                    nst += 1
                    evac(G, sbt[:, :], pa[:, :], r=[pa], w=[sbt])
                    if nm == "q":
                        col0 = t4 * 512
                    else:
                        col0 = hf * 2048 + t4 * 512
                    S.dma("sp", dst[c0 - base:c1 - base, col0:col0 + 512], sbt[:, :], r=[sbt], w=[nm + "T"])
        S.barrier(G.bar)


def phase_b(G):
    nc, S, I = G.nc, G.S, G.I
    with ExitStack() as st:
        sb = lambda n, s, d: st.enter_context(nc.sbuf_tensor(n, s, d))
        ps = lambda n, s, d: st.enter_context(nc.psum_tensor(n, s, d))
        ident = sb("b_ident", [128, 128], BF16)
        tribias = sb("b_tribias", [128, 256], F32)
        tribias_bf = sb("b_tribias_bf", [128, 256], BF16)
        expbig = sb("b_expbig", [64, T], BF16)
        cmask = sb("b_cmask", [128, 32, 128], BF16)
        abias = sb("b_abias", [128, 512], F32)
        cbias = sb("b_cbias", [128, 2, 512], F32)
        ksT = sb("b_ksT", [64, T], BF16)
        kwT = sb("b_kwT", [64, T], BF16)
        cT = sb("b_cT", [64, T], BF16)
        vs = sb("b_vs", [128, 32, 65], BF16)
        vw = sb("b_vw", [128, 32, 65], BF16)
        qT = sb("b_qT", [64, 4, TO], BF16)
        w1 = [sb(f"b_w1{i}", [128, 16, 64], F32) for i in range(2)]
        w1b = [sb(f"b_w1b{i}", [64, 32, 64], BF16) for i in range(2)]
        w2b = [sb(f"b_w2b{i}", [64, 64], BF16) for i in range(2)]
        pec = [sb(f"b_pec{i}", [128, 16], F32) for i in range(2)]
        b1 = [sb(f"b_b1{i}", [64, 1], F32) for i in range(2)]
        hf = sb("b_hf", [64, 256], F32)
        h2 = sb("b_h2", [64, 256], F32)
        gT = sb("b_gT", [64, 256], BF16)
        kcmpT = sb("b_kcmpT", [64, 256], BF16)
        vcx = sb("b_vcx", [128, 2, 129], BF16)
        tmp = [sb(f"b_tmp{i}", [128, 512], F32) for i in range(2)]
        tmp2 = [sb(f"b_tmq{i}", [128, 512], F32) for i in range(2)]
        Pt = [sb(f"b_P{i}", [128, 512], BF16) for i in range(3)]
        glt = sb("b_gl", [128, 48], F32)
        gsig = sb("b_gsig", [128, 48], F32)
        selc = [sb(f"b_selc{i}", [128, 64], F32) for i in range(3)]
        imp = sb("b_imp", [128, 64], F32)
        imp2 = sb("b_imp2", [128, 64], F32)
        m8 = sb("b_m8", [128, 16], F32)
        selb = sb("b_selb", [128, 64], BF16)
        selT = sb("b_selT", [64, 512], BF16)
        tri4 = sb("b_tri4", [128, 512], BF16)
        sums = sb("b_sums", [128, 12], F32)
        coef = sb("b_coef", [128, 12], F32)
        oacc = sb("b_oacc", [128, 256], F32)
        obf = sb("b_obf", [128, 256], BF16)
        ost = sb("b_ost", [128, 2, 128], BF16)
        pS = [ps(f"b_pS{i}", [128, 512], F32) for i in range(2)]
        pM = ps("b_pM", [128, 512], F32)
        PMK = [("pM", j) for j in range(4)]
        pAs = ps("b_pAs", [128, 4, 65], F32)
        pAw = ps("b_pAw", [128, 4, 65], F32)
        pAc = [ps(f"b_pAc{i}", [128, 2, 129], F32) for i in range(2)]
        pT = ps("b_pT", [128, 1024], BF16)

        S.dma("pool", ident[:], I["c_ident"][:, :], w=[ident])
        S.dma("sp", tribias[:], I["c_tribias"][:, :], w=[tribias])
        S.dma("pool", tribias_bf[:], I["c_tribias"][:, :], w=[tribias_bf])
        S.op("pool", lambda: nc.gpsimd.tensor_copy(out=tri4[:].rearrange("p (h q) -> p h q", h=4), in_=tribias_bf[:, 0:128].unsqueeze(1).to_broadcast([128, 4, 128])), r=[tribias_bf], w=[tri4])
        for j in range(4):
            S.dma("pool", expbig[:, j * 1024:(j + 1) * 1024], I["c_expbig"][:, j * 1024:(j + 1) * 1024], w=[expbig])
        cm_v = I["c_cmask"].rearrange("(a p) q -> p a q", p=128)
        for j in range(4):
            S.dma("pool", cmask[:, j * 8:(j + 1) * 8, :], cm_v[:, j * 8:(j + 1) * 8, :], w=[cmask])
        for kv, (nw1, nw2, npe) in enumerate((("cmp_w1_k", "cmp_w2_k", "cmp_pe_k"), ("cmp_w1_v", "cmp_w2_v", "cmp_pe_v"))):
            S.dma("sp", w1[kv][:], I[nw1].rearrange("(p c) o -> p c o", c=16), w=[w1[kv]])
            S.dma("pool", w1b[kv][:], I[nw1].rearrange("(l d) o -> d l o", d=64), w=[w1b[kv]])
            S.dma("pool", w2b[kv][:], I[nw2][:, :], w=[w2b[kv]])
            S.dma("sp", pec[kv][:], I[npe].rearrange("l d -> (l d)").rearrange("(p c) -> p c", c=16), w=[pec[kv]])
            for ch in range(16):
                S.op("pe", lambda kv=kv, ch=ch: nc.tensor.matmul(pM[0:64, 0:1], lhsT=w1[kv][:, ch, :], rhs=pec[kv][:, ch:ch + 1],
                                                                   start=(ch == 0), stop=(ch == 15)), r=[w1[kv], pec[kv]], w=PMK)
            S.op("dve", lambda kv=kv: nc.vector.tensor_copy(out=b1[kv][:], in_=pM[0:64, 0:1]), r=PMK, w=[b1[kv]])
        S.op("dve", lambda: nc.vector.memset(vcx[:], 0.0), w=[vcx])
        S.op("dve", lambda: nc.vector.memset(vcx[:, :, 64:65], 1.0), r=[vcx], w=[vcx])
        S.dma("pool", vcx[:, :, 65:129], I["c_overlap"].rearrange("(ct p) j -> p ct j", p=128), r=[vcx], w=[vcx])
        S.op("dve", lambda: nc.vector.memset(kcmpT[:], 0.0), w=[kcmpT])

        def compress(kv, g):
            src = G.kcT if kv == 0 else G.vcT
            S.dma("sp", cT[:], src[g * 64:(g + 1) * 64, :], r=["kcT", "vcT"], w=[cT])
            cv = cT[:].rearrange("p (c s) -> p c s", s=16)
            for l in range(32):
                rhs = cv[:, 0:255, l] if l < 16 else cv[:, 1:256, l - 16]
                S.op("pe", lambda l=l, rhs=rhs: nc.tensor.matmul(pM[0:64, 0:255], lhsT=w1b[kv][:, l, :], rhs=rhs,
                                                                  start=(l == 0), stop=(l == 31)), r=[w1b[kv], cT], w=PMK)
            S.op("act", lambda: nc.scalar.activation(out=hf[:, 0:255], in_=pM[0:64, 0:255], func=AF.Identity, bias=b1[kv][:, 0:1], scale=1.0),
                 r=PMK + [b1[kv]], w=[hf])
            S.op("dve", lambda: nc.vector.tensor_tensor(out=h2[:, 0:255], in0=hf[:, 0:255], in1=hf[:, 0:255], op=ALU.mult), r=[hf], w=[h2])
            S.op("dve", lambda: nc.vector.tensor_scalar(out=h2[:, 0:255], in0=h2[:, 0:255], scalar1=0.044715, scalar2=1.0, op0=ALU.mult, op1=ALU.add), r=[h2], w=[h2])
            S.op("dve", lambda: nc.vector.tensor_tensor(out=h2[:, 0:255], in0=h2[:, 0:255], in1=hf[:, 0:255], op=ALU.mult), r=[h2, hf], w=[h2])
            S.op("act", lambda: nc.scalar.activation(out=h2[:, 0:255], in_=h2[:, 0:255], func=AF.Tanh, scale=0.7978845608028654), r=[h2], w=[h2])
            S.op("dve", lambda: nc.vector.scalar_tensor_tensor(out=gT[:, 0:255], in0=h2[:, 0:255], scalar=1.0, in1=hf[:, 0:255], op0=ALU.add, op1=ALU.mult),
                 r=[h2, hf], w=[gT])
            if kv == 0:
                S.op("pe", lambda: nc.tensor.matmul(pM[0:64, 0:255], lhsT=w2b[0][:, :], rhs=gT[:, 0:255], start=True, stop=True), r=[w2b[0], gT], w=PMK)
                S.op("act", lambda: nc.scalar.mul(out=kcmpT[:, 0:255], in_=pM[0:64, 0:255], mul=0.5), r=PMK, w=[kcmpT])
            else:
                for ct in range(2):
                    rows = 128 if ct == 0 else 127
                    S.op("pe", lambda ct=ct, rows=rows: nc.tensor.matmul(pM[0:rows, 0:64], lhsT=gT[:, ct * 128:ct * 128 + rows], rhs=w2b[1][:, :],
                                                                          start=True, stop=True), r=[w2b[1], gT], w=PMK)
                    S.op("act", lambda ct=ct, rows=rows: nc.scalar.mul(out=vcx[0:rows, ct, 0:64], in_=pM[0:rows, 0:64], mul=0.5), r=PMK, w=[vcx])

        np_ = [0]
        for g in range(4):
            S.dma("sp", ksT[:], G.ksT[g * 64:(g + 1) * 64, :], r=["ksT"], w=[ksT])
            S.dma("sp", kwT[:], G.kwT[g * 64:(g + 1) * 64, :], r=["kwT"], w=[kwT])
            S.dma("sp", qT[:], G.qT[g * 256:(g + 1) * 256, :].rearrange("(h d) t -> d h t", d=64), r=["qT"], w=[qT])
            vs_v = G.vs_tm.rearrange("(t p) c -> p t c", p=128)
            vw_v = G.vw_tm.rearrange("(t p) c -> p t c", p=128)
            for j in range(4):
                S.dma("sp", vs[:, j * 8:(j + 1) * 8, :], vs_v[:, j * 8:(j + 1) * 8, g * 65:(g + 1) * 65], r=["vs_tm"], w=[vs])
                S.dma("sp", vw[:, j * 8:(j + 1) * 8, :], vw_v[:, j * 8:(j + 1) * 8, g * 65:(g + 1) * 65], r=["vw_tm"], w=[vw])
            S.dma("sp", abias[:], I["c_abias"][g * 128:(g + 1) * 128, :], w=[abias])
            S.dma("sp", cbias[:], I["c_cbias"][g * 256:(g + 1) * 256, :].rearrange("(ct p) x -> p ct x", p=128), w=[cbias])
            compress(0, g)
            compress(1, g)
            for i in range(16):
                q0 = i * 128
                S.dma("sp", glt[:], G.gl_tm[q0:q0 + 128, :], r=["gl_tm"], w=[glt])
                S.op("act", lambda: nc.scalar.activation(out=gsig[:], in_=glt[:], func=AF.Sigmoid), r=[glt], w=[gsig])
                for j, nm in enumerate(("c_selmul", "c_seladd", "c_selvalid")):
                    S.dma("sp", selc[j][:], I[nm][q0:q0 + 128, :], w=[selc[j]])
                gv = gsig[:, 12 * g:12 * g + 12].rearrange("p (h r) -> p h r", r=3)
                qrhs = qT[:, :, q0:q0 + 128]

                def tile_gen(lhsT, bias_ap, mask_fn, offs, pv_fn, rows=128, extra=()):
                    n = np_[0]
                    np_[0] += 1
                    p_s, t1, t2, P = pS[n % 2], tmp[n % 2], tmp2[n % 2], Pt[n % 3]
                    mk = mask_fn(n) if mask_fn is not None else None
                    S.op("pe", lambda: nc.tensor.matmul(p_s[0:rows, :], lhsT=lhsT, rhs=qrhs, start=True, stop=(len(extra) == 0)), r=[ksT, kwT, kcmpT, qT], w=[p_s])
                    for xi, (xl, xr) in enumerate(extra):
                        S.op("pe", lambda xl=xl, xr=xr, xi=xi: nc.tensor.matmul(p_s[0:rows, :], lhsT=xl, rhs=xr, start=False, stop=(xi == len(extra) - 1)),
                             r=[expbig, selT, ident, tri4], w=[p_s])
                    yield
                    S.op("dve", lambda: nc.vector.scalar_tensor_tensor(out=t1[0:rows, :], in0=p_s[0:rows, :], scalar=SCALE, in1=bias_ap,
                                                                        op0=ALU.mult, op1=ALU.add), r=[p_s, abias, cbias], w=[t1])
                    src = t1
                    if mk is not None:
                        mask_ap, mkey = mk
                        S.op("dve", lambda: nc.vector.tensor_tensor(out=t2[0:rows, :].rearrange("p (h q) -> p h q", h=4),
                                                                     in0=t1[0:rows, :].rearrange("p (h q) -> p h q", h=4),
                                                                     in1=mask_ap, op=ALU.add), r=[t1, mkey], w=[t2])
                        src = t2
                    yield
                    for h in range(4):
                        S.op("act", lambda h=h: nc.scalar.activation(out=P[0:rows, h * 128:(h + 1) * 128], in_=src[0:rows, h * 128:(h + 1) * 128],
                                                                      func=AF.Exp, bias=float(offs[h]), scale=1.0), r=[src], w=[P])
                    yield
                    pv_fn(P)

                Pc = []

                def cmp_tile(ct):
                    rows = 128 if ct == 0 else 127
                    offs = [-SLOPES[4 * g + h] * (2048 + 128 * i) for h in range(4)]

                    def pv(P):
                        Pc.append(P)
                        if ct == 0:
                            return
                        for h in range(4):
                            for c2 in range(2):
                                r2 = 128 if c2 == 0 else 127
                                S.op("pe", lambda h=h, c2=c2, r2=r2: nc.tensor.matmul(pAc[h // 2][:, h % 2, :], lhsT=Pc[c2][0:r2, h * 128:(h + 1) * 128],
                                                                                       rhs=vcx[0:r2, c2, :], start=(c2 == 0), stop=(c2 == 1)),
                                     r=[Pc[c2], vcx], w=[pAc[h // 2]])
                    return tile_gen(kcmpT[:, ct * 128:ct * 128 + rows], cbias[0:rows, ct, :],
                                    lambda n: (cmask[0:rows, i * 2 + ct, :].unsqueeze(1).to_broadcast([rows, 4, 128]), cmask), offs, pv, rows)

                def win_tile(wi):
                    kt = 12 + i + wi
                    offs = [-SLOPES[4 * g + h] * 128.0 * (4 - wi) for h in range(4)]
                    if wi == 0:
                        mfn = lambda n: (tribias[:, 128:256].unsqueeze(1).to_broadcast([128, 4, 128]), tribias)
                    elif wi == 4:
                        mfn = lambda n: (tribias[:, 0:128].unsqueeze(1).to_broadcast([128, 4, 128]), tribias)
                    else:
                        mfn = None

                    def pv(P):
                        for h in range(4):
                            S.op("pe", lambda h=h: nc.tensor.matmul(pAw[:, h, :], lhsT=P[:, h * 128:(h + 1) * 128], rhs=vw[:, kt, :],
                                                                     start=(wi == 0), stop=(wi == 4)), r=[P, vw], w=[pAw])
                    return tile_gen(kwT[:, kt * 128:(kt + 1) * 128], abias[:, :], mfn, offs, pv)

                run_pipelined([cmp_tile(0), cmp_tile(1)] + [win_tile(wi) for wi in range(5)], 2)
                for hh in range(2):
                    S.op("dve", lambda hh=hh: nc.vector.tensor_scalar(out=sums[:, 2 * hh:2 * hh + 2], in0=pAc[hh][:, :, 64], scalar1=1e-30, scalar2=None, op0=ALU.max),
                         r=[pAc[hh]], w=[sums])
                S.op("dve", lambda: nc.vector.reciprocal(out=sums[:, 0:4], in_=sums[:, 0:4]), r=[sums], w=[sums])
                S.op("dve", lambda: nc.vector.tensor_tensor(out=coef[:, 0:4], in0=sums[:, 0:4], in1=gv[:, :, 0], op=ALU.mult), r=[sums, gsig], w=[coef])
                for h in range(4):
                    pa = pAc[h // 2]
                    if h == 0:
                        S.op("dve", lambda pa=pa, h=h: nc.vector.tensor_scalar(out=imp[:], in0=pa[:, h % 2, 65:129], scalar1=sums[:, h:h + 1], scalar2=None, op0=ALU.mult),
                             r=[pa, sums], w=[imp])
                    else:
                        S.op("dve", lambda pa=pa, h=h: nc.vector.scalar_tensor_tensor(out=imp[:], in0=pa[:, h % 2, 65:129], scalar=sums[:, h:h + 1], in1=imp[:],
                                                                                       op0=ALU.mult, op1=ALU.add), r=[pa, sums, imp], w=[imp])
                    S.op("dve", lambda pa=pa, h=h: nc.vector.tensor_scalar(out=oacc[:, h * 64:(h + 1) * 64], in0=pa[:, h % 2, 0:64], scalar1=coef[:, h:h + 1], scalar2=None, op0=ALU.mult),
                         r=[pa, coef], w=[oacc])
                S.op("dve", lambda: nc.vector.tensor_tensor(out=imp[:], in0=imp[:], in1=selc[0][:], op=ALU.mult), r=[imp, selc[0]], w=[imp])
                S.op("dve", lambda: nc.vector.tensor_tensor(out=imp[:], in0=imp[:], in1=selc[1][:], op=ALU.add), r=[imp, selc[1]], w=[imp])
                S.op("dve", lambda: nc.vector.max(out=m8[:, 0:8], in_=imp[:]), r=[imp], w=[m8])
                S.op("dve", lambda: nc.vector.match_replace(out=imp2[:], in_to_replace=m8[:, 0:8], in_values=imp[:], imm_value=-3.0e38), r=[imp, m8], w=[imp2])
                S.op("dve", lambda: nc.vector.max(out=m8[:, 8:16], in_=imp2[:]), r=[imp2], w=[m8])
                S.op("dve", lambda: nc.vector.tensor_tensor(out=imp2[:], in0=imp[:], in1=m8[:, 15:16].to_broadcast([128, 64]), op=ALU.is_ge), r=[imp, m8], w=[imp2])
                S.op("dve", lambda: nc.vector.tensor_tensor(out=imp2[:], in0=imp2[:], in1=selc[2][:], op=ALU.mult), r=[imp2, selc[2]], w=[imp2])
                S.op("dve", lambda: nc.vector.tensor_scalar(out=selb[:], in0=imp2[:], scalar1=-1.0, scalar2=None, op0=ALU.add), r=[imp2], w=[selb])
                S.op("pe", lambda: nc.tensor.transpose(pT[0:64, 0:128], selb[:, :], ident[:]), r=[selb, ident], w=[pT])
                S.op("act", lambda: nc.scalar.copy(out=selT[:].rearrange("p (h q) -> p h q", h=4), in_=pT[0:64, 0:128].unsqueeze(1).to_broadcast([64, 4, 128])), r=[pT], w=[selT])
                nkt = 17 + i

                def slc_tile(kt):
                    diag = (kt == nkt - 1)
                    offs = [-SLOPES[4 * g + h] * 128.0 * (nkt - 1 - kt) for h in range(4)]

                    extra = [(expbig[:, kt * 128:(kt + 1) * 128], selT[:, :])]
                    if diag:
                        extra.append((ident[:, :], tri4[:, :]))

                    def pv(P):
                        for h in range(4):
                            S.op("pe", lambda h=h: nc.tensor.matmul(pAs[:, h, :], lhsT=P[:, h * 128:(h + 1) * 128], rhs=vs[:, kt, :],
                                                                     start=(kt == 0), stop=(kt == nkt - 1)), r=[P, vs], w=[pAs])
                    return tile_gen(ksT[:, kt * 128:(kt + 1) * 128], abias[:, :], None, offs, pv, extra=extra)

                run_pipelined([slc_tile(kt) for kt in range(nkt)], 2)
                for bi, pa in ((1, pAs), (2, pAw)):
                    S.op("dve", lambda bi=bi, pa=pa: nc.vector.tensor_scalar(out=sums[:, 4 * bi:4 * bi + 4], in0=pa[:, :, 64], scalar1=1e-30, scalar2=None, op0=ALU.max),
                         r=[pa], w=[sums])
                    S.op("dve", lambda bi=bi: nc.vector.reciprocal(out=sums[:, 4 * bi:4 * bi + 4], in_=sums[:, 4 * bi:4 * bi + 4]), r=[sums], w=[sums])
                    S.op("dve", lambda bi=bi: nc.vector.tensor_tensor(out=coef[:, 4 * bi:4 * bi + 4], in0=sums[:, 4 * bi:4 * bi + 4], in1=gv[:, :, bi], op=ALU.mult),
                         r=[sums, gsig], w=[coef])
                    for h in range(4):
                        S.op("dve", lambda bi=bi, pa=pa, h=h: nc.vector.scalar_tensor_tensor(out=oacc[:, h * 64:(h + 1) * 64], in0=pa[:, h, 0:64],
                                                                                              scalar=coef[:, 4 * bi + h:4 * bi + h + 1], in1=oacc[:, h * 64:(h + 1) * 64],
                                                                                              op0=ALU.mult, op1=ALU.add), r=[pa, coef, oacc], w=[oacc])
                S.op("act", lambda: nc.scalar.copy(out=obf[:], in_=oacc[:]), r=[oacc], w=[obf])
                for j in range(2):
                    S.op("pe", lambda j=j: nc.tensor.transpose(pT[:, 128 + j * 128:256 + j * 128], obf[:, j * 128:(j + 1) * 128], ident[:]), r=[obf, ident], w=[pT])
                S.op("act", lambda: nc.scalar.copy(out=ost[:], in_=pT[:, 128:384].rearrange("p (j q) -> p j q", j=2)), r=[pT], w=[ost])
                S.dma("sp", G.mixT[g * 256:(g + 1) * 256, q0:q0 + 128].rearrange("(j p) q -> p j q", p=128), ost[:], r=[ost], w=["mixT"])
        S.barrier(G.bar)


def phase_c(G):
    nc, S, I = G.nc, G.S, G.I
    with ExitStack() as st:
        sb = lambda n, s, d: st.enter_context(nc.sbuf_tensor(n, s, d))
        ps = lambda n, s, d: st.enter_context(nc.psum_tensor(n, s, d))
        identf = sb("c_identf", [128, 128], F32)
        mu = sb("c_mu", [128, 3520], F32)
        bc = {}
        for nm in ("rwkv_w0", "rwkv_a0", "rwkv_k_k", "rwkv_k_a"):
            bc[nm] = sb("c_" + nm, [128, 1024], F32)
        wup = sb("c_wup", [96, 1024], F32)
        aup = sb("c_aup", [96, 1024], F32)
        gup = sb("c_gup", [128, 2, 1024], F32)
        z = [sb(f"c_z{i}", [128, 3520], F32) for i in range(2)]
        zp = [sb(f"c_zp{i}", [128, 3520], F32) for i in range(2)]
        lT = sb("c_lT", [128, 4, 128], F32)
        wv = sb("c_wv", [128, 1024], F32)
        av = sb("c_av", [128, 1024], F32)
        gv = sb("c_gv", [128, 1024], F32)
        kk = sb("c_kk", [128, 1024], F32)
        sq = sb("c_sq", [128, 1024], F32)
        ss = sb("c_ss", [128, 16], F32)
        km = sb("c_km", [128, 1024], F32)
        bv = sb("c_bv", [128, 1024], F32)
        pT = ps("c_pT", [128, 512], F32)
        pW = [ps(f"c_pW{i}", [128, 512], F32) for i in range(6)]
        S.dma("sp", identf[:], I["c_ident"][:, :], w=[identf])
        S.dma("sp", mu[:], I["rwkv_mu"][0:1, :].partition_broadcast(128), w=[mu])
        for nm in bc:
            S.dma("sp", bc[nm][:], I[nm][0:1, :].partition_broadcast(128), w=[bc[nm]])
        S.dma("sp", wup[:], I["rwkv_w_up"][:, :], w=[wup])
        S.dma("sp", aup[:], I["rwkv_a_up"][:, :], w=[aup])
        S.dma("sp", gup[:], I["rwkv_g_up"].rearrange("(c p) n -> p c n", p=128), w=[gup])
        for tl in range(32):
            tok0 = tl * 128
            own = tl >= 16
            zt, zpt = z[tl % 2], zp[tl % 2]
            S.dma("sp", zt[:], G.zr[tok0:tok0 + 128, :], r=["zr"], w=[zt])
            if tl == 0:
                S.op("pool", lambda: nc.gpsimd.memset(zpt[0:1, :], 0.0), w=[zpt])
                S.dma("sp", zpt[1:128, :], G.zr[0:127, :], r=["zr", zpt], w=[zpt])
            else:
                S.dma("sp", zpt[:], G.zr[tok0 - 1:tok0 + 127, :], r=["zr"], w=[zpt])
            S.op("pool", lambda: nc.gpsimd.tensor_tensor(out=zpt[:], in0=zpt[:], in1=zt[:], op=ALU.subtract), r=[zpt, zt], w=[zpt])
            S.op("dve", lambda: nc.vector.tensor_tensor(out=zpt[:], in0=zpt[:], in1=mu[:], op=ALU.mult), r=[zpt, mu], w=[zpt])
            S.op("pool", lambda: nc.gpsimd.tensor_tensor(out=zt[:], in0=zt[:], in1=zpt[:], op=ALU.add), r=[zpt, zt], w=[zt])
            r_ = zt[:, 0:1024]
            k_ = zt[:, 1024:2048]
            v_ = zt[:, 2048:3072]
            S.op("pe", lambda: nc.tensor.transpose(pT[0:96, 0:128], zt[:, 3072:3168], identf[:]), r=[zt, identf], w=[pT])
            S.op("pe", lambda: nc.tensor.transpose(pT[0:96, 128:256], zt[:, 3168:3264], identf[:]), r=[zt, identf], w=[pT])
            S.op("act", lambda: nc.scalar.activation(out=lT[0:96, 0, :], in_=pT[0:96, 0:128], func=AF.Tanh), r=[pT], w=[lT])
            S.op("dve", lambda: nc.vector.tensor_copy(out=lT[0:96, 1, :], in_=pT[0:96, 128:256]), r=[pT], w=[lT])
            if own:
                for j in range(2):
                    S.op("pe", lambda j=j: nc.tensor.transpose(pT[:, 256 + j * 128:384 + j * 128], zt[:, 3264 + j * 128:3392 + j * 128], identf[:]), r=[zt, identf], w=[pT])
                S.op("act", lambda: nc.scalar.activation(out=lT[:, 2:4, :], in_=pT[:, 256:512].rearrange("p (a b) -> p a b", a=2), func=AF.Sigmoid), r=[pT], w=[lT])
            for hh in range(2):
                cs = slice(hh * 512, (hh + 1) * 512)
                S.op("pe", lambda hh=hh, cs=cs: nc.tensor.matmul(pW[hh][:, :], lhsT=lT[0:96, 0, :], rhs=wup[:, cs], start=True, stop=True), r=[lT, wup], w=[pW[hh]])
                S.op("pe", lambda hh=hh, cs=cs: nc.tensor.matmul(pW[2 + hh][:, :], lhsT=lT[0:96, 1, :], rhs=aup[:, cs], start=True, stop=True), r=[lT, aup], w=[pW[2 + hh]])
                S.op("dve", lambda hh=hh, cs=cs: nc.vector.tensor_tensor(out=wv[:, cs], in0=pW[hh][:, :], in1=bc["rwkv_w0"][:, cs], op=ALU.add), r=[pW[hh], bc["rwkv_w0"]], w=[wv])
                S.op("dve", lambda hh=hh, cs=cs: nc.vector.tensor_tensor(out=av[:, cs], in0=pW[2 + hh][:, :], in1=bc["rwkv_a0"][:, cs], op=ALU.add), r=[pW[2 + hh], bc["rwkv_a0"]], w=[av])
                if own:
                    for j in range(2):
                        S.op("pe", lambda hh=hh, cs=cs, j=j: nc.tensor.matmul(pW[4 + hh][:, :], lhsT=lT[:, 2 + j, :], rhs=gup[:, j, cs], start=(j == 0), stop=(j == 1)),
                             r=[lT, gup], w=[pW[4 + hh]])
                    S.op("act", lambda hh=hh, cs=cs: nc.scalar.copy(out=gv[:, cs], in_=pW[4 + hh][:, :]), r=[pW[4 + hh]], w=[gv])
            S.op("act", lambda: nc.scalar.activation(out=wv[:], in_=wv[:], func=AF.Sigmoid), r=[wv], w=[wv])
            S.op("pool", lambda: nc.gpsimd.tensor_scalar(out=wv[:], in0=wv[:], scalar1=-0.6065306597126334, scalar2=None, op0=ALU.mult), r=[wv], w=[wv])
            S.op("act", lambda: nc.scalar.activation(out=av[:], in_=av[:], func=AF.Sigmoid), r=[av], w=[av])
            S.op("dve", lambda: nc.vector.tensor_tensor(out=kk[:], in0=k_, in1=bc["rwkv_k_k"][:], op=ALU.mult), r=[zt, bc["rwkv_k_k"]], w=[kk])
            S.op("pool", lambda: nc.gpsimd.tensor_tensor(out=sq[:], in0=kk[:], in1=kk[:], op=ALU.mult), r=[kk], w=[sq])
            S.op("dve", lambda: nc.vector.tensor_reduce(out=ss[:], in_=sq[:].rearrange("p (h k) -> p h k", k=64), axis=AX.X, op=ALU.add), r=[sq], w=[ss])
            S.op("act", lambda: nc.scalar.activation(out=ss[:], in_=ss[:], func=AF.Sqrt), r=[ss], w=[ss])
            S.op("dve", lambda: nc.vector.tensor_scalar(out=ss[:], in0=ss[:], scalar1=1e-12, scalar2=None, op0=ALU.max), r=[ss], w=[ss])
            S.op("dve", lambda: nc.vector.reciprocal(out=ss[:], in_=ss[:]), r=[ss], w=[ss])
            S.op("dve", lambda: nc.vector.tensor_tensor(out=kk[:].rearrange("p (h k) -> p h k", k=64), in0=kk[:].rearrange("p (h k) -> p h k", k=64),
                                                        in1=ss[:].unsqueeze(2).to_broadcast([128, 16, 64]), op=ALU.mult), r=[kk, ss], w=[kk])
            S.op("dve", lambda: nc.vector.scalar_tensor_tensor(out=km[:], in0=av[:], scalar=-1.0, in1=bc["rwkv_k_a"][:], op0=ALU.add, op1=ALU.mult), r=[av, bc["rwkv_k_a"]], w=[km])
            S.op("dve", lambda: nc.vector.scalar_tensor_tensor(out=km[:], in0=km[:], scalar=1.0, in1=k_, op0=ALU.add, op1=ALU.mult), r=[km, zt], w=[km])
            S.op("pool", lambda: nc.gpsimd.tensor_tensor(out=bv[:], in0=kk[:], in1=av[:], op=ALU.mult), r=[kk, av], w=[bv])
            rows = slice(tok0, tok0 + 128)
            S.dma("sp", G.rw_r[rows, :], r_, r=[zt], w=["rw_r"])
            S.dma("sp", G.rw_v[rows, :], v_, r=[zt], w=["rw_v"])
            S.dma("sp", G.rw_k[rows, :], km[:], r=[km], w=["rw_k"])
            S.dma("sp", G.rw_kn[rows, :], kk[:], r=[kk], w=["rw_kn"])
            S.dma("sp", G.rw_b[rows, :], bv[:], r=[bv], w=["rw_b"])
            S.dma("sp", G.rw_lw[rows, :], wv[:], r=[wv], w=["rw_lw"])
            if own:
                S.dma("sp", G.rw_g[tok0 - 2048:tok0 - 1920, :], gv[:], r=[gv], w=["rw_g"])
        S.barrier(G.bar)
    with ExitStack() as st:
        sb = lambda n, s, d: st.enter_context(nc.sbuf_tensor(n, s, d))
        ps = lambda n, s, d: st.enter_context(nc.psum_tensor(n, s, d))
        crw = sb("s_crw", [64, 448], F32)
        MM = sb("s_MM", [64, 320], F32)
        srcs = [G.rw_lw, G.rw_kn, G.rw_r, G.rw_b, G.rw_k, G.rw_v]
        keys = ["rw_lw", "rw_kn", "rw_r", "rw_b", "rw_k", "rw_v"]
        blk = [[[sb(f"s_in{p}_{hh}_{a}", [64, 8, 64], F32) for a in range(6)] for hh in range(4)] for p in range(2)]
        Hs = [[sb(f"s_H{h}_{p}", [64, 64], F32) for p in range(2)] for h in range(16)]
        NW = 4
        E = [sb(f"s_E{i}", [64, 256], F32) for i in range(NW)]
        FM = [sb(f"s_FM{i}", [64, 256], F32) for i in range(NW)]
        BK = [sb(f"s_BK{i}", [64, 128], F32) for i in range(NW)]
        GM = [sb(f"s_GM{i}", [64, 320], F32) for i in range(NW)]
        XX = [[sb(f"s_XX{i}_{p}", [64, 128], F32) for p in range(2)] for i in range(NW)]
        P2 = [[sb(f"s_P2{i}_{p}", [64, 128], F32) for p in range(2)] for i in range(NW)]
        RU = [sb(f"s_RU{i}", [64, 64], F32) for i in range(NW)]
        U = [sb(f"s_U{i}", [64, 64], F32) for i in range(NW)]
        ybuf = [[sb(f"s_y{p}_{hh}", [64, 8, 64], F32) for hh in range(4)] for p in range(2)]
        pA = ps("s_pA", [64, 256], F32)
        pB = ps("s_pB", [64, 256], F32)
        pC = ps("s_pC", [64, 320], F32)
        pD = ps("s_pD", [64, 128], F32)
        pE = ps("s_pE", [64, 128], F32)
        pF = ps("s_pF", [64, 128], F32)
        pG = ps("s_pG", [64, 64], F32)
        pH = ps("s_pH", [64, 64], F32)
        S.dma("sp", crw[:], I["c_rw"][:, :], w=[crw])
        S.op("dve", lambda: nc.vector.tensor_copy(out=MM[:, 0:128], in_=crw[:, 128:256]), r=[crw], w=[MM])
        S.op("dve", lambda: nc.vector.tensor_copy(out=MM[:, 128:320], in_=crw[:, 128:320]), r=[crw, MM], w=[MM])
        TB = crw[:, 0:128]
        I64 = crw[:, 320:384]
        INC = crw[:, 384:448]
        for h in range(16):
            S.op("pool", lambda h=h: nc.gpsimd.memset(Hs[h][0][:], 0.0), w=[Hs[h][0]])
        nwk = 0
        for hg in range(4):
            for b8 in range(8):
                par = (hg * 8 + b8) % 2
                for hh in range(4):
                    h = hg * 4 + hh
                    for a in range(6):
                        S.dma("sp", blk[par][hh][a][:], srcs[a][b8 * 512:(b8 + 1) * 512, h * 64:(h + 1) * 64].rearrange("(c t) k -> t c k", t=64),
                              r=[keys[a]], w=[blk[par][hh][a]])
                own = b8 >= 4
                for c in range(8):
                    cg = b8 * 8 + c
                    for hh in range(4):
                        h = hg * 4 + hh
                        LW, KN, R, B, K, V = [blk[par][hh][a][:, c, :] for a in range(6)]
                        tl = blk[par][hh]
                        w_ = nwk % NW
                        nwk += 1
                        e_, fm, bk, gm, ru, u_ = E[w_], FM[w_], BK[w_], GM[w_], RU[w_], U[w_]
                        Hc, Hn = Hs[h][cg % 2], Hs[h][(cg + 1) % 2]
                        S.op("pe", lambda: nc.tensor.matmul(pA[:, 0:128], lhsT=LW, rhs=TB, start=True, stop=True), r=[tl[0], crw], w=[pA])
                        S.op("pe", lambda: nc.tensor.matmul(pA[:, 128:192], lhsT=INC, rhs=LW, start=True, stop=True), r=[tl[0], crw], w=[pA])
                        S.op("act", lambda: nc.scalar.activation(out=e_[:, 0:128], in_=pA[:, 0:128], func=AF.Exp), r=[pA], w=[e_])
                        S.op("act", lambda: nc.scalar.activation(out=e_[:, 128:256].rearrange("p (a b) -> p a b", a=2),
                                                                 in_=pA[:, 0:256].rearrange("p (a b) -> p a b", a=2)[:, :, 0:64], func=AF.Exp, scale=-1.0), r=[pA], w=[e_])
                        for j, (src, ti) in enumerate(((KN, 1), (R, 2), (B, 3), (K, 4))):
                            S.op("pe", lambda j=j, src=src: nc.tensor.transpose(pB[:, j * 64:(j + 1) * 64], src, I64), r=[tl[ti], crw], w=[pB])
                        S.op("dve", lambda: nc.vector.scalar_tensor_tensor(out=fm[:, 0:64], in0=pB[:, 0:64], scalar=-1.0, in1=e_[:, 64:128], op0=ALU.mult, op1=ALU.mult),
                             r=[pB, e_], w=[fm])
                        S.op("dve", lambda: nc.vector.tensor_tensor(out=fm[:, 64:128], in0=pB[:, 64:128], in1=e_[:, 0:64], op=ALU.mult), r=[pB, e_, fm], w=[fm])
                        S.op("dve", lambda: nc.vector.tensor_tensor(out=fm[:, 128:256].rearrange("p (a b) -> p a b", a=2), in0=pB[:, 128:256].rearrange("p (a b) -> p a b", a=2),
                                                                    in1=e_[:, 128:192].unsqueeze(1).to_broadcast([64, 2, 64]), op=ALU.mult), r=[pB, e_, fm], w=[fm])
                        S.op("pool", lambda: nc.gpsimd.tensor_tensor(out=bk[:, 0:64], in0=B, in1=e_[:, 192:256], op=ALU.mult), r=[tl[3], e_], w=[bk])
                        S.op("pool", lambda: nc.gpsimd.tensor_tensor(out=bk[:, 64:128], in0=K, in1=e_[:, 192:256], op=ALU.mult), r=[tl[4], e_, bk], w=[bk])
                        AT, RT, BT, KT = fm[:, 0:64], fm[:, 64:128], fm[:, 128:192], fm[:, 192:256]
                        S.op("pe", lambda: nc.tensor.matmul(pC[:, 0:128], lhsT=BT, rhs=fm[:, 0:128], start=True, stop=True), r=[fm], w=[pC])
                        S.op("pe", lambda: nc.tensor.matmul(pC[:, 128:256], lhsT=KT, rhs=fm[:, 0:128], start=True, stop=True), r=[fm], w=[pC])
                        S.op("pe", lambda: nc.tensor.matmul(pC[:, 256:320], lhsT=AT, rhs=BT, start=True, stop=True), r=[fm], w=[pC])
                        S.op("dve", lambda: nc.vector.tensor_tensor(out=gm[:], in0=pC[:, :], in1=MM[:], op=ALU.mult), r=[pC, MM], w=[gm])
                        N_, MrbT, LakT, MrkT, NT = gm[:, 0:64], gm[:, 64:128], gm[:, 128:192], gm[:, 192:256], gm[:, 256:320]
                        xx = XX[w_]
                        p2 = P2[w_]
                        S.op("pool", lambda: nc.gpsimd.tensor_tensor(out=xx[0][:, 0:64], in0=N_, in1=I64, op=ALU.add), r=[gm, crw], w=[xx[0]])
                        S.op("pool", lambda: nc.gpsimd.tensor_tensor(out=xx[0][:, 64:128], in0=NT, in1=I64, op=ALU.add), r=[gm, crw, xx[0]], w=[xx[0]])
                        Pc, PTc, pk = N_, NT, gm
                        for k in range(5):
                            last = (k == 4)
                            pn = p2[k % 2]
                            S.op("pe", lambda Pc=Pc, PTc=PTc: nc.tensor.matmul(pD[:, 0:64], lhsT=PTc, rhs=Pc, start=True, stop=True), r=[pk], w=[pD])
                            if not last:
                                S.op("pe", lambda Pc=Pc, PTc=PTc: nc.tensor.matmul(pD[:, 64:128], lhsT=Pc, rhs=PTc, start=True, stop=True), r=[pk], w=[pD])
                                S.op("act", lambda pn=pn: nc.scalar.copy(out=pn[:], in_=pD[:, :]), r=[pD], w=[pn])
                            else:
                                S.op("act", lambda pn=pn: nc.scalar.copy(out=pn[:, 0:64], in_=pD[:, 0:64]), r=[pD], w=[pn])
                            xc_, xn_ = xx[k % 2], xx[(k + 1) % 2]
                            S.op("pe", lambda xc_=xc_, pn=pn: nc.tensor.matmul(pE[:, 0:64], lhsT=xc_[:, 64:128], rhs=pn[:, 0:64], start=True, stop=True), r=[xc_, pn], w=[pE])
                            if not last:
                                S.op("pe", lambda xc_=xc_, pn=pn: nc.tensor.matmul(pE[:, 64:128], lhsT=pn[:, 0:64], rhs=xc_[:, 64:128], start=True, stop=True), r=[xc_, pn], w=[pE])
                                S.op("dve", lambda xc_=xc_, xn_=xn_: nc.vector.tensor_tensor(out=xn_[:], in0=pE[:, :], in1=xc_[:], op=ALU.add), r=[pE, xc_], w=[xn_])
                            else:
                                S.op("dve", lambda xc_=xc_, xn_=xn_: nc.vector.tensor_tensor(out=xn_[:, 0:64], in0=pE[:, 0:64], in1=xc_[:, 0:64], op=ALU.add), r=[pE, xc_], w=[xn_])
                            Pc, PTc, pk = pn[:, 0:64], pn[:, 64:128], pn
                        X = xx[1][:, 0:64]
                        xk = xx[1]
                        S.op("pe", lambda: nc.tensor.matmul(pF[:, 0:64], lhsT=AT, rhs=Hc[:], start=True, stop=False), r=[fm, Hc], w=[pF])
                        S.op("pe", lambda: nc.tensor.matmul(pF[:, 0:64], lhsT=LakT, rhs=V, start=False, stop=True), r=[gm, tl[5]], w=[pF])
                        S.op("act", lambda: nc.scalar.copy(out=ru[:], in_=pF[:, 0:64]), r=[pF], w=[ru])
                        S.op("pe", lambda: nc.tensor.matmul(pF[:, 64:128], lhsT=X, rhs=ru[:], start=True, stop=True), r=[xk, ru], w=[pF])
                        S.op("dve", lambda: nc.vector.tensor_copy(out=u_[:], in_=pF[:, 64:128]), r=[pF], w=[u_])
                        if own:
                            yb = ybuf[par][hh]
                            S.op("pe", lambda: nc.tensor.matmul(pG[:, :], lhsT=RT, rhs=Hc[:], start=True, stop=False), r=[fm, Hc], w=[pG])
                            S.op("pe", lambda: nc.tensor.matmul(pG[:, :], lhsT=MrbT, rhs=u_[:], start=False, stop=False), r=[gm, u_], w=[pG])
                            S.op("pe", lambda: nc.tensor.matmul(pG[:, :], lhsT=MrkT, rhs=V, start=False, stop=True), r=[gm, tl[5]], w=[pG])
                            S.op("act", lambda: nc.scalar.copy(out=yb[:, c, :], in_=pG[:, :]), r=[pG], w=[yb])
                        S.op("pe", lambda: nc.tensor.matmul(pH[:, :], lhsT=I64, rhs=Hc[:], start=True, stop=False), r=[crw, Hc], w=[pH])
                        S.op("pe", lambda: nc.tensor.matmul(pH[:, :], lhsT=bk[:, 0:64], rhs=u_[:], start=False, stop=False), r=[bk, u_], w=[pH])
                        S.op("pe", lambda: nc.tensor.matmul(pH[:, :], lhsT=bk[:, 64:128], rhs=V, start=False, stop=True), r=[bk, tl[5]], w=[pH])
                        S.op("dve", lambda: nc.vector.tensor_scalar(out=Hn[:], in0=pH[:, :], scalar1=e_[:, 63:64], scalar2=None, op0=ALU.mult), r=[pH, e_], w=[Hn])
                if own:
                    for hh in range(4):
                        h = hg * 4 + hh
                        r0 = (b8 - 4) * 512
                        S.dma("sp", G.rw_y[r0:r0 + 512, h * 64:(h + 1) * 64].rearrange("(c t) k -> t c k", t=64), ybuf[par][hh][:], r=[ybuf[par][hh]], w=["rw_y"])
        S.barrier(G.bar)
    with ExitStack() as st:
        sb = lambda n, s, d: st.enter_context(nc.sbuf_tensor(n, s, d))
        ps = lambda n, s, d: st.enter_context(nc.psum_tensor(n, s, d))
        ident = sb("p_ident", [128, 128], BF16)
        bc = {}
        for nm in ("rwkv_ln_g", "rwkv_ln_b", "rwkv_r_k"):
            bc[nm] = sb("p_" + nm, [128, 1024], F32)
            S.dma("sp", bc[nm][:], I[nm][0:1, :].partition_broadcast(128), w=[bc[nm]])
        S.dma("pool", ident[:], I["c_ident"][:, :], w=[ident])
        y = [sb(f"p_y{i}", [128, 1024], F32) for i in range(2)]
        rr = [sb(f"p_r{i}", [128, 1024], F32) for i in range(2)]
        kq = [sb(f"p_k{i}", [128, 1024], F32) for i in range(2)]
        vv = [sb(f"p_v{i}", [128, 1024], F32) for i in range(2)]
        gg = [sb(f"p_g{i}", [128, 1024], F32) for i in range(2)]
        sq = sb("p_sq", [128, 1024], F32)
        st1 = sb("p_st1", [128, 16], F32)
        st2 = sb("p_st2", [128, 16], F32)
        st3 = sb("p_st3", [128, 16], F32)
        obf = sb("p_obf", [128, 1024], BF16)
        ost = sb("p_ost", [128, 8, 128], BF16)
        pT = ps("p_pT", [128, 1024], BF16)
        v3 = lambda t: t[:].rearrange("p (h k) -> p h k", k=64)
        b3 = lambda t: t[:].unsqueeze(2).to_broadcast([128, 16, 64])
        for tl in range(16):
            p = tl % 2
            rows = slice(tl * 128, (tl + 1) * 128)
            crow = slice(2048 + tl * 128, 2048 + (tl + 1) * 128)
            yt, rt, kt, vt, gt = y[p], rr[p], kq[p], vv[p], gg[p]
            S.dma("sp", yt[:], G.rw_y[rows, :], r=["rw_y"], w=[yt])
            S.dma("sp", rt[:], G.rw_r[crow, :], r=["rw_r"], w=[rt])
            S.dma("sp", kt[:], G.rw_k[crow, :], r=["rw_k"], w=[kt])
            S.dma("sp", vt[:], G.rw_v[crow, :], r=["rw_v"], w=[vt])
            S.dma("sp", gt[:], G.rw_g[rows, :], r=["rw_g"], w=[gt])
            S.op("dve", lambda: nc.vector.tensor_reduce(out=st1[:], in_=v3(yt), axis=AX.X, op=ALU.add), r=[yt], w=[st1])
            S.op("pool", lambda: nc.gpsimd.tensor_tensor(out=sq[:], in0=yt[:], in1=yt[:], op=ALU.mult), r=[yt], w=[sq])
            S.op("dve", lambda: nc.vector.tensor_reduce(out=st2[:], in_=v3(sq), axis=AX.X, op=ALU.add), r=[sq], w=[st2])
            S.op("dve", lambda: nc.vector.tensor_scalar(out=st1[:], in0=st1[:], scalar1=1.0 / 64.0, scalar2=None, op0=ALU.mult), r=[st1], w=[st1])
            S.op("dve", lambda: nc.vector.tensor_tensor(out=st3[:], in0=st1[:], in1=st1[:], op=ALU.mult), r=[st1], w=[st3])
            S.op("dve", lambda: nc.vector.scalar_tensor_tensor(out=st2[:], in0=st2[:], scalar=1.0 / 64.0, in1=st3[:], op0=ALU.mult, op1=ALU.subtract), r=[st2, st3], w=[st2])
            S.op("dve", lambda: nc.vector.tensor_scalar(out=st2[:], in0=st2[:], scalar1=64e-5, scalar2=None, op0=ALU.add), r=[st2], w=[st2])
            S.op("act", lambda: nc.scalar.activation(out=st2[:], in_=st2[:], func=AF.Sqrt), r=[st2], w=[st2])
            S.op("dve", lambda: nc.vector.reciprocal(out=st2[:], in_=st2[:]), r=[st2], w=[st2])
            S.op("dve", lambda: nc.vector.tensor_tensor(out=v3(yt), in0=v3(yt), in1=b3(st1), op=ALU.subtract), r=[yt, st1], w=[yt])
            S.op("dve", lambda: nc.vector.tensor_tensor(out=v3(yt), in0=v3(yt), in1=b3(st2), op=ALU.mult), r=[yt, st2], w=[yt])
            S.op("pool", lambda: nc.gpsimd.tensor_tensor(out=yt[:], in0=yt[:], in1=bc["rwkv_ln_g"][:], op=ALU.mult), r=[yt, bc["rwkv_ln_g"]], w=[yt])
            S.op("pool", lambda: nc.gpsimd.tensor_tensor(out=yt[:], in0=yt[:], in1=bc["rwkv_ln_b"][:], op=ALU.add), r=[yt, bc["rwkv_ln_b"]], w=[yt])
            S.op("pool", lambda: nc.gpsimd.tensor_tensor(out=rt[:], in0=rt[:], in1=kt[:], op=ALU.mult), r=[rt, kt], w=[rt])
            S.op("pool", lambda: nc.gpsimd.tensor_tensor(out=rt[:], in0=rt[:], in1=bc["rwkv_r_k"][:], op=ALU.mult), r=[rt, bc["rwkv_r_k"]], w=[rt])
            S.op("dve", lambda: nc.vector.tensor_reduce(out=st3[:], in_=v3(rt), axis=AX.X, op=ALU.add), r=[rt], w=[st3])
            S.op("dve", lambda: nc.vector.tensor_tensor(out=v3(vt), in0=v3(vt), in1=b3(st3), op=ALU.mult), r=[vt, st3], w=[vt])
            S.op("pool", lambda: nc.gpsimd.tensor_tensor(out=yt[:], in0=yt[:], in1=vt[:], op=ALU.add), r=[yt, vt], w=[yt])
            S.op("dve", lambda: nc.vector.tensor_tensor(out=obf[:], in0=yt[:], in1=gt[:], op=ALU.mult), r=[yt, gt], w=[obf])
            for j in range(8):
                S.op("pe", lambda j=j: nc.tensor.transpose(pT[:, j * 128:(j + 1) * 128], obf[:, j * 128:(j + 1) * 128], ident[:]), r=[obf, ident], w=[pT])
            S.op("act", lambda: nc.scalar.copy(out=ost[:], in_=pT[:, :].rearrange("p (j q) -> p j q", j=8)), r=[pT], w=[ost])
            S.dma("sp", G.mixT[1024:2048, tl * 128:(tl + 1) * 128].rearrange("(j p) q -> p j q", p=128), ost[:], r=[ost], w=["mixT"])
        S.barrier(G.bar)


def layer_norm_tile(G, x, g_bc, b_bc, sq, st, eps=1e-5):
    nc, S = G.nc, G.S
    S.op("dve", lambda: nc.vector.tensor_reduce(out=st[:, 0:1], in_=x[:], axis=AX.X, op=ALU.add), r=[x], w=[st])
    S.op("pool", lambda: nc.gpsimd.tensor_tensor(out=sq[:], in0=x[:], in1=x[:], op=ALU.mult), r=[x], w=[sq])
    S.op("dve", lambda: nc.vector.tensor_reduce(out=st[:, 1:2], in_=sq[:], axis=AX.X, op=ALU.add), r=[sq, st], w=[st])
    S.op("dve", lambda: nc.vector.tensor_scalar(out=st[:, 0:2], in0=st[:, 0:2], scalar1=1.0 / D, scalar2=None, op0=ALU.mult), r=[st], w=[st])
    S.op("dve", lambda: nc.vector.tensor_tensor(out=st[:, 2:3], in0=st[:, 0:1], in1=st[:, 0:1], op=ALU.mult), r=[st], w=[st])
    S.op("dve", lambda: nc.vector.tensor_tensor(out=st[:, 1:2], in0=st[:, 1:2], in1=st[:, 2:3], op=ALU.subtract), r=[st], w=[st])
    S.op("dve", lambda: nc.vector.tensor_scalar(out=st[:, 1:2], in0=st[:, 1:2], scalar1=eps, scalar2=None, op0=ALU.add), r=[st], w=[st])
    S.op("act", lambda: nc.scalar.activation(out=st[:, 1:2], in_=st[:, 1:2], func=AF.Sqrt), r=[st], w=[st])
    S.op("dve", lambda: nc.vector.reciprocal(out=st[:, 1:2], in_=st[:, 1:2]), r=[st], w=[st])
    S.op("dve", lambda: nc.vector.tensor_scalar(out=x[:], in0=x[:], scalar1=st[:, 0:1], scalar2=st[:, 1:2], op0=ALU.subtract, op1=ALU.mult), r=[x, st], w=[x])
    S.op("pool", lambda: nc.gpsimd.tensor_tensor(out=x[:], in0=x[:], in1=g_bc[:], op=ALU.mult), r=[x, g_bc], w=[x])
    S.op("pool", lambda: nc.gpsimd.tensor_tensor(out=x[:], in0=x[:], in1=b_bc[:], op=ALU.add), r=[x, b_bc], w=[x])


def phase_d(G):
    nc, S, I = G.nc, G.S, G.I
    with ExitStack() as st:
        sb = lambda n, s, d: st.enter_context(nc.sbuf_tensor(n, s, d))
        ps = lambda n, s, d: st.enter_context(nc.psum_tensor(n, s, d))
        ident = sb("d_ident", [128, 128], BF16)
        identf = sb("d_identf", [128, 128], F32)
        mixT = sb("d_mixT", [128, 16, TO], BF16)
        wout = sb("d_wout", [128, 16, D], BF16)
        gbc = sb("d_g", [128, D], F32)
        bbc = sb("d_b", [128, D], F32)
        rw = sb("d_rw", [128, 16, 32], F32)
        rb = sb("d_rb", [128, 32], F32)
        xt = [sb(f"d_x{i}", [128, D], F32) for i in range(2)]
        hp = [sb(f"d_hp{i}", [128, D], F32) for i in range(2)]
        sq = sb("d_sq", [128, D], F32)
        stt = sb("d_st", [128, 4], F32)
        hbf = sb("d_hbf", [128, D], BF16)
        hTs = sb("d_hTs", [128, 16, 128], BF16)
        hT32 = sb("d_hT32", [128, 16, 128], F32)
        lg = sb("d_lg", [128, 32], F32)
        ex = sb("d_ex", [128, 32], F32)
        m8 = sb("d_m8", [128, 8], F32)
        sm = sb("d_sm", [128, 2], F32)
        pO = [ps(f"d_pO{i}", [128, 512], F32) for i in range(4)]
        pTf = [ps(f"d_pTf{i}", [128, 512], F32) for i in range(2)]
        pL = ps("d_pL", [128, 32], F32)
        pTb = ps("d_pTb", [128, 1024], BF16)
        S.dma("pool", ident[:], I["c_ident"][:, :], w=[ident])
        S.dma("sp", identf[:], I["c_ident"][:, :], w=[identf])
        mv = G.mixT.rearrange("(kc p) t -> p kc t", p=128)
        wv = I["w_out"].rearrange("(kc p) c -> p kc c", p=128)
        for j in range(4):
            S.dma("sp", mixT[:, j * 4:(j + 1) * 4, :], mv[:, j * 4:(j + 1) * 4, :], r=["mixT"], w=[mixT])
            S.dma("pool", wout[:, :, j * 512:(j + 1) * 512], wv[:, :, j * 512:(j + 1) * 512], w=[wout])
        S.dma("sp", gbc[:], I["ln1_g"][0:1, :].partition_broadcast(128), w=[gbc])
        S.dma("sp", bbc[:], I["ln1_b"][0:1, :].partition_broadcast(128), w=[bbc])
        S.dma("sp", rw[:], I["router_w"].rearrange("(kc p) e -> p kc e", p=128), w=[rw])
        S.dma("sp", rb[:], I["router_b"][0:1, :].partition_broadcast(128), w=[rb])
        for tl in range(16):
            x_ = xt[tl % 2]
            h_ = hp[tl % 2]
            rows = slice(tl * 128, (tl + 1) * 128)
            S.dma("sp", x_[:], I["xc"][2048 + tl * 128:2048 + (tl + 1) * 128, :], w=[x_])
            for c4 in range(4):
                for kc in range(16):
                    S.op("pe", lambda c4=c4, kc=kc: nc.tensor.matmul(pO[c4][:, :], lhsT=mixT[:, kc, tl * 128:(tl + 1) * 128], rhs=wout[:, kc, c4 * 512:(c4 + 1) * 512],
                                                                       start=(kc == 0), stop=(kc == 15)), r=[mixT, wout], w=[pO[c4]])
                S.op("dve", lambda c4=c4: nc.vector.scalar_tensor_tensor(out=h_[:, c4 * 512:(c4 + 1) * 512], in0=x_[:, c4 * 512:(c4 + 1) * 512], scalar=ALPHA, in1=pO[c4][:, :],
                                                                          op0=ALU.mult, op1=ALU.add), r=[x_, pO[c4]], w=[h_])
            layer_norm_tile(G, h_, gbc, bbc, sq, stt)
            S.dma("sp", G.h1[rows, :], h_[:], r=[h_], w=["h1"])
            S.op("act", lambda: nc.scalar.copy(out=hbf[:], in_=h_[:]), r=[h_], w=[hbf])
            for j in range(2):
                for k8 in range(8):
                    kc = j * 8 + k8
                    S.op("pe", lambda k8=k8, kc=kc: nc.tensor.transpose(pTb[:, k8 * 128:(k8 + 1) * 128], hbf[:, kc * 128:(kc + 1) * 128], ident[:]), r=[hbf, ident], w=[pTb])
                evac(G, hTs[:, j * 8:(j + 1) * 8, :], pTb[:].rearrange("p (a b) -> p a b", a=8), r=[pTb], w=[hTs])
            S.dma("sp", G.h1T[:, rows].rearrange("(kc p) t -> p kc t", p=128), hTs[:], r=[hTs], w=["h1T"])
            for j in range(4):
                pt = pTf[j % 2]
                for k4 in range(4):
                    kc = j * 4 + k4
                    S.op("pe", lambda k4=k4, kc=kc, pt=pt: nc.tensor.transpose(pt[:, k4 * 128:(k4 + 1) * 128], h_[:, kc * 128:(kc + 1) * 128], identf[:]), r=[h_, identf], w=[pt])
                evac(G, hT32[:, j * 4:(j + 1) * 4, :], pt[:].rearrange("p (a b) -> p a b", a=4), r=[pt], w=[hT32])
            for kc in range(16):
                S.op("pe", lambda kc=kc: nc.tensor.matmul(pL[:, :], lhsT=hT32[:, kc, :], rhs=rw[:, kc, :], start=(kc == 0), stop=(kc == 15)), r=[hT32, rw], w=[pL])
            S.op("dve", lambda: nc.vector.tensor_tensor(out=lg[:], in0=pL[:, :], in1=rb[:], op=ALU.add), r=[pL, rb], w=[lg])
            S.op("dve", lambda: nc.vector.max(out=m8[:], in_=lg[:]), r=[lg], w=[m8])
            S.op("dve", lambda: nc.vector.tensor_scalar(out=sm[:, 0:1], in0=m8[:, 0:1], scalar1=-1.0, scalar2=None, op0=ALU.mult), r=[m8], w=[sm])
            S.op("act", lambda: nc.scalar.activation(out=ex[:], in_=lg[:], func=AF.Exp, bias=sm[:, 0:1], scale=1.0), r=[lg, sm], w=[ex])
            S.op("dve", lambda: nc.vector.tensor_tensor(out=lg[:], in0=lg[:], in1=m8[:, 3:4].to_broadcast([128, 32]), op=ALU.is_ge), r=[lg, m8], w=[lg])
            S.op("dve", lambda: nc.vector.tensor_tensor(out=ex[:], in0=ex[:], in1=lg[:], op=ALU.mult), r=[ex, lg], w=[ex])
            S.op("dve", lambda: nc.vector.tensor_reduce(out=sm[:, 1:2], in_=ex[:], axis=AX.X, op=ALU.add), r=[ex, sm], w=[sm])
            S.op("dve", lambda: nc.vector.reciprocal(out=sm[:, 1:2], in_=sm[:, 1:2]), r=[sm], w=[sm])
            S.op("dve", lambda: nc.vector.tensor_scalar(out=ex[:], in0=ex[:], scalar1=sm[:, 1:2], scalar2=None, op0=ALU.mult), r=[ex, sm], w=[ex])
            S.dma("sp", G.gw[rows, :], ex[:], r=[ex], w=["gw"])
        S.barrier(G.bar)


def phase_e(G, experts=32):
    nc, S, I = G.nc, G.S, G.I
    LIM = 7.0
    with ExitStack() as st:
        sb = lambda n, s, d: st.enter_context(nc.sbuf_tensor(n, s, d))
        ps = lambda n, s, d: st.enter_context(nc.psum_tensor(n, s, d))
        identf = sb("e_identf", [128, 128], F32)
        h1T = sb("e_h1T", [128, 16, 1024], BF16)
        Y = sb("e_Y", [128, 8, D], F32)
        gw = sb("e_gw", [128, 8, 32], F32)
        gwT = sb("e_gwT", [32, 8, 128], F32)
        bdn = sb("e_bdn", [32, D], F32)
        bg = sb("e_bg", [128, 512], F32)
        bu = sb("e_bu", [128, 512], F32)
        wg = [sb(f"e_wg{i}", [128, 16, 256], BF16) for i in range(2)]
        wu = [sb(f"e_wu{i}", [128, 16, 256], BF16) for i in range(2)]
        wd = [sb(f"e_wd{i}", [128, 2, D], BF16) for i in range(2)]
        hT = [sb(f"e_hT{i}", [128, 2, 1024], BF16) for i in range(2)]
        gt = [sb(f"e_g{i}", [128, 512], F32) for i in range(2)]
        sg = [sb(f"e_sg{i}", [128, 512], F32) for i in range(2)]
        ut = [sb(f"e_u{i}", [128, 512], F32) for i in range(2)]
        ev = [sb(f"e_ev{i}", [128, 512], F32) for i in range(3)]
        h1t = [sb(f"e_h1t{i}", [128, D], F32) for i in range(1)]
        pGU = [ps(f"e_pGU{i}", [128, 512], F32) for i in range(4)]
        pDn = [ps(f"e_pD{i}", [128, 512], F32) for i in range(4)]
        S.dma("sp", identf[:], I["c_ident"][:, :], w=[identf])
        S.dma("sp", bdn[:], I["exp_b_down"][:, :], w=[bdn])
        S.dma("sp", bg[:], I["exp_b_gate"][:, :], w=[bg])
        S.dma("sp", bu[:], I["exp_b_up"][:, :], w=[bu])
        wgv = I["exp_w_gate"].rearrange("(e kc p) f -> e p kc f", p=128, kc=16)
        wuv = I["exp_w_up"].rearrange("(e kc p) f -> e p kc f", p=128, kc=16)
        wdv = I["exp_w_down"].rearrange("(e fc p) d -> e p fc d", p=128, fc=16)
        nst = 0
        YK = [[("Y", tl, d4) for d4 in range(4)] for tl in range(8)]
        YALL = [k for row in YK for k in row]
        for hf in range(2):
            t0 = hf * 1024
            S.dma("sp", h1T[:], G.h1T[:, t0:t0 + 1024].rearrange("(kc p) t -> p kc t", p=128), r=["h1T"], w=[h1T])
            S.dma("sp", gw[:], G.gw[t0:t0 + 1024, :].rearrange("(t p) e -> p t e", p=128), r=["gw"], w=[gw])
            S.op("pool", lambda: nc.gpsimd.memset(Y[:], 0.0), w=YALL)
            for e in range(experts):
                for fgp in range(8):
                    b = nst % 2
                    nst += 1
                    S.dma("pool", wg[b][:], wgv[e][:, :, fgp * 256:(fgp + 1) * 256], w=[wg[b]])
                    S.dma("pool", wu[b][:], wuv[e][:, :, fgp * 256:(fgp + 1) * 256], w=[wu[b]])
                    S.dma("pool", wd[b][:], wdv[e][:, fgp * 2:(fgp + 1) * 2, :], w=[wd[b]])
                    hb = hT[b]
                    for tc_ in range(2):
                        for f2 in range(2):
                            fc = fgp * 2 + f2
                            bcol = e * 16 + fc
                            pg, pu = pGU[(2 * f2) % 4], pGU[(2 * f2 + 1) % 4]
                            for kc in range(16):
                                S.op("pe", lambda kc=kc, pg=pg: nc.tensor.matmul(pg[:, :], lhsT=wg[b][:, kc, f2 * 128:(f2 + 1) * 128], rhs=h1T[:, kc, tc_ * 512:(tc_ + 1) * 512],
                                                                                  start=(kc == 0), stop=(kc == 15)), r=[wg[b], h1T], w=[pg])
                            for kc in range(16):
                                S.op("pe", lambda kc=kc, pu=pu: nc.tensor.matmul(pu[:, :], lhsT=wu[b][:, kc, f2 * 128:(f2 + 1) * 128], rhs=h1T[:, kc, tc_ * 512:(tc_ + 1) * 512],
                                                                                  start=(kc == 0), stop=(kc == 15)), r=[wu[b], h1T], w=[pu])
                            g_, s_, u_ = gt[f2], sg[f2], ut[f2]
                            S.op("dve", lambda: nc.vector.tensor_scalar(out=g_[:], in0=pg[:, :], scalar1=bg[:, bcol:bcol + 1], scalar2=LIM, op0=ALU.add, op1=ALU.min), r=[pg, bg], w=[g_])
                            S.op("act", lambda: nc.scalar.activation(out=s_[:], in_=g_[:], func=AF.Sigmoid, scale=1.702), r=[g_], w=[s_])
                            S.op("dve", lambda: nc.vector.tensor_scalar(out=u_[:], in0=pu[:, :], scalar1=bu[:, bcol:bcol + 1], scalar2=LIM, op0=ALU.add, op1=ALU.min), r=[pu, bu], w=[u_])
                            S.op("dve", lambda: nc.vector.tensor_scalar(out=u_[:], in0=u_[:], scalar1=-LIM, scalar2=1.0, op0=ALU.max, op1=ALU.add), r=[u_], w=[u_])
                            S.op("dve", lambda: nc.vector.tensor_tensor(out=g_[:], in0=g_[:], in1=s_[:], op=ALU.mult), r=[g_, s_], w=[g_])
                            S.op("dve", lambda: nc.vector.tensor_tensor(out=hb[:, f2, tc_ * 512:(tc_ + 1) * 512], in0=g_[:], in1=u_[:], op=ALU.mult), r=[g_, u_], w=[("hT", b, tc_)])
                    for tl in range(8):
                        for d4 in range(4):
                            pd = pDn[(tl * 4 + d4) % 4]
                            for f2 in range(2):
                                S.op("pe", lambda f2=f2, pd=pd, d4=d4, tl=tl: nc.tensor.matmul(pd[:, :], lhsT=hb[:, f2, tl * 128:(tl + 1) * 128], rhs=wd[b][:, f2, d4 * 512:(d4 + 1) * 512],
                                                                                                 start=(f2 == 0), stop=(f2 == 1)), r=[("hT", b, tl // 4), wd[b]], w=[pd])
                            S.op("dve", lambda pd=pd, tl=tl, d4=d4: nc.vector.scalar_tensor_tensor(out=Y[:, tl, d4 * 512:(d4 + 1) * 512], in0=pd[:, :], scalar=gw[:, tl, e:e + 1],
                                                                                                     in1=Y[:, tl, d4 * 512:(d4 + 1) * 512], op0=ALU.mult, op1=ALU.add),
                                 r=[pd, gw, YK[tl][d4]], w=[YK[tl][d4]])
            for tl in range(8):
                S.op("pe", lambda tl=tl: nc.tensor.transpose(pGU[0][0:32, 0:128], gw[:, tl, :], identf[:]), r=[gw, identf], w=[pGU[0]])
                S.op("dve", lambda tl=tl: nc.vector.tensor_copy(out=gwT[:, tl, :], in_=pGU[0][0:32, 0:128]), r=[pGU[0]], w=[gwT])
                ht = h1t[0]
                rows = slice(t0 + tl * 128, t0 + (tl + 1) * 128)
                S.dma("sp", ht[:], G.h1[rows, :], r=["h1"], w=[ht])
                for d4 in range(4):
                    pd = pDn[d4]
                    S.op("pe", lambda pd=pd, d4=d4, tl=tl: nc.tensor.matmul(pd[:, :], lhsT=gwT[:, tl, :], rhs=bdn[:, d4 * 512:(d4 + 1) * 512], start=True, stop=True), r=[gwT, bdn], w=[pd])
                    S.op("dve", lambda pd=pd, d4=d4, tl=tl: nc.vector.tensor_tensor(out=Y[:, tl, d4 * 512:(d4 + 1) * 512], in0=Y[:, tl, d4 * 512:(d4 + 1) * 512], in1=pd[:, :], op=ALU.add),
                         r=[pd, YK[tl][d4]], w=[YK[tl][d4]])
                S.op("dve", lambda tl=tl, ht=ht: nc.vector.scalar_tensor_tensor(out=ht[:], in0=ht[:], scalar=ALPHA, in1=Y[:, tl, :], op0=ALU.mult, op1=ALU.add), r=[ht] + YK[tl], w=[ht])
                S.dma("sp", G.ypre[rows, :], ht[:], r=[ht], w=["ypre"])
        S.barrier(G.bar)


def phase_f(G):
    nc, S, I = G.nc, G.S, G.I
    with ExitStack() as st:
        sb = lambda n, s, d: st.enter_context(nc.sbuf_tensor(n, s, d))
        ps = lambda n, s, d: st.enter_context(nc.psum_tensor(n, s, d))
        ident = sb("f_ident", [128, 128], BF16)
        pgw = sb("f_pgw", [128, 16, D], BF16)
        plw = sb("f_plw", [128, 2, D], BF16)
        gbc = sb("f_g", [128, D], F32)
        bbc = sb("f_b", [128, D], F32)
        yt = [sb(f"f_y{i}", [128, D], F32) for i in range(2)]
        ot = [sb(f"f_o{i}", [128, D], F32) for i in range(2)]
        sq = sb("f_sq", [128, D], F32)
        stt = sb("f_st", [128, 4], F32)
        hbf = sb("f_hbf", [128, D], BF16)
        pb = [sb(f"f_pb{i}", [128, 256], BF16) for i in range(2)]
        hTs = sb("f_hTs", [128, 16, 128], BF16)
        pTs = sb("f_pTs", [128, 2, 128], BF16)
        sgt = [sb(f"f_sg{i}", [128, 512], F32) for i in range(2)]
        pGt = [ps(f"f_pG{i}", [128, 512], F32) for i in range(2)]
        pPt = [ps(f"f_pP{i}", [128, 512], F32) for i in range(2)]
        pTb = ps("f_pTb", [128, 1024], BF16)
        S.dma("pool", ident[:], I["c_ident"][:, :], w=[ident])
        wv = I["ple_gate_w"].rearrange("(kc p) c -> p kc c", p=128)
        for j in range(4):
            S.dma("pool", pgw[:, :, j * 512:(j + 1) * 512], wv[:, :, j * 512:(j + 1) * 512], w=[pgw])
        S.dma("pool", plw[:], I["ple_w"].rearrange("(kc p) c -> p kc c", p=128), w=[plw])
        S.dma("sp", gbc[:], I["ln2_g"][0:1, :].partition_broadcast(128), w=[gbc])
        S.dma("sp", bbc[:], I["ln2_b"][0:1, :].partition_broadcast(128), w=[bbc])
        for tl in range(16):
            rows = slice(tl * 128, (tl + 1) * 128)
            y_ = yt[tl % 2]
            o_ = ot[tl % 2]
            p_ = pb[tl % 2]
            S.dma("sp", y_[:], G.ypre[rows, :], r=["ypre"], w=[y_])
            S.dma("pool", p_[:], I["p_own"][rows, :], w=[p_])
            layer_norm_tile(G, y_, gbc, bbc, sq, stt)
            S.op("act", lambda: nc.scalar.copy(out=hbf[:], in_=y_[:]), r=[y_], w=[hbf])
            for j in range(2):
                for k8 in range(8):
                    kc = j * 8 + k8
                    S.op("pe", lambda k8=k8, kc=kc: nc.tensor.transpose(pTb[:, k8 * 128:(k8 + 1) * 128], hbf[:, kc * 128:(kc + 1) * 128], ident[:]), r=[hbf, ident], w=[pTb])
                evac(G, hTs[:, j * 8:(j + 1) * 8, :], pTb[:].rearrange("p (a b) -> p a b", a=8), r=[pTb], w=[hTs])
            for j in range(2):
                S.op("pe", lambda j=j: nc.tensor.transpose(pTb[:, j * 128:(j + 1) * 128], p_[:, j * 128:(j + 1) * 128], ident[:]), r=[p_, ident], w=[pTb])
            evac(G, pTs[:], pTb[:, 0:256].rearrange("p (a b) -> p a b", a=2), r=[pTb], w=[pTs])
            for d4 in range(4):
                cs = slice(d4 * 512, (d4 + 1) * 512)
                pg, pp, s_ = pGt[d4 % 2], pPt[d4 % 2], sgt[d4 % 2]
                for kc in range(16):
                    S.op("pe", lambda kc=kc, pg=pg, cs=cs: nc.tensor.matmul(pg[:, :], lhsT=hTs[:, kc, :], rhs=pgw[:, kc, cs], start=(kc == 0), stop=(kc == 15)), r=[hTs, pgw], w=[pg])
                for kc in range(2):
                    S.op("pe", lambda kc=kc, pp=pp, cs=cs: nc.tensor.matmul(pp[:, :], lhsT=pTs[:, kc, :], rhs=plw[:, kc, cs], start=(kc == 0), stop=(kc == 1)), r=[pTs, plw], w=[pp])
                S.op("act", lambda pg=pg, s_=s_: nc.scalar.activation(out=s_[:], in_=pg[:, :], func=AF.Sigmoid), r=[pg], w=[s_])
                S.op("dve", lambda pp=pp, s_=s_: nc.vector.tensor_tensor(out=s_[:], in0=s_[:], in1=pp[:, :], op=ALU.mult), r=[pp, s_], w=[s_])
                S.op("dve", lambda s_=s_, cs=cs: nc.vector.tensor_tensor(out=o_[:, cs], in0=y_[:, cs], in1=s_[:], op=ALU.add), r=[y_, s_], w=[o_])
            S.dma("sp", G.out[rows, :], o_[:], r=[o_], w=["out"])


def make_consts(s):
    c = {}
    kv = np.ones((T,), np.float32)
    if s == 0:
        kv[:2048] = 0.0
    c["c_kvalid"] = np.ascontiguousarray(kv.reshape(32, 128).T)
    c["c_ident"] = np.eye(128, dtype=np.float32)
    k = np.arange(128)[:, None]
    q = np.arange(128)[None, :]
    c["c_trile"] = (k <= q).astype(np.float32)
    c["c_trigt"] = (k > q).astype(np.float32)
    ab = np.zeros((4, 128, 4, 128), np.float32)
    cb = np.zeros((4, 2, 128, 4, 128), np.float32)
    for g in range(4):
        for h in range(4):
            sl = SLOPES[4 * g + h]
            ab[g, :, h, :] = -sl * (q - k)
            for ct in range(2):
                cb[g, ct, :, h, :] = -sl * (q - 16 * (k + 128 * ct) - 31)
    c["c_abias"] = ab.reshape(4 * 128, 512)
    c["c_cbias"] = cb.reshape(4 * 2 * 128, 512)
    cval = np.zeros((256, 1), np.float32)
    cval[(128 if s == 0 else 0):255] = 1.0
    cm = np.zeros((16, 2, 128, 128), np.float32)
    for i in range(16):
        for ct in range(2):
            cc = k + 128 * ct
            d = 2048 + 128 * i + q - 16 * cc - 31
            cm[i, ct] = (d >= 0) * cval[cc[:, 0]]
    c["c_cmask"] = ((cm - 1.0) * BIG).reshape(16 * 2 * 128, 128).astype(np.float32)
    c["c_tribias"] = np.concatenate([(c["c_trile"] - 1.0) * BIG, (c["c_trigt"] - 1.0) * BIG], axis=1).astype(np.float32)
    c["c_expand"] = (np.arange(T)[None, :] // 64 == np.arange(64)[:, None]).astype(np.float32)
    c["c_expbig"] = c["c_expand"] * BIG
    ce = np.arange(256)[:, None] * 16 + 31
    cs = ce - 31
    ss = np.arange(64)[None, :] * 64
    c["c_overlap"] = np.clip(np.minimum(ce, ss + 63) - np.maximum(cs, ss) + 1, 0, None).astype(np.float32)
    j0 = 32 if s == 0 else 0
    qi = np.arange(TO)[:, None]
    cur = 32 + qi // 64
    j = np.arange(64)[None, :]
    invalid = (j < j0) | (j > cur)
    forced = ((j == j0) | (j == cur) | (j == cur - 1)) & ~invalid
    c["c_selmul"] = (~invalid & ~forced).astype(np.float32)
    c["c_seladd"] = np.where(invalid, -BIG, np.where(forced, BIG, 0.0)).astype(np.float32)
    c["c_selvalid"] = (~invalid).astype(np.float32)
    s_ = np.arange(64)[:, None]
    t_ = np.arange(64)[None, :]
    incl = (s_ <= t_).astype(np.float32)
    strict = (s_ < t_).astype(np.float32)
    low = (t_ < s_).astype(np.float32)
    c["c_rw"] = np.concatenate([incl, strict, strict, incl, low, np.eye(64, dtype=np.float32), incl], axis=1)
    return c


def prep_core_inputs(inputs, c, consts_cache={}):
    b, s = c // 2, c % 2
    m = {}
    x = inputs["x"]
    if s == 1:
        m["xc"] = np.ascontiguousarray(x[b])
    else:
        m["xc"] = np.concatenate([np.zeros((2048, D), np.float32), x[b, :2048]], axis=0)
    m["p_own"] = np.ascontiguousarray(inputs["p"][0, b, 2048 * s:2048 * s + 2048])
    for name, shape in INPUT_SPECS:
        if name in ("xc", "p_own") or name.startswith("c_"):
            continue
        a = np.asarray(inputs[name], np.float32)
        if name in ("exp_b_gate", "exp_b_up"):
            a = np.ascontiguousarray(a.reshape(32, 16, 128).transpose(2, 0, 1))
        m[name] = a.reshape(shape)
    if s not in consts_cache:
        consts_cache[s] = make_consts(s)
    m.update(consts_cache[s])
    return m


def kernel(**inputs):
    nc, G = build()
    in_maps = [prep_core_inputs(inputs, c) for c in range(NCORES)]
    res = run_bass_kernel_spmd(nc, in_maps, core_ids=list(range(NCORES)))
    out = np.zeros((4, 4096, D), np.float32)
    for c in range(NCORES):
        b, s = c // 2, c % 2
        out[b, 2048 * s:2048 * s + 2048] = res.results[c]["out"]
    return out
```
